# Optimizing a Trainium2 kernel written in Bass

```python
import math
import jax, jax.numpy as jnp
from jax import lax
import numpy as np

D_MODEL = 1024
BATCH = 16
SEQ = 2048
DEPTH = 2

N_MIXERS = 2
N_NSA_LAYERS = (DEPTH + N_MIXERS - 1) // N_MIXERS
N_DIFF_LAYERS = DEPTH // N_MIXERS

REL_BUCKETS = 32
REL_MAX_DIST = 128
N_BIAS_MAPS = 16

NSA_HEADS = 16
NSA_GROUPS = 4
NSA_HPG = NSA_HEADS // NSA_GROUPS
NSA_HEAD_DIM = D_MODEL // NSA_HEADS
NSA_WIDTH = NSA_HEADS * NSA_HEAD_DIM
NSA_KV = NSA_GROUPS * NSA_HEAD_DIM
CMP_BLOCK = 32
CMP_STRIDE = 16
CMP_HIDDEN = 2 * NSA_HEAD_DIM
SEL_BLOCK = 64
SEL_TOPK = 8
WINDOW = 512
NSA_QBLOCK = 64
NSA_SPLIT_SIZES = [NSA_WIDTH] + [NSA_KV] * 6 + [3 * NSA_HEADS, NSA_WIDTH]
NSA_IN = sum(NSA_SPLIT_SIZES)

DIFF_HEADS = 8
DIFF_HALF = D_MODEL // (2 * DIFF_HEADS)
DIFF_VDIM = 2 * DIFF_HALF
DIFF_WIDTH = DIFF_HEADS * DIFF_VDIM
DIFF_IN = 4 * DIFF_WIDTH
DIFF_QBLOCK = 128

NEG = -1e30
BIG = 1e9
EPS = 1e-6

kernel_name = "hybrid_nsa_diffattn_sandwich"


def rmsnorm(x, g):
    xf = x.astype(jnp.float32)
    y = xf * lax.rsqrt(jnp.mean(xf * xf, axis=-1, keepdims=True) + EPS)
    return (y * g.astype(jnp.float32)).astype(x.dtype)


def rel_bucket(dist):
    n = jnp.maximum(dist, 0)
    max_exact = REL_BUCKETS // 2
    nf = jnp.maximum(n, 1).astype(jnp.float32)
    large = max_exact + (jnp.log(nf / max_exact) / math.log(REL_MAX_DIST / max_exact)
                         * (REL_BUCKETS - max_exact)).astype(jnp.int32)
    large = jnp.minimum(large, REL_BUCKETS - 1)
    return jnp.where(n < max_exact, n, large)


def compress(kv, pe, w1, w2):
    b, s, g, dh = kv.shape
    nc = (s - CMP_BLOCK) // CMP_STRIDE + 1
    idx = jnp.arange(nc)[:, None] * CMP_STRIDE + jnp.arange(CMP_BLOCK)[None, :]
    blocks = kv[:, idx] + pe[None, None, :, None, :]
    flat = jnp.moveaxis(blocks, 3, 2).reshape(b, nc, g, CMP_BLOCK * dh)
    return jax.nn.silu(flat @ w1) @ w2


def nsa_mixer(u, table, w_in, pe_k, w1_k, w2_k, pe_v, w1_v, w2_v, w_out):
    b, s, _ = u.shape
    G, J, dh = NSA_GROUPS, NSA_HPG, NSA_HEAD_DIM
    points = np.cumsum(NSA_SPLIT_SIZES)[:-1].tolist()
    q, kc, vc, ks, vs, kw, vw, gate, z = jnp.split(u @ w_in, points, axis=-1)
    q = q.reshape(b, s, G, J, dh) * (dh ** -0.5)
    kc, vc, ks, vs, kw, vw = [t.reshape(b, s, G, dh) for t in (kc, vc, ks, vs, kw, vw)]
    gate = jax.nn.sigmoid(gate.astype(jnp.float32)).reshape(b, s, G, J, 3)

    k_cmp = compress(kc, pe_k, w1_k, w2_k)
    v_cmp = compress(vc, pe_v, w1_v, w2_v)
    nc = k_cmp.shape[1]
    cmp_lo = jnp.arange(nc) * CMP_STRIDE
    cmp_end = cmp_lo + CMP_BLOCK - 1
    n_sel = s // SEL_BLOCK
    topk = min(SEL_TOPK, n_sel)
    sel_lo = jnp.arange(n_sel) * SEL_BLOCK
    overlap = jnp.clip(jnp.minimum(cmp_lo[:, None] + CMP_BLOCK, sel_lo[None, :] + SEL_BLOCK)
                       - jnp.maximum(cmp_lo[:, None], sel_lo[None, :]), 0).astype(jnp.float32) / CMP_BLOCK

    table_g = table.reshape(REL_BUCKETS, G, J)
    table_gt = jnp.transpose(table_g, (1, 0, 2))
    ks_t = jnp.transpose(ks, (0, 2, 1, 3))
    vs_t = jnp.transpose(vs, (0, 2, 1, 3))
    kw_pad = jnp.pad(kw, ((0, 0), (WINDOW, 0), (0, 0), (0, 0)))
    vw_pad = jnp.pad(vw, ((0, 0), (WINDOW, 0), (0, 0), (0, 0)))
    bi = jnp.arange(b)[:, None, None, None]
    gi = jnp.arange(G)[None, :, None, None]
    blk_n = jnp.arange(n_sel)

    def block(qb):
        start = qb * NSA_QBLOCK
        t = start + jnp.arange(NSA_QBLOCK)
        qblk = lax.dynamic_slice_in_dim(q, start, NSA_QBLOCK, axis=1)

        dist_c = t[:, None] - cmp_end[None, :]
        m_c = dist_c >= 0
        bias_c = jnp.transpose(table_g[rel_bucket(dist_c)], (2, 3, 0, 1))
        s_c = jnp.einsum('bqgjd,bcgd->bgjqc', qblk, k_cmp).astype(jnp.float32) + bias_c
        p_c = jax.nn.softmax(jnp.where(m_c, s_c, NEG), axis=-1) * jnp.any(m_c, axis=-1)[:, None]
        o_c = jnp.einsum('bgjqc,bcgd->bqgjd', p_c.astype(v_cmp.dtype), v_cmp)

        imp = jnp.einsum('bgjqc,cn->bgqn', p_c, overlap)
        cur = t // SEL_BLOCK
        forced = (blk_n[None, :] == 0) | (blk_n[None, :] == cur[:, None]) | (blk_n[None, :] == cur[:, None] - 1)
        future = blk_n[None, :] > cur[:, None]
        imp = jnp.where(forced, BIG, jnp.where(future, -BIG, imp))
        _, sel = lax.top_k(imp, topk)
        pos = (sel[..., None] * SEL_BLOCK + jnp.arange(SEL_BLOCK)).reshape(b, G, NSA_QBLOCK, topk * SEL_BLOCK)
        k_g = ks_t[bi, gi, pos]
        v_g = vs_t[bi, gi, pos]
        dist_s = t[None, None, :, None] - pos
        bias_s = jnp.moveaxis(table_gt[gi, rel_bucket(dist_s)], -1, 2)
        s_s = jnp.einsum('bqgjd,bgqkd->bgjqk', qblk, k_g).astype(jnp.float32) + bias_s
        p_s = jax.nn.softmax(jnp.where((dist_s >= 0)[:, :, None], s_s, NEG), axis=-1)
        o_s = jnp.einsum('bgjqk,bgqkd->bqgjd', p_s.astype(v_g.dtype), v_g)

        k_win = lax.dynamic_slice_in_dim(kw_pad, start, NSA_QBLOCK + WINDOW, axis=1)
        v_win = lax.dynamic_slice_in_dim(vw_pad, start, NSA_QBLOCK + WINDOW, axis=1)
        pos_w = start - WINDOW + jnp.arange(NSA_QBLOCK + WINDOW)
        dist_w = t[:, None] - pos_w[None, :]
        m_w = (dist_w >= 0) & (dist_w < WINDOW) & (pos_w[None, :] >= 0)
        bias_w = jnp.transpose(table_g[rel_bucket(dist_w)], (2, 3, 0, 1))
        s_w = jnp.einsum('bqgjd,bkgd->bgjqk', qblk, k_win).astype(jnp.float32) + bias_w
        p_w = jax.nn.softmax(jnp.where(m_w, s_w, NEG), axis=-1)
        o_w = jnp.einsum('bgjqk,bkgd->bqgjd', p_w.astype(v_win.dtype), v_win)
        return o_c, o_s, o_w

    o_c, o_s, o_w = lax.map(block, jnp.arange(s // NSA_QBLOCK))
    o_c, o_s, o_w = [jnp.moveaxis(o, 0, 1).reshape(b, s, G, J, dh) for o in (o_c, o_s, o_w)]
    o = gate[..., 0:1] * o_c + gate[..., 1:2] * o_s + gate[..., 2:3] * o_w
    o = o.astype(u.dtype).reshape(b, s, NSA_WIDTH) * jax.nn.silu(z)
    return o @ w_out


def diff_mixer(u, table, w_in, lq1, lk1, lq2, lk2, subln, w_out, lambda_init):
    b, s, _ = u.shape
    H, d, dv = DIFF_HEADS, DIFF_HALF, DIFF_VDIM
    q, k, v, z = jnp.split(u @ w_in, 4, axis=-1)
    q = q.reshape(b, s, H, 2, d) * (d ** -0.5)
    k = k.reshape(b, s, H, 2, d)
    v = v.reshape(b, s, H, dv)
    lam = (jnp.exp(jnp.sum(lq1.astype(jnp.float32) * lk1.astype(jnp.float32)))
           - jnp.exp(jnp.sum(lq2.astype(jnp.float32) * lk2.astype(jnp.float32))) + lambda_init)
    table_h = table.reshape(REL_BUCKETS, H, 2)
    kpos = jnp.arange(s)

    def block(qb):
        start = qb * DIFF_QBLOCK
        t = start + jnp.arange(DIFF_QBLOCK)
        qblk = lax.dynamic_slice_in_dim(q, start, DIFF_QBLOCK, axis=1)
        dist = t[:, None] - kpos[None, :]
        bias = jnp.transpose(table_h[rel_bucket(dist)], (2, 3, 0, 1))
        sc = jnp.einsum('bqhmd,bkhmd->bhmqk', qblk, k).astype(jnp.float32) + bias
        p = jax.nn.softmax(jnp.where(dist >= 0, sc, NEG), axis=-1)
        a = p[:, :, 0] - lam * p[:, :, 1]
        return jnp.einsum('bhqk,bkhe->bqhe', a.astype(v.dtype), v)

    o = lax.map(block, jnp.arange(s // DIFF_QBLOCK))
    o = jnp.moveaxis(o, 0, 1).reshape(b, s, H, dv)
    o = rmsnorm(o, subln.reshape(H, dv)) * (1.0 - lambda_init)
    o = o.reshape(b, s, DIFF_WIDTH) * jax.nn.silu(z)
    return o @ w_out


def setup_inputs(seed: int = 0) -> dict:
    key = jax.random.key(seed)
    ks = jax.random.split(key, 20)
    nrm = jax.random.normal
    f32 = jnp.float32
    A, Bn = N_NSA_LAYERS, N_DIFF_LAYERS
    ld = CMP_BLOCK * NSA_HEAD_DIM
    return {
        "x": nrm(ks[0], (BATCH, SEQ, D_MODEL), f32),
        "rel_bias_table": 0.3 * nrm(ks[1], (REL_BUCKETS, N_BIAS_MAPS), f32),
        "norm_pre": 1.0 + 0.05 * nrm(ks[2], (DEPTH, D_MODEL), f32),
        "norm_post": 1.0 + 0.05 * nrm(ks[3], (DEPTH, D_MODEL), f32),
        "nsa_w_in": nrm(ks[4], (A, D_MODEL, NSA_IN), f32) * D_MODEL ** -0.5,
        "nsa_cmp_pe_k": 0.5 * nrm(ks[5], (A, CMP_BLOCK, NSA_HEAD_DIM), f32),
        "nsa_cmp_w1_k": nrm(ks[6], (A, ld, CMP_HIDDEN), f32) * ld ** -0.5,
        "nsa_cmp_w2_k": nrm(ks[7], (A, CMP_HIDDEN, NSA_HEAD_DIM), f32) * CMP_HIDDEN ** -0.5,
        "nsa_cmp_pe_v": 0.5 * nrm(ks[8], (A, CMP_BLOCK, NSA_HEAD_DIM), f32),
        "nsa_cmp_w1_v": nrm(ks[9], (A, ld, CMP_HIDDEN), f32) * ld ** -0.5,
        "nsa_cmp_w2_v": nrm(ks[10], (A, CMP_HIDDEN, NSA_HEAD_DIM), f32) * CMP_HIDDEN ** -0.5,
        "nsa_w_out": nrm(ks[11], (A, NSA_WIDTH, D_MODEL), f32) * NSA_WIDTH ** -0.5,
        "diff_w_in": nrm(ks[12], (Bn, D_MODEL, DIFF_IN), f32) * D_MODEL ** -0.5,
        "diff_lambda_q1": 0.1 * nrm(ks[13], (Bn, DIFF_HALF), f32),
        "diff_lambda_k1": 0.1 * nrm(ks[14], (Bn, DIFF_HALF), f32),
        "diff_lambda_q2": 0.1 * nrm(ks[15], (Bn, DIFF_HALF), f32),
        "diff_lambda_k2": 0.1 * nrm(ks[16], (Bn, DIFF_HALF), f32),
        "diff_subln": 1.0 + 0.05 * nrm(ks[17], (Bn, DIFF_WIDTH), f32),
        "diff_w_out": nrm(ks[18], (Bn, DIFF_WIDTH, D_MODEL), f32) * DIFF_WIDTH ** -0.5,
    }


def reference(x, rel_bias_table, norm_pre, norm_post, nsa_w_in, nsa_cmp_pe_k, nsa_cmp_w1_k,
              nsa_cmp_w2_k, nsa_cmp_pe_v, nsa_cmp_w1_v, nsa_cmp_w2_v, nsa_w_out, diff_w_in,
              diff_lambda_q1, diff_lambda_k1, diff_lambda_q2, diff_lambda_k2, diff_subln, diff_w_out):
    for i in range(DEPTH):
        u = rmsnorm(x, norm_pre[i])
        j = i // N_MIXERS
        if i % N_MIXERS == 0:
            y = nsa_mixer(u, rel_bias_table, nsa_w_in[j], nsa_cmp_pe_k[j], nsa_cmp_w1_k[j],
                          nsa_cmp_w2_k[j], nsa_cmp_pe_v[j], nsa_cmp_w1_v[j], nsa_cmp_w2_v[j],
                          nsa_w_out[j])
        else:
            lambda_init = 0.8 - 0.6 * math.exp(-0.3 * i)
            y = diff_mixer(u, rel_bias_table, diff_w_in[j], diff_lambda_q1[j], diff_lambda_k1[j],
                           diff_lambda_q2[j], diff_lambda_k2[j], diff_subln[j], diff_w_out[j],
                           lambda_init)
        x = x + rmsnorm(y, norm_post[i])
    return x
```

```python
import math
from contextlib import ExitStack

import numpy as np
import ml_dtypes

import concourse.bass as bass
import concourse.mybir as mybir
from concourse.bass_utils import run_bass_kernel_spmd

F32 = mybir.dt.float32
BF16 = mybir.dt.bfloat16
AF = mybir.ActivationFunctionType
ALU = mybir.AluOpType
AX = mybir.AxisListType

D = 1024
NSA_IN = 3632
EPS = 1e-6
PEN = -30000.0
LAMBDA_INIT = 0.8 - 0.6 * math.exp(-0.3 * 1)


class Buf:
    __slots__ = ("name", "writers", "readers")

    def __init__(self, name=""):
        self.name = name
        self.writers = []
        self.readers = []


class Op:
    __slots__ = ("eng", "fn", "dma", "deps", "needs_sig", "sig")

    def __init__(self, eng, fn, dma):
        self.eng = eng
        self.fn = fn
        self.dma = dma
        self.deps = []
        self.needs_sig = False
        self.sig = None


ENGS = ("pe", "act", "dve", "pool", "sp")


class Prog:
    def __init__(self, nc, n_dma_sems=48):
        self.nc = nc
        self.ops = {e: [] for e in ENGS}
        self.n_dma_sems = n_dma_sems
        self.dma_last = [None] * n_dma_sems
        self.dma_cnt = [0] * n_dma_sems
        self.dma_rr = 0
        self.pending = {}

    def barrier(self):
        lasts = []
        for e in ENGS:
            for op in reversed(self.ops[e]):
                if not op.dma:
                    op.needs_sig = True
                    lasts.append(op)
                    break
        for j in range(self.n_dma_sems):
            if self.dma_last[j] is not None:
                lasts.append(self.dma_last[j])
        self.pending = {e: list(lasts) for e in ENGS}

    def add(self, eng, fn, reads=(), writes=(), dma=False, join=False):
        op = Op(eng, fn, dma)
        if self.pending.get(eng):
            op.deps.extend(self.pending.pop(eng))
        for b in reads:
            for w in b.writers:
                self._dep(w, op, True)
        for b in writes:
            if not join:
                for w in b.writers:
                    self._dep(w, op, False)
            for r in b.readers:
                self._dep(r, op, False)
        for b in writes:
            if join:
                b.writers.append(op)
            else:
                b.writers = [op]
                b.readers = []
        for b in reads:
            b.readers.append(op)
        if dma:
            j = self.dma_rr
            self.dma_rr = (j + 1) % self.n_dma_sems
            prev = self.dma_last[j]
            if prev is not None:
                op.deps.append(prev)
            self.dma_cnt[j] += 1
            op.sig = (("d", j), 16 * self.dma_cnt[j])
            op.needs_sig = True
            self.dma_last[j] = op
        self.ops[eng].append(op)
        return op

    def _dep(self, p, c, raw):
        if p is c:
            return
        if (not p.dma) and (not c.dma) and p.eng == c.eng:
            if p.eng == "pe" or not raw:
                return
        p.needs_sig = True
        c.deps.append(p)

    def emit(self):
        nc = self.nc
        with ExitStack() as es:
            esem = {e: es.enter_context(nc.semaphore("s_" + e)) for e in ENGS}
            dsem = [es.enter_context(nc.semaphore("d%d" % j)) for j in range(self.n_dma_sems)]
            for e in ENGS:
                cnt = 0
                for op in self.ops[e]:
                    if op.dma:
                        continue
                    if op.needs_sig:
                        cnt += 1
                        op.sig = (("e", e), cnt)

            def semof(key):
                return esem[key[1]] if key[0] == "e" else dsem[key[1]]

            block = es.enter_context(nc.Block())
            engobj = {"pe": "tensor", "act": "scalar", "dve": "vector", "pool": "gpsimd", "sp": "sync"}

            def make(e):
                def body(eng):
                    waited = {}
                    for op in self.ops[e]:
                        need = {}
                        for p in op.deps:
                            k, v = p.sig
                            if need.get(k, 0) < v:
                                need[k] = v
                        for k, v in need.items():
                            if waited.get(k, 0) >= v:
                                continue
                            eng.wait_ge(semof(k), v)
                            waited[k] = v
                        ins = op.fn(eng)
                        if op.needs_sig:
                            k, v = op.sig
                            ins.then_inc(semof(k), 16 if op.dma else 1)
                    if e == "sp":
                        for j in range(self.n_dma_sems):
                            if self.dma_cnt[j] > 0:
                                eng.wait_ge(dsem[j], 16 * self.dma_cnt[j])
                return body

            for e in ENGS:
                getattr(block, engobj[e])(make(e))


def _rel_bucket(n):
    n = np.maximum(n, 0)
    nf = np.maximum(n, 1).astype(np.float32)
    large = 16 + (np.log(nf / np.float32(16)) / np.float32(math.log(8.0)) * np.float32(16)).astype(np.int32)
    large = np.minimum(large, 31)
    return np.where(n < 16, n, large)


def host_consts(S):
    NT = S // 128
    ncmp = S // 16 - 1
    bf = ml_dtypes.bfloat16
    c = {}
    c["c_ident"] = np.eye(128, dtype=np.float32).astype(bf)
    b = _rel_bucket(np.arange(128))
    oh = np.zeros((32, 128), np.float32)
    oh[b, np.arange(128)] = 1.0
    oh[31, :] -= 1.0
    c["c_onehot"] = oh
    cmp_lo = np.arange(ncmp) * 16
    sel_lo = np.arange(S // 64) * 64
    ov = np.clip(np.minimum(cmp_lo[:, None] + 32, sel_lo[None, :] + 64)
                 - np.maximum(cmp_lo[:, None], sel_lo[None, :]), 0, None).astype(np.float32) / 32.0
    ovp = np.zeros((127, 32), np.float32)
    ovp[:ncmp, :S // 64] = ov
    c["c_ovl"] = ovp.astype(bf)
    X = np.zeros((32, S), np.float32)
    X[np.arange(S) // 64, np.arange(S)] = 1.0
    c["c_X"] = X.astype(bf)
    k = np.arange(128)[:, None]
    q = np.arange(128)[None, :]
    c["c_M4"] = (q < k).astype(np.float32).astype(bf)
    t = np.arange(S)
    cur = t // 64
    n = np.arange(32)[None, :]
    forced = (n == 0) | (n == cur[:, None]) | (n == cur[:, None] - 1)
    future = n > cur[:, None]
    keep = (~(forced | future)).astype(np.float32)
    addc = np.where(forced, 1e9, np.where(future, -1e9, 0.0)).astype(np.float32)
    c["c_keep"] = np.ascontiguousarray(keep.reshape(NT, 128, 32).transpose(1, 0, 2))
    c["c_addc"] = np.ascontiguousarray(addc.reshape(NT, 128, 32).transpose(1, 0, 2))
    return c


def build(nseq, S, layers=(0, 1)):
    NT = S // 128
    NQ = S // 512
    NCMP = S // 16 - 1
    nc = bass.Bass("TRN2", target_bir_lowering=False)
    p = Prog(nc)

    def din(name, shape, dt=F32):
        return nc.dram_tensor(name, list(shape), dt, kind="ExternalInput").ap()

    x_in = din("x", [nseq, S, D])
    table = din("rel_bias_table", [32, 16])
    norm_pre = din("norm_pre", [2, D])
    norm_post = din("norm_post", [2, D])
    nsa_w_in = din("nsa_w_in", [D, NSA_IN])
    pe_k = din("nsa_cmp_pe_k", [32, 64])
    w1_k = din("nsa_cmp_w1_k", [2048, 128])
    w2_k = din("nsa_cmp_w2_k", [128, 64])
    pe_v = din("nsa_cmp_pe_v", [32, 64])
    w1_v = din("nsa_cmp_w1_v", [2048, 128])
    w2_v = din("nsa_cmp_w2_v", [128, 64])
    nsa_w_out = din("nsa_w_out", [D, D])
    diff_w_in = din("diff_w_in", [D, 4096])
    lq1 = din("diff_lambda_q1", [1, 64])
    lk1 = din("diff_lambda_k1", [1, 64])
    lq2 = din("diff_lambda_q2", [1, 64])
    lk2 = din("diff_lambda_k2", [1, 64])
    subln = din("diff_subln", [1, D])
    diff_w_out = din("diff_w_out", [D, D])
    c_ident = din("c_ident", [128, 128], BF16)
    c_onehot = din("c_onehot", [32, 128])
    c_ovl = din("c_ovl", [127, 32], BF16)
    c_X = din("c_X", [32, S], BF16)
    c_M4 = din("c_M4", [128, 128], BF16)
    c_keep = din("c_keep", [128, NT, 32])
    c_addc = din("c_addc", [128, NT, 32])
    out = nc.dram_tensor("out", [nseq, S, D], F32, kind="ExternalOutput").ap()
    x1_d = nc.dram_tensor("x1_scr", [nseq, S, D], F32).ap()
    De = nc.dram_tensor("De_scr", [16, 128, 512], BF16).ap()
    Dc = nc.dram_tensor("Dc_scr", [16, 127, 4096], BF16).ap()
    B_x1 = [[Buf("x1d%d_%d" % (s, i)) for i in range(NT)] for s in range(nseq)]
    B_out = [[Buf("out%d_%d" % (s, i)) for i in range(NT)] for s in range(nseq)]
    B_De = Buf("De")
    B_Dc = Buf("Dc")
    NOB = Buf("const_in")

    es = ExitStack()
    cur_es = [es]
    import os as _os
    DEBUG = bool(_os.environ.get("KDEBUG"))
    dbg_seen = set()

    def dbg(name, tt, ap, dt=F32):
        if not DEBUG or name in dbg_seen:
            return
        dbg_seen.add(name)
        o = nc.dram_tensor("dbg_" + name, list(ap.shape), dt, kind="ExternalOutput").ap()
        p.add("sp", lambda e: e.dma_start(out=o, in_=ap), reads=[tt.b], writes=[Buf()], dma=True)

    class TT:
        def __init__(self, t, name):
            self.t = t
            self.b = Buf(name)

        def __getitem__(self, k):
            return self.t[k]

    def sb(name, shape, dt=F32):
        return TT(cur_es[0].enter_context(nc.sbuf_tensor(name, list(shape), dt)), name)

    banks = [TT(es.enter_context(nc.psum_tensor("bank%d" % i, [128, 512], F32)), "bank%d" % i) for i in range(8)]

    def DMA(eng, out_ap, in_ap, reads, writes, join=False, **kw):
        p.add(eng, lambda e: e.dma_start(out=out_ap, in_=in_ap, **kw), reads=reads, writes=writes, dma=True, join=join)

    def MM(out_ap, lhsT, rhs, start, stop, reads, writes, skip=False):
        if skip:
            p.add("pe", lambda e: e.matmul(out_ap, lhsT=lhsT, rhs=rhs, start=start, stop=stop, skip_group_check=True), reads=reads, writes=writes)
        else:
            p.add("pe", lambda e: e.matmul(out_ap, lhsT=lhsT, rhs=rhs, start=start, stop=stop), reads=reads, writes=writes)

    def TR(out_ap, in_ap, reads, writes):
        p.add("pe", lambda e: e.transpose(out=out_ap, in_=in_ap, identity=ident[:, :]), reads=list(reads) + [ident.b], writes=writes)

    def ACT(out_ap, in_ap, func, reads, writes, **kw):
        p.add("act", lambda e: e.activation(out=out_ap, in_=in_ap, func=func, **kw), reads=reads, writes=writes)

    def V(eng, name, reads, writes, *a, **kw):
        p.add(eng, lambda e: getattr(e, name)(*a, **kw), reads=reads, writes=writes)

    evac_rr = [0]

    def EVAC(out_ap, in_ap, reads, writes, scale=None):
        evac_rr[0] ^= 1
        if evac_rr[0]:
            if scale is None:
                ACT(out_ap, in_ap, AF.Copy, reads, writes)
            else:
                ACT(out_ap, in_ap, AF.Copy, reads, writes, scale=float(scale))
        else:
            if scale is None:
                V("dve", "tensor_copy", reads, writes, out=out_ap, in_=in_ap)
            else:
                V("dve", "tensor_scalar", reads, writes, out=out_ap, in0=in_ap, scalar1=float(scale), scalar2=None, op0=ALU.mult)

    ident = sb("ident", [128, 128], BF16)
    DMA("sp", ident[:, :], c_ident[:, :], [NOB], [ident.b])
    M4 = sb("M4", [128, 128], BF16)
    DMA("sp", M4[:, :], c_M4[:, :], [NOB], [M4.b])
    Ee = sb("Ee", [128, 16, 256], BF16)
    uT = sb("uT", [128, 8, S], BF16)
    oT = sb("oT", [128, 8, S], BF16)
    xt = [sb("xt%d" % i, [128, D]) for i in range(2)]
    ub = [sb("ub%d" % i, [128, D], BF16) for i in range(2)]
    stat = [sb("stat%d" % i, [128, 8]) for i in range(4)]
    PT = [sb("PT%d" % i, [128, 512], BF16) for i in range(4)]
    yout = [sb("yout0", [128, D])]
    cst = {}

    def setup_bias():
        tab = sb("tab", [32, 16])
        oh = sb("oh", [32, 128])
        fse = sb("fse", [16, 512], BF16)
        fsc = sb("fsc", [16, 4096], BF16)
        DMA("sp", tab[:, :], table[:, :], [NOB], [tab.b])
        DMA("sp", oh[:, :], c_onehot[:, :], [NOB], [oh.b])
        MM(banks[0][0:16, 0:128], tab[:, :], oh[:, :], True, True, [tab.b, oh.b], [banks[0].b])
        V("pool", "memset", [], [fse.b], fse[:, :], 0.0)
        V("pool", "memset", [fse.b], [fse.b], fse[:, 128:384], 1.0)
        V("pool", "memset", [], [fsc.b], fsc[:, :], 0.0)
        V("pool", "memset", [fsc.b], [fsc.b], fsc[:, 159:2064], 1.0)
        ACT(fse[:, 0:128], banks[0][0:16, 0:128], AF.Exp, [banks[0].b, fse.b], [fse.b])
        ACT(fsc[:, 31:159], banks[0][0:16, 0:128], AF.Exp, [banks[0].b, fsc.b], [fsc.b])
        DMA("sp", De, fse[:, :].unsqueeze(1).broadcast_to([16, 128, 512]), [fse.b], [B_De])
        DMA("sp", Dc, fsc[:, :].unsqueeze(1).broadcast_to([16, 127, 4096]), [fsc.b], [B_Dc])
        for h in range(16):
            DMA("sp", Ee[:, h, :], bass.AP(De.tensor, h * 128 * 512, [[511, 128], [1, 256]]), [B_De], [Ee.b], join=(h > 0))

    def load_norm_gains(l):
        cst["gpre"] = sb("gpre%d" % l, [128, D])
        cst["gpost"] = sb("gpost%d" % l, [128, D])
        DMA("sp", cst["gpre"][:, :], norm_pre[l:l + 1, :].partition_broadcast(128), [NOB], [cst["gpre"].b])
        DMA("sp", cst["gpost"][:, :], norm_post[l:l + 1, :].partition_broadcast(128), [NOB], [cst["gpost"].b])

    def Ec_src(h, c0, ncols):
        return bass.AP(Dc.tensor, h * 127 * 4096 + c0, [[4080, NCMP], [1, ncols]])

    STG_N = 1024
    stg = [sb("stg%d" % i, [128, STG_N]) for i in range(2)]
    stg_rr = [0]

    def LOADW(dst_ap, src_ap, dst_buf, join=False):
        shp = list(dst_ap.shape)
        P_ = shp[0]
        mid = 1
        for d_ in shp[1:-1]:
            mid *= d_
        last = shp[-1]
        step = max(1, STG_N // mid)
        bp = dst_ap.base_partition()
        first = True
        for c0 in range(0, last, step):
            c1 = min(last, c0 + step)
            n = mid * (c1 - c0)
            assert n <= STG_N, shp
            st_ = stg[stg_rr[0]]
            stg_rr[0] = (stg_rr[0] + 1) % len(stg)
            sv = st_[bp:bp + P_, 0:n]
            if len(shp) == 3:
                sv = sv.rearrange("p (a b) -> p a b", a=shp[1])
                d_ap, s_ap = dst_ap[:, :, c0:c1], src_ap[:, :, c0:c1]
            else:
                d_ap, s_ap = dst_ap[:, c0:c1], src_ap[:, c0:c1]
            DMA("sp", sv, s_ap, [NOB], [st_.b])
            p.add("pool", (lambda d_ap, sv: (lambda e: e.tensor_copy(out=d_ap, in_=sv)))(d_ap, sv), reads=[st_.b], writes=[dst_buf],
                  join=(join or not first))
            first = False

    stat_rr = [0]

    def get_stat():
        stat_rr[0] = (stat_rr[0] + 1) % 4
        return stat[stat_rr[0]]

    def phase_norm_T(src_ap_fn, src_bufs, g_tile):
        for i in range(NT):
            xb_ = xt[i % 2]
            u_ = ub[i % 2]
            DMA("sp", xb_[:, :], src_ap_fn(i), src_bufs(i), [xb_.b])
            st = get_stat()
            ACT(u_[:, :], xb_[:, :], AF.Square, [xb_.b], [u_.b, st.b], accum_out=st[:, 0:1])
            V("dve", "tensor_scalar", [st.b], [st.b], out=st[:, 1:2], in0=st[:, 0:1], scalar1=1.0 / D, scalar2=EPS, op0=ALU.mult, op1=ALU.add)
            ACT(st[:, 2:3], st[:, 1:2], AF.Sqrt, [st.b], [st.b])
            V("dve", "reciprocal", [st.b], [st.b], out=st[:, 3:4], in_=st[:, 2:3])
            V("dve", "scalar_tensor_tensor", [xb_.b, st.b, g_tile.b], [u_.b], out=u_[:, :], in0=xb_[:, :], scalar=st[:, 3:4], in1=g_tile[:, :], op0=ALU.mult, op1=ALU.mult)
            bk = banks[6 + (i % 2)]
            bkv = bk[:, :].bitcast(BF16)
            for c in range(8):
                TR(bkv[:, c * 128:(c + 1) * 128], u_[:, c * 128:(c + 1) * 128], [u_.b], [bk.b])
            EVAC(uT[:, :, i * 128:(i + 1) * 128], bkv.rearrange("p (c t) -> p c t", c=8), [bk.b], [uT.b])

    def proj_fm(wt, wb, out_ap_fn, out_buf, scale=None, bank_ids=(5, 6, 7)):
        for Q in range(NQ):
            bk = banks[bank_ids[Q % len(bank_ids)]]
            for c in range(8):
                MM(bk[:, :], wt[:, c, :], uT[:, c, Q * 512:(Q + 1) * 512], c == 0, c == 7, [wb, uT.b], [bk.b])
            EVAC(out_ap_fn(Q), bk[:, :], [bk.b], [out_buf], scale=scale)

    def phase_out(wbig, w_out_d, gp, res_fn, res_bufs, dst_fn, dst_bufs):
        wo3 = w_out_d.rearrange("(c p) n -> p c n", p=128)
        for q4 in range(4):
            LOADW(wbig[:, :, 256 * q4:256 * q4 + 256], wo3[:, :, 256 * q4:256 * q4 + 256], wbig.b, join=(q4 > 0))
        for i in range(NT):
            xb_ = xt[i % 2]
            DMA("sp", xb_[:, :], res_fn(i), res_bufs(i), [xb_.b])
            bk = [banks[4 + 2 * (i % 2)], banks[5 + 2 * (i % 2)]]
            for half in range(2):
                for c in range(8):
                    MM(bk[half][:, :], oT[:, c, i * 128:(i + 1) * 128], wbig[:, c, half * 512:(half + 1) * 512], c == 0, c == 7, [oT.b, wbig.b], [bk[half].b])
            st = get_stat()
            ACT(ub[0][:, 0:512], bk[0][:, :], AF.Square, [bk[0].b], [ub[0].b, st.b], accum_out=st[:, 0:1])
            ACT(ub[0][:, 512:1024], bk[1][:, :], AF.Square, [bk[1].b], [ub[0].b, st.b], accum_out=st[:, 1:2])
            V("dve", "tensor_tensor", [st.b], [st.b], out=st[:, 2:3], in0=st[:, 0:1], in1=st[:, 1:2], op=ALU.add)
            V("dve", "tensor_scalar", [st.b], [st.b], out=st[:, 3:4], in0=st[:, 2:3], scalar1=1.0 / D, scalar2=EPS, op0=ALU.mult, op1=ALU.add)
            ACT(st[:, 4:5], st[:, 3:4], AF.Sqrt, [st.b], [st.b])
            V("dve", "reciprocal", [st.b], [st.b], out=st[:, 5:6], in_=st[:, 4:5])
            yo = yout[0]
            for half in range(2):
                sl = slice(half * 512, (half + 1) * 512)
                V("dve", "scalar_tensor_tensor", [bk[half].b, st.b, gp.b], [yo.b], out=yo[:, sl], in0=bk[half][:, :], scalar=st[:, 5:6], in1=gp[:, sl], op0=ALU.mult, op1=ALU.mult)
            V("pool", "tensor_tensor", [yo.b, xb_.b], [yo.b], out=yo[:, :], in0=yo[:, :], in1=xb_[:, :], op=ALU.add)
            DMA("sp", dst_fn(i), yo[:, :], [yo.b], dst_bufs(i))


    class Stream:
        pass

    S_BANKS = [banks[0], banks[1], banks[2]]
    s_rr = [0]
    pt_rr = [0]
    LOOK = 2

    def run_streams(streams):
        pending = []

        def emit_pv(item):
            st_, kj, c0, c1, ptb = item
            for r in range(c0, c1):
                qi = 4 * st_.Q + r
                first = max(0, qi - 4) if st_.band else 0
                o_ap, o_tt = st_.O[r]
                if not hasattr(st_, "started"):
                    st_.started = set()
                is_first = id(o_tt) not in st_.started
                st_.started.add(id(o_tt))
                MM(o_ap, ptb[:, r * 128:(r + 1) * 128], st_.v_ap(kj), is_first, kj == qi,
                   [ptb.b] + st_.v_bufs, [o_tt.b], skip=True)
            if kj == st_.last_kj:
                st_.finalize(st_)

        for st_ in streams:
            Q = st_.Q
            kj_lo = max(0, 4 * Q - 4) if st_.band else 0
            st_.last_kj = 4 * Q + 3
            for kj in range(kj_lo, 4 * Q + 4):
                rd0 = kj - 4 * Q
                c0 = max(0, rd0)
                c1 = min(4, rd0 + 5) if st_.band else 4
                sbk = S_BANKS[s_rr[0]]
                s_rr[0] = (s_rr[0] + 1) % len(S_BANKS)
                ptb = PT[pt_rr[0]]
                pt_rr[0] = (pt_rr[0] + 1) % len(PT)
                cols = slice(c0 * 128, c1 * 128)
                gcols = slice(Q * 512 + c0 * 128, Q * 512 + c1 * 128)
                MM(sbk[:, cols], st_.k_ap(kj), st_.q_ap(gcols), True, st_.pen is None, st_.qk_bufs, [sbk.b])
                if st_.pen is not None:
                    MM(sbk[:, cols], cst["Xs"][0:32, kj * 128:(kj + 1) * 128], st_.pen[0:32, gcols], False, True,
                       [cst["Xs"].b, st_.pen_buf], [sbk.b])
                ACT(ptb[:, cols], sbk[:, cols], AF.Exp, [sbk.b], [ptb.b])
                if -1 <= rd0 <= 3:
                    lo = max(rd0, 0)
                    hi = min(rd0 + 2, 4)
                    eo = (lo - rd0) * 128
                    V("dve", "tensor_tensor", [ptb.b, Ee.b], [ptb.b], out=ptb[:, lo * 128:hi * 128], in0=ptb[:, lo * 128:hi * 128],
                      in1=Ee[:, st_.emap, eo:eo + (hi - lo) * 128], op=ALU.mult)
                if st_.band:
                    r4 = rd0 + 4
                    if 0 <= r4 <= 3:
                        V("dve", "tensor_tensor", [ptb.b, M4.b], [ptb.b], out=ptb[:, r4 * 128:(r4 + 1) * 128], in0=ptb[:, r4 * 128:(r4 + 1) * 128],
                          in1=M4[:, :], op=ALU.mult)
                pending.append((st_, kj, c0, c1, ptb))
                if len(pending) > LOOK:
                    emit_pv(pending.pop(0))
        while pending:
            emit_pv(pending.pop(0))

    def nsa_setup():
        t = {}
        load_norm_gains(0)
        cst["Xs"] = sb("Xs", [32, S], BF16)
        DMA("sp", cst["Xs"][:, :], c_X[:, :], [NOB], [cst["Xs"].b])
        t["keep"] = sb("keep", [128, NT, 32])
        t["addc"] = sb("addc", [128, NT, 32])
        DMA("sp", t["keep"][:, :, :], c_keep[:, :, :], [NOB], [t["keep"].b])
        DMA("sp", t["addc"][:, :, :], c_addc[:, :, :], [NOB], [t["addc"].b])
        t["w1"] = sb("w1", [128, 32, 128], BF16)
        for lh in range(2):
            ls = slice(16 * lh, 16 * lh + 16)
            LOADW(t["w1"][0:64, ls, :], w1_k.rearrange("(l d) h -> d l h", d=64)[:, ls, :], t["w1"].b, join=(lh > 0))
            LOADW(t["w1"][64:128, ls, :], w1_v.rearrange("(l d) h -> d l h", d=64)[:, ls, :], t["w1"].b, join=True)
        t["w2k"] = sb("w2k", [128, 2, 64], BF16)
        for e_ in range(2):
            LOADW(t["w2k"][:, e_, :], w2_k[:, :], t["w2k"].b, join=(e_ > 0))
        t["w2v"] = sb("w2v", [128, 64], BF16)
        LOADW(t["w2v"][:, :], w2_v[:, :], t["w2v"].b)
        pes = sb("pes", [32, 128])
        DMA("sp", pes[:, 0:64], pe_k[:, :], [NOB], [pes.b])
        DMA("sp", pes[:, 64:128], pe_v[:, :], [NOB], [pes.b], join=True)
        pesb = sb("pesb", [32, 128], BF16)
        V("dve", "tensor_copy", [pes.b], [pesb.b], out=pesb[:, :], in_=pes[:, :])
        peT = sb("peT", [128, 32], BF16)
        bkv = banks[3][:, :].bitcast(BF16)
        p.add("pe", lambda e: e.transpose(out=bkv[:, 0:32], in_=pesb[:, :], identity=ident[0:32, 0:32]), reads=[pesb.b, ident.b], writes=[banks[3].b])
        V("dve", "tensor_copy", [banks[3].b], [peT.b], out=peT[:, :], in_=bkv[:, 0:32])
        t["peh"] = sb("peh", [128, 2])
        for kv in range(2):
            ps_ = slice(64 * kv, 64 * kv + 64)
            for l in range(32):
                MM(banks[4 + kv][:, 0:1], t["w1"][ps_, l, :], peT[ps_, l:l + 1], l == 0, l == 31, [t["w1"].b, peT.b], [banks[4 + kv].b])
        for kv in range(2):
            V("dve", "tensor_copy", [banks[4 + kv].b], [t["peh"].b], out=t["peh"][:, kv:kv + 1], in_=banks[4 + kv][:, 0:1])
        t["Vca"] = sb("Vca", [127, 97], BF16)
        V("pool", "memset", [], [t["Vca"].b], t["Vca"][:, :], 1.0)
        DMA("sp", t["Vca"][:, 65:97], c_ovl[:, :], [t["Vca"].b], [t["Vca"].b])
        t["wq"] = sb("wq", [128, 8, 256], BF16)
        t["wc"] = sb("wc", [128, 8, 128], BF16)
        t["wk"] = sb("wk", [128, 8, 256], BF16)
        t["wv"] = sb("wv", [128, 8, 140], BF16)
        t["wz"] = sb("wz", [128, 8, 256], BF16)
        t["qT"] = [sb("qT%d" % i, [128, S], BF16) for i in range(2)]
        t["cT"] = sb("cT", [128, S], BF16)
        t["ksT"] = sb("ksT", [128, S], BF16)
        t["kwT"] = sb("kwT", [128, S], BF16)
        t["vsa"] = sb("vsa", [128, NT, 65], BF16)
        t["vwa"] = sb("vwa", [128, NT, 65], BF16)
        V("pool", "memset", [], [t["vsa"].b], t["vsa"][:, :, :], 1.0)
        V("pool", "memset", [], [t["vwa"].b], t["vwa"][:, :, :], 1.0)
        t["gate"] = sb("gate", [128, NT, 12])
        t["penT"] = sb("penT", [32, S], BF16)
        accraw = sb("acc", [128, max(NT * 256, 4096)])
        t["acc"] = TT(accraw[:, 0:NT * 256].rearrange("p (i c) -> p i c", i=NT), "acc")
        t["acc"].b = accraw.b
        t["wbig"] = TT(accraw[:, 0:4096].bitcast(BF16).rearrange("p (c n) -> p c n", c=8), "wbig")
        t["wbig"].b = accraw.b
        t["ha"] = [sb("ha%d" % i, [128, 128], BF16) for i in range(2)]
        t["kcmp"] = sb("kcmp", [128, 128], BF16)
        t["Ec"] = [sb("Ec%d" % i, [127, 512], BF16) for i in range(4)]
        t["cm"] = sb("cm", [128, 4, 4, 97])
        t["sm"] = [sb("sm0", [128, 32]), sb("sm1", [128, 512]), sb("sm2", [128, 288])]
        t["penb"] = sb("penb", [128, 4, 32], BF16)
        t["tmp"] = [sb("tmpo%d" % i, [128, 4, 64]) for i in range(2)]
        t["zs"] = [sb("zs%d" % i, [128, 256]) for i in range(2)]
        t["og"] = [sb("og%d" % i, [128, 256], BF16) for i in range(2)]
        return t

    def nsa_layer(t, s):
        phase_norm_T(lambda i: x_in[s, i * 128:(i + 1) * 128, :], lambda i: [NOB], cst["gpre"])
        wv3 = nsa_w_in.rearrange("(c p) n -> p c n", p=128)
        for g in range(4):
            LOADW(t["wq"][:, :, :], wv3[:, :, 256 * g:256 * g + 256], t["wq"].b)
            LOADW(t["wc"][:, :, 0:64], wv3[:, :, 1024 + 64 * g:1024 + 64 * g + 64], t["wc"].b)
            LOADW(t["wc"][:, :, 64:128], wv3[:, :, 1280 + 64 * g:1280 + 64 * g + 64], t["wc"].b, join=True)
            for e_ in range(2):
                LOADW(t["wk"][:, :, 64 * e_:64 * e_ + 64], wv3[:, :, 1536 + 64 * g:1536 + 64 * g + 64], t["wk"].b, join=(e_ > 0))
                LOADW(t["wk"][:, :, 128 + 64 * e_:128 + 64 * e_ + 64], wv3[:, :, 2048 + 64 * g:2048 + 64 * g + 64], t["wk"].b, join=True)
            LOADW(t["wv"][:, :, 0:64], wv3[:, :, 1792 + 64 * g:1792 + 64 * g + 64], t["wv"].b)
            LOADW(t["wv"][:, :, 64:128], wv3[:, :, 2304 + 64 * g:2304 + 64 * g + 64], t["wv"].b, join=True)
            LOADW(t["wv"][:, :, 128:140], wv3[:, :, 2560 + 12 * g:2560 + 12 * g + 12], t["wv"].b, join=True)
            LOADW(t["wz"][:, :, :], wv3[:, :, 2608 + 256 * g:2608 + 256 * g + 256], t["wz"].b)
            for pr in range(2):
                qt_ = t["qT"][pr]
                proj_fm(t["wq"][:, :, 128 * pr:128 * pr + 128], t["wq"].b, lambda Q, qt_=qt_: qt_[:, Q * 512:(Q + 1) * 512], qt_.b, scale=0.125)
            proj_fm(t["wc"][:, :, :], t["wc"].b, lambda Q: t["cT"][:, Q * 512:(Q + 1) * 512], t["cT"].b)
            proj_fm(t["wk"][:, :, 0:128], t["wk"].b, lambda Q: t["ksT"][:, Q * 512:(Q + 1) * 512], t["ksT"].b)
            proj_fm(t["wk"][:, :, 128:256], t["wk"].b, lambda Q: t["kwT"][:, Q * 512:(Q + 1) * 512], t["kwT"].b)
            for i in range(NT):
                bk = banks[3 + (i % 2)]
                for c in range(8):
                    MM(bk[:, 0:140], uT[:, c, i * 128:(i + 1) * 128], t["wv"][:, c, :], c == 0, c == 7, [uT.b, t["wv"].b], [bk.b])
                V("dve", "tensor_copy", [bk.b], [t["vsa"].b], out=t["vsa"][:, i, 0:64], in_=bk[:, 0:64])
                V("dve", "tensor_copy", [bk.b], [t["vwa"].b], out=t["vwa"][:, i, 0:64], in_=bk[:, 64:128])
                ACT(t["gate"][:, i, :], bk[:, 128:140], AF.Sigmoid, [bk.b], [t["gate"].b])
            dbg("qT0", t["qT"][0], t["qT"][0][:, :], BF16)
            dbg("cT", t["cT"], t["cT"][:, :], BF16)
            dbg("ksT", t["ksT"], t["ksT"][:, :], BF16)
            dbg("vsa", t["vsa"], t["vsa"][:, :, :], BF16)
            dbg("gate", t["gate"], t["gate"][:, :, :])
            for kv in range(2):
                ps_ = slice(64 * kv, 64 * kv + 64)
                bk = banks[3 + kv]
                for l in range(32):
                    MM(bk[:, 0:NCMP], t["w1"][ps_, l, :], t["cT"][ps_, l:l + 16 * (NCMP - 1) + 1:16], l == 0, l == 31, [t["w1"].b, t["cT"].b], [bk.b])
                ACT(t["ha"][kv][:, 0:NCMP], bk[:, 0:NCMP], AF.Silu, [bk.b, t["peh"].b], [t["ha"][kv].b], bias=t["peh"][:, kv:kv + 1])
            MM(banks[5][:, 0:NCMP], t["w2k"][:, :, :].rearrange("p e d -> p (e d)"), t["ha"][0][:, 0:NCMP], True, True, [t["w2k"].b, t["ha"][0].b], [banks[5].b])
            V("dve", "tensor_copy", [banks[5].b], [t["kcmp"].b], out=t["kcmp"][:, 0:NCMP], in_=banks[5][:, 0:NCMP])
            MM(banks[6][0:NCMP, 0:64], t["ha"][1][:, 0:NCMP], t["w2v"][:, :], True, True, [t["w2v"].b, t["ha"][1].b], [banks[6].b])
            V("dve", "tensor_copy", [banks[6].b], [t["Vca"].b], out=t["Vca"][0:NCMP, 0:64], in_=banks[6][0:NCMP, 0:64])
            dbg("ha0", t["ha"][0], t["ha"][0][:, 0:NCMP], BF16)
            dbg("kcmp", t["kcmp"], t["kcmp"][:, 0:NCMP], BF16)
            dbg("Vca", t["Vca"], t["Vca"][0:NCMP, :], BF16)
            dbg("peh", t["peh"], t["peh"][:, :])
            ec_rr = 0
            for Q in range(NQ):
                for j in range(4):
                    h = 4 * g + j
                    pr, e_ = j // 2, j % 2
                    ps_ = slice(64 * e_, 64 * e_ + 64)
                    ec = t["Ec"][ec_rr % 4]
                    ec_rr += 1
                    DMA("sp", ec[0:NCMP, :], Ec_src(h, Q * 512, 512), [B_Dc], [ec.b])
                    sbk = S_BANKS[s_rr[0]]
                    s_rr[0] = (s_rr[0] + 1) % 3
                    ptb = PT[pt_rr[0]]
                    pt_rr[0] = (pt_rr[0] + 1) % len(PT)
                    MM(sbk[0:NCMP, :], t["kcmp"][ps_, 0:NCMP], t["qT"][pr][ps_, Q * 512:(Q + 1) * 512], True, True, [t["kcmp"].b, t["qT"][pr].b], [sbk.b])
                    ACT(ptb[0:NCMP, :], sbk[0:NCMP, :], AF.Exp, [sbk.b], [ptb.b])
                    V("dve", "tensor_tensor", [ptb.b, ec.b], [ptb.b], out=ptb[0:NCMP, :], in0=ptb[0:NCMP, :], in1=ec[0:NCMP, :], op=ALU.mult)
                    ob = banks[3 + (j % 2)]
                    for r in range(4):
                        MM(ob[:, r * 97:(r + 1) * 97], ptb[0:NCMP, r * 128:(r + 1) * 128], t["Vca"][0:NCMP, :], True, True, [ptb.b, t["Vca"].b], [ob.b])
                    V("dve", "tensor_copy", [ob.b], [t["cm"].b], out=t["cm"][:, :, j, :], in_=ob[:, 0:388].rearrange("p (r c) -> p r c", r=4))
                cm = t["cm"]
                sm0, sm1, sm2 = t["sm"]
                rinv = sm0[:, 0:16].rearrange("p (r j) -> p r j", r=4)
                coef = sm0[:, 16:32].rearrange("p (r j) -> p r j", r=4)
                V("dve", "tensor_scalar", [cm.b], [sm0.b], out=rinv, in0=cm[:, :, :, 64], scalar1=1e-30, scalar2=None, op0=ALU.add)
                V("dve", "reciprocal", [sm0.b], [sm0.b], out=rinv, in_=rinv)
                gv = t["gate"][:, 4 * Q:4 * Q + 4, :].rearrange("p r (j b) -> p r j b", b=3)
                V("dve", "tensor_tensor", [sm0.b, t["gate"].b], [sm0.b], out=coef, in0=rinv, in1=gv[:, :, :, 0], op=ALU.mult)
                accv = t["acc"][:, 4 * Q:4 * Q + 4, :].rearrange("p r (j d) -> p r j d", j=4)
                V("dve", "tensor_tensor", [cm.b, sm0.b], [t["acc"].b], out=accv, in0=cm[:, :, :, 0:64],
                  in1=coef.unsqueeze(3).broadcast_to([128, 4, 4, 64]), op=ALU.mult)
                impw = sm1[:, :].rearrange("p (r j n) -> p r j n", r=4, j=4)
                V("dve", "tensor_tensor", [cm.b, sm0.b], [sm1.b], out=impw, in0=cm[:, :, :, 65:97],
                  in1=rinv.unsqueeze(3).broadcast_to([128, 4, 4, 32]), op=ALU.mult)
                imp = sm2[:, 0:128].rearrange("p (r n) -> p r n", r=4)
                V("dve", "tensor_tensor", [sm1.b], [sm2.b], out=imp, in0=impw[:, :, 0, :], in1=impw[:, :, 1, :], op=ALU.add)
                V("dve", "tensor_tensor", [sm1.b, sm2.b], [sm2.b], out=imp, in0=imp, in1=impw[:, :, 2, :], op=ALU.add)
                V("dve", "tensor_tensor", [sm1.b, sm2.b], [sm2.b], out=imp, in0=imp, in1=impw[:, :, 3, :], op=ALU.add)
                V("dve", "tensor_tensor", [sm2.b, t["keep"].b], [sm2.b], out=imp, in0=imp, in1=t["keep"][:, 4 * Q:4 * Q + 4, :], op=ALU.mult)
                V("dve", "tensor_tensor", [sm2.b, t["addc"].b], [sm2.b], out=imp, in0=imp, in1=t["addc"][:, 4 * Q:4 * Q + 4, :], op=ALU.add)
                top8 = sm2[:, 128:160].rearrange("p (r e) -> p r e", r=4)
                for r in range(4):
                    V("dve", "max", [sm2.b], [sm2.b], out=top8[:, r, :], in_=imp[:, r, :])
                pen32 = sm2[:, 160:288].rearrange("p (r n) -> p r n", r=4)
                V("dve", "tensor_tensor", [sm2.b], [sm2.b], out=pen32, in0=imp, in1=top8[:, :, 7:8].broadcast_to([128, 4, 32]), op=ALU.is_lt)
                V("dve", "tensor_scalar", [sm2.b], [t["penb"].b], out=t["penb"][:, :, :], in0=pen32, scalar1=PEN, scalar2=None, op0=ALU.mult)
                bkp = banks[5]
                bkpv = bkp[:, :].bitcast(BF16)
                for r in range(4):
                    TR(bkpv[0:32, r * 128:(r + 1) * 128], t["penb"][:, r, :], [t["penb"].b], [bkp.b])
                V("dve", "tensor_copy", [bkp.b], [t["penT"].b], out=t["penT"][:, Q * 512:(Q + 1) * 512], in_=bkpv[0:32, 0:512])
            dbg("cm", t["cm"], t["cm"][:, :, :, :])
            dbg("penT", t["penT"], t["penT"][:, :], BF16)
            dbg("acc_c", t["acc"], t["acc"][:, :, :])
            dbg("Ec0", t["Ec"][0], t["Ec"][0][0:NCMP, :], BF16)
            streams = []
            o_rr = 0
            for Q in range(NQ):
                for j in range(4):
                    for br in (1, 2):
                        st_ = Stream()
                        st_.Q = Q
                        st_.band = (br == 2)
                        pr, e_ = j // 2, j % 2
                        ps_ = slice(64 * e_, 64 * e_ + 64)
                        qt_ = t["qT"][pr]
                        kt_ = t["ksT"] if br == 1 else t["kwT"]
                        va_ = t["vsa"] if br == 1 else t["vwa"]
                        st_.q_ap = lambda gc, qt_=qt_, ps_=ps_: qt_[ps_, gc]
                        st_.k_ap = lambda kj, kt_=kt_, ps_=ps_: kt_[ps_, kj * 128:(kj + 1) * 128]
                        st_.qk_bufs = [qt_.b, kt_.b]
                        st_.pen = t["penT"] if br == 1 else None
                        st_.pen_buf = t["penT"].b
                        st_.v_ap = lambda kj, va_=va_: va_[:, kj, :]
                        st_.v_bufs = [va_.b]
                        st_.emap = 4 * g + j
                        ob = banks[3 + (o_rr % 2)]
                        o_rr += 1
                        st_.O = [(ob[:, r * 65:(r + 1) * 65], ob) for r in range(4)]
                        st_.ob = ob
                        st_.j = j
                        st_.br = br

                        def fin(st_):
                            ob = st_.ob
                            Q, j, br = st_.Q, st_.j, st_.br
                            ov = ob[:, 0:260].rearrange("p (r c) -> p r c", r=4)
                            sm = get_stat()
                            V("dve", "reciprocal", [ob.b], [sm.b], out=sm[:, 0:4], in_=ov[:, :, 64])
                            gv = t["gate"][:, 4 * Q:4 * Q + 4, :].rearrange("p r (j b) -> p r j b", b=3)
                            V("dve", "tensor_tensor", [sm.b, t["gate"].b], [sm.b], out=sm[:, 4:8], in0=sm[:, 0:4], in1=gv[:, :, j, br], op=ALU.mult)
                            tm = t["tmp"][(2 * j + br) % 2]
                            V("dve", "tensor_tensor", [ob.b, sm.b], [tm.b], out=tm[:, :, :], in0=ov[:, :, 0:64],
                              in1=sm[:, 4:8].unsqueeze(2).broadcast_to([128, 4, 64]), op=ALU.mult)
                            accv = t["acc"][:, 4 * Q:4 * Q + 4, 64 * j:64 * j + 64]
                            V("pool", "tensor_tensor", [tm.b, t["acc"].b], [t["acc"].b], out=accv, in0=accv, in1=tm[:, :, :], op=ALU.add)
                        st_.finalize = fin
                        streams.append(st_)
            run_streams(streams)
            dbg("acc_f", t["acc"], t["acc"][:, :, :])
            for i in range(NT):
                bk = banks[5 + (i % 2)]
                for c in range(8):
                    MM(bk[:, 0:256], uT[:, c, i * 128:(i + 1) * 128], t["wz"][:, c, :], c == 0, c == 7, [uT.b, t["wz"].b], [bk.b])
                zs = t["zs"][i % 2]
                og = t["og"][i % 2]
                ACT(zs[:, :], bk[:, 0:256], AF.Silu, [bk.b], [zs.b])
                V("dve", "tensor_tensor", [zs.b, t["acc"].b], [og.b], out=og[:, :], in0=t["acc"][:, i, :], in1=zs[:, :], op=ALU.mult)
                bkv = bk[:, :].bitcast(BF16)
                for pr in range(2):
                    TR(bkv[:, 512 + pr * 128:512 + (pr + 1) * 128], og[:, pr * 128:(pr + 1) * 128], [og.b], [bk.b])
                EVAC(oT[:, 2 * g:2 * g + 2, i * 128:(i + 1) * 128], bkv[:, 512:768].rearrange("p (c t) -> p c t", c=2), [bk.b], [oT.b])
        dbg("oT", oT, oT[:, :, :], BF16)
        dbg("uT", uT, uT[:, :, :], BF16)
        dbg("Ee", Ee, Ee[:, :, :], BF16)
        dst, dstb = (x1_d, B_x1[s]) if 1 in layers else (out, B_out[s])
        phase_out(t["wbig"], nsa_w_out, cst["gpost"], lambda i: x_in[s, i * 128:(i + 1) * 128, :], lambda i: [NOB],
                  lambda i: dst[s, i * 128:(i + 1) * 128, :], lambda i: [dstb[i]])

    def diff_setup():
        t = {}
        load_norm_gains(1)
        t["wbig"] = sb("wbigd", [128, 8, 1024], BF16)
        lam4 = sb("lam4", [128, 4, 64])
        for idx, src in enumerate((lq1, lk1, lq2, lk2)):
            DMA("sp", lam4[:, idx, :], src[0:1, :].partition_broadcast(128), [NOB], [lam4.b], join=(idx > 0))
        lm = sb("lm", [128, 8])
        prod = sb("lprod", [128, 2, 64])
        V("dve", "tensor_tensor", [lam4.b], [prod.b], out=prod[:, 0, :], in0=lam4[:, 0, :], in1=lam4[:, 1, :], op=ALU.mult)
        V("dve", "tensor_tensor", [lam4.b], [prod.b], out=prod[:, 1, :], in0=lam4[:, 2, :], in1=lam4[:, 3, :], op=ALU.mult)
        V("dve", "reduce_sum", [prod.b], [lm.b], out=lm[:, 0:2], in_=prod[:, :, :], axis=AX.X)
        ACT(lm[:, 2:4], lm[:, 0:2], AF.Exp, [lm.b], [lm.b])
        V("dve", "tensor_tensor", [lm.b], [lm.b], out=lm[:, 4:5], in0=lm[:, 3:4], in1=lm[:, 2:3], op=ALU.subtract)
        V("dve", "tensor_scalar", [lm.b], [lm.b], out=lm[:, 5:6], in0=lm[:, 4:5], scalar1=-LAMBDA_INIT, scalar2=None, op0=ALU.add)
        t["lm"] = lm
        t["subln"] = sb("sublnb", [128, D])
        DMA("sp", t["subln"][:, :], subln[0:1, :].partition_broadcast(128), [NOB], [t["subln"].b])
        t["w"] = [sb("wd%d" % i, [128, 8, 512], BF16) for i in range(2)]
        t["qT"] = sb("dqT", [128, S], BF16)
        t["kT"] = sb("dkT", [128, S], BF16)
        t["va"] = sb("dva", [128, NT, 129], BF16)
        V("pool", "memset", [], [t["va"].b], t["va"][:, :, :], 1.0)
        t["od"] = sb("od", [128, NT, 128])
        t["zs"] = sb("dzs", [128, NT, 128])
        t["tmp"] = [sb("dtmp%d" % i, [128, 2, 128]) for i in range(2)]
        t["sq"] = sb("dsq", [128, NT, 128])
        t["rs"] = sb("drs", [128, 4, NT])
        t["og"] = sb("dog", [128, NT, 128], BF16)
        return t

    def diff_layer(t, s):
        phase_norm_T(lambda i: x1_d[s, i * 128:(i + 1) * 128, :], lambda i: [B_x1[s][i]], cst["gpre"])
        wv3 = diff_w_in.rearrange("(c p) n -> p c n", p=128)
        lm = t["lm"]
        for h in range(8):
            w = t["w"][h % 2]
            for part in range(4):
                LOADW(w[:, :, 128 * part:128 * part + 128], wv3[:, :, 1024 * part + 128 * h:1024 * part + 128 * h + 128], w.b, join=(part > 0))
            proj_fm(w[:, :, 0:128], w.b, lambda Q: t["qT"][:, Q * 512:(Q + 1) * 512], t["qT"].b, scale=0.125, bank_ids=(7,))
            proj_fm(w[:, :, 128:256], w.b, lambda Q: t["kT"][:, Q * 512:(Q + 1) * 512], t["kT"].b, bank_ids=(7,))
            for i4 in range(NT // 4):
                bk = banks[7]
                for r in range(4):
                    i = 4 * i4 + r
                    for c in range(8):
                        MM(bk[:, r * 128:(r + 1) * 128], uT[:, c, i * 128:(i + 1) * 128], w[:, c, 256:384], c == 0, c == 7, [uT.b, w.b], [bk.b])
                V("dve", "tensor_copy", [bk.b], [t["va"].b], out=t["va"][:, 4 * i4:4 * i4 + 4, 0:128], in_=bk[:, :].rearrange("p (r c) -> p r c", r=4))
                for r in range(4):
                    i = 4 * i4 + r
                    for c in range(8):
                        MM(bk[:, r * 128:(r + 1) * 128], uT[:, c, i * 128:(i + 1) * 128], w[:, c, 384:512], c == 0, c == 7, [uT.b, w.b], [bk.b])
                ACT(t["zs"][:, 4 * i4:4 * i4 + 4, :], bk[:, :].rearrange("p (r c) -> p r c", r=4), AF.Silu, [bk.b], [t["zs"].b])
            streams = []
            for Q in range(NQ):
                for m in range(2):
                    st_ = Stream()
                    st_.Q = Q
                    st_.band = False
                    ps_ = slice(64 * m, 64 * m + 64)
                    st_.q_ap = lambda gc, ps_=ps_: t["qT"][ps_, gc]
                    st_.k_ap = lambda kj, ps_=ps_: t["kT"][ps_, kj * 128:(kj + 1) * 128]
                    st_.qk_bufs = [t["qT"].b, t["kT"].b]
                    st_.pen = None
                    st_.pen_buf = None
                    st_.v_ap = lambda kj: t["va"][:, kj, :]
                    st_.v_bufs = [t["va"].b]
                    st_.emap = 2 * h + m
                    obs = [banks[3 + 2 * m], banks[4 + 2 * m]]
                    st_.O = [(obs[r // 2][:, (r % 2) * 129:(r % 2) * 129 + 129], obs[r // 2]) for r in range(4)]
                    st_.obs = obs
                    st_.m = m

                    def fin(st_):
                        Q, m = st_.Q, st_.m
                        for half in range(2):
                            ob = st_.obs[half]
                            ov = ob[:, 0:258].rearrange("p (r c) -> p r c", r=2)
                            i0 = 4 * Q + 2 * half
                            sm = get_stat()
                            V("dve", "reciprocal", [ob.b], [sm.b], out=sm[:, 0:2], in_=ov[:, :, 128])
                            if m == 0:
                                V("dve", "tensor_tensor", [ob.b, sm.b], [t["od"].b], out=t["od"][:, i0:i0 + 2, :], in0=ov[:, :, 0:128],
                                  in1=sm[:, 0:2].unsqueeze(2).broadcast_to([128, 2, 128]), op=ALU.mult)
                            else:
                                V("dve", "tensor_scalar", [sm.b, lm.b], [sm.b], out=sm[:, 2:4], in0=sm[:, 0:2], scalar1=lm[:, 5:6], scalar2=None, op0=ALU.mult)
                                tm = t["tmp"][half]
                                V("dve", "tensor_tensor", [ob.b, sm.b], [tm.b], out=tm[:, :, :], in0=ov[:, :, 0:128],
                                  in1=sm[:, 2:4].unsqueeze(2).broadcast_to([128, 2, 128]), op=ALU.mult)
                                V("pool", "tensor_tensor", [tm.b, t["od"].b], [t["od"].b], out=t["od"][:, i0:i0 + 2, :], in0=t["od"][:, i0:i0 + 2, :],
                                  in1=tm[:, :, :], op=ALU.add)
                    st_.finalize = fin
                    streams.append(st_)
            run_streams(streams)
            od, sq, rs = t["od"], t["sq"], t["rs"]
            V("pool", "tensor_tensor", [od.b], [sq.b], out=sq[:, :, :], in0=od[:, :, :], in1=od[:, :, :], op=ALU.mult)
            V("dve", "reduce_sum", [sq.b], [rs.b], out=rs[:, 0, :], in_=sq[:, :, :], axis=AX.X)
            V("dve", "tensor_scalar", [rs.b], [rs.b], out=rs[:, 1, :], in0=rs[:, 0, :], scalar1=1.0 / 128, scalar2=EPS, op0=ALU.mult, op1=ALU.add)
            ACT(rs[:, 2, :], rs[:, 1, :], AF.Sqrt, [rs.b], [rs.b])
            V("dve", "reciprocal", [rs.b], [rs.b], out=rs[:, 3, :], in_=rs[:, 2, :])
            V("dve", "tensor_tensor", [od.b, rs.b], [sq.b], out=sq[:, :, :], in0=od[:, :, :], in1=rs[:, 3, :].unsqueeze(2).broadcast_to([128, NT, 128]), op=ALU.mult)
            V("dve", "scalar_tensor_tensor", [sq.b, t["subln"].b], [sq.b], out=sq[:, :, :], in0=sq[:, :, :], scalar=1.0 - LAMBDA_INIT,
              in1=t["subln"][:, 128 * h:128 * h + 128].unsqueeze(1).broadcast_to([128, NT, 128]), op0=ALU.mult, op1=ALU.mult)
            V("dve", "tensor_tensor", [sq.b, t["zs"].b], [t["og"].b], out=t["og"][:, :, :], in0=sq[:, :, :], in1=t["zs"][:, :, :], op=ALU.mult)
            for i4 in range(NT // 4):
                bk = banks[7]
                bkv = bk[:, :].bitcast(BF16)
                for r in range(4):
                    TR(bkv[:, r * 128:(r + 1) * 128], t["og"][:, 4 * i4 + r, :], [t["og"].b], [bk.b])
                EVAC(oT[:, h, i4 * 512:(i4 + 1) * 512], bkv[:, 0:512], [bk.b], [oT.b])
        phase_out(t["wbig"], diff_w_out, cst["gpost"], lambda i: x1_d[s, i * 128:(i + 1) * 128, :], lambda i: [B_x1[s][i]],
                  lambda i: out[s, i * 128:(i + 1) * 128, :], lambda i: [B_out[s][i]])

    with es:
        with ExitStack() as es1:
            cur_es[0] = es1
            setup_bias()
        cur_es[0] = es
        p.barrier()
        if 0 in layers:
            with ExitStack() as es2:
                cur_es[0] = es2
                nt_ = nsa_setup()
                for s in range(nseq):
                    nsa_layer(nt_, s)
            cur_es[0] = es
            p.barrier()
        if 1 in layers:
            with ExitStack() as es3:
                cur_es[0] = es3
                dt_ = diff_setup()
                for s in range(nseq):
                    if 0 not in layers:
                        for i in range(NT):
                            xb_ = xt[i % 2]
                            DMA("sp", xb_[:, :], x_in[s, i * 128:(i + 1) * 128, :], [NOB], [xb_.b])
                            DMA("sp", x1_d[s, i * 128:(i + 1) * 128, :], xb_[:, :], [xb_.b], [B_x1[s][i]])
                    diff_layer(dt_, s)
            cur_es[0] = es
        p.emit()
    return nc


INPUT_NAMES = ["rel_bias_table", "norm_pre", "norm_post", "nsa_w_in", "nsa_cmp_pe_k", "nsa_cmp_w1_k", "nsa_cmp_w2_k",
               "nsa_cmp_pe_v", "nsa_cmp_w1_v", "nsa_cmp_w2_v", "nsa_w_out", "diff_w_in", "diff_lambda_q1", "diff_lambda_k1",
               "diff_lambda_q2", "diff_lambda_k2", "diff_subln", "diff_w_out"]


def make_in_maps(inputs, n_cores, nseq, S):
    consts = host_consts(S)
    shared = {}
    for k in INPUT_NAMES:
        a = np.ascontiguousarray(np.asarray(inputs[k], dtype=np.float32))
        if a.ndim == 3:
            a = a[0]
        elif k.startswith("diff_lambda") or k == "diff_subln":
            a = a.reshape(1, -1)
        shared[k] = np.ascontiguousarray(a)
    shared.update(consts)
    x = np.asarray(inputs["x"], dtype=np.float32)
    maps = []
    for c in range(n_cores):
        m = dict(shared)
        m["x"] = np.ascontiguousarray(x[c * nseq:(c + 1) * nseq])
        maps.append(m)
    return maps


def kernel(**inputs):
    x = np.asarray(inputs["x"])
    B, S, _ = x.shape
    n_cores = 8
    nseq = B // n_cores
    nc = build(nseq, S)
    in_maps = make_in_maps(inputs, n_cores, nseq, S)
    res = run_bass_kernel_spmd(nc, in_maps, core_ids=list(range(n_cores)))
    return np.concatenate([np.asarray(r["out"]) for r in res.results], axis=0).astype(np.float32)
```

```python
import math
from contextlib import ExitStack

import numpy as np
import ml_dtypes

import concourse.bass as bass
import concourse.mybir as mybir
from concourse.bass_utils import run_bass_kernel_spmd

F32 = mybir.dt.float32
BF16 = mybir.dt.bfloat16
AF = mybir.ActivationFunctionType
ALU = mybir.AluOpType
AX = mybir.AxisListType

D = 1024
NSA_IN = 3632
EPS = 1e-6
PEN = -30000.0
LAMBDA_INIT = 0.8 - 0.6 * math.exp(-0.3 * 1)


class Buf:
    __slots__ = ("name", "writers", "readers", "prev")

    def __init__(self, name=""):
        self.name = name
        self.writers = []
        self.readers = []
        self.prev = []


class Op:
    __slots__ = ("eng", "fn", "dma", "deps", "needs_sig", "sig")

    def __init__(self, eng, fn, dma):
        self.eng = eng
        self.fn = fn
        self.dma = dma
        self.deps = []
        self.needs_sig = False
        self.sig = None


ENGS = ("pe", "act", "dve", "pool", "sp")


class Prog:
    def __init__(self, nc, n_dma_sems=48):
        self.nc = nc
        self.ops = {e: [] for e in ENGS}
        self.n_dma_sems = n_dma_sems
        self.dma_last = [None] * n_dma_sems
        self.dma_cnt = [0] * n_dma_sems
        self.dma_rr = 0
        self.pending = {}

    def barrier(self):
        lasts = []
        for e in ENGS:
            for op in reversed(self.ops[e]):
                if not op.dma:
                    op.needs_sig = True
                    lasts.append(op)
                    break
        for j in range(self.n_dma_sems):
            if self.dma_last[j] is not None:
                lasts.append(self.dma_last[j])
        self.pending = {e: list(lasts) for e in ENGS}

    def add(self, eng, fn, reads=(), writes=(), dma=False, join=False):
        op = Op(eng, fn, dma)
        if self.pending.get(eng):
            op.deps.extend(self.pending.pop(eng))
        for b in reads:
            for w in b.writers:
                self._dep(w, op, True)
        for b in writes:
            if not join:
                for w in b.writers:
                    self._dep(w, op, False)
            else:
                for r in b.prev:
                    self._dep(r, op, False)
            for r in b.readers:
                self._dep(r, op, False)
        for b in writes:
            if join:
                b.writers.append(op)
            else:
                b.prev = list(b.writers) + list(b.readers)
                b.writers = [op]
                b.readers = []
        for b in reads:
            b.readers.append(op)
        if dma:
            j = self.dma_rr
            self.dma_rr = (j + 1) % self.n_dma_sems
            prev = self.dma_last[j]
            if prev is not None:
                op.deps.append(prev)
            self.dma_cnt[j] += 1
            op.sig = (("d", j), 16 * self.dma_cnt[j])
            op.needs_sig = True
            self.dma_last[j] = op
        self.ops[eng].append(op)
        return op

    def _dep(self, p, c, raw):
        if p is c:
            return
        if (not p.dma) and (not c.dma) and p.eng == c.eng:
            if p.eng == "pe" or not raw:
                return
        p.needs_sig = True
        c.deps.append(p)

    def emit(self):
        nc = self.nc
        with ExitStack() as es:
            esem = {e: es.enter_context(nc.semaphore("s_" + e)) for e in ENGS}
            dsem = [es.enter_context(nc.semaphore("d%d" % j)) for j in range(self.n_dma_sems)]
            for e in ENGS:
                cnt = 0
                for op in self.ops[e]:
                    if op.dma:
                        continue
                    if op.needs_sig:
                        cnt += 1
                        op.sig = (("e", e), cnt)

            def semof(key):
                return esem[key[1]] if key[0] == "e" else dsem[key[1]]

            block = es.enter_context(nc.Block())
            engobj = {"pe": "tensor", "act": "scalar", "dve": "vector", "pool": "gpsimd", "sp": "sync"}

            def make(e):
                def body(eng):
                    waited = {}
                    for op in self.ops[e]:
                        need = {}
                        for p in op.deps:
                            k, v = p.sig
                            if need.get(k, 0) < v:
                                need[k] = v
                        for k, v in need.items():
                            if waited.get(k, 0) >= v:
                                continue
                            eng.wait_ge(semof(k), v)
                            waited[k] = v
                        ins = op.fn(eng)
                        if op.needs_sig:
                            k, v = op.sig
                            ins.then_inc(semof(k), 16 if op.dma else 1)
                    if e == "sp":
                        for j in range(self.n_dma_sems):
                            if self.dma_cnt[j] > 0:
                                eng.wait_ge(dsem[j], 16 * self.dma_cnt[j])
                return body

            for e in ENGS:
                getattr(block, engobj[e])(make(e))


def _rel_bucket(n):
    n = np.maximum(n, 0)
    nf = np.maximum(n, 1).astype(np.float32)
    large = 16 + (np.log(nf / np.float32(16)) / np.float32(math.log(8.0)) * np.float32(16)).astype(np.int32)
    large = np.minimum(large, 31)
    return np.where(n < 16, n, large)


def host_consts(S):
    NT = S // 128
    ncmp = S // 16 - 1
    bf = ml_dtypes.bfloat16
    c = {}
    c["c_ident"] = np.eye(128, dtype=np.float32).astype(bf)
    b = _rel_bucket(np.arange(128))
    oh = np.zeros((32, 128), np.float32)
    oh[b, np.arange(128)] = 1.0
    oh[31, :] -= 1.0
    c["c_onehot"] = oh
    cmp_lo = np.arange(ncmp) * 16
    sel_lo = np.arange(S // 64) * 64
    ov = np.clip(np.minimum(cmp_lo[:, None] + 32, sel_lo[None, :] + 64)
                 - np.maximum(cmp_lo[:, None], sel_lo[None, :]), 0, None).astype(np.float32) / 32.0
    ovp = np.zeros((127, 32), np.float32)
    ovp[:ncmp, :S // 64] = ov
    c["c_ovl"] = ovp.astype(bf)
    X = np.zeros((32, S), np.float32)
    X[np.arange(S) // 64, np.arange(S)] = 1.0
    c["c_X"] = X.astype(bf)
    k = np.arange(128)[:, None]
    q = np.arange(128)[None, :]
    c["c_M4"] = (q < k).astype(np.float32).astype(bf)
    t = np.arange(S)
    cur = t // 64
    n = np.arange(32)[None, :]
    forced = (n == 0) | (n == cur[:, None]) | (n == cur[:, None] - 1)
    future = n > cur[:, None]
    keep = (~(forced | future)).astype(np.float32)
    addc = np.where(forced, 1e9, np.where(future, -1e9, 0.0)).astype(np.float32)
    c["c_keep"] = np.ascontiguousarray(keep.reshape(NT, 128, 32).transpose(1, 0, 2))
    c["c_addc"] = np.ascontiguousarray(addc.reshape(NT, 128, 32).transpose(1, 0, 2))
    return c


def build(nseq, S, layers=(0, 1)):
    NT = S // 128
    NQ = S // 512
    NCMP = S // 16 - 1
    nc = bass.Bass("TRN2", target_bir_lowering=False)
    p = Prog(nc)

    def din(name, shape, dt=F32):
        return nc.dram_tensor(name, list(shape), dt, kind="ExternalInput").ap()

    x_in = din("x", [nseq, S, D])
    table = din("rel_bias_table", [32, 16])
    norm_pre = din("norm_pre", [2, D])
    norm_post = din("norm_post", [2, D])
    nsa_w_in = din("nsa_w_in", [D, NSA_IN])
    pe_k = din("nsa_cmp_pe_k", [32, 64])
    w1_k = din("nsa_cmp_w1_k", [2048, 128])
    w2_k = din("nsa_cmp_w2_k", [128, 64])
    pe_v = din("nsa_cmp_pe_v", [32, 64])
    w1_v = din("nsa_cmp_w1_v", [2048, 128])
    w2_v = din("nsa_cmp_w2_v", [128, 64])
    nsa_w_out = din("nsa_w_out", [D, D])
    diff_w_in = din("diff_w_in", [D, 4096])
    lq1 = din("diff_lambda_q1", [1, 64])
    lk1 = din("diff_lambda_k1", [1, 64])
    lq2 = din("diff_lambda_q2", [1, 64])
    lk2 = din("diff_lambda_k2", [1, 64])
    subln = din("diff_subln", [1, D])
    diff_w_out = din("diff_w_out", [D, D])
    c_ident = din("c_ident", [128, 128], BF16)
    c_onehot = din("c_onehot", [32, 128])
    c_ovl = din("c_ovl", [127, 32], BF16)
    c_X = din("c_X", [32, S], BF16)
    c_M4 = din("c_M4", [128, 128], BF16)
    c_keep = din("c_keep", [128, NT, 32])
    c_addc = din("c_addc", [128, NT, 32])
    out = nc.dram_tensor("out", [nseq, S, D], F32, kind="ExternalOutput").ap()
    x1_d = nc.dram_tensor("x1_scr", [nseq, S, D], F32).ap()
    De = nc.dram_tensor("De_scr", [16, 128, 512], BF16).ap()
    Dc = nc.dram_tensor("Dc_scr", [16, 127, 4096], BF16).ap()
    B_x1 = [[Buf("x1d%d_%d" % (s, i)) for i in range(NT)] for s in range(nseq)]
    B_out = [[Buf("out%d_%d" % (s, i)) for i in range(NT)] for s in range(nseq)]
    B_De = Buf("De")
    B_Dc = Buf("Dc")
    NOB = Buf("const_in")

    es = ExitStack()
    cur_es = [es]
    import os as _os
    DEBUG = bool(_os.environ.get("KDEBUG"))
    dbg_seen = set()

    def dbg(name, tt, ap, dt=F32):
        if not DEBUG or name in dbg_seen:
            return
        dbg_seen.add(name)
        o = nc.dram_tensor("dbg_" + name, list(ap.shape), dt, kind="ExternalOutput").ap()
        p.add("sp", lambda e: e.dma_start(out=o, in_=ap), reads=[tt.b], writes=[Buf()], dma=True)

    class TT:
        def __init__(self, t, name):
            self.t = t
            self.b = Buf(name)

        def __getitem__(self, k):
            return self.t[k]

    def sb(name, shape, dt=F32):
        return TT(cur_es[0].enter_context(nc.sbuf_tensor(name, list(shape), dt)), name)

    banks = [TT(es.enter_context(nc.psum_tensor("bank%d" % i, [128, 512], F32)), "bank%d" % i) for i in range(8)]

    def DMA(eng, out_ap, in_ap, reads, writes, join=False, **kw):
        p.add(eng, lambda e: e.dma_start(out=out_ap, in_=in_ap, **kw), reads=reads, writes=writes, dma=True, join=join)

    def MM(out_ap, lhsT, rhs, start, stop, reads, writes, skip=False):
        if skip:
            p.add("pe", lambda e: e.matmul(out_ap, lhsT=lhsT, rhs=rhs, start=start, stop=stop, skip_group_check=True), reads=reads, writes=writes)
        else:
            p.add("pe", lambda e: e.matmul(out_ap, lhsT=lhsT, rhs=rhs, start=start, stop=stop), reads=reads, writes=writes)

    def TR(out_ap, in_ap, reads, writes):
        p.add("pe", lambda e: e.transpose(out=out_ap, in_=in_ap, identity=ident[:, :]), reads=list(reads) + [ident.b], writes=writes)

    def ACT(out_ap, in_ap, func, reads, writes, **kw):
        p.add("act", lambda e: e.activation(out=out_ap, in_=in_ap, func=func, **kw), reads=reads, writes=writes)

    def V(eng, name, reads, writes, *a, **kw):
        p.add(eng, lambda e: getattr(e, name)(*a, **kw), reads=reads, writes=writes)

    evac_rr = [0]

    def EVAC(out_ap, in_ap, reads, writes, scale=None):
        evac_rr[0] ^= 1
        if evac_rr[0]:
            if scale is None:
                ACT(out_ap, in_ap, AF.Copy, reads, writes)
            else:
                ACT(out_ap, in_ap, AF.Copy, reads, writes, scale=float(scale))
        else:
            if scale is None:
                V("dve", "tensor_copy", reads, writes, out=out_ap, in_=in_ap)
            else:
                V("dve", "tensor_scalar", reads, writes, out=out_ap, in0=in_ap, scalar1=float(scale), scalar2=None, op0=ALU.mult)

    ident = sb("ident", [128, 128], BF16)
    DMA("sp", ident[:, :], c_ident[:, :], [NOB], [ident.b])
    M4 = sb("M4", [128, 128], BF16)
    DMA("sp", M4[:, :], c_M4[:, :], [NOB], [M4.b])
    Ee = sb("Ee", [128, 16, 256], BF16)
    uT = sb("uT", [128, 8, S], BF16)
    oT = sb("oT", [128, 8, S], BF16)
    xt = [sb("xt%d" % i, [128, D]) for i in range(2)]
    ub = [sb("ub%d" % i, [128, D], BF16) for i in range(2)]
    stat = [sb("stat%d" % i, [128, 8]) for i in range(4)]
    PT = [sb("PT%d" % i, [128, 512], BF16) for i in range(4)]
    yout = [sb("yout0", [128, D])]
    cst = {}

    def setup_bias():
        tab = sb("tab", [32, 16])
        oh = sb("oh", [32, 128])
        fse = sb("fse", [16, 512], BF16)
        fsc = sb("fsc", [16, 4096], BF16)
        DMA("sp", tab[:, :], table[:, :], [NOB], [tab.b])
        DMA("sp", oh[:, :], c_onehot[:, :], [NOB], [oh.b])
        MM(banks[0][0:16, 0:128], tab[:, :], oh[:, :], True, True, [tab.b, oh.b], [banks[0].b])
        V("pool", "memset", [], [fse.b], fse[:, :], 0.0)
        V("pool", "memset", [fse.b], [fse.b], fse[:, 128:384], 1.0)
        V("pool", "memset", [], [fsc.b], fsc[:, :], 0.0)
        V("pool", "memset", [fsc.b], [fsc.b], fsc[:, 159:2064], 1.0)
        ACT(fse[:, 0:128], banks[0][0:16, 0:128], AF.Exp, [banks[0].b, fse.b], [fse.b])
        ACT(fsc[:, 31:159], banks[0][0:16, 0:128], AF.Exp, [banks[0].b, fsc.b], [fsc.b])
        DMA("sp", De, fse[:, :].unsqueeze(1).broadcast_to([16, 128, 512]), [fse.b], [B_De])
        DMA("sp", Dc, fsc[:, :].unsqueeze(1).broadcast_to([16, 127, 4096]), [fsc.b], [B_Dc])
        for h in range(16):
            DMA("sp", Ee[:, h, :], bass.AP(De.tensor, h * 128 * 512, [[511, 128], [1, 256]]), [B_De], [Ee.b], join=(h > 0))

    def load_norm_gains(l):
        cst["gpre"] = sb("gpre%d" % l, [128, D])
        cst["gpost"] = sb("gpost%d" % l, [128, D])
        DMA("sp", cst["gpre"][:, :], norm_pre[l:l + 1, :].partition_broadcast(128), [NOB], [cst["gpre"].b])
        DMA("sp", cst["gpost"][:, :], norm_post[l:l + 1, :].partition_broadcast(128), [NOB], [cst["gpost"].b])

    def Ec_src(h, c0, ncols):
        return bass.AP(Dc.tensor, h * 127 * 4096 + c0, [[4080, NCMP], [1, ncols]])

    STG_N = 1024
    stgs = {}
    cast_rr = [0]

    def CAST(out_ap, in_ap, reads, writes, join=False):
        e = ("dve", "pool", "act")[cast_rr[0] % 3]
        cast_rr[0] += 1
        if e == "act":
            p.add("act", lambda en: en.activation(out=out_ap, in_=in_ap, func=AF.Copy), reads=reads, writes=writes, join=join)
        else:
            p.add(e, lambda en: en.tensor_copy(out=out_ap, in_=in_ap), reads=reads, writes=writes, join=join)

    def LOADW(dst_ap, src_ap, dst_buf, join=False):
        stg = stgs["stg"]
        shp = list(dst_ap.shape)
        P_ = shp[0]
        mid = 1
        for d_ in shp[1:-1]:
            mid *= d_
        last = shp[-1]
        step = max(1, STG_N // mid)
        bp = dst_ap.base_partition()
        first = True
        for c0 in range(0, last, step):
            c1 = min(last, c0 + step)
            n = mid * (c1 - c0)
            assert n <= STG_N, shp
            st_ = stg[stgs["rr"]]
            stgs["rr"] = (stgs["rr"] + 1) % len(stg)
            sv = st_[bp:bp + P_, 0:n]
            if len(shp) == 3:
                sv = sv.rearrange("p (a b) -> p a b", a=shp[1])
                d_ap, s_ap = dst_ap[:, :, c0:c1], src_ap[:, :, c0:c1]
            else:
                d_ap, s_ap = dst_ap[:, c0:c1], src_ap[:, c0:c1]
            DMA("sp", sv, s_ap, [NOB], [st_.b])
            CAST(d_ap, sv, [st_.b], [dst_buf], join=(join or not first))
            first = False

    GA = 652
    WG = nc.dram_tensor("WG_scr", [4, 128, 8, 908], BF16).ap()
    WD = nc.dram_tensor("WD_scr", [8, 128, 8, 512], BF16).ap()
    WO = nc.dram_tensor("WO_scr", [2, 128, 8, 1024], BF16).ap()
    W1S = nc.dram_tensor("W1_scr", [128, 32, 128], BF16).ap()
    W2S = nc.dram_tensor("W2_scr", [128, 128], BF16).ap()
    B_WG = [Buf("WG%d" % g) for g in range(4)]
    B_WD = [Buf("WD%d" % h) for h in range(8)]
    B_WO = [Buf("WO%d" % l) for l in range(2)]
    B_W1 = Buf("W1S")
    B_W2 = Buf("W2S")

    def setup_weights():
        stgs["stg"] = [sb("stg%d" % i, [128, STG_N]) for i in range(4)]
        stgs["rr"] = 0
        asm = [sb("asm%d" % i, [128, 8, 1024], BF16) for i in range(2)]
        k = 0
        wv3 = nsa_w_in.rearrange("(c p) n -> p c n", p=128)
        if 0 in layers:
            for g in range(4):
                a_ = asm[k % 2]
                k += 1
                pieces = [(0, 256 * g, 256), (256, 1024 + 64 * g, 64), (320, 1280 + 64 * g, 64), (384, 1536 + 64 * g, 64),
                          (448, 2048 + 64 * g, 64), (512, 1792 + 64 * g, 64), (576, 2304 + 64 * g, 64), (640, 2560 + 12 * g, 12),
                          (652, 2608 + 256 * g, 256)]
                for pi, (d0, s0, n) in enumerate(pieces):
                    LOADW(a_[:, :, d0:d0 + n], wv3[:, :, s0:s0 + n], a_.b, join=(pi > 0))
                DMA("sp", WG[g], a_[:, :, 0:908], [a_.b], [B_WG[g]])
            a_ = asm[k % 2]
            k += 1
            for lh in range(2):
                ls = slice(16 * lh, 16 * lh + 16)
                LOADW(a_[0:64, :, :].rearrange("p c n -> p (c n)")[:, 0:4096].rearrange("p (l h) -> p l h", l=32)[:, ls, :],
                      w1_k.rearrange("(l d) h -> d l h", d=64)[:, ls, :], a_.b, join=(lh > 0))
                LOADW(a_[64:128, :, :].rearrange("p c n -> p (c n)")[:, 0:4096].rearrange("p (l h) -> p l h", l=32)[:, ls, :],
                      w1_v.rearrange("(l d) h -> d l h", d=64)[:, ls, :], a_.b, join=True)
            LOADW(a_[:, 4, 0:64], w2_k[:, :], a_.b, join=True)
            LOADW(a_[:, 4, 64:128], w2_v[:, :], a_.b, join=True)
            DMA("sp", W1S, a_[:, :, :].rearrange("p c n -> p (c n)")[:, 0:4096].rearrange("p (l h) -> p l h", l=32), [a_.b], [B_W1])
            DMA("sp", W2S, a_[:, 4, 0:128], [a_.b], [B_W2])
        if 1 in layers:
            dv3 = diff_w_in.rearrange("(c p) n -> p c n", p=128)
            for h in range(8):
                a_ = asm[k % 2]
                k += 1
                for part in range(4):
                    LOADW(a_[:, :, 128 * part:128 * part + 128], dv3[:, :, 1024 * part + 128 * h:1024 * part + 128 * h + 128], a_.b, join=(part > 0))
                DMA("sp", WD[h], a_[:, :, 0:512], [a_.b], [B_WD[h]])
        for l, w_ in enumerate((nsa_w_out, diff_w_out)):
            if l not in layers:
                continue
            a_ = asm[k % 2]
            k += 1
            wo3 = w_.rearrange("(c p) n -> p c n", p=128)
            for q4 in range(4):
                LOADW(a_[:, :, 256 * q4:256 * q4 + 256], wo3[:, :, 256 * q4:256 * q4 + 256], a_.b, join=(q4 > 0))
            DMA("sp", WO[l], a_[:, :, :], [a_.b], [B_WO[l]])

    stat_rr = [0]

    def get_stat():
        stat_rr[0] = (stat_rr[0] + 1) % 4
        return stat[stat_rr[0]]

    def phase_norm_T(src_ap_fn, src_bufs, g_tile):
        for i in range(NT):
            xb_ = xt[i % 2]
            u_ = ub[i % 2]
            DMA("sp", xb_[:, :], src_ap_fn(i), src_bufs(i), [xb_.b])
            st = get_stat()
            ACT(u_[:, :], xb_[:, :], AF.Square, [xb_.b], [u_.b, st.b], accum_out=st[:, 0:1])
            V("dve", "tensor_scalar", [st.b], [st.b], out=st[:, 1:2], in0=st[:, 0:1], scalar1=1.0 / D, scalar2=EPS, op0=ALU.mult, op1=ALU.add)
            ACT(st[:, 2:3], st[:, 1:2], AF.Sqrt, [st.b], [st.b])
            V("dve", "reciprocal", [st.b], [st.b], out=st[:, 3:4], in_=st[:, 2:3])
            V("dve", "scalar_tensor_tensor", [xb_.b, st.b, g_tile.b], [u_.b], out=u_[:, :], in0=xb_[:, :], scalar=st[:, 3:4], in1=g_tile[:, :], op0=ALU.mult, op1=ALU.mult)
            bk = banks[6 + (i % 2)]
            bkv = bk[:, :].bitcast(BF16)
            for c in range(8):
                TR(bkv[:, c * 128:(c + 1) * 128], u_[:, c * 128:(c + 1) * 128], [u_.b], [bk.b])
            EVAC(uT[:, :, i * 128:(i + 1) * 128], bkv.rearrange("p (c t) -> p c t", c=8), [bk.b], [uT.b])

    pf_rr = [0]

    def proj_fm(wt, wb, outs, scale=None, bank_ids=(5, 6, 7), M=128):
        for Q in range(NQ):
            bk = banks[bank_ids[pf_rr[0] % len(bank_ids)]]
            pf_rr[0] += 1
            for c in range(8):
                MM(bk[0:M, :], wt[:, c, :], uT[:, c, Q * 512:(Q + 1) * 512], c == 0, c == 7, [wb, uT.b], [bk.b])
            for (rs_, fn_, ob_) in outs:
                EVAC(fn_(Q), bk[rs_, :], [bk.b], [ob_], scale=scale)

    def phase_out(wbig, w_out_d, gp, res_fn, res_bufs, dst_fn, dst_bufs):
        if w_out_d is not None:
            DMA("sp", wbig[:, :, :], WO[w_out_d], [B_WO[w_out_d]], [wbig.b])
        for i in range(NT):
            xb_ = xt[i % 2]
            DMA("sp", xb_[:, :], res_fn(i), res_bufs(i), [xb_.b])
            bk = [banks[4 + 2 * (i % 2)], banks[5 + 2 * (i % 2)]]
            for half in range(2):
                for c in range(8):
                    MM(bk[half][:, :], oT[:, c, i * 128:(i + 1) * 128], wbig[:, c, half * 512:(half + 1) * 512], c == 0, c == 7, [oT.b, wbig.b], [bk[half].b])
            st = get_stat()
            ACT(ub[0][:, 0:512], bk[0][:, :], AF.Square, [bk[0].b], [ub[0].b, st.b], accum_out=st[:, 0:1])
            ACT(ub[0][:, 512:1024], bk[1][:, :], AF.Square, [bk[1].b], [ub[0].b, st.b], accum_out=st[:, 1:2])
            V("dve", "tensor_tensor", [st.b], [st.b], out=st[:, 2:3], in0=st[:, 0:1], in1=st[:, 1:2], op=ALU.add)
            V("dve", "tensor_scalar", [st.b], [st.b], out=st[:, 3:4], in0=st[:, 2:3], scalar1=1.0 / D, scalar2=EPS, op0=ALU.mult, op1=ALU.add)
            ACT(st[:, 4:5], st[:, 3:4], AF.Sqrt, [st.b], [st.b])
            V("dve", "reciprocal", [st.b], [st.b], out=st[:, 5:6], in_=st[:, 4:5])
            yo = yout[0]
            for half in range(2):
                sl = slice(half * 512, (half + 1) * 512)
                V("dve", "scalar_tensor_tensor", [bk[half].b, st.b, gp.b], [yo.b], out=yo[:, sl], in0=bk[half][:, :], scalar=st[:, 5:6], in1=gp[:, sl], op0=ALU.mult, op1=ALU.mult)
            V("pool", "tensor_tensor", [yo.b, xb_.b], [yo.b], out=yo[:, :], in0=yo[:, :], in1=xb_[:, :], op=ALU.add)
            DMA("sp", dst_fn(i), yo[:, :], [yo.b], dst_bufs(i))


    class Stream:
        pass

    S_BANKS = [banks[0], banks[1], banks[2]]
    s_rr = [0]
    pt_rr = [0]
    LOOK = 2

    def run_streams(streams):
        pending = []

        def emit_pv(item):
            st_, kj, c0, c1, ptb = item
            for r in range(c0, c1):
                qi = 4 * st_.Q + r
                first = max(0, qi - 4) if st_.band else 0
                o_ap, o_tt = st_.O[r]
                if not hasattr(st_, "started"):
                    st_.started = set()
                is_first = id(o_tt) not in st_.started
                st_.started.add(id(o_tt))
                MM(o_ap, ptb[:, r * 128:(r + 1) * 128], st_.v_ap(kj), is_first, kj == qi,
                   [ptb.b] + st_.v_bufs, [o_tt.b], skip=True)
            if kj == st_.last_kj:
                st_.finalize(st_)

        for st_ in streams:
            Q = st_.Q
            kj_lo = max(0, 4 * Q - 4) if st_.band else 0
            st_.last_kj = 4 * Q + 3
            for kj in range(kj_lo, 4 * Q + 4):
                rd0 = kj - 4 * Q
                c0 = max(0, rd0)
                c1 = min(4, rd0 + 5) if st_.band else 4
                sbk = S_BANKS[s_rr[0]]
                s_rr[0] = (s_rr[0] + 1) % len(S_BANKS)
                ptb = PT[pt_rr[0]]
                pt_rr[0] = (pt_rr[0] + 1) % len(PT)
                cols = slice(c0 * 128, c1 * 128)
                gcols = slice(Q * 512 + c0 * 128, Q * 512 + c1 * 128)
                MM(sbk[:, cols], st_.k_ap(kj), st_.q_ap(gcols), True, st_.pen is None, st_.qk_bufs, [sbk.b])
                if st_.pen is not None:
                    MM(sbk[:, cols], cst["Xs"][0:32, kj * 128:(kj + 1) * 128], st_.pen[0:32, gcols], False, True,
                       [cst["Xs"].b, st_.pen_buf], [sbk.b])
                ACT(ptb[:, cols], sbk[:, cols], AF.Exp, [sbk.b], [ptb.b])
                if -1 <= rd0 <= 3:
                    lo = max(rd0, 0)
                    hi = min(rd0 + 2, 4)
                    eo = (lo - rd0) * 128
                    V("dve", "tensor_tensor", [ptb.b, Ee.b], [ptb.b], out=ptb[:, lo * 128:hi * 128], in0=ptb[:, lo * 128:hi * 128],
                      in1=Ee[:, st_.emap, eo:eo + (hi - lo) * 128], op=ALU.mult)
                if st_.band:
                    r4 = rd0 + 4
                    if 0 <= r4 <= 3:
                        V("dve", "tensor_tensor", [ptb.b, M4.b], [ptb.b], out=ptb[:, r4 * 128:(r4 + 1) * 128], in0=ptb[:, r4 * 128:(r4 + 1) * 128],
                          in1=M4[:, :], op=ALU.mult)
                pending.append((st_, kj, c0, c1, ptb))
                if len(pending) > LOOK:
                    emit_pv(pending.pop(0))
        while pending:
            emit_pv(pending.pop(0))

    def nsa_setup():
        t = {}
        load_norm_gains(0)
        t["keep"] = sb("keep", [128, NT, 32])
        t["addc"] = sb("addc", [128, NT, 32])
        DMA("sp", t["keep"][:, :, :], c_keep[:, :, :], [NOB], [t["keep"].b])
        DMA("sp", t["addc"][:, :, :], c_addc[:, :, :], [NOB], [t["addc"].b])
        t["w1"] = sb("w1", [128, 32, 128], BF16)
        DMA("sp", t["w1"][:, :, :], W1S, [B_W1], [t["w1"].b])
        t["w2"] = sb("w2", [128, 128], BF16)
        DMA("sp", t["w2"][:, :], W2S, [B_W2], [t["w2"].b])
        t["w2k"] = sb("w2k", [128, 128], BF16)
        V("pool", "memset", [], [t["w2k"].b], t["w2k"][:, :], 0.0)
        V("pool", "tensor_copy", [t["w2"].b, t["w2k"].b], [t["w2k"].b], out=t["w2k"][:, 0:64], in_=t["w2"][:, 0:64])
        pes = sb("pes", [32, 128])
        DMA("sp", pes[:, 0:64], pe_k[:, :], [NOB], [pes.b])
        DMA("sp", pes[:, 64:128], pe_v[:, :], [NOB], [pes.b], join=True)
        pesb = sb("pesb", [32, 128], BF16)
        V("dve", "tensor_copy", [pes.b], [pesb.b], out=pesb[:, :], in_=pes[:, :])
        peT = sb("peT", [128, 32], BF16)
        bkv = banks[3][:, :].bitcast(BF16)
        p.add("pe", lambda e: e.transpose(out=bkv[:, 0:32], in_=pesb[:, :], identity=ident[0:32, 0:32]), reads=[pesb.b, ident.b], writes=[banks[3].b])
        V("dve", "tensor_copy", [banks[3].b], [peT.b], out=peT[:, :], in_=bkv[:, 0:32])
        t["peh"] = sb("peh", [128, 2])
        for kv in range(2):
            ps_ = slice(64 * kv, 64 * kv + 64)
            for l in range(32):
                MM(banks[4 + kv][:, 0:1], t["w1"][ps_, l, :], peT[ps_, l:l + 1], l == 0, l == 31, [t["w1"].b, peT.b], [banks[4 + kv].b])
        for kv in range(2):
            V("dve", "tensor_copy", [banks[4 + kv].b], [t["peh"].b], out=t["peh"][:, kv:kv + 1], in_=banks[4 + kv][:, 0:1])
        t["Vca"] = sb("Vca", [127, 97], BF16)
        V("pool", "memset", [], [t["Vca"].b], t["Vca"][:, :], 1.0)
        DMA("sp", t["Vca"][:, 65:97], c_ovl[:, :], [t["Vca"].b], [t["Vca"].b])
        t["wgA"] = sb("wgA", [128, 8, GA], BF16)
        t["wgZ"] = sb("wgZ", [128, 8, 256], BF16)
        t["qa"] = [sb("qa%d" % i, [128, S], BF16) for i in range(4)]
        for i in range(4):
            V("pool", "memset", [], [t["qa"][i].b], t["qa"][i][:, :], 0.0)
        t["cT"] = sb("cT", [128, S], BF16)
        t["ksx"] = sb("ksx", [128, S], BF16)
        V("pool", "memset", [], [t["ksx"].b], t["ksx"][:, :], 0.0)
        DMA("sp", t["ksx"][64:96, :], c_X[:, :], [t["ksx"].b], [t["ksx"].b])
        t["kwz"] = sb("kwz", [128, S], BF16)
        V("pool", "memset", [], [t["kwz"].b], t["kwz"][:, :], 0.0)
        t["vsa"] = sb("vsa", [128, NT, 65], BF16)
        t["vwa"] = sb("vwa", [128, NT, 65], BF16)
        V("pool", "memset", [], [t["vsa"].b], t["vsa"][:, :, :], 1.0)
        V("pool", "memset", [], [t["vwa"].b], t["vwa"][:, :, :], 1.0)
        t["gate"] = sb("gate", [128, NT, 12])
        accraw = sb("acc", [128, max(NT * 256, 4096)])
        t["acc"] = TT(accraw[:, 0:NT * 256].rearrange("p (i c) -> p i c", i=NT), "acc")
        t["acc"].b = accraw.b
        t["wbig"] = TT(accraw[:, 0:4096].bitcast(BF16).rearrange("p (c n) -> p c n", c=8), "wbig")
        t["wbig"].b = accraw.b
        t["ha"] = [sb("ha%d" % i, [128, 128], BF16) for i in range(2)]
        t["kcmp"] = sb("kcmp", [128, 128], BF16)
        t["Ec"] = [sb("Ec%d" % i, [127, 512], BF16) for i in range(3)]
        t["cm"] = sb("cm", [128, 4, 4, 97])
        t["sm"] = [sb("sm0", [128, 32]), sb("sm1", [128, 512]), sb("sm2", [128, 288])]
        t["penb"] = sb("penb", [128, 4, 96], BF16)
        V("pool", "memset", [], [t["penb"].b], t["penb"][:, :, :], 0.0)
        t["tmp"] = [sb("tmpo%d" % i, [128, 4, 64]) for i in range(2)]
        t["zs"] = [sb("zs%d" % i, [128, 256]) for i in range(2)]
        t["og"] = [sb("og%d" % i, [128, 256], BF16) for i in range(2)]
        DMA("sp", t["wgA"][:, :, :], WG[0][:, :, 0:GA], [B_WG[0]], [t["wgA"].b])
        DMA("sp", t["wgZ"][:, :, :], WG[0][:, :, GA:908], [B_WG[0]], [t["wgZ"].b])
        return t

    def nsa_layer(t, s):
        phase_norm_T(lambda i: x_in[s, i * 128:(i + 1) * 128, :], lambda i: [NOB], cst["gpre"])
        wgA, wgZ = t["wgA"], t["wgZ"]
        for g in range(4):
            g_next = (g + 1) % 4
            has_next = (g < 3) or (s + 1 < nseq)
            for j in range(4):
                qa_ = t["qa"][j]
                proj_fm(wgA[:, :, 64 * j:64 * j + 64], wgA.b, [(slice(0, 64), (lambda Q, qa_=qa_: qa_[0:64, Q * 512:(Q + 1) * 512]), qa_.b)], scale=0.125, M=64)
            proj_fm(wgA[:, :, 256:384], wgA.b, [(slice(0, 128), (lambda Q: t["cT"][:, Q * 512:(Q + 1) * 512]), t["cT"].b)])
            proj_fm(wgA[:, :, 384:448], wgA.b, [(slice(0, 64), (lambda Q: t["ksx"][0:64, Q * 512:(Q + 1) * 512]), t["ksx"].b)], M=64)
            proj_fm(wgA[:, :, 448:512], wgA.b, [(slice(0, 64), (lambda Q: t["kwz"][0:64, Q * 512:(Q + 1) * 512]), t["kwz"].b)], M=64)
            for i in range(NT):
                bk = banks[3 + (i % 2)]
                for c in range(8):
                    MM(bk[:, 0:140], uT[:, c, i * 128:(i + 1) * 128], wgA[:, c, 512:652], c == 0, c == 7, [uT.b, wgA.b], [bk.b])
                V("dve", "tensor_copy", [bk.b], [t["vsa"].b], out=t["vsa"][:, i, 0:64], in_=bk[:, 0:64])
                V("dve", "tensor_copy", [bk.b], [t["vwa"].b], out=t["vwa"][:, i, 0:64], in_=bk[:, 64:128])
                ACT(t["gate"][:, i, :], bk[:, 128:140], AF.Sigmoid, [bk.b], [t["gate"].b])
            if has_next:
                DMA("sp", wgA[:, :, :], WG[g_next][:, :, 0:GA], [B_WG[g_next]], [wgA.b])
            dbg("qT0", t["qa"][0], t["qa"][0][:, :], BF16)
            dbg("cT", t["cT"], t["cT"][:, :], BF16)
            dbg("vsa", t["vsa"], t["vsa"][:, :, :], BF16)
            dbg("gate", t["gate"], t["gate"][:, :, :])
            for kv in range(2):
                ps_ = slice(64 * kv, 64 * kv + 64)
                bk = banks[3 + kv]
                for l in range(32):
                    MM(bk[:, 0:NCMP], t["w1"][ps_, l, :], t["cT"][ps_, l:l + 16 * (NCMP - 1) + 1:16], l == 0, l == 31, [t["w1"].b, t["cT"].b], [bk.b])
                ACT(t["ha"][kv][:, 0:NCMP], bk[:, 0:NCMP], AF.Silu, [bk.b, t["peh"].b], [t["ha"][kv].b], bias=t["peh"][:, kv:kv + 1])
            MM(banks[5][:, 0:NCMP], t["w2k"][:, :], t["ha"][0][:, 0:NCMP], True, True, [t["w2k"].b, t["ha"][0].b], [banks[5].b])
            V("dve", "tensor_copy", [banks[5].b], [t["kcmp"].b], out=t["kcmp"][:, 0:NCMP], in_=banks[5][:, 0:NCMP])
            MM(banks[6][0:NCMP, 0:64], t["ha"][1][:, 0:NCMP], t["w2"][:, 64:128], True, True, [t["w2"].b, t["ha"][1].b], [banks[6].b])
            V("dve", "tensor_copy", [banks[6].b], [t["Vca"].b], out=t["Vca"][0:NCMP, 0:64], in_=banks[6][0:NCMP, 0:64])
            dbg("kcmp", t["kcmp"], t["kcmp"][:, 0:NCMP], BF16)
            dbg("Vca", t["Vca"], t["Vca"][0:NCMP, :], BF16)
            ec_rr = 0
            for Q in range(NQ):
                for j in range(4):
                    h = 4 * g + j
                    qa_ = t["qa"][j]
                    ec = t["Ec"][ec_rr % 3]
                    ec_rr += 1
                    DMA("sp", ec[0:NCMP, :], Ec_src(h, Q * 512, 512), [B_Dc], [ec.b])
                    sbk = S_BANKS[s_rr[0]]
                    s_rr[0] = (s_rr[0] + 1) % 3
                    ptb = PT[pt_rr[0]]
                    pt_rr[0] = (pt_rr[0] + 1) % len(PT)
                    MM(sbk[0:NCMP, :], t["kcmp"][:, 0:NCMP], qa_[:, Q * 512:(Q + 1) * 512], True, True, [t["kcmp"].b, qa_.b], [sbk.b])
                    ACT(ptb[0:NCMP, :], sbk[0:NCMP, :], AF.Exp, [sbk.b], [ptb.b])
                    V("dve", "tensor_tensor", [ptb.b, ec.b], [ptb.b], out=ptb[0:NCMP, :], in0=ptb[0:NCMP, :], in1=ec[0:NCMP, :], op=ALU.mult)
                    ob = banks[3 + (j % 2)]
                    for r in range(4):
                        MM(ob[:, r * 97:(r + 1) * 97], ptb[0:NCMP, r * 128:(r + 1) * 128], t["Vca"][0:NCMP, :], True, True, [ptb.b, t["Vca"].b], [ob.b])
                    V("dve", "tensor_copy", [ob.b], [t["cm"].b], out=t["cm"][:, :, j, :], in_=ob[:, 0:388].rearrange("p (r c) -> p r c", r=4))
                cm = t["cm"]
                sm0, sm1, sm2 = t["sm"]
                rinv = sm0[:, 0:16].rearrange("p (r j) -> p r j", r=4)
                coef = sm0[:, 16:32].rearrange("p (r j) -> p r j", r=4)
                V("dve", "tensor_scalar", [cm.b], [sm0.b], out=rinv, in0=cm[:, :, :, 64], scalar1=1e-30, scalar2=None, op0=ALU.add)
                V("dve", "reciprocal", [sm0.b], [sm0.b], out=rinv, in_=rinv)
                gv = t["gate"][:, 4 * Q:4 * Q + 4, :].rearrange("p r (j b) -> p r j b", b=3)
                V("dve", "tensor_tensor", [sm0.b, t["gate"].b], [sm0.b], out=coef, in0=rinv, in1=gv[:, :, :, 0], op=ALU.mult)
                accv = t["acc"][:, 4 * Q:4 * Q + 4, :].rearrange("p r (j d) -> p r j d", j=4)
                V("dve", "tensor_tensor", [cm.b, sm0.b], [t["acc"].b], out=accv, in0=cm[:, :, :, 0:64],
                  in1=coef.unsqueeze(3).broadcast_to([128, 4, 4, 64]), op=ALU.mult)
                impw = sm1[:, :].rearrange("p (r j n) -> p r j n", r=4, j=4)
                V("dve", "tensor_tensor", [cm.b, sm0.b], [sm1.b], out=impw, in0=cm[:, :, :, 65:97],
                  in1=rinv.unsqueeze(3).broadcast_to([128, 4, 4, 32]), op=ALU.mult)
                imp = sm2[:, 0:128].rearrange("p (r n) -> p r n", r=4)
                V("dve", "tensor_tensor", [sm1.b], [sm2.b], out=imp, in0=impw[:, :, 0, :], in1=impw[:, :, 1, :], op=ALU.add)
                V("dve", "tensor_tensor", [sm1.b, sm2.b], [sm2.b], out=imp, in0=imp, in1=impw[:, :, 2, :], op=ALU.add)
                V("dve", "tensor_tensor", [sm1.b, sm2.b], [sm2.b], out=imp, in0=imp, in1=impw[:, :, 3, :], op=ALU.add)
                V("dve", "tensor_tensor", [sm2.b, t["keep"].b], [sm2.b], out=imp, in0=imp, in1=t["keep"][:, 4 * Q:4 * Q + 4, :], op=ALU.mult)
                V("dve", "tensor_tensor", [sm2.b, t["addc"].b], [sm2.b], out=imp, in0=imp, in1=t["addc"][:, 4 * Q:4 * Q + 4, :], op=ALU.add)
                top8 = sm2[:, 128:160].rearrange("p (r e) -> p r e", r=4)
                for r in range(4):
                    V("dve", "max", [sm2.b], [sm2.b], out=top8[:, r, :], in_=imp[:, r, :])
                pen32 = sm2[:, 160:288].rearrange("p (r n) -> p r n", r=4)
                V("dve", "tensor_tensor", [sm2.b], [sm2.b], out=pen32, in0=imp, in1=top8[:, :, 7:8].broadcast_to([128, 4, 32]), op=ALU.is_lt)
                V("dve", "tensor_scalar", [sm2.b], [t["penb"].b], out=t["penb"][:, :, 64:96], in0=pen32, scalar1=PEN, scalar2=None, op0=ALU.mult)
                bkp = banks[5]
                bkpv = bkp[:, :].bitcast(BF16)
                for r in range(4):
                    TR(bkpv[0:96, r * 128:(r + 1) * 128], t["penb"][:, r, :], [t["penb"].b], [bkp.b])
                cols = slice(Q * 512, (Q + 1) * 512)
                V("dve", "tensor_copy", [bkp.b], [t["qa"][0].b], out=t["qa"][0][64:96, cols], in_=bkpv[64:96, 0:512])
                for j in range(1, 4):
                    V("pool", "tensor_copy", [t["qa"][0].b], [t["qa"][j].b], out=t["qa"][j][64:96, cols], in_=t["qa"][0][64:96, cols])
            dbg("cm", t["cm"], t["cm"][:, :, :, :])
            dbg("acc_c", t["acc"], t["acc"][:, :, :])
            streams = []
            o_rr = 0
            for Q in range(NQ):
                for j in range(4):
                    for br in (1, 2):
                        st_ = Stream()
                        st_.Q = Q
                        st_.band = (br == 2)
                        qa_ = t["qa"][j]
                        kt_ = t["ksx"] if br == 1 else t["kwz"]
                        va_ = t["vsa"] if br == 1 else t["vwa"]
                        st_.q_ap = lambda gc, qa_=qa_: qa_[:, gc]
                        st_.k_ap = lambda kj, kt_=kt_: kt_[:, kj * 128:(kj + 1) * 128]
                        st_.qk_bufs = [qa_.b, kt_.b]
                        st_.pen = None
                        st_.pen_buf = None
                        st_.v_ap = lambda kj, va_=va_: va_[:, kj, :]
                        st_.v_bufs = [va_.b]
                        st_.emap = 4 * g + j
                        ob = banks[3 + (o_rr % 2)]
                        o_rr += 1
                        st_.O = [(ob[:, r * 65:(r + 1) * 65], ob) for r in range(4)]
                        st_.ob = ob
                        st_.j = j
                        st_.br = br

                        def fin(st_):
                            ob = st_.ob
                            Q, j, br = st_.Q, st_.j, st_.br
                            ov = ob[:, 0:260].rearrange("p (r c) -> p r c", r=4)
                            sm = get_stat()
                            V("dve", "reciprocal", [ob.b], [sm.b], out=sm[:, 0:4], in_=ov[:, :, 64])
                            gv = t["gate"][:, 4 * Q:4 * Q + 4, :].rearrange("p r (j b) -> p r j b", b=3)
                            V("dve", "tensor_tensor", [sm.b, t["gate"].b], [sm.b], out=sm[:, 4:8], in0=sm[:, 0:4], in1=gv[:, :, j, br], op=ALU.mult)
                            tm = t["tmp"][(2 * j + br) % 2]
                            V("dve", "tensor_tensor", [ob.b, sm.b], [tm.b], out=tm[:, :, :], in0=ov[:, :, 0:64],
                              in1=sm[:, 4:8].unsqueeze(2).broadcast_to([128, 4, 64]), op=ALU.mult)
                            accv = t["acc"][:, 4 * Q:4 * Q + 4, 64 * j:64 * j + 64]
                            V("pool", "tensor_tensor", [tm.b, t["acc"].b], [t["acc"].b], out=accv, in0=accv, in1=tm[:, :, :], op=ALU.add)
                        st_.finalize = fin
                        streams.append(st_)
            run_streams(streams)
            dbg("acc_f", t["acc"], t["acc"][:, :, :])
            for i in range(NT):
                bk = banks[5 + (i % 2)]
                for c in range(8):
                    MM(bk[:, 0:256], uT[:, c, i * 128:(i + 1) * 128], wgZ[:, c, :], c == 0, c == 7, [uT.b, wgZ.b], [bk.b])
                zs = t["zs"][i % 2]
                og = t["og"][i % 2]
                ACT(zs[:, :], bk[:, 0:256], AF.Silu, [bk.b], [zs.b])
                V("dve", "tensor_tensor", [zs.b, t["acc"].b], [og.b], out=og[:, :], in0=t["acc"][:, i, :], in1=zs[:, :], op=ALU.mult)
                bkv = bk[:, :].bitcast(BF16)
                for pr in range(2):
                    TR(bkv[:, 512 + pr * 128:512 + (pr + 1) * 128], og[:, pr * 128:(pr + 1) * 128], [og.b], [bk.b])
                EVAC(oT[:, 2 * g:2 * g + 2, i * 128:(i + 1) * 128], bkv[:, 512:768].rearrange("p (c t) -> p c t", c=2), [bk.b], [oT.b])
            if has_next:
                DMA("sp", wgZ[:, :, :], WG[g_next][:, :, GA:908], [B_WG[g_next]], [wgZ.b])
        dbg("oT", oT, oT[:, :, :], BF16)
        dst, dstb = (x1_d, B_x1[s]) if 1 in layers else (out, B_out[s])
        phase_out(t["wbig"], 0, cst["gpost"], lambda i: x_in[s, i * 128:(i + 1) * 128, :], lambda i: [NOB],
                  lambda i: dst[s, i * 128:(i + 1) * 128, :], lambda i: [dstb[i]])

    def diff_setup():
        t = {}
        load_norm_gains(1)
        t["wbig"] = sb("wbigd", [128, 8, 1024], BF16)
        DMA("sp", t["wbig"][:, :, :], WO[1], [B_WO[1]], [t["wbig"].b])
        lam4 = sb("lam4", [128, 4, 64])
        for idx, src in enumerate((lq1, lk1, lq2, lk2)):
            DMA("sp", lam4[:, idx, :], src[0:1, :].partition_broadcast(128), [NOB], [lam4.b], join=(idx > 0))
        lm = sb("lm", [128, 8])
        prod = sb("lprod", [128, 2, 64])
        V("dve", "tensor_tensor", [lam4.b], [prod.b], out=prod[:, 0, :], in0=lam4[:, 0, :], in1=lam4[:, 1, :], op=ALU.mult)
        V("dve", "tensor_tensor", [lam4.b], [prod.b], out=prod[:, 1, :], in0=lam4[:, 2, :], in1=lam4[:, 3, :], op=ALU.mult)
        V("dve", "reduce_sum", [prod.b], [lm.b], out=lm[:, 0:2], in_=prod[:, :, :], axis=AX.X)
        ACT(lm[:, 2:4], lm[:, 0:2], AF.Exp, [lm.b], [lm.b])
        V("dve", "tensor_tensor", [lm.b], [lm.b], out=lm[:, 4:5], in0=lm[:, 3:4], in1=lm[:, 2:3], op=ALU.subtract)
        V("dve", "tensor_scalar", [lm.b], [lm.b], out=lm[:, 5:6], in0=lm[:, 4:5], scalar1=-LAMBDA_INIT, scalar2=None, op0=ALU.add)
        t["lm"] = lm
        t["subln"] = sb("sublnb", [128, D])
        DMA("sp", t["subln"][:, :], subln[0:1, :].partition_broadcast(128), [NOB], [t["subln"].b])
        t["w"] = [sb("wd%d" % i, [128, 8, 512], BF16) for i in range(2)]
        t["qT"] = sb("dqT", [128, S], BF16)
        t["kz"] = [sb("dkz%d" % i, [128, S], BF16) for i in range(2)]
        for i in range(2):
            V("pool", "memset", [], [t["kz"][i].b], t["kz"][i][:, :], 0.0)
        t["va"] = sb("dva", [128, NT, 129], BF16)
        V("pool", "memset", [], [t["va"].b], t["va"][:, :, :], 1.0)
        t["od"] = sb("od", [128, NT, 128])
        t["zs"] = sb("dzs", [128, NT, 128])
        t["tmp"] = [sb("dtmp%d" % i, [128, 2, 128]) for i in range(2)]
        t["sq"] = sb("dsq", [128, NT, 128])
        t["rs"] = sb("drs", [128, 4, NT])
        t["og"] = sb("dog", [128, NT, 128], BF16)
        return t

    def diff_layer(t, s):
        phase_norm_T(lambda i: x1_d[s, i * 128:(i + 1) * 128, :], lambda i: [B_x1[s][i]], cst["gpre"])
        wv3 = diff_w_in.rearrange("(c p) n -> p c n", p=128)
        lm = t["lm"]
        if s == 0:
            DMA("sp", t["w"][0][:, :, :], WD[0], [B_WD[0]], [t["w"][0].b])
        for h in range(8):
            w = t["w"][h % 2]
            if h < 7 or s + 1 < nseq:
                hn = (h + 1) % 8
                DMA("sp", t["w"][hn % 2][:, :, :], WD[hn], [B_WD[hn]], [t["w"][hn % 2].b])
            proj_fm(w[:, :, 0:128], w.b, [(slice(0, 128), (lambda Q: t["qT"][:, Q * 512:(Q + 1) * 512]), t["qT"].b)], scale=0.125, bank_ids=(7,))
            proj_fm(w[:, :, 128:256], w.b, [(slice(0, 64), (lambda Q: t["kz"][0][0:64, Q * 512:(Q + 1) * 512]), t["kz"][0].b),
                                            (slice(64, 128), (lambda Q: t["kz"][1][64:128, Q * 512:(Q + 1) * 512]), t["kz"][1].b)], bank_ids=(7,))
            for i4 in range(NT // 4):
                bk = banks[7]
                for r in range(4):
                    i = 4 * i4 + r
                    for c in range(8):
                        MM(bk[:, r * 128:(r + 1) * 128], uT[:, c, i * 128:(i + 1) * 128], w[:, c, 256:384], c == 0, c == 7, [uT.b, w.b], [bk.b])
                V("dve", "tensor_copy", [bk.b], [t["va"].b], out=t["va"][:, 4 * i4:4 * i4 + 4, 0:128], in_=bk[:, :].rearrange("p (r c) -> p r c", r=4))
                for r in range(4):
                    i = 4 * i4 + r
                    for c in range(8):
                        MM(bk[:, r * 128:(r + 1) * 128], uT[:, c, i * 128:(i + 1) * 128], w[:, c, 384:512], c == 0, c == 7, [uT.b, w.b], [bk.b])
                ACT(t["zs"][:, 4 * i4:4 * i4 + 4, :], bk[:, :].rearrange("p (r c) -> p r c", r=4), AF.Silu, [bk.b], [t["zs"].b])
            streams = []
            for Q in range(NQ):
                for m in range(2):
                    st_ = Stream()
                    st_.Q = Q
                    st_.band = False
                    kz_ = t["kz"][m]
                    st_.q_ap = lambda gc: t["qT"][:, gc]
                    st_.k_ap = lambda kj, kz_=kz_: kz_[:, kj * 128:(kj + 1) * 128]
                    st_.qk_bufs = [t["qT"].b, kz_.b]
                    st_.pen = None
                    st_.pen_buf = None
                    st_.v_ap = lambda kj: t["va"][:, kj, :]
                    st_.v_bufs = [t["va"].b]
                    st_.emap = 2 * h + m
                    obs = [banks[3 + 2 * m], banks[4 + 2 * m]]
                    st_.O = [(obs[r // 2][:, (r % 2) * 129:(r % 2) * 129 + 129], obs[r // 2]) for r in range(4)]
                    st_.obs = obs
                    st_.m = m

                    def fin(st_):
                        Q, m = st_.Q, st_.m
                        for half in range(2):
                            ob = st_.obs[half]
                            ov = ob[:, 0:258].rearrange("p (r c) -> p r c", r=2)
                            i0 = 4 * Q + 2 * half
                            sm = get_stat()
                            V("dve", "reciprocal", [ob.b], [sm.b], out=sm[:, 0:2], in_=ov[:, :, 128])
                            if m == 0:
                                V("dve", "tensor_tensor", [ob.b, sm.b], [t["od"].b], out=t["od"][:, i0:i0 + 2, :], in0=ov[:, :, 0:128],
                                  in1=sm[:, 0:2].unsqueeze(2).broadcast_to([128, 2, 128]), op=ALU.mult)
                            else:
                                V("dve", "tensor_scalar", [sm.b, lm.b], [sm.b], out=sm[:, 2:4], in0=sm[:, 0:2], scalar1=lm[:, 5:6], scalar2=None, op0=ALU.mult)
                                tm = t["tmp"][half]
                                V("dve", "tensor_tensor", [ob.b, sm.b], [tm.b], out=tm[:, :, :], in0=ov[:, :, 0:128],
                                  in1=sm[:, 2:4].unsqueeze(2).broadcast_to([128, 2, 128]), op=ALU.mult)
                                V("pool", "tensor_tensor", [tm.b, t["od"].b], [t["od"].b], out=t["od"][:, i0:i0 + 2, :], in0=t["od"][:, i0:i0 + 2, :],
                                  in1=tm[:, :, :], op=ALU.add)
                    st_.finalize = fin
                    streams.append(st_)
            run_streams(streams)
            od, sq, rs = t["od"], t["sq"], t["rs"]
            V("pool", "tensor_tensor", [od.b], [sq.b], out=sq[:, :, :], in0=od[:, :, :], in1=od[:, :, :], op=ALU.mult)
            V("dve", "reduce_sum", [sq.b], [rs.b], out=rs[:, 0, :], in_=sq[:, :, :], axis=AX.X)
            V("dve", "tensor_scalar", [rs.b], [rs.b], out=rs[:, 1, :], in0=rs[:, 0, :], scalar1=1.0 / 128, scalar2=EPS, op0=ALU.mult, op1=ALU.add)
            ACT(rs[:, 2, :], rs[:, 1, :], AF.Sqrt, [rs.b], [rs.b])
            V("dve", "reciprocal", [rs.b], [rs.b], out=rs[:, 3, :], in_=rs[:, 2, :])
            V("dve", "tensor_tensor", [od.b, rs.b], [sq.b], out=sq[:, :, :], in0=od[:, :, :], in1=rs[:, 3, :].unsqueeze(2).broadcast_to([128, NT, 128]), op=ALU.mult)
            V("dve", "scalar_tensor_tensor", [sq.b, t["subln"].b], [sq.b], out=sq[:, :, :], in0=sq[:, :, :], scalar=1.0 - LAMBDA_INIT,
              in1=t["subln"][:, 128 * h:128 * h + 128].unsqueeze(1).broadcast_to([128, NT, 128]), op0=ALU.mult, op1=ALU.mult)
            V("dve", "tensor_tensor", [sq.b, t["zs"].b], [t["og"].b], out=t["og"][:, :, :], in0=sq[:, :, :], in1=t["zs"][:, :, :], op=ALU.mult)
            for i4 in range(NT // 4):
                bk = banks[7]
                bkv = bk[:, :].bitcast(BF16)
                for r in range(4):
                    TR(bkv[:, r * 128:(r + 1) * 128], t["og"][:, 4 * i4 + r, :], [t["og"].b], [bk.b])
                EVAC(oT[:, h, i4 * 512:(i4 + 1) * 512], bkv[:, 0:512], [bk.b], [oT.b])
        phase_out(t["wbig"], None, cst["gpost"], lambda i: x1_d[s, i * 128:(i + 1) * 128, :], lambda i: [B_x1[s][i]],
                  lambda i: out[s, i * 128:(i + 1) * 128, :], lambda i: [B_out[s][i]])

    with es:
        with ExitStack() as es1:
            cur_es[0] = es1
            setup_bias()
            setup_weights()
        cur_es[0] = es
        p.barrier()
        if 0 in layers:
            with ExitStack() as es2:
                cur_es[0] = es2
                nt_ = nsa_setup()
                for s in range(nseq):
                    nsa_layer(nt_, s)
            cur_es[0] = es
            p.barrier()
        if 1 in layers:
            with ExitStack() as es3:
                cur_es[0] = es3
                dt_ = diff_setup()
                for s in range(nseq):
                    if 0 not in layers:
                        for i in range(NT):
                            xb_ = xt[i % 2]
                            DMA("sp", xb_[:, :], x_in[s, i * 128:(i + 1) * 128, :], [NOB], [xb_.b])
                            DMA("sp", x1_d[s, i * 128:(i + 1) * 128, :], xb_[:, :], [xb_.b], [B_x1[s][i]])
                    diff_layer(dt_, s)
            cur_es[0] = es
        p.emit()
    return nc


INPUT_NAMES = ["rel_bias_table", "norm_pre", "norm_post", "nsa_w_in", "nsa_cmp_pe_k", "nsa_cmp_w1_k", "nsa_cmp_w2_k",
               "nsa_cmp_pe_v", "nsa_cmp_w1_v", "nsa_cmp_w2_v", "nsa_w_out", "diff_w_in", "diff_lambda_q1", "diff_lambda_k1",
               "diff_lambda_q2", "diff_lambda_k2", "diff_subln", "diff_w_out"]


def make_in_maps(inputs, n_cores, nseq, S):
    consts = host_consts(S)
    shared = {}
    for k in INPUT_NAMES:
        a = np.ascontiguousarray(np.asarray(inputs[k], dtype=np.float32))
        if a.ndim == 3:
            a = a[0]
        elif k.startswith("diff_lambda") or k == "diff_subln":
            a = a.reshape(1, -1)
        shared[k] = np.ascontiguousarray(a)
    shared.update(consts)
    x = np.asarray(inputs["x"], dtype=np.float32)
    maps = []
    for c in range(n_cores):
        m = dict(shared)
        m["x"] = np.ascontiguousarray(x[c * nseq:(c + 1) * nseq])
        maps.append(m)
    return maps


def kernel(**inputs):
    x = np.asarray(inputs["x"])
    B, S, _ = x.shape
    n_cores = 8
    nseq = B // n_cores
    nc = build(nseq, S)
    in_maps = make_in_maps(inputs, n_cores, nseq, S)
    res = run_bass_kernel_spmd(nc, in_maps, core_ids=list(range(n_cores)))
    return np.concatenate([np.asarray(r["out"]) for r in res.results], axis=0).astype(np.float32)
```

```python
import math
from contextlib import ExitStack

import numpy as np
import ml_dtypes

import concourse.bass as bass
import concourse.mybir as mybir
from concourse.bass_utils import run_bass_kernel_spmd

F32 = mybir.dt.float32
BF16 = mybir.dt.bfloat16
AF = mybir.ActivationFunctionType
ALU = mybir.AluOpType
AX = mybir.AxisListType

D = 1024
NSA_IN = 3632
EPS = 1e-6
PEN = -30000.0
LAMBDA_INIT = 0.8 - 0.6 * math.exp(-0.3 * 1)


class Buf:
    __slots__ = ("name", "writers", "readers", "prev")

    def __init__(self, name=""):
        self.name = name
        self.writers = []
        self.readers = []
        self.prev = []


class Op:
    __slots__ = ("eng", "fn", "dma", "deps", "needs_sig", "sig")

    def __init__(self, eng, fn, dma):
        self.eng = eng
        self.fn = fn
        self.dma = dma
        self.deps = []
        self.needs_sig = False
        self.sig = None


ENGS = ("pe", "act", "dve", "pool", "sp")


class Prog:
    def __init__(self, nc, n_dma_sems=48):
        self.nc = nc
        self.ops = {e: [] for e in ENGS}
        self.n_dma_sems = n_dma_sems
        self.dma_last = [None] * n_dma_sems
        self.dma_cnt = [0] * n_dma_sems
        self.dma_rr = 0
        self.pending = {}

    def barrier(self):
        lasts = []
        for e in ENGS:
            for op in reversed(self.ops[e]):
                if not op.dma:
                    op.needs_sig = True
                    lasts.append(op)
                    break
        for j in range(self.n_dma_sems):
            if self.dma_last[j] is not None:
                lasts.append(self.dma_last[j])
        self.pending = {e: list(lasts) for e in ENGS}

    def add(self, eng, fn, reads=(), writes=(), dma=False, join=False):
        op = Op(eng, fn, dma)
        if self.pending.get(eng):
            op.deps.extend(self.pending.pop(eng))
        for b in reads:
            for w in b.writers:
                self._dep(w, op, True)
        for b in writes:
            if not join:
                for w in b.writers:
                    self._dep(w, op, False)
            else:
                for r in b.prev:
                    self._dep(r, op, False)
            for r in b.readers:
                self._dep(r, op, False)
        for b in writes:
            if join:
                b.writers.append(op)
            else:
                b.prev = list(b.writers) + list(b.readers)
                b.writers = [op]
                b.readers = []
        for b in reads:
            b.readers.append(op)
        if dma:
            j = self.dma_rr
            self.dma_rr = (j + 1) % self.n_dma_sems
            prev = self.dma_last[j]
            if prev is not None:
                op.deps.append(prev)
            self.dma_cnt[j] += 1
            op.sig = (("d", j), 16 * self.dma_cnt[j])
            op.needs_sig = True
            self.dma_last[j] = op
        self.ops[eng].append(op)
        return op

    def _dep(self, p, c, raw):
        if p is c:
            return
        if (not p.dma) and (not c.dma) and p.eng == c.eng:
            if p.eng == "pe" or not raw:
                return
        p.needs_sig = True
        c.deps.append(p)

    def emit(self):
        nc = self.nc
        with ExitStack() as es:
            esem = {e: es.enter_context(nc.semaphore("s_" + e)) for e in ENGS}
            dsem = [es.enter_context(nc.semaphore("d%d" % j)) for j in range(self.n_dma_sems)]
            for e in ENGS:
                cnt = 0
                for op in self.ops[e]:
                    if op.dma:
                        continue
                    if op.needs_sig:
                        cnt += 1
                        op.sig = (("e", e), cnt)

            def semof(key):
                return esem[key[1]] if key[0] == "e" else dsem[key[1]]

            block = es.enter_context(nc.Block())
            engobj = {"pe": "tensor", "act": "scalar", "dve": "vector", "pool": "gpsimd", "sp": "sync"}

            def make(e):
                def body(eng):
                    waited = {}
                    for op in self.ops[e]:
                        need = {}
                        for p in op.deps:
                            k, v = p.sig
                            if need.get(k, 0) < v:
                                need[k] = v
                        for k, v in need.items():
                            if waited.get(k, 0) >= v:
                                continue
                            eng.wait_ge(semof(k), v)
                            waited[k] = v
                        ins = op.fn(eng)
                        if op.needs_sig:
                            k, v = op.sig
                            ins.then_inc(semof(k), 16 if op.dma else 1)
                    if e == "sp":
                        for j in range(self.n_dma_sems):
                            if self.dma_cnt[j] > 0:
                                eng.wait_ge(dsem[j], 16 * self.dma_cnt[j])
                return body

            for e in ENGS:
                getattr(block, engobj[e])(make(e))


def _rel_bucket(n):
    n = np.maximum(n, 0)
    nf = np.maximum(n, 1).astype(np.float32)
    large = 16 + (np.log(nf / np.float32(16)) / np.float32(math.log(8.0)) * np.float32(16)).astype(np.int32)
    large = np.minimum(large, 31)
    return np.where(n < 16, n, large)


def host_consts(S):
    NT = S // 128
    ncmp = S // 16 - 1
    bf = ml_dtypes.bfloat16
    c = {}
    c["c_ident"] = np.eye(128, dtype=np.float32).astype(bf)
    b = _rel_bucket(np.arange(128))
    oh = np.zeros((32, 128), np.float32)
    oh[b, np.arange(128)] = 1.0
    oh[31, :] -= 1.0
    c["c_onehot"] = oh
    cmp_lo = np.arange(ncmp) * 16
    sel_lo = np.arange(S // 64) * 64
    ov = np.clip(np.minimum(cmp_lo[:, None] + 32, sel_lo[None, :] + 64)
                 - np.maximum(cmp_lo[:, None], sel_lo[None, :]), 0, None).astype(np.float32) / 32.0
    ovp = np.zeros((127, 32), np.float32)
    ovp[:ncmp, :S // 64] = ov
    c["c_ovl"] = ovp.astype(bf)
    X = np.zeros((32, S), np.float32)
    X[np.arange(S) // 64, np.arange(S)] = 1.0
    c["c_X"] = X.astype(bf)
    k = np.arange(128)[:, None]
    q = np.arange(128)[None, :]
    c["c_M4"] = (q < k).astype(np.float32).astype(bf)
    t = np.arange(S)
    cur = t // 64
    n = np.arange(32)[None, :]
    forced = (n == 0) | (n == cur[:, None]) | (n == cur[:, None] - 1)
    future = n > cur[:, None]
    keep = (~(forced | future)).astype(np.float32)
    addc = np.where(forced, 1e9, np.where(future, -1e9, 0.0)).astype(np.float32)
    c["c_keep"] = np.ascontiguousarray(keep.reshape(NT, 128, 32).transpose(1, 0, 2))
    c["c_addc"] = np.ascontiguousarray(addc.reshape(NT, 128, 32).transpose(1, 0, 2))
    return c


def build(nseq, S, layers=(0, 1)):
    NT = S // 128
    NQ = S // 512
    NCMP = S // 16 - 1
    nc = bass.Bass("TRN2", target_bir_lowering=False)
    p = Prog(nc)

    def din(name, shape, dt=F32):
        return nc.dram_tensor(name, list(shape), dt, kind="ExternalInput").ap()

    x_in = din("x", [nseq, S, D])
    table = din("rel_bias_table", [32, 16])
    norm_pre = din("norm_pre", [2, D])
    norm_post = din("norm_post", [2, D])
    nsa_w_in = din("nsa_w_in", [D, NSA_IN])
    pe_k = din("nsa_cmp_pe_k", [32, 64])
    w1_k = din("nsa_cmp_w1_k", [2048, 128])
    w2_k = din("nsa_cmp_w2_k", [128, 64])
    pe_v = din("nsa_cmp_pe_v", [32, 64])
    w1_v = din("nsa_cmp_w1_v", [2048, 128])
    w2_v = din("nsa_cmp_w2_v", [128, 64])
    nsa_w_out = din("nsa_w_out", [D, D])
    diff_w_in = din("diff_w_in", [D, 4096])
    lq1 = din("diff_lambda_q1", [1, 64])
    lk1 = din("diff_lambda_k1", [1, 64])
    lq2 = din("diff_lambda_q2", [1, 64])
    lk2 = din("diff_lambda_k2", [1, 64])
    subln = din("diff_subln", [1, D])
    diff_w_out = din("diff_w_out", [D, D])
    c_ident = din("c_ident", [128, 128], BF16)
    c_onehot = din("c_onehot", [32, 128])
    c_ovl = din("c_ovl", [127, 32], BF16)
    c_X = din("c_X", [32, S], BF16)
    c_M4 = din("c_M4", [128, 128], BF16)
    c_keep = din("c_keep", [128, NT, 32])
    c_addc = din("c_addc", [128, NT, 32])
    out = nc.dram_tensor("out", [nseq, S, D], F32, kind="ExternalOutput").ap()
    x1_d = nc.dram_tensor("x1_scr", [nseq, S, D], F32).ap()
    De = nc.dram_tensor("De_scr", [16, 128, 512], BF16).ap()
    Dc = nc.dram_tensor("Dc_scr", [16, 127, 4096], BF16).ap()
    B_x1 = [[Buf("x1d%d_%d" % (s, i)) for i in range(NT)] for s in range(nseq)]
    B_out = [[Buf("out%d_%d" % (s, i)) for i in range(NT)] for s in range(nseq)]
    B_De = Buf("De")
    B_Dc = Buf("Dc")
    NOB = Buf("const_in")

    import os as _os
    es = ExitStack()
    cur_es = [es]
    DEBUG = bool(_os.environ.get("KDEBUG"))
    dbg_seen = set()

    def dbg(name, tt, ap, dt=F32):
        if not DEBUG or name in dbg_seen:
            return
        dbg_seen.add(name)
        o = nc.dram_tensor("dbg_" + name, list(ap.shape), dt, kind="ExternalOutput").ap()
        p.add("sp", lambda e: e.dma_start(out=o, in_=ap), reads=[tt.b], writes=[Buf()], dma=True)

    class TT:
        def __init__(self, t, name):
            self.t = t
            self.b = Buf(name)

        def __getitem__(self, k):
            return self.t[k]

    def sb(name, shape, dt=F32):
        if _os.environ.get("KDEBUG_SB"):
            print("SB", name, shape, "remaining", nc.sbuf_bytes_remaining)
        return TT(cur_es[0].enter_context(nc.sbuf_tensor(name, list(shape), dt)), name)

    banks = [TT(es.enter_context(nc.psum_tensor("bank%d" % i, [128, 512], F32)), "bank%d" % i) for i in range(8)]

    def DMA(eng, out_ap, in_ap, reads, writes, join=False, **kw):
        p.add(eng, lambda e: e.dma_start(out=out_ap, in_=in_ap, **kw), reads=reads, writes=writes, dma=True, join=join)

    def MM(out_ap, lhsT, rhs, start, stop, reads, writes, skip=False):
        if skip:
            p.add("pe", lambda e: e.matmul(out_ap, lhsT=lhsT, rhs=rhs, start=start, stop=stop, skip_group_check=True), reads=reads, writes=writes)
        else:
            p.add("pe", lambda e: e.matmul(out_ap, lhsT=lhsT, rhs=rhs, start=start, stop=stop), reads=reads, writes=writes)

    def TR(out_ap, in_ap, reads, writes):
        p.add("pe", lambda e: e.transpose(out=out_ap, in_=in_ap, identity=ident[:, :]), reads=list(reads) + [ident.b], writes=writes)

    def ACT(out_ap, in_ap, func, reads, writes, **kw):
        p.add("act", lambda e: e.activation(out=out_ap, in_=in_ap, func=func, **kw), reads=reads, writes=writes)

    def V(eng, name, reads, writes, *a, **kw):
        p.add(eng, lambda e: getattr(e, name)(*a, **kw), reads=reads, writes=writes)

    evac_rr = [0]

    def EVAC(out_ap, in_ap, reads, writes, scale=None):
        evac_rr[0] ^= 1
        if evac_rr[0]:
            if scale is None:
                ACT(out_ap, in_ap, AF.Copy, reads, writes)
            else:
                ACT(out_ap, in_ap, AF.Copy, reads, writes, scale=float(scale))
        else:
            if scale is None:
                V("dve", "tensor_copy", reads, writes, out=out_ap, in_=in_ap)
            else:
                V("dve", "tensor_scalar", reads, writes, out=out_ap, in0=in_ap, scalar1=float(scale), scalar2=None, op0=ALU.mult)

    ident = sb("ident", [128, 128], BF16)
    DMA("sp", ident[:, :], c_ident[:, :], [NOB], [ident.b])
    M4 = sb("M4", [128, 128], BF16)
    DMA("sp", M4[:, :], c_M4[:, :], [NOB], [M4.b])
    Ee = sb("Ee", [128, 16, 256], BF16)
    uT = sb("uT", [128, 8, S], BF16)
    oT = sb("oT", [128, 8, S], BF16)
    xt = [sb("xt%d" % i, [128, D]) for i in range(2)]
    ub = [sb("ub%d" % i, [128, D], BF16) for i in range(2)]
    stat = [sb("stat%d" % i, [128, 8]) for i in range(4)]
    PT = [sb("PT%d" % i, [128, 512], BF16) for i in range(4)]
    yout = [sb("yout0", [128, D])]
    cst = {}

    def setup_bias():
        tab = sb("tab", [32, 16])
        oh = sb("oh", [32, 128])
        fse = sb("fse", [16, 512], BF16)
        fsc = sb("fsc", [16, 4096], BF16)
        DMA("sp", tab[:, :], table[:, :], [NOB], [tab.b])
        DMA("sp", oh[:, :], c_onehot[:, :], [NOB], [oh.b])
        MM(banks[0][0:16, 0:128], tab[:, :], oh[:, :], True, True, [tab.b, oh.b], [banks[0].b])
        V("pool", "memset", [], [fse.b], fse[:, :], 0.0)
        V("pool", "memset", [fse.b], [fse.b], fse[:, 128:384], 1.0)
        V("pool", "memset", [], [fsc.b], fsc[:, :], 0.0)
        V("pool", "memset", [fsc.b], [fsc.b], fsc[:, 159:2064], 1.0)
        ACT(fse[:, 0:128], banks[0][0:16, 0:128], AF.Exp, [banks[0].b, fse.b], [fse.b])
        ACT(fsc[:, 31:159], banks[0][0:16, 0:128], AF.Exp, [banks[0].b, fsc.b], [fsc.b])
        DMA("sp", De, fse[:, :].unsqueeze(1).broadcast_to([16, 128, 512]), [fse.b], [B_De])
        DMA("sp", Dc, fsc[:, :].unsqueeze(1).broadcast_to([16, 127, 4096]), [fsc.b], [B_Dc])
        for h in range(16):
            DMA("sp", Ee[:, h, :], bass.AP(De.tensor, h * 128 * 512, [[511, 128], [1, 256]]), [B_De], [Ee.b], join=(h > 0))

    def load_norm_gains(l):
        cst["gpre"] = sb("gpre%d" % l, [128, D])
        cst["gpost"] = sb("gpost%d" % l, [128, D])
        DMA("sp", cst["gpre"][:, :], norm_pre[l:l + 1, :].partition_broadcast(128), [NOB], [cst["gpre"].b])
        DMA("sp", cst["gpost"][:, :], norm_post[l:l + 1, :].partition_broadcast(128), [NOB], [cst["gpost"].b])

    def Ec_src(h, c0, ncols):
        return bass.AP(Dc.tensor, h * 127 * 4096 + c0, [[4080, NCMP], [1, ncols]])

    STG_N = 1024
    stgs = {}
    cast_rr = [0]

    cast_engs = [("dve", "act")]

    def CAST(out_ap, in_ap, reads, writes, join=False):
        e = cast_engs[0][cast_rr[0] % len(cast_engs[0])]
        cast_rr[0] += 1
        if e == "act":
            p.add("act", lambda en: en.activation(out=out_ap, in_=in_ap, func=AF.Copy), reads=reads, writes=writes, join=join)
        else:
            p.add(e, lambda en: en.tensor_copy(out=out_ap, in_=in_ap), reads=reads, writes=writes, join=join)

    def LOADW(dst_ap, src_ap, dst_buf, join=False):
        stg = stgs["stg"]
        shp = list(dst_ap.shape)
        P_ = shp[0]
        mid = 1
        for d_ in shp[1:-1]:
            mid *= d_
        last = shp[-1]
        step = max(1, STG_N // mid)
        bp = dst_ap.base_partition()
        first = True
        for c0 in range(0, last, step):
            c1 = min(last, c0 + step)
            n = mid * (c1 - c0)
            assert n <= STG_N, shp
            st_ = stg[stgs["rr"]]
            stgs["rr"] = (stgs["rr"] + 1) % len(stg)
            sv = st_[bp:bp + P_, 0:n]
            if len(shp) == 3:
                sv = sv.rearrange("p (a b) -> p a b", a=shp[1])
                d_ap, s_ap = dst_ap[:, :, c0:c1], src_ap[:, :, c0:c1]
            else:
                d_ap, s_ap = dst_ap[:, c0:c1], src_ap[:, c0:c1]
            DMA("sp", sv, s_ap, [NOB], [st_.b])
            CAST(d_ap, sv, [st_.b], [dst_buf], join=(join or not first))
            first = False

    GA = 652
    WG = nc.dram_tensor("WG_scr", [4, 128, 8, 908], BF16).ap()
    WD = nc.dram_tensor("WD_scr", [8, 128, 8, 512], BF16).ap()
    WO = nc.dram_tensor("WO_scr", [2, 128, 8, 1024], BF16).ap()
    W1S = nc.dram_tensor("W1_scr", [128, 32, 128], BF16).ap()
    W2S = nc.dram_tensor("W2_scr", [128, 128], BF16).ap()
    B_WG = [Buf("WG%d" % g) for g in range(4)]
    B_WD = [Buf("WD%d" % h) for h in range(8)]
    B_WO = [Buf("WO%d" % l) for l in range(2)]
    B_W1 = Buf("W1S")
    B_W2 = Buf("W2S")

    def setup_weights():
        stgs["stg"] = [sb("stg%d" % i, [128, STG_N]) for i in range(4)]
        stgs["rr"] = 0
        asm = [sb("asm%d" % i, [128, 8, 1024], BF16) for i in range(2)]
        k = 0
        wv3 = nsa_w_in.rearrange("(c p) n -> p c n", p=128)
        if 0 in layers:
            for g in range(4):
                a_ = asm[k % 2]
                k += 1
                pieces = [(0, 256 * g, 256), (256, 1024 + 64 * g, 64), (320, 1280 + 64 * g, 64), (384, 1536 + 64 * g, 64),
                          (448, 2048 + 64 * g, 64), (512, 1792 + 64 * g, 64), (576, 2304 + 64 * g, 64), (640, 2560 + 12 * g, 12),
                          (652, 2608 + 256 * g, 256)]
                for pi, (d0, s0, n) in enumerate(pieces):
                    LOADW(a_[:, :, d0:d0 + n], wv3[:, :, s0:s0 + n], a_.b, join=(pi > 0))
                DMA("sp", WG[g], a_[:, :, 0:908], [a_.b], [B_WG[g]])
            a_ = asm[k % 2]
            k += 1
            for lh in range(2):
                ls = slice(16 * lh, 16 * lh + 16)
                LOADW(a_[0:64, :, :].rearrange("p c n -> p (c n)")[:, 0:4096].rearrange("p (l h) -> p l h", l=32)[:, ls, :],
                      w1_k.rearrange("(l d) h -> d l h", d=64)[:, ls, :], a_.b, join=(lh > 0))
                LOADW(a_[64:128, :, :].rearrange("p c n -> p (c n)")[:, 0:4096].rearrange("p (l h) -> p l h", l=32)[:, ls, :],
                      w1_v.rearrange("(l d) h -> d l h", d=64)[:, ls, :], a_.b, join=True)
            LOADW(a_[:, 4, 0:64], w2_k[:, :], a_.b, join=True)
            LOADW(a_[:, 4, 64:128], w2_v[:, :], a_.b, join=True)
            DMA("sp", W1S, a_[:, :, :].rearrange("p c n -> p (c n)")[:, 0:4096].rearrange("p (l h) -> p l h", l=32), [a_.b], [B_W1])
            DMA("sp", W2S, a_[:, 4, 0:128], [a_.b], [B_W2])
        for l, w_ in enumerate((nsa_w_out,)):
            if l not in layers:
                continue
            a_ = asm[k % 2]
            k += 1
            wo3 = w_.rearrange("(c p) n -> p c n", p=128)
            for q4 in range(4):
                LOADW(a_[:, :, 256 * q4:256 * q4 + 256], wo3[:, :, 256 * q4:256 * q4 + 256], a_.b, join=(q4 > 0))
            DMA("sp", WO[l], a_[:, :, :], [a_.b], [B_WO[l]])

    stat_rr = [0]

    def get_stat():
        stat_rr[0] = (stat_rr[0] + 1) % 4
        return stat[stat_rr[0]]

    def phase_norm_T(src_ap_fn, src_bufs, g_tile):
        for i in range(NT):
            xb_ = xt[i % 2]
            u_ = ub[i % 2]
            DMA("sp", xb_[:, :], src_ap_fn(i), src_bufs(i), [xb_.b])
            st = get_stat()
            ACT(u_[:, :], xb_[:, :], AF.Square, [xb_.b], [u_.b, st.b], accum_out=st[:, 0:1])
            V("dve", "tensor_scalar", [st.b], [st.b], out=st[:, 1:2], in0=st[:, 0:1], scalar1=1.0 / D, scalar2=EPS, op0=ALU.mult, op1=ALU.add)
            ACT(st[:, 2:3], st[:, 1:2], AF.Sqrt, [st.b], [st.b])
            V("dve", "reciprocal", [st.b], [st.b], out=st[:, 3:4], in_=st[:, 2:3])
            V("dve", "scalar_tensor_tensor", [xb_.b, st.b, g_tile.b], [u_.b], out=u_[:, :], in0=xb_[:, :], scalar=st[:, 3:4], in1=g_tile[:, :], op0=ALU.mult, op1=ALU.mult)
            bk = banks[6 + (i % 2)]
            bkv = bk[:, :].bitcast(BF16)
            for c in range(8):
                TR(bkv[:, c * 128:(c + 1) * 128], u_[:, c * 128:(c + 1) * 128], [u_.b], [bk.b])
            EVAC(uT[:, :, i * 128:(i + 1) * 128], bkv.rearrange("p (c t) -> p c t", c=8), [bk.b], [uT.b])

    pf_rr = [0]

    def proj_fm(wt, wb, outs, scale=None, bank_ids=(5, 6, 7), M=128):
        for Q in range(NQ):
            bk = banks[bank_ids[pf_rr[0] % len(bank_ids)]]
            pf_rr[0] += 1
            for c in range(8):
                MM(bk[0:M, :], wt[:, c, :], uT[:, c, Q * 512:(Q + 1) * 512], c == 0, c == 7, [wb, uT.b], [bk.b])
            for (rs_, fn_, ob_) in outs:
                EVAC(fn_(Q), bk[rs_, :], [bk.b], [ob_], scale=scale)

    def phase_out(wbig, w_out_d, gp, res_fn, res_bufs, dst_fn, dst_bufs):
        if w_out_d is not None:
            DMA("sp", wbig[:, :, :], WO[w_out_d], [B_WO[w_out_d]], [wbig.b])
        for i in range(NT):
            xb_ = xt[i % 2]
            DMA("sp", xb_[:, :], res_fn(i), res_bufs(i), [xb_.b])
            bk = [banks[4 + 2 * (i % 2)], banks[5 + 2 * (i % 2)]]
            for half in range(2):
                for c in range(8):
                    MM(bk[half][:, :], oT[:, c, i * 128:(i + 1) * 128], wbig[:, c, half * 512:(half + 1) * 512], c == 0, c == 7, [oT.b, wbig.b], [bk[half].b])
            st = get_stat()
            ACT(ub[0][:, 0:512], bk[0][:, :], AF.Square, [bk[0].b], [ub[0].b, st.b], accum_out=st[:, 0:1])
            ACT(ub[0][:, 512:1024], bk[1][:, :], AF.Square, [bk[1].b], [ub[0].b, st.b], accum_out=st[:, 1:2])
            V("dve", "tensor_tensor", [st.b], [st.b], out=st[:, 2:3], in0=st[:, 0:1], in1=st[:, 1:2], op=ALU.add)
            V("dve", "tensor_scalar", [st.b], [st.b], out=st[:, 3:4], in0=st[:, 2:3], scalar1=1.0 / D, scalar2=EPS, op0=ALU.mult, op1=ALU.add)
            ACT(st[:, 4:5], st[:, 3:4], AF.Sqrt, [st.b], [st.b])
            V("dve", "reciprocal", [st.b], [st.b], out=st[:, 5:6], in_=st[:, 4:5])
            yo = yout[0]
            for half in range(2):
                sl = slice(half * 512, (half + 1) * 512)
                V("dve", "scalar_tensor_tensor", [bk[half].b, st.b, gp.b], [yo.b], out=yo[:, sl], in0=bk[half][:, :], scalar=st[:, 5:6], in1=gp[:, sl], op0=ALU.mult, op1=ALU.mult)
            V("pool", "tensor_tensor", [yo.b, xb_.b], [yo.b], out=yo[:, :], in0=yo[:, :], in1=xb_[:, :], op=ALU.add)
            DMA("sp", dst_fn(i), yo[:, :], [yo.b], dst_bufs(i))


    class Stream:
        pass

    S_BANKS = [banks[0], banks[1], banks[2]]
    s_rr = [0]
    pt_rr = [0]
    LOOK = 2

    def run_streams(streams, filler=None, every=3):
        pending = []
        ntile = [0]

        def emit_pv(item):
            st_, kj, c0, c1, ptb = item
            for r in range(c0, c1):
                qi = 4 * st_.Q + r
                first = max(0, qi - 4) if st_.band else 0
                o_ap, o_tt = st_.O[r]
                if not hasattr(st_, "started"):
                    st_.started = set()
                is_first = id(o_tt) not in st_.started
                st_.started.add(id(o_tt))
                MM(o_ap, ptb[:, r * 128:(r + 1) * 128], st_.v_ap(kj), is_first, kj == qi,
                   [ptb.b] + st_.v_bufs, [o_tt.b], skip=True)
            if kj == st_.last_kj:
                st_.finalize(st_)

        for st_ in streams:
            Q = st_.Q
            kj_lo = max(0, 4 * Q - 4) if st_.band else 0
            st_.last_kj = 4 * Q + 3
            for kj in range(kj_lo, 4 * Q + 4):
                rd0 = kj - 4 * Q
                c0 = max(0, rd0)
                c1 = min(4, rd0 + 5) if st_.band else 4
                sbk = S_BANKS[s_rr[0]]
                s_rr[0] = (s_rr[0] + 1) % len(S_BANKS)
                ptb = PT[pt_rr[0]]
                pt_rr[0] = (pt_rr[0] + 1) % len(PT)
                cols = slice(c0 * 128, c1 * 128)
                gcols = slice(Q * 512 + c0 * 128, Q * 512 + c1 * 128)
                MM(sbk[:, cols], st_.k_ap(kj), st_.q_ap(gcols), True, st_.pen is None, st_.qk_bufs, [sbk.b])
                if st_.pen is not None:
                    MM(sbk[:, cols], cst["Xs"][0:32, kj * 128:(kj + 1) * 128], st_.pen[0:32, gcols], False, True,
                       [cst["Xs"].b, st_.pen_buf], [sbk.b])
                ACT(ptb[:, cols], sbk[:, cols], AF.Exp, [sbk.b], [ptb.b])
                if -1 <= rd0 <= 3:
                    lo = max(rd0, 0)
                    hi = min(rd0 + 2, 4)
                    eo = (lo - rd0) * 128
                    V("dve", "tensor_tensor", [ptb.b, Ee.b], [ptb.b], out=ptb[:, lo * 128:hi * 128], in0=ptb[:, lo * 128:hi * 128],
                      in1=Ee[:, st_.emap, eo:eo + (hi - lo) * 128], op=ALU.mult)
                if st_.band:
                    r4 = rd0 + 4
                    if 0 <= r4 <= 3:
                        V("dve", "tensor_tensor", [ptb.b, M4.b], [ptb.b], out=ptb[:, r4 * 128:(r4 + 1) * 128], in0=ptb[:, r4 * 128:(r4 + 1) * 128],
                          in1=M4[:, :], op=ALU.mult)
                pending.append((st_, kj, c0, c1, ptb))
                if len(pending) > LOOK:
                    emit_pv(pending.pop(0))
                ntile[0] += 1
                if filler is not None and ntile[0] % every == 0:
                    next(filler, None)
        while pending:
            emit_pv(pending.pop(0))
        if filler is not None:
            for _ in filler:
                pass

    def nsa_setup():
        t = {}
        load_norm_gains(0)
        t["keep"] = sb("keep", [128, NT, 32])
        t["addc"] = sb("addc", [128, NT, 32])
        DMA("sp", t["keep"][:, :, :], c_keep[:, :, :], [NOB], [t["keep"].b])
        DMA("sp", t["addc"][:, :, :], c_addc[:, :, :], [NOB], [t["addc"].b])
        t["w1"] = sb("w1", [128, 32, 128], BF16)
        DMA("sp", t["w1"][:, :, :], W1S, [B_W1], [t["w1"].b])
        t["w2"] = sb("w2", [128, 128], BF16)
        DMA("sp", t["w2"][:, :], W2S, [B_W2], [t["w2"].b])
        t["w2k"] = sb("w2k", [128, 128], BF16)
        V("pool", "memset", [], [t["w2k"].b], t["w2k"][:, :], 0.0)
        V("pool", "tensor_copy", [t["w2"].b, t["w2k"].b], [t["w2k"].b], out=t["w2k"][:, 0:64], in_=t["w2"][:, 0:64])
        pes = sb("pes", [32, 128])
        DMA("sp", pes[:, 0:64], pe_k[:, :], [NOB], [pes.b])
        DMA("sp", pes[:, 64:128], pe_v[:, :], [NOB], [pes.b], join=True)
        pesb = sb("pesb", [32, 128], BF16)
        V("dve", "tensor_copy", [pes.b], [pesb.b], out=pesb[:, :], in_=pes[:, :])
        peT = sb("peT", [128, 32], BF16)
        bkv = banks[3][:, :].bitcast(BF16)
        p.add("pe", lambda e: e.transpose(out=bkv[:, 0:32], in_=pesb[:, :], identity=ident[0:32, 0:32]), reads=[pesb.b, ident.b], writes=[banks[3].b])
        V("dve", "tensor_copy", [banks[3].b], [peT.b], out=peT[:, :], in_=bkv[:, 0:32])
        t["peh"] = sb("peh", [128, 2])
        for kv in range(2):
            ps_ = slice(64 * kv, 64 * kv + 64)
            for l in range(32):
                MM(banks[4 + kv][:, 0:1], t["w1"][ps_, l, :], peT[ps_, l:l + 1], l == 0, l == 31, [t["w1"].b, peT.b], [banks[4 + kv].b])
        for kv in range(2):
            V("dve", "tensor_copy", [banks[4 + kv].b], [t["peh"].b], out=t["peh"][:, kv:kv + 1], in_=banks[4 + kv][:, 0:1])
        t["Vca"] = sb("Vca", [127, 97], BF16)
        V("pool", "memset", [], [t["Vca"].b], t["Vca"][:, :], 1.0)
        DMA("sp", t["Vca"][:, 65:97], c_ovl[:, :], [t["Vca"].b], [t["Vca"].b])
        t["wgA"] = sb("wgA", [128, 8, GA], BF16)
        t["wgZ"] = sb("wgZ", [128, 8, 256], BF16)
        t["qa"] = [sb("qa%d" % i, [128, S], BF16) for i in range(4)]
        for i in range(4):
            V("pool", "memset", [], [t["qa"][i].b], t["qa"][i][:, :], 0.0)
        t["cT"] = sb("cT", [128, S], BF16)
        t["ksx"] = sb("ksx", [128, S], BF16)
        V("pool", "memset", [], [t["ksx"].b], t["ksx"][:, :], 0.0)
        DMA("sp", t["ksx"][64:96, :], c_X[:, :], [t["ksx"].b], [t["ksx"].b])
        t["kwz"] = sb("kwz", [128, S], BF16)
        V("pool", "memset", [], [t["kwz"].b], t["kwz"][:, :], 0.0)
        t["vsa"] = sb("vsa", [128, NT, 65], BF16)
        t["vwa"] = sb("vwa", [128, NT, 65], BF16)
        V("pool", "memset", [], [t["vsa"].b], t["vsa"][:, :, :], 1.0)
        V("pool", "memset", [], [t["vwa"].b], t["vwa"][:, :, :], 1.0)
        t["gate"] = sb("gate", [128, NT, 12])
        accraw = sb("acc", [128, max(NT * 256, 4096)])
        t["acc"] = TT(accraw[:, 0:NT * 256].rearrange("p (i c) -> p i c", i=NT), "acc")
        t["acc"].b = accraw.b
        t["wbig"] = TT(accraw[:, 0:4096].bitcast(BF16).rearrange("p (c n) -> p c n", c=8), "wbig")
        t["wbig"].b = accraw.b
        t["ha"] = [sb("ha%d" % i, [128, 128], BF16) for i in range(2)]
        t["kcmp"] = sb("kcmp", [128, 128], BF16)
        t["Ec"] = [sb("Ec%d" % i, [127, 512], BF16) for i in range(3)]
        t["cm"] = sb("cm", [128, 4, 4, 97])
        t["sm"] = [sb("sm0", [128, 32]), sb("sm1", [128, 512]), sb("sm2", [128, 288])]
        t["penb"] = sb("penb", [128, 4, 96], BF16)
        V("pool", "memset", [], [t["penb"].b], t["penb"][:, :, :], 0.0)
        t["tmp"] = [sb("tmpo%d" % i, [128, 4, 64]) for i in range(2)]
        t["zs"] = [sb("zs%d" % i, [128, 256]) for i in range(2)]
        t["og"] = [sb("og%d" % i, [128, 256], BF16) for i in range(2)]
        DMA("sp", t["wgA"][:, :, :], WG[0][:, :, 0:GA], [B_WG[0]], [t["wgA"].b])
        DMA("sp", t["wgZ"][:, :, :], WG[0][:, :, GA:908], [B_WG[0]], [t["wgZ"].b])
        return t

    def nsa_layer(t, s):
        phase_norm_T(lambda i: x_in[s, i * 128:(i + 1) * 128, :], lambda i: [NOB], cst["gpre"])
        wgA, wgZ = t["wgA"], t["wgZ"]
        for g in range(4):
            g_next = (g + 1) % 4
            has_next = (g < 3) or (s + 1 < nseq)
            for j in range(4):
                qa_ = t["qa"][j]
                proj_fm(wgA[:, :, 64 * j:64 * j + 64], wgA.b, [(slice(0, 64), (lambda Q, qa_=qa_: qa_[0:64, Q * 512:(Q + 1) * 512]), qa_.b)], scale=0.125, M=64)
            proj_fm(wgA[:, :, 256:384], wgA.b, [(slice(0, 128), (lambda Q: t["cT"][:, Q * 512:(Q + 1) * 512]), t["cT"].b)])
            proj_fm(wgA[:, :, 384:448], wgA.b, [(slice(0, 64), (lambda Q: t["ksx"][0:64, Q * 512:(Q + 1) * 512]), t["ksx"].b)], M=64)
            proj_fm(wgA[:, :, 448:512], wgA.b, [(slice(0, 64), (lambda Q: t["kwz"][0:64, Q * 512:(Q + 1) * 512]), t["kwz"].b)], M=64)
            for i in range(NT):
                bk = banks[3 + (i % 2)]
                for c in range(8):
                    MM(bk[:, 0:140], uT[:, c, i * 128:(i + 1) * 128], wgA[:, c, 512:652], c == 0, c == 7, [uT.b, wgA.b], [bk.b])
                V("dve", "tensor_copy", [bk.b], [t["vsa"].b], out=t["vsa"][:, i, 0:64], in_=bk[:, 0:64])
                V("dve", "tensor_copy", [bk.b], [t["vwa"].b], out=t["vwa"][:, i, 0:64], in_=bk[:, 64:128])
                ACT(t["gate"][:, i, :], bk[:, 128:140], AF.Sigmoid, [bk.b], [t["gate"].b])
            if has_next:
                DMA("sp", wgA[:, :, :], WG[g_next][:, :, 0:GA], [B_WG[g_next]], [wgA.b])
            dbg("qT0", t["qa"][0], t["qa"][0][:, :], BF16)
            dbg("cT", t["cT"], t["cT"][:, :], BF16)
            dbg("vsa", t["vsa"], t["vsa"][:, :, :], BF16)
            dbg("gate", t["gate"], t["gate"][:, :, :])
            for kv in range(2):
                ps_ = slice(64 * kv, 64 * kv + 64)
                bk = banks[3 + kv]
                for l in range(32):
                    MM(bk[:, 0:NCMP], t["w1"][ps_, l, :], t["cT"][ps_, l:l + 16 * (NCMP - 1) + 1:16], l == 0, l == 31, [t["w1"].b, t["cT"].b], [bk.b])
                ACT(t["ha"][kv][:, 0:NCMP], bk[:, 0:NCMP], AF.Silu, [bk.b, t["peh"].b], [t["ha"][kv].b], bias=t["peh"][:, kv:kv + 1])
            MM(banks[5][:, 0:NCMP], t["w2k"][:, :], t["ha"][0][:, 0:NCMP], True, True, [t["w2k"].b, t["ha"][0].b], [banks[5].b])
            V("dve", "tensor_copy", [banks[5].b], [t["kcmp"].b], out=t["kcmp"][:, 0:NCMP], in_=banks[5][:, 0:NCMP])
            MM(banks[6][0:NCMP, 0:64], t["ha"][1][:, 0:NCMP], t["w2"][:, 64:128], True, True, [t["w2"].b, t["ha"][1].b], [banks[6].b])
            V("dve", "tensor_copy", [banks[6].b], [t["Vca"].b], out=t["Vca"][0:NCMP, 0:64], in_=banks[6][0:NCMP, 0:64])
            dbg("kcmp", t["kcmp"], t["kcmp"][:, 0:NCMP], BF16)
            dbg("Vca", t["Vca"], t["Vca"][0:NCMP, :], BF16)
            ec_rr = 0
            for Q in range(NQ):
                for j in range(4):
                    h = 4 * g + j
                    qa_ = t["qa"][j]
                    ec = t["Ec"][ec_rr % 3]
                    ec_rr += 1
                    DMA("sp", ec[0:NCMP, :], Ec_src(h, Q * 512, 512), [B_Dc], [ec.b])
                    sbk = S_BANKS[s_rr[0]]
                    s_rr[0] = (s_rr[0] + 1) % 3
                    ptb = PT[pt_rr[0]]
                    pt_rr[0] = (pt_rr[0] + 1) % len(PT)
                    MM(sbk[0:NCMP, :], t["kcmp"][:, 0:NCMP], qa_[:, Q * 512:(Q + 1) * 512], True, True, [t["kcmp"].b, qa_.b], [sbk.b])
                    ACT(ptb[0:NCMP, :], sbk[0:NCMP, :], AF.Exp, [sbk.b], [ptb.b])
                    V("dve", "tensor_tensor", [ptb.b, ec.b], [ptb.b], out=ptb[0:NCMP, :], in0=ptb[0:NCMP, :], in1=ec[0:NCMP, :], op=ALU.mult)
                    ob = banks[3 + (j % 2)]
                    for r in range(4):
                        MM(ob[:, r * 97:(r + 1) * 97], ptb[0:NCMP, r * 128:(r + 1) * 128], t["Vca"][0:NCMP, :], True, True, [ptb.b, t["Vca"].b], [ob.b])
                    V("dve", "tensor_copy", [ob.b], [t["cm"].b], out=t["cm"][:, :, j, :], in_=ob[:, 0:388].rearrange("p (r c) -> p r c", r=4))
                cm = t["cm"]
                sm0, sm1, sm2 = t["sm"]
                rinv = sm0[:, 0:16].rearrange("p (r j) -> p r j", r=4)
                coef = sm0[:, 16:32].rearrange("p (r j) -> p r j", r=4)
                V("dve", "tensor_scalar", [cm.b], [sm0.b], out=rinv, in0=cm[:, :, :, 64], scalar1=1e-30, scalar2=None, op0=ALU.add)
                V("dve", "reciprocal", [sm0.b], [sm0.b], out=rinv, in_=rinv)
                gv = t["gate"][:, 4 * Q:4 * Q + 4, :].rearrange("p r (j b) -> p r j b", b=3)
                V("dve", "tensor_tensor", [sm0.b, t["gate"].b], [sm0.b], out=coef, in0=rinv, in1=gv[:, :, :, 0], op=ALU.mult)
                accv = t["acc"][:, 4 * Q:4 * Q + 4, :].rearrange("p r (j d) -> p r j d", j=4)
                V("dve", "tensor_tensor", [cm.b, sm0.b], [t["acc"].b], out=accv, in0=cm[:, :, :, 0:64],
                  in1=coef.unsqueeze(3).broadcast_to([128, 4, 4, 64]), op=ALU.mult)
                impw = sm1[:, :].rearrange("p (r j n) -> p r j n", r=4, j=4)
                V("dve", "tensor_tensor", [cm.b, sm0.b], [sm1.b], out=impw, in0=cm[:, :, :, 65:97],
                  in1=rinv.unsqueeze(3).broadcast_to([128, 4, 4, 32]), op=ALU.mult)
                imp = sm2[:, 0:128].rearrange("p (r n) -> p r n", r=4)
                V("dve", "tensor_tensor", [sm1.b], [sm2.b], out=imp, in0=impw[:, :, 0, :], in1=impw[:, :, 1, :], op=ALU.add)
                V("dve", "tensor_tensor", [sm1.b, sm2.b], [sm2.b], out=imp, in0=imp, in1=impw[:, :, 2, :], op=ALU.add)
                V("dve", "tensor_tensor", [sm1.b, sm2.b], [sm2.b], out=imp, in0=imp, in1=impw[:, :, 3, :], op=ALU.add)
                V("dve", "tensor_tensor", [sm2.b, t["keep"].b], [sm2.b], out=imp, in0=imp, in1=t["keep"][:, 4 * Q:4 * Q + 4, :], op=ALU.mult)
                V("dve", "tensor_tensor", [sm2.b, t["addc"].b], [sm2.b], out=imp, in0=imp, in1=t["addc"][:, 4 * Q:4 * Q + 4, :], op=ALU.add)
                top8 = sm2[:, 128:160].rearrange("p (r e) -> p r e", r=4)
                for r in range(4):
                    V("dve", "max", [sm2.b], [sm2.b], out=top8[:, r, :], in_=imp[:, r, :])
                pen32 = sm2[:, 160:288].rearrange("p (r n) -> p r n", r=4)
                V("dve", "tensor_tensor", [sm2.b], [sm2.b], out=pen32, in0=imp, in1=top8[:, :, 7:8].broadcast_to([128, 4, 32]), op=ALU.is_lt)
                V("dve", "tensor_scalar", [sm2.b], [t["penb"].b], out=t["penb"][:, :, 64:96], in0=pen32, scalar1=PEN, scalar2=None, op0=ALU.mult)
                bkp = banks[5]
                bkpv = bkp[:, :].bitcast(BF16)
                for r in range(4):
                    TR(bkpv[0:96, r * 128:(r + 1) * 128], t["penb"][:, r, :], [t["penb"].b], [bkp.b])
                cols = slice(Q * 512, (Q + 1) * 512)
                V("dve", "tensor_copy", [bkp.b], [t["qa"][0].b], out=t["qa"][0][64:96, cols], in_=bkpv[64:96, 0:512])
                for j in range(1, 4):
                    V("pool", "tensor_copy", [t["qa"][0].b], [t["qa"][j].b], out=t["qa"][j][64:96, cols], in_=t["qa"][0][64:96, cols])
            dbg("cm", t["cm"], t["cm"][:, :, :, :])
            dbg("acc_c", t["acc"], t["acc"][:, :, :])
            streams = []
            o_rr = 0
            for Q in range(NQ):
                for j in range(4):
                    for br in (1, 2):
                        st_ = Stream()
                        st_.Q = Q
                        st_.band = (br == 2)
                        qa_ = t["qa"][j]
                        kt_ = t["ksx"] if br == 1 else t["kwz"]
                        va_ = t["vsa"] if br == 1 else t["vwa"]
                        st_.q_ap = lambda gc, qa_=qa_: qa_[:, gc]
                        st_.k_ap = lambda kj, kt_=kt_: kt_[:, kj * 128:(kj + 1) * 128]
                        st_.qk_bufs = [qa_.b, kt_.b]
                        st_.pen = None
                        st_.pen_buf = None
                        st_.v_ap = lambda kj, va_=va_: va_[:, kj, :]
                        st_.v_bufs = [va_.b]
                        st_.emap = 4 * g + j
                        ob = banks[3 + (o_rr % 2)]
                        o_rr += 1
                        st_.O = [(ob[:, r * 65:(r + 1) * 65], ob) for r in range(4)]
                        st_.ob = ob
                        st_.j = j
                        st_.br = br

                        def fin(st_):
                            ob = st_.ob
                            Q, j, br = st_.Q, st_.j, st_.br
                            ov = ob[:, 0:260].rearrange("p (r c) -> p r c", r=4)
                            sm = get_stat()
                            V("dve", "reciprocal", [ob.b], [sm.b], out=sm[:, 0:4], in_=ov[:, :, 64])
                            gv = t["gate"][:, 4 * Q:4 * Q + 4, :].rearrange("p r (j b) -> p r j b", b=3)
                            V("dve", "tensor_tensor", [sm.b, t["gate"].b], [sm.b], out=sm[:, 4:8], in0=sm[:, 0:4], in1=gv[:, :, j, br], op=ALU.mult)
                            tm = t["tmp"][(2 * j + br) % 2]
                            V("dve", "tensor_tensor", [ob.b, sm.b], [tm.b], out=tm[:, :, :], in0=ov[:, :, 0:64],
                              in1=sm[:, 4:8].unsqueeze(2).broadcast_to([128, 4, 64]), op=ALU.mult)
                            accv = t["acc"][:, 4 * Q:4 * Q + 4, 64 * j:64 * j + 64]
                            V("pool", "tensor_tensor", [tm.b, t["acc"].b], [t["acc"].b], out=accv, in0=accv, in1=tm[:, :, :], op=ALU.add)
                        st_.finalize = fin
                        streams.append(st_)
            run_streams(streams)
            dbg("acc_f", t["acc"], t["acc"][:, :, :])
            for i in range(NT):
                bk = banks[5 + (i % 2)]
                for c in range(8):
                    MM(bk[:, 0:256], uT[:, c, i * 128:(i + 1) * 128], wgZ[:, c, :], c == 0, c == 7, [uT.b, wgZ.b], [bk.b])
                zs = t["zs"][i % 2]
                og = t["og"][i % 2]
                ACT(zs[:, :], bk[:, 0:256], AF.Silu, [bk.b], [zs.b])
                V("dve", "tensor_tensor", [zs.b, t["acc"].b], [og.b], out=og[:, :], in0=t["acc"][:, i, :], in1=zs[:, :], op=ALU.mult)
                bkv = bk[:, :].bitcast(BF16)
                for pr in range(2):
                    TR(bkv[:, 512 + pr * 128:512 + (pr + 1) * 128], og[:, pr * 128:(pr + 1) * 128], [og.b], [bk.b])
                EVAC(oT[:, 2 * g:2 * g + 2, i * 128:(i + 1) * 128], bkv[:, 512:768].rearrange("p (c t) -> p c t", c=2), [bk.b], [oT.b])
            if has_next:
                DMA("sp", wgZ[:, :, :], WG[g_next][:, :, GA:908], [B_WG[g_next]], [wgZ.b])
        dbg("oT", oT, oT[:, :, :], BF16)
        dst, dstb = (x1_d, B_x1[s]) if 1 in layers else (out, B_out[s])
        phase_out(t["wbig"], 0, cst["gpost"], lambda i: x_in[s, i * 128:(i + 1) * 128, :], lambda i: [NOB],
                  lambda i: dst[s, i * 128:(i + 1) * 128, :], lambda i: [dstb[i]])

    def diff_setup():
        t = {}
        load_norm_gains(1)
        stgs["stg"] = [sb("dstg%d" % i, [128, STG_N]) for i in range(2)]
        stgs["rr"] = 0
        cast_engs[0] = ("pool",)
        raw = sb("dscr", [128, 4096])
        t["sq"] = TT(raw[:, 0:NT * 128].rearrange("p (i c) -> p i c", i=NT), "sq")
        t["og"] = TT(raw[:, 2048:3072].bitcast(BF16)[:, 0:NT * 128].rearrange("p (i c) -> p i c", i=NT), "og")
        t["wbig"] = TT(raw[:, :].bitcast(BF16).rearrange("p (c n) -> p c n", c=8), "wbig")
        t["sq"].b = t["og"].b = t["wbig"].b = raw.b
        lam4 = sb("lam4", [128, 4, 64])
        for idx, src in enumerate((lq1, lk1, lq2, lk2)):
            DMA("sp", lam4[:, idx, :], src[0:1, :].partition_broadcast(128), [NOB], [lam4.b], join=(idx > 0))
        lm = sb("lm", [128, 8])
        prod = sb("lprod", [128, 2, 64])
        V("dve", "tensor_tensor", [lam4.b], [prod.b], out=prod[:, 0, :], in0=lam4[:, 0, :], in1=lam4[:, 1, :], op=ALU.mult)
        V("dve", "tensor_tensor", [lam4.b], [prod.b], out=prod[:, 1, :], in0=lam4[:, 2, :], in1=lam4[:, 3, :], op=ALU.mult)
        V("dve", "reduce_sum", [prod.b], [lm.b], out=lm[:, 0:2], in_=prod[:, :, :], axis=AX.X)
        ACT(lm[:, 2:4], lm[:, 0:2], AF.Exp, [lm.b], [lm.b])
        V("dve", "tensor_tensor", [lm.b], [lm.b], out=lm[:, 4:5], in0=lm[:, 3:4], in1=lm[:, 2:3], op=ALU.subtract)
        V("dve", "tensor_scalar", [lm.b], [lm.b], out=lm[:, 5:6], in0=lm[:, 4:5], scalar1=-LAMBDA_INIT, scalar2=None, op0=ALU.add)
        t["lm"] = lm
        t["subln"] = sb("sublnb", [128, D])
        DMA("sp", t["subln"][:, :], subln[0:1, :].partition_broadcast(128), [NOB], [t["subln"].b])
        t["w"] = [sb("wd%d" % i, [128, 8, 512], BF16) for i in range(2)]
        t["qT"] = [sb("dqT%d" % i, [128, S], BF16) for i in range(2)]
        t["kz"] = [[sb("dkz%d_%d" % (b_, i), [128, S], BF16) for i in range(2)] for b_ in range(2)]
        for b_ in range(2):
            for i in range(2):
                V("pool", "memset", [], [t["kz"][b_][i].b], t["kz"][b_][i][:, :], 0.0)
        t["va"] = [sb("dva%d" % i, [128, NT, 129], BF16) for i in range(2)]
        for i in range(2):
            V("pool", "memset", [], [t["va"][i].b], t["va"][i][:, :, :], 1.0)
        t["od"] = [sb("od%d" % i, [128, NT, 128]) for i in range(2)]
        t["zs"] = [sb("dzs%d" % i, [128, NT, 128], BF16) for i in range(2)]
        t["tmp"] = [sb("dtmp%d" % i, [128, 2, 128]) for i in range(2)]
        t["rs"] = sb("drs", [128, 4, NT])
        return t

    def diff_layer(t, s):
        phase_norm_T(lambda i: x1_d[s, i * 128:(i + 1) * 128, :], lambda i: [B_x1[s][i]], cst["gpre"])
        dv3 = diff_w_in.rearrange("(c p) n -> p c n", p=128)
        lm = t["lm"]

        def load_w(h):
            w = t["w"][h % 2]
            for part in range(4):
                LOADW(w[:, :, 128 * part:128 * part + 128], dv3[:, :, 1024 * part + 128 * h:1024 * part + 128 * h + 128], w.b, join=(part > 0))

        def proj_gen(h):
            hb = h % 2
            w = t["w"][hb]
            qT, kz, va, zs = t["qT"][hb], t["kz"][hb], t["va"][hb], t["zs"][hb]
            for Q in range(NQ):
                bk = banks[7]
                for c in range(8):
                    MM(bk[:, :], w[:, c, 0:128], uT[:, c, Q * 512:(Q + 1) * 512], c == 0, c == 7, [w.b, uT.b], [bk.b])
                EVAC(qT[:, Q * 512:(Q + 1) * 512], bk[:, :], [bk.b], [qT.b], scale=0.125)
                yield
            for Q in range(NQ):
                bk = banks[7]
                for c in range(8):
                    MM(bk[:, :], w[:, c, 128:256], uT[:, c, Q * 512:(Q + 1) * 512], c == 0, c == 7, [w.b, uT.b], [bk.b])
                EVAC(kz[0][0:64, Q * 512:(Q + 1) * 512], bk[0:64, :], [bk.b], [kz[0].b])
                EVAC(kz[1][64:128, Q * 512:(Q + 1) * 512], bk[64:128, :], [bk.b], [kz[1].b])
                yield
            for i4 in range(NT // 4):
                bk = banks[7]
                for r in range(4):
                    i = 4 * i4 + r
                    for c in range(8):
                        MM(bk[:, r * 128:(r + 1) * 128], uT[:, c, i * 128:(i + 1) * 128], w[:, c, 256:384], c == 0, c == 7, [uT.b, w.b], [bk.b])
                V("dve", "tensor_copy", [bk.b], [va.b], out=va[:, 4 * i4:4 * i4 + 4, 0:128], in_=bk[:, :].rearrange("p (r c) -> p r c", r=4))
                yield
                for r in range(4):
                    i = 4 * i4 + r
                    for c in range(8):
                        MM(bk[:, r * 128:(r + 1) * 128], uT[:, c, i * 128:(i + 1) * 128], w[:, c, 384:512], c == 0, c == 7, [uT.b, w.b], [bk.b])
                ACT(zs[:, 4 * i4:4 * i4 + 4, :], bk[:, :].rearrange("p (r c) -> p r c", r=4), AF.Silu, [bk.b], [zs.b])
                yield
            if h + 2 < 8:
                load_w(h + 2)
            yield

        def tail_gen(h):
            hb = h % 2
            od, zs, sq, rs = t["od"][hb], t["zs"][hb], t["sq"], t["rs"]
            V("pool", "tensor_tensor", [od.b], [sq.b], out=sq[:, :, :], in0=od[:, :, :], in1=od[:, :, :], op=ALU.mult)
            V("dve", "reduce_sum", [sq.b], [rs.b], out=rs[:, 0, :], in_=sq[:, :, :], axis=AX.X)
            yield
            V("dve", "tensor_scalar", [rs.b], [rs.b], out=rs[:, 1, :], in0=rs[:, 0, :], scalar1=1.0 / 128, scalar2=EPS, op0=ALU.mult, op1=ALU.add)
            ACT(rs[:, 2, :], rs[:, 1, :], AF.Sqrt, [rs.b], [rs.b])
            V("dve", "reciprocal", [rs.b], [rs.b], out=rs[:, 3, :], in_=rs[:, 2, :])
            yield
            V("dve", "tensor_tensor", [od.b, rs.b], [sq.b], out=sq[:, :, :], in0=od[:, :, :], in1=rs[:, 3, :].unsqueeze(2).broadcast_to([128, NT, 128]), op=ALU.mult)
            yield
            V("dve", "scalar_tensor_tensor", [sq.b, t["subln"].b], [sq.b], out=sq[:, :, :], in0=sq[:, :, :], scalar=1.0 - LAMBDA_INIT,
              in1=t["subln"][:, 128 * h:128 * h + 128].unsqueeze(1).broadcast_to([128, NT, 128]), op0=ALU.mult, op1=ALU.mult)
            yield
            V("dve", "tensor_tensor", [sq.b, zs.b], [t["og"].b], out=t["og"][:, :, :], in0=sq[:, :, :], in1=zs[:, :, :], op=ALU.mult)
            yield
            for i4 in range(NT // 4):
                bk = banks[7]
                bkv = bk[:, :].bitcast(BF16)
                for r in range(4):
                    TR(bkv[:, r * 128:(r + 1) * 128], t["og"][:, 4 * i4 + r, :], [t["og"].b], [bk.b])
                EVAC(oT[:, h, i4 * 512:(i4 + 1) * 512], bkv[:, 0:512], [bk.b], [oT.b])
                yield

        def chain(*gens):
            for g_ in gens:
                if g_ is not None:
                    for _ in g_:
                        yield

        def head_streams(h):
            hb = h % 2
            qT, kz, va, od = t["qT"][hb], t["kz"][hb], t["va"][hb], t["od"][hb]
            streams = []
            for Q in range(NQ):
                for m in range(2):
                    st_ = Stream()
                    st_.Q = Q
                    st_.band = False
                    kz_ = kz[m]
                    st_.q_ap = lambda gc, qT=qT: qT[:, gc]
                    st_.k_ap = lambda kj, kz_=kz_: kz_[:, kj * 128:(kj + 1) * 128]
                    st_.qk_bufs = [qT.b, kz_.b]
                    st_.pen = None
                    st_.pen_buf = None
                    st_.v_ap = lambda kj, va=va: va[:, kj, :]
                    st_.v_bufs = [va.b]
                    st_.emap = 2 * h + m
                    obs = [banks[3 + 2 * m], banks[4 + 2 * m]]
                    st_.O = [(obs[r // 2][:, (r % 2) * 129:(r % 2) * 129 + 129], obs[r // 2]) for r in range(4)]
                    st_.obs = obs
                    st_.m = m
                    st_.od = od

                    def fin(st_):
                        Q, m, od = st_.Q, st_.m, st_.od
                        for half in range(2):
                            ob = st_.obs[half]
                            ov = ob[:, 0:258].rearrange("p (r c) -> p r c", r=2)
                            i0 = 4 * Q + 2 * half
                            sm = get_stat()
                            V("dve", "reciprocal", [ob.b], [sm.b], out=sm[:, 0:2], in_=ov[:, :, 128])
                            if m == 0:
                                V("dve", "tensor_tensor", [ob.b, sm.b], [od.b], out=od[:, i0:i0 + 2, :], in0=ov[:, :, 0:128],
                                  in1=sm[:, 0:2].unsqueeze(2).broadcast_to([128, 2, 128]), op=ALU.mult)
                            else:
                                V("dve", "tensor_scalar", [sm.b, lm.b], [sm.b], out=sm[:, 2:4], in0=sm[:, 0:2], scalar1=lm[:, 5:6], scalar2=None, op0=ALU.mult)
                                tm = t["tmp"][half]
                                V("dve", "tensor_tensor", [ob.b, sm.b], [tm.b], out=tm[:, :, :], in0=ov[:, :, 0:128],
                                  in1=sm[:, 2:4].unsqueeze(2).broadcast_to([128, 2, 128]), op=ALU.mult)
                                V("pool", "tensor_tensor", [tm.b, od.b], [od.b], out=od[:, i0:i0 + 2, :], in0=od[:, i0:i0 + 2, :],
                                  in1=tm[:, :, :], op=ALU.add)
                    st_.finalize = fin
                    streams.append(st_)
            return streams

        load_w(0)
        load_w(1)
        for _ in proj_gen(0):
            pass
        for h in range(8):
            fill = chain(tail_gen(h - 1) if h > 0 else None, proj_gen(h + 1) if h < 7 else None)
            run_streams(head_streams(h), filler=fill, every=3)
        for _ in tail_gen(7):
            pass
        cast_engs[0] = ("dve", "act")
        wo3 = diff_w_out.rearrange("(c p) n -> p c n", p=128)
        for q4 in range(4):
            LOADW(t["wbig"][:, :, 256 * q4:256 * q4 + 256], wo3[:, :, 256 * q4:256 * q4 + 256], t["wbig"].b, join=(q4 > 0))
        cast_engs[0] = ("pool",)
        phase_out(t["wbig"], None, cst["gpost"], lambda i: x1_d[s, i * 128:(i + 1) * 128, :], lambda i: [B_x1[s][i]],
                  lambda i: out[s, i * 128:(i + 1) * 128, :], lambda i: [B_out[s][i]])

    with es:
        with ExitStack() as es1:
            cur_es[0] = es1
            setup_bias()
            setup_weights()
        cur_es[0] = es
        p.barrier()
        if 0 in layers:
            with ExitStack() as es2:
                cur_es[0] = es2
                nt_ = nsa_setup()
                for s in range(nseq):
                    nsa_layer(nt_, s)
            cur_es[0] = es
            p.barrier()
        if 1 in layers:
            with ExitStack() as es3:
                cur_es[0] = es3
                dt_ = diff_setup()
                for s in range(nseq):
                    if 0 not in layers:
                        for i in range(NT):
                            xb_ = xt[i % 2]
                            DMA("sp", xb_[:, :], x_in[s, i * 128:(i + 1) * 128, :], [NOB], [xb_.b])
                            DMA("sp", x1_d[s, i * 128:(i + 1) * 128, :], xb_[:, :], [xb_.b], [B_x1[s][i]])
                    diff_layer(dt_, s)
            cur_es[0] = es
        p.emit()
    return nc


INPUT_NAMES = ["rel_bias_table", "norm_pre", "norm_post", "nsa_w_in", "nsa_cmp_pe_k", "nsa_cmp_w1_k", "nsa_cmp_w2_k",
               "nsa_cmp_pe_v", "nsa_cmp_w1_v", "nsa_cmp_w2_v", "nsa_w_out", "diff_w_in", "diff_lambda_q1", "diff_lambda_k1",
               "diff_lambda_q2", "diff_lambda_k2", "diff_subln", "diff_w_out"]


def make_in_maps(inputs, n_cores, nseq, S):
    consts = host_consts(S)
    shared = {}
    for k in INPUT_NAMES:
        a = np.ascontiguousarray(np.asarray(inputs[k], dtype=np.float32))
        if a.ndim == 3:
            a = a[0]
        elif k.startswith("diff_lambda") or k == "diff_subln":
            a = a.reshape(1, -1)
        shared[k] = np.ascontiguousarray(a)
    shared.update(consts)
    x = np.asarray(inputs["x"], dtype=np.float32)
    maps = []
    for c in range(n_cores):
        m = dict(shared)
        m["x"] = np.ascontiguousarray(x[c * nseq:(c + 1) * nseq])
        maps.append(m)
    return maps


def kernel(**inputs):
    x = np.asarray(inputs["x"])
    B, S, _ = x.shape
    n_cores = 8
    nseq = B // n_cores
    nc = build(nseq, S)
    in_maps = make_in_maps(inputs, n_cores, nseq, S)
    res = run_bass_kernel_spmd(nc, in_maps, core_ids=list(range(n_cores)))
    return np.concatenate([np.asarray(r["out"]) for r in res.results], axis=0).astype(np.float32)
```

```python
import math
from contextlib import ExitStack

import numpy as np
import ml_dtypes

import concourse.bass as bass
import concourse.mybir as mybir
from concourse.bass_utils import run_bass_kernel_spmd

F32 = mybir.dt.float32
BF16 = mybir.dt.bfloat16
AF = mybir.ActivationFunctionType
ALU = mybir.AluOpType
AX = mybir.AxisListType

D = 1024
NSA_IN = 3632
EPS = 1e-6
PEN = -30000.0
LAMBDA_INIT = 0.8 - 0.6 * math.exp(-0.3 * 1)


class Buf:
    __slots__ = ("name", "writers", "readers", "prev")

    def __init__(self, name=""):
        self.name = name
        self.writers = []
        self.readers = []
        self.prev = []


class Op:
    __slots__ = ("eng", "fn", "dma", "deps", "needs_sig", "sig")

    def __init__(self, eng, fn, dma):
        self.eng = eng
        self.fn = fn
        self.dma = dma
        self.deps = []
        self.needs_sig = False
        self.sig = None


ENGS = ("pe", "act", "dve", "pool", "sp")


class Prog:
    def __init__(self, nc, n_dma_sems=48):
        self.nc = nc
        self.ops = {e: [] for e in ENGS}
        self.n_dma_sems = n_dma_sems
        self.dma_last = [None] * n_dma_sems
        self.dma_cnt = [0] * n_dma_sems
        self.dma_rr = 0
        self.pending = {}

    def barrier(self):
        lasts = []
        for e in ENGS:
            for op in reversed(self.ops[e]):
                if not op.dma:
                    op.needs_sig = True
                    lasts.append(op)
                    break
        for j in range(self.n_dma_sems):
            if self.dma_last[j] is not None:
                lasts.append(self.dma_last[j])
        self.pending = {e: list(lasts) for e in ENGS}

    def add(self, eng, fn, reads=(), writes=(), dma=False, join=False):
        op = Op(eng, fn, dma)
        if self.pending.get(eng):
            op.deps.extend(self.pending.pop(eng))
        for b in reads:
            for w in b.writers:
                self._dep(w, op, True)
        for b in writes:
            if not join:
                for w in b.writers:
                    self._dep(w, op, False)
            else:
                for r in b.prev:
                    self._dep(r, op, False)
            for r in b.readers:
                self._dep(r, op, False)
        for b in writes:
            if join:
                b.writers.append(op)
            else:
                b.prev = list(b.writers) + list(b.readers)
                b.writers = [op]
                b.readers = []
        for b in reads:
            b.readers.append(op)
        if dma:
            j = self.dma_rr
            self.dma_rr = (j + 1) % self.n_dma_sems
            prev = self.dma_last[j]
            if prev is not None:
                op.deps.append(prev)
            self.dma_cnt[j] += 1
            op.sig = (("d", j), 16 * self.dma_cnt[j])
            op.needs_sig = True
            self.dma_last[j] = op
        self.ops[eng].append(op)
        return op

    def _dep(self, p, c, raw):
        if p is c:
            return
        if (not p.dma) and (not c.dma) and p.eng == c.eng:
            if p.eng == "pe" or not raw:
                return
        p.needs_sig = True
        c.deps.append(p)

    def emit(self):
        nc = self.nc
        with ExitStack() as es:
            esem = {e: es.enter_context(nc.semaphore("s_" + e)) for e in ENGS}
            dsem = [es.enter_context(nc.semaphore("d%d" % j)) for j in range(self.n_dma_sems)]
            for e in ENGS:
                cnt = 0
                for op in self.ops[e]:
                    if op.dma:
                        continue
                    if op.needs_sig:
                        cnt += 1
                        op.sig = (("e", e), cnt)

            def semof(key):
                return esem[key[1]] if key[0] == "e" else dsem[key[1]]

            block = es.enter_context(nc.Block())
            engobj = {"pe": "tensor", "act": "scalar", "dve": "vector", "pool": "gpsimd", "sp": "sync"}

            def make(e):
                def body(eng):
                    waited = {}
                    for op in self.ops[e]:
                        need = {}
                        for p in op.deps:
                            k, v = p.sig
                            if need.get(k, 0) < v:
                                need[k] = v
                        for k, v in need.items():
                            if waited.get(k, 0) >= v:
                                continue
                            eng.wait_ge(semof(k), v)
                            waited[k] = v
                        ins = op.fn(eng)
                        if op.needs_sig:
                            k, v = op.sig
                            ins.then_inc(semof(k), 16 if op.dma else 1)
                    if e == "sp":
                        for j in range(self.n_dma_sems):
                            if self.dma_cnt[j] > 0:
                                eng.wait_ge(dsem[j], 16 * self.dma_cnt[j])
                return body

            for e in ENGS:
                getattr(block, engobj[e])(make(e))


def _rel_bucket(n):
    n = np.maximum(n, 0)
    nf = np.maximum(n, 1).astype(np.float32)
    large = 16 + (np.log(nf / np.float32(16)) / np.float32(math.log(8.0)) * np.float32(16)).astype(np.int32)
    large = np.minimum(large, 31)
    return np.where(n < 16, n, large)


def host_consts(S):
    NT = S // 128
    ncmp = S // 16 - 1
    bf = ml_dtypes.bfloat16
    c = {}
    c["c_ident"] = np.eye(128, dtype=np.float32).astype(bf)
    b = _rel_bucket(np.arange(128))
    oh = np.zeros((32, 128), np.float32)
    oh[b, np.arange(128)] = 1.0
    oh[31, :] -= 1.0
    c["c_onehot"] = oh
    cmp_lo = np.arange(ncmp) * 16
    sel_lo = np.arange(S // 64) * 64
    ov = np.clip(np.minimum(cmp_lo[:, None] + 32, sel_lo[None, :] + 64)
                 - np.maximum(cmp_lo[:, None], sel_lo[None, :]), 0, None).astype(np.float32) / 32.0
    ovp = np.zeros((127, 32), np.float32)
    ovp[:ncmp, :S // 64] = ov
    c["c_ovl"] = ovp.astype(bf)
    X = np.zeros((32, S), np.float32)
    X[np.arange(S) // 64, np.arange(S)] = 1.0
    c["c_X"] = X.astype(bf)
    k = np.arange(128)[:, None]
    q = np.arange(128)[None, :]
    c["c_M4"] = (q < k).astype(np.float32).astype(bf)
    t = np.arange(S)
    cur = t // 64
    n = np.arange(32)[None, :]
    forced = (n == 0) | (n == cur[:, None]) | (n == cur[:, None] - 1)
    future = n > cur[:, None]
    keep = (~(forced | future)).astype(np.float32)
    addc = np.where(forced, 1e9, np.where(future, -1e9, 0.0)).astype(np.float32)
    c["c_keep"] = np.ascontiguousarray(keep.reshape(NT, 128, 32).transpose(1, 0, 2))
    c["c_addc"] = np.ascontiguousarray(addc.reshape(NT, 128, 32).transpose(1, 0, 2))
    return c


def build(nseq, S, layers=(0, 1)):
    NT = S // 128
    NQ = S // 512
    NCMP = S // 16 - 1
    nc = bass.Bass("TRN2", target_bir_lowering=False)
    p = Prog(nc)

    def din(name, shape, dt=F32):
        return nc.dram_tensor(name, list(shape), dt, kind="ExternalInput").ap()

    x_in = din("x", [nseq, S, D])
    table = din("rel_bias_table", [32, 16])
    norm_pre = din("norm_pre", [2, D])
    norm_post = din("norm_post", [2, D])
    nsa_w_in = din("nsa_w_in", [D, NSA_IN])
    pe_k = din("nsa_cmp_pe_k", [32, 64])
    w1_k = din("nsa_cmp_w1_k", [2048, 128])
    w2_k = din("nsa_cmp_w2_k", [128, 64])
    pe_v = din("nsa_cmp_pe_v", [32, 64])
    w1_v = din("nsa_cmp_w1_v", [2048, 128])
    w2_v = din("nsa_cmp_w2_v", [128, 64])
    nsa_w_out = din("nsa_w_out", [D, D])
    diff_w_in = din("diff_w_in", [D, 4096])
    lq1 = din("diff_lambda_q1", [1, 64])
    lk1 = din("diff_lambda_k1", [1, 64])
    lq2 = din("diff_lambda_q2", [1, 64])
    lk2 = din("diff_lambda_k2", [1, 64])
    subln = din("diff_subln", [1, D])
    diff_w_out = din("diff_w_out", [D, D])
    c_ident = din("c_ident", [128, 128], BF16)
    c_onehot = din("c_onehot", [32, 128])
    c_ovl = din("c_ovl", [127, 32], BF16)
    c_X = din("c_X", [32, S], BF16)
    c_M4 = din("c_M4", [128, 128], BF16)
    c_keep = din("c_keep", [128, NT, 32])
    c_addc = din("c_addc", [128, NT, 32])
    out = nc.dram_tensor("out", [nseq, S, D], F32, kind="ExternalOutput").ap()
    x1_d = nc.dram_tensor("x1_scr", [nseq, S, D], F32).ap()
    De = nc.dram_tensor("De_scr", [16, 128, 512], BF16).ap()
    Dc = nc.dram_tensor("Dc_scr", [16, 127, 4096], BF16).ap()
    B_x1 = [[Buf("x1d%d_%d" % (s, i)) for i in range(NT)] for s in range(nseq)]
    B_out = [[Buf("out%d_%d" % (s, i)) for i in range(NT)] for s in range(nseq)]
    B_De = Buf("De")
    B_Dc = Buf("Dc")
    NOB = Buf("const_in")

    import os as _os
    es = ExitStack()
    cur_es = [es]
    DEBUG = bool(_os.environ.get("KDEBUG"))
    dbg_seen = set()

    def dbg(name, tt, ap, dt=F32):
        if not DEBUG or name in dbg_seen:
            return
        dbg_seen.add(name)
        o = nc.dram_tensor("dbg_" + name, list(ap.shape), dt, kind="ExternalOutput").ap()
        p.add("sp", lambda e: e.dma_start(out=o, in_=ap), reads=[tt.b], writes=[Buf()], dma=True)

    class TT:
        def __init__(self, t, name):
            self.t = t
            self.b = Buf(name)

        def __getitem__(self, k):
            return self.t[k]

    def sb(name, shape, dt=F32):
        if _os.environ.get("KDEBUG_SB"):
            print("SB", name, shape, "remaining", nc.sbuf_bytes_remaining)
        return TT(cur_es[0].enter_context(nc.sbuf_tensor(name, list(shape), dt)), name)

    banks = [TT(es.enter_context(nc.psum_tensor("bank%d" % i, [128, 512], F32)), "bank%d" % i) for i in range(8)]

    def DMA(eng, out_ap, in_ap, reads, writes, join=False, **kw):
        p.add(eng, lambda e: e.dma_start(out=out_ap, in_=in_ap, **kw), reads=reads, writes=writes, dma=True, join=join)

    def MM(out_ap, lhsT, rhs, start, stop, reads, writes, skip=False):
        if skip:
            p.add("pe", lambda e: e.matmul(out_ap, lhsT=lhsT, rhs=rhs, start=start, stop=stop, skip_group_check=True), reads=reads, writes=writes)
        else:
            p.add("pe", lambda e: e.matmul(out_ap, lhsT=lhsT, rhs=rhs, start=start, stop=stop), reads=reads, writes=writes)

    def TR(out_ap, in_ap, reads, writes):
        p.add("pe", lambda e: e.transpose(out=out_ap, in_=in_ap, identity=ident[:, :]), reads=list(reads) + [ident.b], writes=writes)

    def ACT(out_ap, in_ap, func, reads, writes, **kw):
        p.add("act", lambda e: e.activation(out=out_ap, in_=in_ap, func=func, **kw), reads=reads, writes=writes)

    def V(eng, name, reads, writes, *a, **kw):
        p.add(eng, lambda e: getattr(e, name)(*a, **kw), reads=reads, writes=writes)

    evac_rr = [0]

    def EVAC(out_ap, in_ap, reads, writes, scale=None):
        evac_rr[0] ^= 1
        if evac_rr[0]:
            if scale is None:
                ACT(out_ap, in_ap, AF.Copy, reads, writes)
            else:
                ACT(out_ap, in_ap, AF.Copy, reads, writes, scale=float(scale))
        else:
            if scale is None:
                V("dve", "tensor_copy", reads, writes, out=out_ap, in_=in_ap)
            else:
                V("dve", "tensor_scalar", reads, writes, out=out_ap, in0=in_ap, scalar1=float(scale), scalar2=None, op0=ALU.mult)

    ident = sb("ident", [128, 128], BF16)
    DMA("sp", ident[:, :], c_ident[:, :], [NOB], [ident.b])
    M4 = sb("M4", [128, 128], BF16)
    DMA("sp", M4[:, :], c_M4[:, :], [NOB], [M4.b])
    Ee = sb("Ee", [128, 16, 256], BF16)
    uT = sb("uT", [128, 8, S], BF16)
    oT = sb("oT", [128, 8, S], BF16)
    xt = [sb("xt%d" % i, [128, D]) for i in range(2)]
    ub = [sb("ub%d" % i, [128, D], BF16) for i in range(2)]
    stat = [sb("stat%d" % i, [128, 8]) for i in range(4)]
    NPT = int(_os.environ.get("K_NPT", "6"))
    PT = [sb("PT%d" % i, [128, 512], BF16) for i in range(NPT)]
    yout = [sb("yout0", [128, D])]
    cst = {}

    def setup_bias():
        tab = sb("tab", [32, 16])
        oh = sb("oh", [32, 128])
        fse = sb("fse", [16, 512], BF16)
        fsc = sb("fsc", [16, 4096], BF16)
        DMA("sp", tab[:, :], table[:, :], [NOB], [tab.b])
        DMA("sp", oh[:, :], c_onehot[:, :], [NOB], [oh.b])
        MM(banks[0][0:16, 0:128], tab[:, :], oh[:, :], True, True, [tab.b, oh.b], [banks[0].b])
        V("pool", "memset", [], [fse.b], fse[:, :], 0.0)
        V("pool", "memset", [fse.b], [fse.b], fse[:, 128:384], 1.0)
        V("pool", "memset", [], [fsc.b], fsc[:, :], 0.0)
        V("pool", "memset", [fsc.b], [fsc.b], fsc[:, 159:2064], 1.0)
        ACT(fse[:, 0:128], banks[0][0:16, 0:128], AF.Exp, [banks[0].b, fse.b], [fse.b])
        ACT(fsc[:, 31:159], banks[0][0:16, 0:128], AF.Exp, [banks[0].b, fsc.b], [fsc.b])
        DMA("sp", De, fse[:, :].unsqueeze(1).broadcast_to([16, 128, 512]), [fse.b], [B_De])
        DMA("sp", Dc, fsc[:, :].unsqueeze(1).broadcast_to([16, 127, 4096]), [fsc.b], [B_Dc])
        for h in range(16):
            DMA("sp", Ee[:, h, :], bass.AP(De.tensor, h * 128 * 512, [[511, 128], [1, 256]]), [B_De], [Ee.b], join=(h > 0))

    def load_norm_gains(l):
        cst["gpre"] = sb("gpre%d" % l, [128, D])
        cst["gpost"] = sb("gpost%d" % l, [128, D])
        DMA("sp", cst["gpre"][:, :], norm_pre[l:l + 1, :].partition_broadcast(128), [NOB], [cst["gpre"].b])
        DMA("sp", cst["gpost"][:, :], norm_post[l:l + 1, :].partition_broadcast(128), [NOB], [cst["gpost"].b])

    def Ec_src(h, c0, ncols):
        return bass.AP(Dc.tensor, h * 127 * 4096 + c0, [[4080, NCMP], [1, ncols]])

    STG_N = 1024
    stgs = {}
    cast_rr = [0]

    cast_engs = [("dve", "act")]

    def CAST(out_ap, in_ap, reads, writes, join=False):
        e = cast_engs[0][cast_rr[0] % len(cast_engs[0])]
        cast_rr[0] += 1
        if e == "act":
            p.add("act", lambda en: en.activation(out=out_ap, in_=in_ap, func=AF.Copy), reads=reads, writes=writes, join=join)
        else:
            p.add(e, lambda en: en.tensor_copy(out=out_ap, in_=in_ap), reads=reads, writes=writes, join=join)

    def LOADW(dst_ap, src_ap, dst_buf, join=False):
        stg = stgs["stg"]
        shp = list(dst_ap.shape)
        P_ = shp[0]
        mid = 1
        for d_ in shp[1:-1]:
            mid *= d_
        last = shp[-1]
        step = max(1, STG_N // mid)
        bp = dst_ap.base_partition()
        first = True
        for c0 in range(0, last, step):
            c1 = min(last, c0 + step)
            n = mid * (c1 - c0)
            assert n <= STG_N, shp
            st_ = stg[stgs["rr"]]
            stgs["rr"] = (stgs["rr"] + 1) % len(stg)
            sv = st_[bp:bp + P_, 0:n]
            if len(shp) == 3:
                sv = sv.rearrange("p (a b) -> p a b", a=shp[1])
                d_ap, s_ap = dst_ap[:, :, c0:c1], src_ap[:, :, c0:c1]
            else:
                d_ap, s_ap = dst_ap[:, c0:c1], src_ap[:, c0:c1]
            DMA("sp", sv, s_ap, [NOB], [st_.b])
            CAST(d_ap, sv, [st_.b], [dst_buf], join=(join or not first))
            first = False

    GA = 652
    WG = nc.dram_tensor("WG_scr", [4, 128, 8, 908], BF16).ap()
    WD = nc.dram_tensor("WD_scr", [8, 128, 8, 512], BF16).ap()
    WO = nc.dram_tensor("WO_scr", [2, 128, 8, 1024], BF16).ap()
    W1S = nc.dram_tensor("W1_scr", [128, 32, 128], BF16).ap()
    W2S = nc.dram_tensor("W2_scr", [128, 128], BF16).ap()
    B_WG = [Buf("WG%d" % g) for g in range(4)]
    B_WD = [Buf("WD%d" % h) for h in range(8)]
    B_WO = [Buf("WO%d" % l) for l in range(2)]
    B_W1 = Buf("W1S")
    B_W2 = Buf("W2S")

    def setup_weights():
        stgs["stg"] = [sb("stg%d" % i, [128, STG_N]) for i in range(4)]
        stgs["rr"] = 0
        asm = [sb("asm%d" % i, [128, 8, 1024], BF16) for i in range(2)]
        k = 0
        wv3 = nsa_w_in.rearrange("(c p) n -> p c n", p=128)
        if 0 in layers:
            for g in range(4):
                a_ = asm[k % 2]
                k += 1
                pieces = [(0, 256 * g, 256), (256, 1024 + 64 * g, 64), (320, 1280 + 64 * g, 64), (384, 1536 + 64 * g, 64),
                          (448, 2048 + 64 * g, 64), (512, 1792 + 64 * g, 64), (576, 2304 + 64 * g, 64), (640, 2560 + 12 * g, 12),
                          (652, 2608 + 256 * g, 256)]
                for pi, (d0, s0, n) in enumerate(pieces):
                    LOADW(a_[:, :, d0:d0 + n], wv3[:, :, s0:s0 + n], a_.b, join=(pi > 0))
                DMA("sp", WG[g], a_[:, :, 0:908], [a_.b], [B_WG[g]])
            a_ = asm[k % 2]
            k += 1
            for lh in range(2):
                ls = slice(16 * lh, 16 * lh + 16)
                LOADW(a_[0:64, :, :].rearrange("p c n -> p (c n)")[:, 0:4096].rearrange("p (l h) -> p l h", l=32)[:, ls, :],
                      w1_k.rearrange("(l d) h -> d l h", d=64)[:, ls, :], a_.b, join=(lh > 0))
                LOADW(a_[64:128, :, :].rearrange("p c n -> p (c n)")[:, 0:4096].rearrange("p (l h) -> p l h", l=32)[:, ls, :],
                      w1_v.rearrange("(l d) h -> d l h", d=64)[:, ls, :], a_.b, join=True)
            LOADW(a_[:, 4, 0:64], w2_k[:, :], a_.b, join=True)
            LOADW(a_[:, 4, 64:128], w2_v[:, :], a_.b, join=True)
            DMA("sp", W1S, a_[:, :, :].rearrange("p c n -> p (c n)")[:, 0:4096].rearrange("p (l h) -> p l h", l=32), [a_.b], [B_W1])
            DMA("sp", W2S, a_[:, 4, 0:128], [a_.b], [B_W2])
        for l, w_ in enumerate((nsa_w_out,)):
            if l not in layers:
                continue
            a_ = asm[k % 2]
            k += 1
            wo3 = w_.rearrange("(c p) n -> p c n", p=128)
            for q4 in range(4):
                LOADW(a_[:, :, 256 * q4:256 * q4 + 256], wo3[:, :, 256 * q4:256 * q4 + 256], a_.b, join=(q4 > 0))
            DMA("sp", WO[l], a_[:, :, :], [a_.b], [B_WO[l]])

    stat_rr = [0]

    def get_stat():
        stat_rr[0] = (stat_rr[0] + 1) % 4
        return stat[stat_rr[0]]

    def phase_norm_T(src_ap_fn, src_bufs, g_tile):
        for i in range(NT):
            xb_ = xt[i % 2]
            u_ = ub[i % 2]
            DMA("sp", xb_[:, :], src_ap_fn(i), src_bufs(i), [xb_.b])
            st = get_stat()
            ACT(u_[:, :], xb_[:, :], AF.Square, [xb_.b], [u_.b, st.b], accum_out=st[:, 0:1])
            V("dve", "tensor_scalar", [st.b], [st.b], out=st[:, 1:2], in0=st[:, 0:1], scalar1=1.0 / D, scalar2=EPS, op0=ALU.mult, op1=ALU.add)
            ACT(st[:, 2:3], st[:, 1:2], AF.Sqrt, [st.b], [st.b])
            V("dve", "reciprocal", [st.b], [st.b], out=st[:, 3:4], in_=st[:, 2:3])
            V("dve", "scalar_tensor_tensor", [xb_.b, st.b, g_tile.b], [u_.b], out=u_[:, :], in0=xb_[:, :], scalar=st[:, 3:4], in1=g_tile[:, :], op0=ALU.mult, op1=ALU.mult)
            bk = banks[6 + (i % 2)]
            bkv = bk[:, :].bitcast(BF16)
            for c in range(8):
                TR(bkv[:, c * 128:(c + 1) * 128], u_[:, c * 128:(c + 1) * 128], [u_.b], [bk.b])
            EVAC(uT[:, :, i * 128:(i + 1) * 128], bkv.rearrange("p (c t) -> p c t", c=8), [bk.b], [uT.b])

    pf_rr = [0]

    def proj_fm(wt, wb, outs, scale=None, bank_ids=(5, 6, 7), M=128):
        for Q in range(NQ):
            bk = banks[bank_ids[pf_rr[0] % len(bank_ids)]]
            pf_rr[0] += 1
            for c in range(8):
                MM(bk[0:M, :], wt[:, c, :], uT[:, c, Q * 512:(Q + 1) * 512], c == 0, c == 7, [wb, uT.b], [bk.b])
            for (rs_, fn_, ob_) in outs:
                EVAC(fn_(Q), bk[rs_, :], [bk.b], [ob_], scale=scale)

    def phase_out(wbig, w_out_d, gp, res_fn, res_bufs, dst_fn, dst_bufs):
        if w_out_d is not None:
            DMA("sp", wbig[:, :, :], WO[w_out_d], [B_WO[w_out_d]], [wbig.b])
        for i in range(NT):
            xb_ = xt[i % 2]
            DMA("sp", xb_[:, :], res_fn(i), res_bufs(i), [xb_.b])
            bk = [banks[4 + 2 * (i % 2)], banks[5 + 2 * (i % 2)]]
            for half in range(2):
                for c in range(8):
                    MM(bk[half][:, :], oT[:, c, i * 128:(i + 1) * 128], wbig[:, c, half * 512:(half + 1) * 512], c == 0, c == 7, [oT.b, wbig.b], [bk[half].b])
            st = get_stat()
            ACT(ub[0][:, 0:512], bk[0][:, :], AF.Square, [bk[0].b], [ub[0].b, st.b], accum_out=st[:, 0:1])
            ACT(ub[0][:, 512:1024], bk[1][:, :], AF.Square, [bk[1].b], [ub[0].b, st.b], accum_out=st[:, 1:2])
            V("dve", "tensor_tensor", [st.b], [st.b], out=st[:, 2:3], in0=st[:, 0:1], in1=st[:, 1:2], op=ALU.add)
            V("dve", "tensor_scalar", [st.b], [st.b], out=st[:, 3:4], in0=st[:, 2:3], scalar1=1.0 / D, scalar2=EPS, op0=ALU.mult, op1=ALU.add)
            ACT(st[:, 4:5], st[:, 3:4], AF.Sqrt, [st.b], [st.b])
            V("dve", "reciprocal", [st.b], [st.b], out=st[:, 5:6], in_=st[:, 4:5])
            yo = yout[0]
            for half in range(2):
                sl = slice(half * 512, (half + 1) * 512)
                V("dve", "scalar_tensor_tensor", [bk[half].b, st.b, gp.b], [yo.b], out=yo[:, sl], in0=bk[half][:, :], scalar=st[:, 5:6], in1=gp[:, sl], op0=ALU.mult, op1=ALU.mult)
            V("pool", "tensor_tensor", [yo.b, xb_.b], [yo.b], out=yo[:, :], in0=yo[:, :], in1=xb_[:, :], op=ALU.add)
            DMA("sp", dst_fn(i), yo[:, :], [yo.b], dst_bufs(i))


    class Stream:
        pass

    S_BANKS = [banks[0], banks[1], banks[2]]
    s_rr = [0]
    pt_rr = [0]
    LOOK = int(_os.environ.get("K_LOOK", "4"))

    def run_streams(streams, filler=None, every=3):
        pending = []
        ntile = [0]

        def emit_pv(item):
            st_, kj, c0, c1, ptb = item
            for r in range(c0, c1):
                qi = 4 * st_.Q + r
                first = max(0, qi - 4) if st_.band else 0
                o_ap, o_tt = st_.O[r]
                if not hasattr(st_, "started"):
                    st_.started = set()
                is_first = id(o_tt) not in st_.started
                st_.started.add(id(o_tt))
                MM(o_ap, ptb[:, r * 128:(r + 1) * 128], st_.v_ap(kj), is_first, kj == qi,
                   [ptb.b] + st_.v_bufs, [o_tt.b], skip=True)
            if kj == st_.last_kj:
                st_.finalize(st_)

        for st_ in streams:
            Q = st_.Q
            kj_lo = max(0, 4 * Q - 4) if st_.band else 0
            st_.last_kj = 4 * Q + 3
            for kj in range(kj_lo, 4 * Q + 4):
                rd0 = kj - 4 * Q
                c0 = max(0, rd0)
                c1 = min(4, rd0 + 5) if st_.band else 4
                sbk = S_BANKS[s_rr[0]]
                s_rr[0] = (s_rr[0] + 1) % len(S_BANKS)
                ptb = PT[pt_rr[0]]
                pt_rr[0] = (pt_rr[0] + 1) % len(PT)
                cols = slice(c0 * 128, c1 * 128)
                gcols = slice(Q * 512 + c0 * 128, Q * 512 + c1 * 128)
                MM(sbk[:, cols], st_.k_ap(kj), st_.q_ap(gcols), True, st_.pen is None, st_.qk_bufs, [sbk.b])
                if st_.pen is not None:
                    MM(sbk[:, cols], cst["Xs"][0:32, kj * 128:(kj + 1) * 128], st_.pen[0:32, gcols], False, True,
                       [cst["Xs"].b, st_.pen_buf], [sbk.b])
                ACT(ptb[:, cols], sbk[:, cols], AF.Exp, [sbk.b], [ptb.b])
                if -1 <= rd0 <= 3:
                    lo = max(rd0, 0)
                    hi = min(rd0 + 2, 4)
                    eo = (lo - rd0) * 128
                    V("dve", "tensor_tensor", [ptb.b, Ee.b], [ptb.b], out=ptb[:, lo * 128:hi * 128], in0=ptb[:, lo * 128:hi * 128],
                      in1=Ee[:, st_.emap, eo:eo + (hi - lo) * 128], op=ALU.mult)
                if st_.band:
                    r4 = rd0 + 4
                    if 0 <= r4 <= 3:
                        V("dve", "tensor_tensor", [ptb.b, M4.b], [ptb.b], out=ptb[:, r4 * 128:(r4 + 1) * 128], in0=ptb[:, r4 * 128:(r4 + 1) * 128],
                          in1=M4[:, :], op=ALU.mult)
                pending.append((st_, kj, c0, c1, ptb))
                if len(pending) > LOOK:
                    emit_pv(pending.pop(0))
                ntile[0] += 1
                if filler is not None and ntile[0] % every == 0:
                    next(filler, None)
        while pending:
            emit_pv(pending.pop(0))
        if filler is not None:
            for _ in filler:
                pass

    def nsa_setup():
        t = {}
        load_norm_gains(0)
        t["keep"] = sb("keep", [128, NT, 32])
        t["addc"] = sb("addc", [128, NT, 32])
        DMA("sp", t["keep"][:, :, :], c_keep[:, :, :], [NOB], [t["keep"].b])
        DMA("sp", t["addc"][:, :, :], c_addc[:, :, :], [NOB], [t["addc"].b])
        t["w1"] = sb("w1", [128, 32, 128], BF16)
        DMA("sp", t["w1"][:, :, :], W1S, [B_W1], [t["w1"].b])
        t["w2"] = sb("w2", [128, 128], BF16)
        DMA("sp", t["w2"][:, :], W2S, [B_W2], [t["w2"].b])
        t["w2k"] = sb("w2k", [128, 128], BF16)
        V("pool", "memset", [], [t["w2k"].b], t["w2k"][:, :], 0.0)
        V("pool", "tensor_copy", [t["w2"].b, t["w2k"].b], [t["w2k"].b], out=t["w2k"][:, 0:64], in_=t["w2"][:, 0:64])
        pes = sb("pes", [32, 128])
        DMA("sp", pes[:, 0:64], pe_k[:, :], [NOB], [pes.b])
        DMA("sp", pes[:, 64:128], pe_v[:, :], [NOB], [pes.b], join=True)
        pesb = sb("pesb", [32, 128], BF16)
        V("dve", "tensor_copy", [pes.b], [pesb.b], out=pesb[:, :], in_=pes[:, :])
        peT = sb("peT", [128, 32], BF16)
        bkv = banks[3][:, :].bitcast(BF16)
        p.add("pe", lambda e: e.transpose(out=bkv[:, 0:32], in_=pesb[:, :], identity=ident[0:32, 0:32]), reads=[pesb.b, ident.b], writes=[banks[3].b])
        V("dve", "tensor_copy", [banks[3].b], [peT.b], out=peT[:, :], in_=bkv[:, 0:32])
        t["peh"] = sb("peh", [128, 2])
        for kv in range(2):
            ps_ = slice(64 * kv, 64 * kv + 64)
            for l in range(32):
                MM(banks[4 + kv][:, 0:1], t["w1"][ps_, l, :], peT[ps_, l:l + 1], l == 0, l == 31, [t["w1"].b, peT.b], [banks[4 + kv].b])
        for kv in range(2):
            V("dve", "tensor_copy", [banks[4 + kv].b], [t["peh"].b], out=t["peh"][:, kv:kv + 1], in_=banks[4 + kv][:, 0:1])
        t["Vca"] = sb("Vca", [127, 97], BF16)
        V("pool", "memset", [], [t["Vca"].b], t["Vca"][:, :], 1.0)
        DMA("sp", t["Vca"][:, 65:97], c_ovl[:, :], [t["Vca"].b], [t["Vca"].b])
        t["wgA"] = sb("wgA", [128, 8, GA], BF16)
        t["wgZ"] = sb("wgZ", [128, 8, 256], BF16)
        t["qa"] = [sb("qa%d" % i, [128, S], BF16) for i in range(4)]
        for i in range(4):
            V("pool", "memset", [], [t["qa"][i].b], t["qa"][i][:, :], 0.0)
        t["cT"] = sb("cT", [128, S], BF16)
        t["ksx"] = sb("ksx", [128, S], BF16)
        V("pool", "memset", [], [t["ksx"].b], t["ksx"][:, :], 0.0)
        DMA("sp", t["ksx"][64:96, :], c_X[:, :], [t["ksx"].b], [t["ksx"].b])
        t["kwz"] = sb("kwz", [128, S], BF16)
        V("pool", "memset", [], [t["kwz"].b], t["kwz"][:, :], 0.0)
        t["vsa"] = sb("vsa", [128, NT, 65], BF16)
        t["vwa"] = sb("vwa", [128, NT, 65], BF16)
        V("pool", "memset", [], [t["vsa"].b], t["vsa"][:, :, :], 1.0)
        V("pool", "memset", [], [t["vwa"].b], t["vwa"][:, :, :], 1.0)
        t["gate"] = sb("gate", [128, NT, 12])
        accraw = sb("acc", [128, max(NT * 256, 4096)])
        t["acc"] = TT(accraw[:, 0:NT * 256].rearrange("p (i c) -> p i c", i=NT), "acc")
        t["acc"].b = accraw.b
        t["wbig"] = TT(accraw[:, 0:4096].bitcast(BF16).rearrange("p (c n) -> p c n", c=8), "wbig")
        t["wbig"].b = accraw.b
        t["ha"] = [sb("ha%d" % i, [128, 128], BF16) for i in range(2)]
        t["kcmp"] = sb("kcmp", [128, 128], BF16)
        t["Ec"] = [sb("Ec%d" % i, [127, 512], BF16) for i in range(3)]
        t["cm"] = sb("cm", [128, 4, 4, 97])
        t["sm"] = [sb("sm0", [128, 32]), sb("sm1", [128, 512]), sb("sm2", [128, 288])]
        t["penb"] = sb("penb", [128, 4, 96], BF16)
        V("pool", "memset", [], [t["penb"].b], t["penb"][:, :, :], 0.0)
        t["tmp"] = [sb("tmpo%d" % i, [128, 4, 64]) for i in range(2)]
        t["th"] = [sb("th%d" % i, [128, 256], BF16) for i in range(2)]
        t["zsg"] = sb("zsg", [128, NT, 256], BF16)
        t["og"] = [sb("og%d" % i, [128, 256], BF16) for i in range(2)]
        DMA("sp", t["wgA"][:, :, :], WG[0][:, :, 0:GA], [B_WG[0]], [t["wgA"].b])
        DMA("sp", t["wgZ"][:, :, :], WG[0][:, :, GA:908], [B_WG[0]], [t["wgZ"].b])
        return t

    def nsa_layer(t, s):
        phase_norm_T(lambda i: x_in[s, i * 128:(i + 1) * 128, :], lambda i: [NOB], cst["gpre"])
        wgA, wgZ = t["wgA"], t["wgZ"]
        for g in range(4):
            g_next = (g + 1) % 4
            has_next = (g < 3) or (s + 1 < nseq)
            for j in range(4):
                qa_ = t["qa"][j]
                proj_fm(wgA[:, :, 64 * j:64 * j + 64], wgA.b, [(slice(0, 64), (lambda Q, qa_=qa_: qa_[0:64, Q * 512:(Q + 1) * 512]), qa_.b)], scale=0.125, M=64)
            proj_fm(wgA[:, :, 256:384], wgA.b, [(slice(0, 128), (lambda Q: t["cT"][:, Q * 512:(Q + 1) * 512]), t["cT"].b)])
            proj_fm(wgA[:, :, 384:448], wgA.b, [(slice(0, 64), (lambda Q: t["ksx"][0:64, Q * 512:(Q + 1) * 512]), t["ksx"].b)], M=64)
            proj_fm(wgA[:, :, 448:512], wgA.b, [(slice(0, 64), (lambda Q: t["kwz"][0:64, Q * 512:(Q + 1) * 512]), t["kwz"].b)], M=64)
            for i in range(NT):
                bk = banks[3 + (i % 2)]
                for c in range(8):
                    MM(bk[:, 0:140], uT[:, c, i * 128:(i + 1) * 128], wgA[:, c, 512:652], c == 0, c == 7, [uT.b, wgA.b], [bk.b])
                V("dve", "tensor_copy", [bk.b], [t["vsa"].b], out=t["vsa"][:, i, 0:64], in_=bk[:, 0:64])
                V("dve", "tensor_copy", [bk.b], [t["vwa"].b], out=t["vwa"][:, i, 0:64], in_=bk[:, 64:128])
                ACT(t["gate"][:, i, :], bk[:, 128:140], AF.Sigmoid, [bk.b], [t["gate"].b])
            if has_next:
                DMA("sp", wgA[:, :, :], WG[g_next][:, :, 0:GA], [B_WG[g_next]], [wgA.b])
            dbg("qT0", t["qa"][0], t["qa"][0][:, :], BF16)
            dbg("cT", t["cT"], t["cT"][:, :], BF16)
            dbg("vsa", t["vsa"], t["vsa"][:, :, :], BF16)
            dbg("gate", t["gate"], t["gate"][:, :, :])
            for kv in range(2):
                ps_ = slice(64 * kv, 64 * kv + 64)
                bk = banks[3 + kv]
                for l in range(32):
                    MM(bk[:, 0:NCMP], t["w1"][ps_, l, :], t["cT"][ps_, l:l + 16 * (NCMP - 1) + 1:16], l == 0, l == 31, [t["w1"].b, t["cT"].b], [bk.b])
                ACT(t["ha"][kv][:, 0:NCMP], bk[:, 0:NCMP], AF.Silu, [bk.b, t["peh"].b], [t["ha"][kv].b], bias=t["peh"][:, kv:kv + 1])
            MM(banks[5][:, 0:NCMP], t["w2k"][:, :], t["ha"][0][:, 0:NCMP], True, True, [t["w2k"].b, t["ha"][0].b], [banks[5].b])
            V("dve", "tensor_copy", [banks[5].b], [t["kcmp"].b], out=t["kcmp"][:, 0:NCMP], in_=banks[5][:, 0:NCMP])
            MM(banks[6][0:NCMP, 0:64], t["ha"][1][:, 0:NCMP], t["w2"][:, 64:128], True, True, [t["w2"].b, t["ha"][1].b], [banks[6].b])
            V("dve", "tensor_copy", [banks[6].b], [t["Vca"].b], out=t["Vca"][0:NCMP, 0:64], in_=banks[6][0:NCMP, 0:64])
            dbg("kcmp", t["kcmp"], t["kcmp"][:, 0:NCMP], BF16)
            dbg("Vca", t["Vca"], t["Vca"][0:NCMP, :], BF16)
            def zproj_gen():
                for i in range(NT):
                    bk = banks[6 + (i % 2)]
                    for c in range(8):
                        MM(bk[:, 0:256], uT[:, c, i * 128:(i + 1) * 128], wgZ[:, c, :], c == 0, c == 7, [uT.b, wgZ.b], [bk.b])
                    th = t["th"][i % 2]
                    ACT(th[:, :], bk[:, 0:256], AF.Tanh, [bk.b], [th.b], scale=0.5)
                    V("dve", "scalar_tensor_tensor", [th.b, bk.b], [t["zsg"].b], out=t["zsg"][:, i, :], in0=th[:, :], scalar=1.0, in1=bk[:, 0:256],
                      op0=ALU.add, op1=ALU.mult)
                    yield
                if has_next:
                    DMA("sp", wgZ[:, :, :], WG[g_next][:, :, GA:908], [B_WG[g_next]], [wgZ.b])
                yield
            zgen = zproj_gen()
            ec_rr = 0
            for Q in range(NQ):
                for j in range(4):
                    next(zgen, None)
                    h = 4 * g + j
                    qa_ = t["qa"][j]
                    ec = t["Ec"][ec_rr % 3]
                    ec_rr += 1
                    DMA("sp", ec[0:NCMP, :], Ec_src(h, Q * 512, 512), [B_Dc], [ec.b])
                    sbk = S_BANKS[s_rr[0]]
                    s_rr[0] = (s_rr[0] + 1) % 3
                    ptb = PT[pt_rr[0]]
                    pt_rr[0] = (pt_rr[0] + 1) % len(PT)
                    MM(sbk[0:NCMP, :], t["kcmp"][:, 0:NCMP], qa_[:, Q * 512:(Q + 1) * 512], True, True, [t["kcmp"].b, qa_.b], [sbk.b])
                    ACT(ptb[0:NCMP, :], sbk[0:NCMP, :], AF.Exp, [sbk.b], [ptb.b])
                    V("dve", "tensor_tensor", [ptb.b, ec.b], [ptb.b], out=ptb[0:NCMP, :], in0=ptb[0:NCMP, :], in1=ec[0:NCMP, :], op=ALU.mult)
                    ob = banks[3 + (j % 2)]
                    for r in range(4):
                        MM(ob[:, r * 97:(r + 1) * 97], ptb[0:NCMP, r * 128:(r + 1) * 128], t["Vca"][0:NCMP, :], True, True, [ptb.b, t["Vca"].b], [ob.b])
                    V("dve", "tensor_copy", [ob.b], [t["cm"].b], out=t["cm"][:, :, j, :], in_=ob[:, 0:388].rearrange("p (r c) -> p r c", r=4))
                cm = t["cm"]
                sm0, sm1, sm2 = t["sm"]
                rinv = sm0[:, 0:16].rearrange("p (r j) -> p r j", r=4)
                coef = sm0[:, 16:32].rearrange("p (r j) -> p r j", r=4)
                V("dve", "tensor_scalar", [cm.b], [sm0.b], out=rinv, in0=cm[:, :, :, 64], scalar1=1e-30, scalar2=None, op0=ALU.add)
                V("dve", "reciprocal", [sm0.b], [sm0.b], out=rinv, in_=rinv)
                gv = t["gate"][:, 4 * Q:4 * Q + 4, :].rearrange("p r (j b) -> p r j b", b=3)
                V("dve", "tensor_tensor", [sm0.b, t["gate"].b], [sm0.b], out=coef, in0=rinv, in1=gv[:, :, :, 0], op=ALU.mult)
                accv = t["acc"][:, 4 * Q:4 * Q + 4, :].rearrange("p r (j d) -> p r j d", j=4)
                V("dve", "tensor_tensor", [cm.b, sm0.b], [t["acc"].b], out=accv, in0=cm[:, :, :, 0:64],
                  in1=coef.unsqueeze(3).broadcast_to([128, 4, 4, 64]), op=ALU.mult)
                next(zgen, None)
                impw = sm1[:, :].rearrange("p (r j n) -> p r j n", r=4, j=4)
                V("dve", "tensor_tensor", [cm.b, sm0.b], [sm1.b], out=impw, in0=cm[:, :, :, 65:97],
                  in1=rinv.unsqueeze(3).broadcast_to([128, 4, 4, 32]), op=ALU.mult)
                imp = sm2[:, 0:128].rearrange("p (r n) -> p r n", r=4)
                V("dve", "tensor_tensor", [sm1.b], [sm2.b], out=imp, in0=impw[:, :, 0, :], in1=impw[:, :, 1, :], op=ALU.add)
                V("dve", "tensor_tensor", [sm1.b, sm2.b], [sm2.b], out=imp, in0=imp, in1=impw[:, :, 2, :], op=ALU.add)
                V("dve", "tensor_tensor", [sm1.b, sm2.b], [sm2.b], out=imp, in0=imp, in1=impw[:, :, 3, :], op=ALU.add)
                V("dve", "tensor_tensor", [sm2.b, t["keep"].b], [sm2.b], out=imp, in0=imp, in1=t["keep"][:, 4 * Q:4 * Q + 4, :], op=ALU.mult)
                V("dve", "tensor_tensor", [sm2.b, t["addc"].b], [sm2.b], out=imp, in0=imp, in1=t["addc"][:, 4 * Q:4 * Q + 4, :], op=ALU.add)
                next(zgen, None)
                top8 = sm2[:, 128:160].rearrange("p (r e) -> p r e", r=4)
                for r in range(4):
                    V("dve", "max", [sm2.b], [sm2.b], out=top8[:, r, :], in_=imp[:, r, :])
                pen32 = sm2[:, 160:288].rearrange("p (r n) -> p r n", r=4)
                V("dve", "tensor_tensor", [sm2.b], [sm2.b], out=pen32, in0=imp, in1=top8[:, :, 7:8].broadcast_to([128, 4, 32]), op=ALU.is_lt)
                V("dve", "tensor_scalar", [sm2.b], [t["penb"].b], out=t["penb"][:, :, 64:96], in0=pen32, scalar1=PEN, scalar2=None, op0=ALU.mult)
                next(zgen, None)
                bkp = banks[5]
                bkpv = bkp[:, :].bitcast(BF16)
                for r in range(4):
                    TR(bkpv[0:96, r * 128:(r + 1) * 128], t["penb"][:, r, :], [t["penb"].b], [bkp.b])
                cols = slice(Q * 512, (Q + 1) * 512)
                V("dve", "tensor_copy", [bkp.b], [t["qa"][0].b], out=t["qa"][0][64:96, cols], in_=bkpv[64:96, 0:512])
                for j in range(1, 4):
                    V("pool", "tensor_copy", [t["qa"][0].b], [t["qa"][j].b], out=t["qa"][j][64:96, cols], in_=t["qa"][0][64:96, cols])
            for _ in zgen:
                pass
            dbg("cm", t["cm"], t["cm"][:, :, :, :])
            dbg("acc_c", t["acc"], t["acc"][:, :, :])
            streams = []
            o_rr = 0
            for Q in range(NQ):
                for j in range(4):
                    for br in (1, 2):
                        st_ = Stream()
                        st_.Q = Q
                        st_.band = (br == 2)
                        qa_ = t["qa"][j]
                        kt_ = t["ksx"] if br == 1 else t["kwz"]
                        va_ = t["vsa"] if br == 1 else t["vwa"]
                        st_.q_ap = lambda gc, qa_=qa_: qa_[:, gc]
                        st_.k_ap = lambda kj, kt_=kt_: kt_[:, kj * 128:(kj + 1) * 128]
                        st_.qk_bufs = [qa_.b, kt_.b]
                        st_.pen = None
                        st_.pen_buf = None
                        st_.v_ap = lambda kj, va_=va_: va_[:, kj, :]
                        st_.v_bufs = [va_.b]
                        st_.emap = 4 * g + j
                        ob = banks[3 + (o_rr % 2)]
                        o_rr += 1
                        st_.O = [(ob[:, r * 65:(r + 1) * 65], ob) for r in range(4)]
                        st_.ob = ob
                        st_.j = j
                        st_.br = br

                        def fin(st_):
                            ob = st_.ob
                            Q, j, br = st_.Q, st_.j, st_.br
                            ov = ob[:, 0:260].rearrange("p (r c) -> p r c", r=4)
                            sm = get_stat()
                            V("dve", "reciprocal", [ob.b], [sm.b], out=sm[:, 0:4], in_=ov[:, :, 64])
                            gv = t["gate"][:, 4 * Q:4 * Q + 4, :].rearrange("p r (j b) -> p r j b", b=3)
                            V("dve", "tensor_tensor", [sm.b, t["gate"].b], [sm.b], out=sm[:, 4:8], in0=sm[:, 0:4], in1=gv[:, :, j, br], op=ALU.mult)
                            tm = t["tmp"][(2 * j + br) % 2]
                            V("dve", "tensor_tensor", [ob.b, sm.b], [tm.b], out=tm[:, :, :], in0=ov[:, :, 0:64],
                              in1=sm[:, 4:8].unsqueeze(2).broadcast_to([128, 4, 64]), op=ALU.mult)
                            accv = t["acc"][:, 4 * Q:4 * Q + 4, 64 * j:64 * j + 64]
                            V("pool", "tensor_tensor", [tm.b, t["acc"].b], [t["acc"].b], out=accv, in0=accv, in1=tm[:, :, :], op=ALU.add)
                        st_.finalize = fin
                        streams.append(st_)
            run_streams(streams)
            dbg("acc_f", t["acc"], t["acc"][:, :, :])
            for i in range(NT):
                bk = banks[5 + (i % 2)]
                og = t["og"][i % 2]
                V("dve", "scalar_tensor_tensor", [t["zsg"].b, t["acc"].b], [og.b], out=og[:, :], in0=t["acc"][:, i, :], scalar=0.5, in1=t["zsg"][:, i, :],
                  op0=ALU.mult, op1=ALU.mult)
                bkv = bk[:, :].bitcast(BF16)
                for pr in range(2):
                    TR(bkv[:, 512 + pr * 128:512 + (pr + 1) * 128], og[:, pr * 128:(pr + 1) * 128], [og.b], [bk.b])
                EVAC(oT[:, 2 * g:2 * g + 2, i * 128:(i + 1) * 128], bkv[:, 512:768].rearrange("p (c t) -> p c t", c=2), [bk.b], [oT.b])
        dbg("oT", oT, oT[:, :, :], BF16)
        dst, dstb = (x1_d, B_x1[s]) if 1 in layers else (out, B_out[s])
        phase_out(t["wbig"], 0, cst["gpost"], lambda i: x_in[s, i * 128:(i + 1) * 128, :], lambda i: [NOB],
                  lambda i: dst[s, i * 128:(i + 1) * 128, :], lambda i: [dstb[i]])

    def diff_setup():
        t = {}
        load_norm_gains(1)
        stgs["stg"] = [sb("dstg%d" % i, [128, STG_N]) for i in range(2)]
        stgs["rr"] = 0
        cast_engs[0] = ("pool",)
        raw = sb("dscr", [128, 4096])
        t["sq"] = TT(raw[:, 0:NT * 128].rearrange("p (i c) -> p i c", i=NT), "sq")
        t["og"] = TT(raw[:, 2048:3072].bitcast(BF16)[:, 0:NT * 128].rearrange("p (i c) -> p i c", i=NT), "og")
        t["wbig"] = TT(raw[:, :].bitcast(BF16).rearrange("p (c n) -> p c n", c=8), "wbig")
        t["sq"].b = t["og"].b = t["wbig"].b = raw.b
        lam4 = sb("lam4", [128, 4, 64])
        for idx, src in enumerate((lq1, lk1, lq2, lk2)):
            DMA("sp", lam4[:, idx, :], src[0:1, :].partition_broadcast(128), [NOB], [lam4.b], join=(idx > 0))
        lm = sb("lm", [128, 8])
        prod = sb("lprod", [128, 2, 64])
        V("dve", "tensor_tensor", [lam4.b], [prod.b], out=prod[:, 0, :], in0=lam4[:, 0, :], in1=lam4[:, 1, :], op=ALU.mult)
        V("dve", "tensor_tensor", [lam4.b], [prod.b], out=prod[:, 1, :], in0=lam4[:, 2, :], in1=lam4[:, 3, :], op=ALU.mult)
        V("dve", "reduce_sum", [prod.b], [lm.b], out=lm[:, 0:2], in_=prod[:, :, :], axis=AX.X)
        ACT(lm[:, 2:4], lm[:, 0:2], AF.Exp, [lm.b], [lm.b])
        V("dve", "tensor_tensor", [lm.b], [lm.b], out=lm[:, 4:5], in0=lm[:, 3:4], in1=lm[:, 2:3], op=ALU.subtract)
        V("dve", "tensor_scalar", [lm.b], [lm.b], out=lm[:, 5:6], in0=lm[:, 4:5], scalar1=-LAMBDA_INIT, scalar2=None, op0=ALU.add)
        t["lm"] = lm
        t["subln"] = sb("sublnb", [128, D])
        DMA("sp", t["subln"][:, :], subln[0:1, :].partition_broadcast(128), [NOB], [t["subln"].b])
        t["w"] = [sb("wd%d" % i, [128, 8, 512], BF16) for i in range(2)]
        t["qT"] = [sb("dqT%d" % i, [128, S], BF16) for i in range(2)]
        t["kz"] = [[sb("dkz%d_%d" % (b_, i), [128, S], BF16) for i in range(2)] for b_ in range(2)]
        for b_ in range(2):
            for i in range(2):
                V("pool", "memset", [], [t["kz"][b_][i].b], t["kz"][b_][i][:, :], 0.0)
        t["va"] = [sb("dva%d" % i, [128, NT, 129], BF16) for i in range(2)]
        for i in range(2):
            V("pool", "memset", [], [t["va"][i].b], t["va"][i][:, :, :], 1.0)
        t["od"] = [sb("od%d" % i, [128, NT, 128]) for i in range(2)]
        t["zs"] = [sb("dzs%d" % i, [128, NT, 128], BF16) for i in range(2)]
        t["tmp"] = [sb("dtmp%d" % i, [128, 2, 128]) for i in range(2)]
        t["rs"] = sb("drs", [128, 4, NT])
        t["th"] = [sb("dth0", [128, 512], BF16)] * 2
        return t

    def diff_layer(t, s):
        phase_norm_T(lambda i: x1_d[s, i * 128:(i + 1) * 128, :], lambda i: [B_x1[s][i]], cst["gpre"])
        dv3 = diff_w_in.rearrange("(c p) n -> p c n", p=128)
        lm = t["lm"]

        def load_w(h):
            w = t["w"][h % 2]
            for part in range(4):
                LOADW(w[:, :, 128 * part:128 * part + 128], dv3[:, :, 1024 * part + 128 * h:1024 * part + 128 * h + 128], w.b, join=(part > 0))

        def proj_gen(h):
            hb = h % 2
            w = t["w"][hb]
            qT, kz, va, zs = t["qT"][hb], t["kz"][hb], t["va"][hb], t["zs"][hb]
            for Q in range(NQ):
                bk = banks[7]
                for c in range(8):
                    MM(bk[:, :], w[:, c, 0:128], uT[:, c, Q * 512:(Q + 1) * 512], c == 0, c == 7, [w.b, uT.b], [bk.b])
                EVAC(qT[:, Q * 512:(Q + 1) * 512], bk[:, :], [bk.b], [qT.b], scale=0.125)
                yield
            for Q in range(NQ):
                bk = banks[7]
                for c in range(8):
                    MM(bk[:, :], w[:, c, 128:256], uT[:, c, Q * 512:(Q + 1) * 512], c == 0, c == 7, [w.b, uT.b], [bk.b])
                EVAC(kz[0][0:64, Q * 512:(Q + 1) * 512], bk[0:64, :], [bk.b], [kz[0].b])
                EVAC(kz[1][64:128, Q * 512:(Q + 1) * 512], bk[64:128, :], [bk.b], [kz[1].b])
                yield
            for i4 in range(NT // 4):
                bk = banks[7]
                for r in range(4):
                    i = 4 * i4 + r
                    for c in range(8):
                        MM(bk[:, r * 128:(r + 1) * 128], uT[:, c, i * 128:(i + 1) * 128], w[:, c, 256:384], c == 0, c == 7, [uT.b, w.b], [bk.b])
                V("dve", "tensor_copy", [bk.b], [va.b], out=va[:, 4 * i4:4 * i4 + 4, 0:128], in_=bk[:, :].rearrange("p (r c) -> p r c", r=4))
                yield
                for r in range(4):
                    i = 4 * i4 + r
                    for c in range(8):
                        MM(bk[:, r * 128:(r + 1) * 128], uT[:, c, i * 128:(i + 1) * 128], w[:, c, 384:512], c == 0, c == 7, [uT.b, w.b], [bk.b])
                th = t["th"][i4 % 2]
                ACT(th[:, :], bk[:, :], AF.Tanh, [bk.b], [th.b], scale=0.5)
                V("dve", "scalar_tensor_tensor", [th.b, bk.b], [zs.b], out=zs[:, 4 * i4:4 * i4 + 4, :], in0=th[:, :].rearrange("p (r c) -> p r c", r=4),
                  scalar=1.0, in1=bk[:, :].rearrange("p (r c) -> p r c", r=4), op0=ALU.add, op1=ALU.mult)
                yield
            if h + 2 < 8:
                load_w(h + 2)
            yield

        def tail_gen(h):
            hb = h % 2
            od, zs, sq, rs = t["od"][hb], t["zs"][hb], t["sq"], t["rs"]
            V("pool", "tensor_tensor", [od.b], [sq.b], out=sq[:, :, :], in0=od[:, :, :], in1=od[:, :, :], op=ALU.mult)
            V("dve", "reduce_sum", [sq.b], [rs.b], out=rs[:, 0, :], in_=sq[:, :, :], axis=AX.X)
            yield
            V("dve", "tensor_scalar", [rs.b], [rs.b], out=rs[:, 1, :], in0=rs[:, 0, :], scalar1=1.0 / 128, scalar2=EPS, op0=ALU.mult, op1=ALU.add)
            ACT(rs[:, 2, :], rs[:, 1, :], AF.Sqrt, [rs.b], [rs.b])
            V("dve", "reciprocal", [rs.b], [rs.b], out=rs[:, 3, :], in_=rs[:, 2, :])
            yield
            V("dve", "tensor_tensor", [od.b, rs.b], [sq.b], out=sq[:, :, :], in0=od[:, :, :], in1=rs[:, 3, :].unsqueeze(2).broadcast_to([128, NT, 128]), op=ALU.mult)
            yield
            V("dve", "scalar_tensor_tensor", [sq.b, t["subln"].b], [sq.b], out=sq[:, :, :], in0=sq[:, :, :], scalar=0.5 * (1.0 - LAMBDA_INIT),
              in1=t["subln"][:, 128 * h:128 * h + 128].unsqueeze(1).broadcast_to([128, NT, 128]), op0=ALU.mult, op1=ALU.mult)
            yield
            V("dve", "tensor_tensor", [sq.b, zs.b], [t["og"].b], out=t["og"][:, :, :], in0=sq[:, :, :], in1=zs[:, :, :], op=ALU.mult)
            yield
            for i4 in range(NT // 4):
                bk = banks[7]
                bkv = bk[:, :].bitcast(BF16)
                for r in range(4):
                    TR(bkv[:, r * 128:(r + 1) * 128], t["og"][:, 4 * i4 + r, :], [t["og"].b], [bk.b])
                EVAC(oT[:, h, i4 * 512:(i4 + 1) * 512], bkv[:, 0:512], [bk.b], [oT.b])
                yield

        def chain(*gens):
            for g_ in gens:
                if g_ is not None:
                    for _ in g_:
                        yield

        def head_streams(h):
            hb = h % 2
            qT, kz, va, od = t["qT"][hb], t["kz"][hb], t["va"][hb], t["od"][hb]
            streams = []
            for Q in range(NQ):
                for m in range(2):
                    st_ = Stream()
                    st_.Q = Q
                    st_.band = False
                    kz_ = kz[m]
                    st_.q_ap = lambda gc, qT=qT: qT[:, gc]
                    st_.k_ap = lambda kj, kz_=kz_: kz_[:, kj * 128:(kj + 1) * 128]
                    st_.qk_bufs = [qT.b, kz_.b]
                    st_.pen = None
                    st_.pen_buf = None
                    st_.v_ap = lambda kj, va=va: va[:, kj, :]
                    st_.v_bufs = [va.b]
                    st_.emap = 2 * h + m
                    obs = [banks[3 + 2 * m], banks[4 + 2 * m]]
                    st_.O = [(obs[r // 2][:, (r % 2) * 129:(r % 2) * 129 + 129], obs[r // 2]) for r in range(4)]
                    st_.obs = obs
                    st_.m = m
                    st_.od = od

                    def fin(st_):
                        Q, m, od = st_.Q, st_.m, st_.od
                        for half in range(2):
                            ob = st_.obs[half]
                            ov = ob[:, 0:258].rearrange("p (r c) -> p r c", r=2)
                            i0 = 4 * Q + 2 * half
                            sm = get_stat()
                            V("dve", "reciprocal", [ob.b], [sm.b], out=sm[:, 0:2], in_=ov[:, :, 128])
                            if m == 0:
                                V("dve", "tensor_tensor", [ob.b, sm.b], [od.b], out=od[:, i0:i0 + 2, :], in0=ov[:, :, 0:128],
                                  in1=sm[:, 0:2].unsqueeze(2).broadcast_to([128, 2, 128]), op=ALU.mult)
                            else:
                                V("dve", "tensor_scalar", [sm.b, lm.b], [sm.b], out=sm[:, 2:4], in0=sm[:, 0:2], scalar1=lm[:, 5:6], scalar2=None, op0=ALU.mult)
                                tm = t["tmp"][half]
                                V("dve", "tensor_tensor", [ob.b, sm.b], [tm.b], out=tm[:, :, :], in0=ov[:, :, 0:128],
                                  in1=sm[:, 2:4].unsqueeze(2).broadcast_to([128, 2, 128]), op=ALU.mult)
                                V("pool", "tensor_tensor", [tm.b, od.b], [od.b], out=od[:, i0:i0 + 2, :], in0=od[:, i0:i0 + 2, :],
                                  in1=tm[:, :, :], op=ALU.add)
                    st_.finalize = fin
                    streams.append(st_)
            return streams

        load_w(0)
        load_w(1)
        for _ in proj_gen(0):
            pass
        for h in range(8):
            fill = chain(tail_gen(h - 1) if h > 0 else None, proj_gen(h + 1) if h < 7 else None)
            run_streams(head_streams(h), filler=fill, every=3)
        for _ in tail_gen(7):
            pass
        cast_engs[0] = ("dve", "act")
        wo3 = diff_w_out.rearrange("(c p) n -> p c n", p=128)
        for q4 in range(4):
            LOADW(t["wbig"][:, :, 256 * q4:256 * q4 + 256], wo3[:, :, 256 * q4:256 * q4 + 256], t["wbig"].b, join=(q4 > 0))
        cast_engs[0] = ("pool",)
        phase_out(t["wbig"], None, cst["gpost"], lambda i: x1_d[s, i * 128:(i + 1) * 128, :], lambda i: [B_x1[s][i]],
                  lambda i: out[s, i * 128:(i + 1) * 128, :], lambda i: [B_out[s][i]])

    with es:
        with ExitStack() as es1:
            cur_es[0] = es1
            setup_bias()
            setup_weights()
        cur_es[0] = es
        p.barrier()
        if 0 in layers:
            with ExitStack() as es2:
                cur_es[0] = es2
                nt_ = nsa_setup()
                for s in range(nseq):
                    nsa_layer(nt_, s)
            cur_es[0] = es
            p.barrier()
        if 1 in layers:
            with ExitStack() as es3:
                cur_es[0] = es3
                dt_ = diff_setup()
                for s in range(nseq):
                    if 0 not in layers:
                        for i in range(NT):
                            xb_ = xt[i % 2]
                            DMA("sp", xb_[:, :], x_in[s, i * 128:(i + 1) * 128, :], [NOB], [xb_.b])
                            DMA("sp", x1_d[s, i * 128:(i + 1) * 128, :], xb_[:, :], [xb_.b], [B_x1[s][i]])
                    diff_layer(dt_, s)
            cur_es[0] = es
        p.emit()
    return nc


INPUT_NAMES = ["rel_bias_table", "norm_pre", "norm_post", "nsa_w_in", "nsa_cmp_pe_k", "nsa_cmp_w1_k", "nsa_cmp_w2_k",
               "nsa_cmp_pe_v", "nsa_cmp_w1_v", "nsa_cmp_w2_v", "nsa_w_out", "diff_w_in", "diff_lambda_q1", "diff_lambda_k1",
               "diff_lambda_q2", "diff_lambda_k2", "diff_subln", "diff_w_out"]


def make_in_maps(inputs, n_cores, nseq, S):
    consts = host_consts(S)
    shared = {}
    for k in INPUT_NAMES:
        a = np.ascontiguousarray(np.asarray(inputs[k], dtype=np.float32))
        if a.ndim == 3:
            a = a[0]
        elif k.startswith("diff_lambda") or k == "diff_subln":
            a = a.reshape(1, -1)
        shared[k] = np.ascontiguousarray(a)
    shared.update(consts)
    x = np.asarray(inputs["x"], dtype=np.float32)
    maps = []
    for c in range(n_cores):
        m = dict(shared)
        m["x"] = np.ascontiguousarray(x[c * nseq:(c + 1) * nseq])
        maps.append(m)
    return maps


def kernel(**inputs):
    x = np.asarray(inputs["x"])
    B, S, _ = x.shape
    n_cores = 8
    nseq = B // n_cores
    nc = build(nseq, S)
    in_maps = make_in_maps(inputs, n_cores, nseq, S)
    res = run_bass_kernel_spmd(nc, in_maps, core_ids=list(range(n_cores)))
    return np.concatenate([np.asarray(r["out"]) for r in res.results], axis=0).astype(np.float32)
```

```python
import math
from contextlib import ExitStack

import numpy as np
import ml_dtypes

import concourse.bass as bass
import concourse.mybir as mybir
from concourse.bass_utils import run_bass_kernel_spmd

F32 = mybir.dt.float32
BF16 = mybir.dt.bfloat16
AF = mybir.ActivationFunctionType
ALU = mybir.AluOpType
AX = mybir.AxisListType

D = 1024
NSA_IN = 3632
EPS = 1e-6
PEN = -30000.0
LAMBDA_INIT = 0.8 - 0.6 * math.exp(-0.3 * 1)


class Buf:
    __slots__ = ("name", "writers", "readers", "prev")

    def __init__(self, name=""):
        self.name = name
        self.writers = []
        self.readers = []
        self.prev = []


class Op:
    __slots__ = ("eng", "fn", "dma", "deps", "needs_sig", "sig")

    def __init__(self, eng, fn, dma):
        self.eng = eng
        self.fn = fn
        self.dma = dma
        self.deps = []
        self.needs_sig = False
        self.sig = None


ENGS = ("pe", "act", "dve", "pool", "sp")


class Prog:
    def __init__(self, nc, n_dma_sems=48):
        self.nc = nc
        self.ops = {e: [] for e in ENGS}
        self.n_dma_sems = n_dma_sems
        self.dma_last = [None] * n_dma_sems
        self.dma_cnt = [0] * n_dma_sems
        self.dma_rr = 0
        self.pending = {}

    def barrier(self):
        lasts = []
        for e in ENGS:
            for op in reversed(self.ops[e]):
                if not op.dma:
                    op.needs_sig = True
                    lasts.append(op)
                    break
        for j in range(self.n_dma_sems):
            if self.dma_last[j] is not None:
                lasts.append(self.dma_last[j])
        self.pending = {e: list(lasts) for e in ENGS}

    def add(self, eng, fn, reads=(), writes=(), dma=False, join=False):
        op = Op(eng, fn, dma)
        if self.pending.get(eng):
            op.deps.extend(self.pending.pop(eng))
        for b in reads:
            for w in b.writers:
                self._dep(w, op, True)
        for b in writes:
            if not join:
                for w in b.writers:
                    self._dep(w, op, False)
            else:
                for r in b.prev:
                    self._dep(r, op, False)
            for r in b.readers:
                self._dep(r, op, False)
        for b in writes:
            if join:
                b.writers.append(op)
            else:
                b.prev = list(b.writers) + list(b.readers)
                b.writers = [op]
                b.readers = []
        for b in reads:
            b.readers.append(op)
        if dma:
            j = self.dma_rr
            self.dma_rr = (j + 1) % self.n_dma_sems
            prev = self.dma_last[j]
            if prev is not None:
                op.deps.append(prev)
            self.dma_cnt[j] += 1
            op.sig = (("d", j), 16 * self.dma_cnt[j])
            op.needs_sig = True
            self.dma_last[j] = op
        self.ops[eng].append(op)
        return op

    def _dep(self, p, c, raw):
        if p is c:
            return
        if (not p.dma) and (not c.dma) and p.eng == c.eng:
            if p.eng == "pe" or not raw:
                return
        p.needs_sig = True
        c.deps.append(p)

    def emit(self):
        nc = self.nc
        with ExitStack() as es:
            esem = {e: es.enter_context(nc.semaphore("s_" + e)) for e in ENGS}
            dsem = [es.enter_context(nc.semaphore("d%d" % j)) for j in range(self.n_dma_sems)]
            for e in ENGS:
                cnt = 0
                for op in self.ops[e]:
                    if op.dma:
                        continue
                    if op.needs_sig:
                        cnt += 1
                        op.sig = (("e", e), cnt)

            def semof(key):
                return esem[key[1]] if key[0] == "e" else dsem[key[1]]

            block = es.enter_context(nc.Block())
            engobj = {"pe": "tensor", "act": "scalar", "dve": "vector", "pool": "gpsimd", "sp": "sync"}

            def make(e):
                def body(eng):
                    waited = {}
                    for op in self.ops[e]:
                        need = {}
                        for p in op.deps:
                            k, v = p.sig
                            if need.get(k, 0) < v:
                                need[k] = v
                        for k, v in need.items():
                            if waited.get(k, 0) >= v:
                                continue
                            eng.wait_ge(semof(k), v)
                            waited[k] = v
                        ins = op.fn(eng)
                        if op.needs_sig:
                            k, v = op.sig
                            ins.then_inc(semof(k), 16 if op.dma else 1)
                    if e == "sp":
                        for j in range(self.n_dma_sems):
                            if self.dma_cnt[j] > 0:
                                eng.wait_ge(dsem[j], 16 * self.dma_cnt[j])
                return body

            for e in ENGS:
                getattr(block, engobj[e])(make(e))


def _rel_bucket(n):
    n = np.maximum(n, 0)
    nf = np.maximum(n, 1).astype(np.float32)
    large = 16 + (np.log(nf / np.float32(16)) / np.float32(math.log(8.0)) * np.float32(16)).astype(np.int32)
    large = np.minimum(large, 31)
    return np.where(n < 16, n, large)


def host_consts(S):
    NT = S // 128
    ncmp = S // 16 - 1
    bf = ml_dtypes.bfloat16
    c = {}
    c["c_ident"] = np.eye(128, dtype=np.float32).astype(bf)
    b = _rel_bucket(np.arange(128))
    oh = np.zeros((32, 128), np.float32)
    oh[b, np.arange(128)] = 1.0
    oh[31, :] -= 1.0
    c["c_onehot"] = oh
    cmp_lo = np.arange(ncmp) * 16
    sel_lo = np.arange(S // 64) * 64
    ov = np.clip(np.minimum(cmp_lo[:, None] + 32, sel_lo[None, :] + 64)
                 - np.maximum(cmp_lo[:, None], sel_lo[None, :]), 0, None).astype(np.float32) / 32.0
    ovp = np.zeros((127, 32), np.float32)
    ovp[:ncmp, :S // 64] = ov
    c["c_ovl"] = ovp.astype(bf)
    X = np.zeros((32, S), np.float32)
    X[np.arange(S) // 64, np.arange(S)] = 1.0
    c["c_X"] = X.astype(bf)
    k = np.arange(128)[:, None]
    q = np.arange(128)[None, :]
    c["c_M4"] = (q < k).astype(np.float32).astype(bf)
    t = np.arange(S)
    cur = t // 64
    n = np.arange(32)[None, :]
    forced = (n == 0) | (n == cur[:, None]) | (n == cur[:, None] - 1)
    future = n > cur[:, None]
    keep = (~(forced | future)).astype(np.float32)
    addc = np.where(forced, 1e9, np.where(future, -1e9, 0.0)).astype(np.float32)
    c["c_keep"] = np.ascontiguousarray(keep.reshape(NT, 128, 32).transpose(1, 0, 2))
    c["c_addc"] = np.ascontiguousarray(addc.reshape(NT, 128, 32).transpose(1, 0, 2))
    return c


def build(nseq, S, layers=(0, 1)):
    NT = S // 128
    NQ = S // 512
    NCMP = S // 16 - 1
    nc = bass.Bass("TRN2", target_bir_lowering=False)
    p = Prog(nc)

    def din(name, shape, dt=F32):
        return nc.dram_tensor(name, list(shape), dt, kind="ExternalInput").ap()

    x_in = din("x", [nseq, S, D])
    table = din("rel_bias_table", [32, 16])
    norm_pre = din("norm_pre", [2, D])
    norm_post = din("norm_post", [2, D])
    nsa_w_in = din("nsa_w_in", [D, NSA_IN])
    pe_k = din("nsa_cmp_pe_k", [32, 64])
    w1_k = din("nsa_cmp_w1_k", [2048, 128])
    w2_k = din("nsa_cmp_w2_k", [128, 64])
    pe_v = din("nsa_cmp_pe_v", [32, 64])
    w1_v = din("nsa_cmp_w1_v", [2048, 128])
    w2_v = din("nsa_cmp_w2_v", [128, 64])
    nsa_w_out = din("nsa_w_out", [D, D])
    diff_w_in = din("diff_w_in", [D, 4096])
    lq1 = din("diff_lambda_q1", [1, 64])
    lk1 = din("diff_lambda_k1", [1, 64])
    lq2 = din("diff_lambda_q2", [1, 64])
    lk2 = din("diff_lambda_k2", [1, 64])
    subln = din("diff_subln", [1, D])
    diff_w_out = din("diff_w_out", [D, D])
    c_ident = din("c_ident", [128, 128], BF16)
    c_onehot = din("c_onehot", [32, 128])
    c_ovl = din("c_ovl", [127, 32], BF16)
    c_X = din("c_X", [32, S], BF16)
    c_M4 = din("c_M4", [128, 128], BF16)
    c_keep = din("c_keep", [128, NT, 32])
    c_addc = din("c_addc", [128, NT, 32])
    out = nc.dram_tensor("out", [nseq, S, D], F32, kind="ExternalOutput").ap()
    x1_d = nc.dram_tensor("x1_scr", [nseq, S, D], F32).ap()
    De = nc.dram_tensor("De_scr", [16, 128, 512], BF16).ap()
    Dc = nc.dram_tensor("Dc_scr", [16, 64, 2048], BF16).ap()
    B_x1 = [[Buf("x1d%d_%d" % (s, i)) for i in range(NT)] for s in range(nseq)]
    B_out = [[Buf("out%d_%d" % (s, i)) for i in range(NT)] for s in range(nseq)]
    B_De = Buf("De")
    B_Dc = Buf("Dc")
    NOB = Buf("const_in")

    import os as _os
    es = ExitStack()
    cur_es = [es]
    DEBUG = bool(_os.environ.get("KDEBUG"))
    dbg_seen = set()

    def dbg(name, tt, ap, dt=F32):
        if not DEBUG or name in dbg_seen:
            return
        dbg_seen.add(name)
        o = nc.dram_tensor("dbg_" + name, list(ap.shape), dt, kind="ExternalOutput").ap()
        p.add("sp", lambda e: e.dma_start(out=o, in_=ap), reads=[tt.b], writes=[Buf()], dma=True)

    class TT:
        def __init__(self, t, name):
            self.t = t
            self.b = Buf(name)

        def __getitem__(self, k):
            return self.t[k]

    def sb(name, shape, dt=F32):
        if _os.environ.get("KDEBUG_SB"):
            print("SB", name, shape, "remaining", nc.sbuf_bytes_remaining)
        return TT(cur_es[0].enter_context(nc.sbuf_tensor(name, list(shape), dt)), name)

    banks = [TT(es.enter_context(nc.psum_tensor("bank%d" % i, [128, 512], F32)), "bank%d" % i) for i in range(8)]

    def DMA(eng, out_ap, in_ap, reads, writes, join=False, **kw):
        p.add(eng, lambda e: e.dma_start(out=out_ap, in_=in_ap, **kw), reads=reads, writes=writes, dma=True, join=join)

    def MM(out_ap, lhsT, rhs, start, stop, reads, writes, skip=False):
        if skip:
            p.add("pe", lambda e: e.matmul(out_ap, lhsT=lhsT, rhs=rhs, start=start, stop=stop, skip_group_check=True), reads=reads, writes=writes)
        else:
            p.add("pe", lambda e: e.matmul(out_ap, lhsT=lhsT, rhs=rhs, start=start, stop=stop), reads=reads, writes=writes)

    def TR(out_ap, in_ap, reads, writes):
        p.add("pe", lambda e: e.transpose(out=out_ap, in_=in_ap, identity=ident[:, :]), reads=list(reads) + [ident.b], writes=writes)

    def ACT(out_ap, in_ap, func, reads, writes, **kw):
        p.add("act", lambda e: e.activation(out=out_ap, in_=in_ap, func=func, **kw), reads=reads, writes=writes)

    def V(eng, name, reads, writes, *a, **kw):
        p.add(eng, lambda e: getattr(e, name)(*a, **kw), reads=reads, writes=writes)

    evac_rr = [0]

    def EVAC(out_ap, in_ap, reads, writes, scale=None):
        evac_rr[0] ^= 1
        if evac_rr[0]:
            if scale is None:
                ACT(out_ap, in_ap, AF.Copy, reads, writes)
            else:
                ACT(out_ap, in_ap, AF.Copy, reads, writes, scale=float(scale))
        else:
            if scale is None:
                V("dve", "tensor_copy", reads, writes, out=out_ap, in_=in_ap)
            else:
                V("dve", "tensor_scalar", reads, writes, out=out_ap, in0=in_ap, scalar1=float(scale), scalar2=None, op0=ALU.mult)

    ident = sb("ident", [128, 128], BF16)
    DMA("sp", ident[:, :], c_ident[:, :], [NOB], [ident.b])
    M4 = sb("M4", [128, 128], BF16)
    DMA("sp", M4[:, :], c_M4[:, :], [NOB], [M4.b])
    Ee = sb("Ee", [128, 16, 256], BF16)
    uT = sb("uT", [128, 8, S], BF16)
    oT = sb("oT", [128, 8, S], BF16)
    xt = [sb("xt%d" % i, [128, D]) for i in range(2)]
    ub = [sb("ub%d" % i, [128, D], BF16) for i in range(2)]
    stat = [sb("stat%d" % i, [128, 8]) for i in range(4)]
    NPT = 8
    PT = [sb("PT%d" % i, [128, 512], BF16) for i in range(NPT)]
    yout = [sb("yout0", [128, D])]
    cst = {}

    def setup_bias():
        tab = sb("tab", [32, 16])
        oh = sb("oh", [32, 128])
        fse = sb("fse", [16, 512], BF16)
        fsc = sb("fsc", [16, 2048], BF16)
        DMA("sp", tab[:, :], table[:, :], [NOB], [tab.b])
        DMA("sp", oh[:, :], c_onehot[:, :], [NOB], [oh.b])
        MM(banks[0][0:16, 0:128], tab[:, :], oh[:, :], True, True, [tab.b, oh.b], [banks[0].b])
        V("pool", "memset", [], [fse.b], fse[:, :], 0.0)
        V("pool", "memset", [fse.b], [fse.b], fse[:, 128:384], 1.0)
        V("pool", "memset", [], [fsc.b], fsc[:, :], 0.0)
        V("pool", "memset", [fsc.b], [fsc.b], fsc[:, 0:512], 1.0)
        V("pool", "memset", [fsc.b], [fsc.b], fsc[:, 1695:2048], 1.0)
        ACT(fse[:, 0:128], banks[0][0:16, 0:128], AF.Exp, [banks[0].b, fse.b], [fse.b])
        ACT(fsc[:, 1567:1695], banks[0][0:16, 0:128], AF.Exp, [banks[0].b, fsc.b], [fsc.b])
        DMA("act", De, fse[:, :].unsqueeze(1).broadcast_to([16, 128, 512]), [fse.b], [B_De])
        DMA("act", Dc, fsc[:, :].unsqueeze(1).broadcast_to([16, 64, 2048]), [fsc.b], [B_Dc])

    def setup_bias_load():
        while stgs.get("jobs"):
            stgs["jobs"].pop(0)()
        for h in range(16):
            DMA("sp", Ee[:, h, :], bass.AP(De.tensor, h * 128 * 512, [[511, 128], [1, 256]]), [B_De], [Ee.b], join=(h > 0))

    def load_norm_gains(l):
        cst["gpre"] = sb("gpre%d" % l, [128, D])
        cst["gpost"] = sb("gpost%d" % l, [128, D])
        DMA("sp", cst["gpre"][:, :], norm_pre[l:l + 1, :].partition_broadcast(128), [NOB], [cst["gpre"].b])
        DMA("sp", cst["gpost"][:, :], norm_post[l:l + 1, :].partition_broadcast(128), [NOB], [cst["gpost"].b])

    def Ec_rows(Q):
        p0 = max(0, 32 * (Q - 1))
        p1 = min(NCMP, 32 * Q + 32)
        return p0, p1

    def Ec_src(h, Q):
        p0, p1 = Ec_rows(Q)
        i0 = p0 - 32 * Q + 32
        return bass.AP(Dc.tensor, h * 64 * 2048 + i0 * 2032, [[2032, p1 - p0], [1, 512]])

    STG_N = 1024
    stgs = {}
    cast_rr = [0]

    cast_engs = [("dve", "act")]

    def CAST(out_ap, in_ap, reads, writes, join=False):
        e = cast_engs[0][cast_rr[0] % len(cast_engs[0])]
        cast_rr[0] += 1
        if e == "act":
            p.add("act", lambda en: en.activation(out=out_ap, in_=in_ap, func=AF.Copy), reads=reads, writes=writes, join=join)
        else:
            p.add(e, lambda en: en.tensor_copy(out=out_ap, in_=in_ap), reads=reads, writes=writes, join=join)

    def LOADW(dst_ap, src_ap, dst_buf, join=False):
        stg = stgs["stg"]
        shp = list(dst_ap.shape)
        P_ = shp[0]
        mid = 1
        for d_ in shp[1:-1]:
            mid *= d_
        last = shp[-1]
        step = max(1, STG_N // mid)
        bp = dst_ap.base_partition()
        first = True
        for c0 in range(0, last, step):
            c1 = min(last, c0 + step)
            n = mid * (c1 - c0)
            assert n <= STG_N, shp
            st_ = stg[stgs["rr"]]
            stgs["rr"] = (stgs["rr"] + 1) % len(stg)
            sv = st_[bp:bp + P_, 0:n]
            if len(shp) == 3:
                sv = sv.rearrange("p (a b) -> p a b", a=shp[1])
                d_ap, s_ap = dst_ap[:, :, c0:c1], src_ap[:, :, c0:c1]
            else:
                d_ap, s_ap = dst_ap[:, c0:c1], src_ap[:, c0:c1]
            dq = stgs.get("queues", ("sp",))
            DMA(dq[stgs["rr"] % len(dq)], sv, s_ap, [NOB], [st_.b])
            CAST(d_ap, sv, [st_.b], [dst_buf], join=(join or not first))
            first = False
            if stgs.get("jobs"):
                stgs["jobs"].pop(0)()

    GA = 652
    WG = nc.dram_tensor("WG_scr", [4, 128, 8, 908], BF16).ap()
    WD = nc.dram_tensor("WD_scr", [8, 128, 8, 512], BF16).ap()
    WO = nc.dram_tensor("WO_scr", [2, 128, 8, 1024], BF16).ap()
    W1S = nc.dram_tensor("W1_scr", [128, 32, 128], BF16).ap()
    W2S = nc.dram_tensor("W2_scr", [128, 128], BF16).ap()
    B_WG = [Buf("WG%d" % g) for g in range(4)]
    B_WD = [Buf("WD%d" % h) for h in range(8)]
    B_WO = [Buf("WO%d" % l) for l in range(2)]
    B_W1 = Buf("W1S")
    B_W2 = Buf("W2S")

    def setup_weights():
        stgs["stg"] = [sb("stg%d" % i, [128, STG_N]) for i in range(4)]
        stgs["rr"] = 0
        stgs["queues"] = ("sp",)
        asm = [sb("asm%d" % i, [128, 8, 1024], BF16) for i in range(2)]
        k = 0
        wv3 = nsa_w_in.rearrange("(c p) n -> p c n", p=128)
        if 0 in layers:
            for g in range(4):
                a_ = asm[k % 2]
                k += 1
                pieces = [(0, 256 * g, 256), (256, 1024 + 64 * g, 64), (320, 1280 + 64 * g, 64), (384, 1536 + 64 * g, 64),
                          (448, 2048 + 64 * g, 64), (512, 1792 + 64 * g, 64), (576, 2304 + 64 * g, 64), (640, 2560 + 12 * g, 12),
                          (652, 2608 + 256 * g, 256)]
                for pi, (d0, s0, n) in enumerate(pieces):
                    LOADW(a_[:, :, d0:d0 + n], wv3[:, :, s0:s0 + n], a_.b, join=(pi > 0))
                DMA("act", WG[g], a_[:, :, 0:908], [a_.b], [B_WG[g]])
            a_ = asm[k % 2]
            k += 1
            for lh in range(2):
                ls = slice(16 * lh, 16 * lh + 16)
                LOADW(a_[0:64, :, :].rearrange("p c n -> p (c n)")[:, 0:4096].rearrange("p (l h) -> p l h", l=32)[:, ls, :],
                      w1_k.rearrange("(l d) h -> d l h", d=64)[:, ls, :], a_.b, join=(lh > 0))
                LOADW(a_[64:128, :, :].rearrange("p c n -> p (c n)")[:, 0:4096].rearrange("p (l h) -> p l h", l=32)[:, ls, :],
                      w1_v.rearrange("(l d) h -> d l h", d=64)[:, ls, :], a_.b, join=True)
            LOADW(a_[:, 4, 0:64], w2_k[:, :], a_.b, join=True)
            LOADW(a_[:, 4, 64:128], w2_v[:, :], a_.b, join=True)
            DMA("act", W1S, a_[:, :, :].rearrange("p c n -> p (c n)")[:, 0:4096].rearrange("p (l h) -> p l h", l=32), [a_.b], [B_W1])
            DMA("act", W2S, a_[:, 4, 0:128], [a_.b], [B_W2])
        for l, w_ in enumerate((nsa_w_out,)):
            if l not in layers:
                continue
            a_ = asm[k % 2]
            k += 1
            wo3 = w_.rearrange("(c p) n -> p c n", p=128)
            for q4 in range(4):
                LOADW(a_[:, :, 256 * q4:256 * q4 + 256], wo3[:, :, 256 * q4:256 * q4 + 256], a_.b, join=(q4 > 0))
            DMA("act", WO[l], a_[:, :, :], [a_.b], [B_WO[l]])

    stat_rr = [0]

    def get_stat():
        stat_rr[0] = (stat_rr[0] + 1) % 4
        return stat[stat_rr[0]]

    def phase_norm_T(src_ap_fn, src_bufs, g_tile):
        for i in range(NT):
            xb_ = xt[i % 2]
            u_ = ub[i % 2]
            DMA("sp", xb_[:, :], src_ap_fn(i), src_bufs(i), [xb_.b])
            st = get_stat()
            ACT(u_[:, :], xb_[:, :], AF.Square, [xb_.b], [u_.b, st.b], accum_out=st[:, 0:1])
            V("dve", "tensor_scalar", [st.b], [st.b], out=st[:, 1:2], in0=st[:, 0:1], scalar1=1.0 / D, scalar2=EPS, op0=ALU.mult, op1=ALU.add)
            ACT(st[:, 2:3], st[:, 1:2], AF.Sqrt, [st.b], [st.b])
            V("dve", "reciprocal", [st.b], [st.b], out=st[:, 3:4], in_=st[:, 2:3])
            V("dve", "scalar_tensor_tensor", [xb_.b, st.b, g_tile.b], [u_.b], out=u_[:, :], in0=xb_[:, :], scalar=st[:, 3:4], in1=g_tile[:, :], op0=ALU.mult, op1=ALU.mult)
            bk = banks[6 + (i % 2)]
            bkv = bk[:, :].bitcast(BF16)
            for c in range(8):
                TR(bkv[:, c * 128:(c + 1) * 128], u_[:, c * 128:(c + 1) * 128], [u_.b], [bk.b])
            EVAC(uT[:, :, i * 128:(i + 1) * 128], bkv.rearrange("p (c t) -> p c t", c=8), [bk.b], [uT.b])

    pf_rr = [0]

    def proj_fm(wt, wb, outs, scale=None, bank_ids=(5, 6, 7), M=128):
        for Q in range(NQ):
            bk = banks[bank_ids[pf_rr[0] % len(bank_ids)]]
            pf_rr[0] += 1
            for c in range(8):
                MM(bk[0:M, :], wt[:, c, :], uT[:, c, Q * 512:(Q + 1) * 512], c == 0, c == 7, [wb, uT.b], [bk.b])
            for (rs_, fn_, ob_) in outs:
                EVAC(fn_(Q), bk[rs_, :], [bk.b], [ob_], scale=scale)

    def phase_out(wbig, w_out_d, gp, res_fn, res_bufs, dst_fn, dst_bufs):
        if w_out_d is not None:
            DMA("sp", wbig[:, :, :], WO[w_out_d], [B_WO[w_out_d]], [wbig.b])
        for i in range(NT):
            xb_ = xt[i % 2]
            DMA("sp", xb_[:, :], res_fn(i), res_bufs(i), [xb_.b])
            bk = [banks[2 + 2 * (i % 3)], banks[3 + 2 * (i % 3)]]
            for half in range(2):
                for c in range(8):
                    MM(bk[half][:, :], oT[:, c, i * 128:(i + 1) * 128], wbig[:, c, half * 512:(half + 1) * 512], c == 0, c == 7, [oT.b, wbig.b], [bk[half].b])
            st = get_stat()
            ACT(ub[0][:, 0:512], bk[0][:, :], AF.Square, [bk[0].b], [ub[0].b, st.b], accum_out=st[:, 0:1])
            ACT(ub[0][:, 512:1024], bk[1][:, :], AF.Square, [bk[1].b], [ub[0].b, st.b], accum_out=st[:, 1:2])
            V("dve", "tensor_tensor", [st.b], [st.b], out=st[:, 2:3], in0=st[:, 0:1], in1=st[:, 1:2], op=ALU.add)
            V("dve", "tensor_scalar", [st.b], [st.b], out=st[:, 3:4], in0=st[:, 2:3], scalar1=1.0 / D, scalar2=EPS, op0=ALU.mult, op1=ALU.add)
            ACT(st[:, 4:5], st[:, 3:4], AF.Sqrt, [st.b], [st.b])
            V("dve", "reciprocal", [st.b], [st.b], out=st[:, 5:6], in_=st[:, 4:5])
            yo = yout[0]
            for half in range(2):
                sl = slice(half * 512, (half + 1) * 512)
                V("dve", "scalar_tensor_tensor", [bk[half].b, st.b, gp.b], [yo.b], out=yo[:, sl], in0=bk[half][:, :], scalar=st[:, 5:6], in1=gp[:, sl], op0=ALU.mult, op1=ALU.mult)
            V("pool", "tensor_tensor", [yo.b, xb_.b], [xb_.b], out=xb_[:, :], in0=yo[:, :], in1=xb_[:, :], op=ALU.add)
            DMA("sp", dst_fn(i), xb_[:, :], [xb_.b], dst_bufs(i))


    class Stream:
        pass

    S_BANKS = [banks[0], banks[1], banks[2]]
    s_rr = [0]
    pt_rr = [0]
    LOOK = 6

    def run_streams(streams, filler=None, every=3):
        pending = []
        ntile = [0]

        def emit_pv(item):
            st_, kj, c0, c1, ptb = item
            for r in range(c0, c1):
                qi = 4 * st_.Q + r
                first = max(0, qi - 4) if st_.band else 0
                o_ap, o_tt = st_.O[r]
                if not hasattr(st_, "started"):
                    st_.started = set()
                is_first = id(o_tt) not in st_.started
                st_.started.add(id(o_tt))
                MM(o_ap, ptb[:, r * 128:(r + 1) * 128], st_.v_ap(kj), is_first, kj == qi,
                   [ptb.b] + st_.v_bufs, [o_tt.b], skip=True)
            if kj == st_.last_kj:
                st_.finalize(st_)

        for st_ in streams:
            Q = st_.Q
            kj_lo = max(0, 4 * Q - 4) if st_.band else 0
            st_.last_kj = 4 * Q + 3
            for kj in range(kj_lo, 4 * Q + 4):
                rd0 = kj - 4 * Q
                c0 = max(0, rd0)
                c1 = min(4, rd0 + 5) if st_.band else 4
                sbk = S_BANKS[s_rr[0]]
                s_rr[0] = (s_rr[0] + 1) % len(S_BANKS)
                ptb = PT[pt_rr[0]]
                pt_rr[0] = (pt_rr[0] + 1) % len(PT)
                cols = slice(c0 * 128, c1 * 128)
                gcols = slice(Q * 512 + c0 * 128, Q * 512 + c1 * 128)
                MM(sbk[:, cols], st_.k_ap(kj), st_.q_ap(gcols), True, st_.pen is None, st_.qk_bufs, [sbk.b])
                if st_.pen is not None:
                    MM(sbk[:, cols], cst["Xs"][0:32, kj * 128:(kj + 1) * 128], st_.pen[0:32, gcols], False, True,
                       [cst["Xs"].b, st_.pen_buf], [sbk.b])
                ACT(ptb[:, cols], sbk[:, cols], AF.Exp, [sbk.b], [ptb.b])
                if -1 <= rd0 <= 3:
                    lo = max(rd0, 0)
                    hi = min(rd0 + 2, 4)
                    eo = (lo - rd0) * 128
                    V("dve", "tensor_tensor", [ptb.b, Ee.b], [ptb.b], out=ptb[:, lo * 128:hi * 128], in0=ptb[:, lo * 128:hi * 128],
                      in1=Ee[:, st_.emap, eo:eo + (hi - lo) * 128], op=ALU.mult)
                if st_.band:
                    r4 = rd0 + 4
                    if 0 <= r4 <= 3:
                        V("dve", "tensor_tensor", [ptb.b, M4.b], [ptb.b], out=ptb[:, r4 * 128:(r4 + 1) * 128], in0=ptb[:, r4 * 128:(r4 + 1) * 128],
                          in1=M4[:, :], op=ALU.mult)
                pending.append((st_, kj, c0, c1, ptb))
                if len(pending) > LOOK:
                    emit_pv(pending.pop(0))
                ntile[0] += 1
                if filler is not None and ntile[0] % every == 0:
                    next(filler, None)
        while pending:
            emit_pv(pending.pop(0))
        if filler is not None:
            for _ in filler:
                pass

    def nsa_setup():
        t = {}
        load_norm_gains(0)
        t["keep"] = sb("keep", [128, NT, 32])
        t["addc"] = sb("addc", [128, NT, 32])
        DMA("sp", t["keep"][:, :, :], c_keep[:, :, :], [NOB], [t["keep"].b])
        DMA("sp", t["addc"][:, :, :], c_addc[:, :, :], [NOB], [t["addc"].b])
        t["w1"] = sb("w1", [128, 32, 128], BF16)
        DMA("sp", t["w1"][:, :, :], W1S, [B_W1], [t["w1"].b])
        t["w2"] = sb("w2", [128, 128], BF16)
        DMA("sp", t["w2"][:, :], W2S, [B_W2], [t["w2"].b])
        t["w2k"] = sb("w2k", [128, 128], BF16)
        V("pool", "memset", [], [t["w2k"].b], t["w2k"][:, :], 0.0)
        V("pool", "tensor_copy", [t["w2"].b, t["w2k"].b], [t["w2k"].b], out=t["w2k"][:, 0:64], in_=t["w2"][:, 0:64])
        pes = sb("pes", [32, 128])
        DMA("sp", pes[:, 0:64], pe_k[:, :], [NOB], [pes.b])
        DMA("sp", pes[:, 64:128], pe_v[:, :], [NOB], [pes.b], join=True)
        pesb = sb("pesb", [32, 128], BF16)
        V("dve", "tensor_copy", [pes.b], [pesb.b], out=pesb[:, :], in_=pes[:, :])
        peT = sb("peT", [128, 32], BF16)
        bkv = banks[3][:, :].bitcast(BF16)
        p.add("pe", lambda e: e.transpose(out=bkv[:, 0:32], in_=pesb[:, :], identity=ident[0:32, 0:32]), reads=[pesb.b, ident.b], writes=[banks[3].b])
        V("dve", "tensor_copy", [banks[3].b], [peT.b], out=peT[:, :], in_=bkv[:, 0:32])
        t["peh"] = sb("peh", [128, 2])
        for kv in range(2):
            ps_ = slice(64 * kv, 64 * kv + 64)
            for l in range(32):
                MM(banks[4 + kv][:, 0:1], t["w1"][ps_, l, :], peT[ps_, l:l + 1], l == 0, l == 31, [t["w1"].b, peT.b], [banks[4 + kv].b])
        for kv in range(2):
            V("dve", "tensor_copy", [banks[4 + kv].b], [t["peh"].b], out=t["peh"][:, kv:kv + 1], in_=banks[4 + kv][:, 0:1])
        t["Vca"] = sb("Vca", [127, 97], BF16)
        V("pool", "memset", [], [t["Vca"].b], t["Vca"][:, :], 1.0)
        DMA("sp", t["Vca"][:, 65:97], c_ovl[:, :], [t["Vca"].b], [t["Vca"].b])
        t["wgA"] = sb("wgA", [128, 8, GA], BF16)
        t["wgZ"] = sb("wgZ", [128, 8, 256], BF16)
        t["qa"] = [sb("qa%d" % i, [128, S], BF16) for i in range(4)]
        for i in range(4):
            V("pool", "memset", [], [t["qa"][i].b], t["qa"][i][:, :], 0.0)
        t["cT"] = sb("cT", [128, S], BF16)
        t["ksx"] = sb("ksx", [128, S], BF16)
        V("pool", "memset", [], [t["ksx"].b], t["ksx"][:, :], 0.0)
        DMA("sp", t["ksx"][64:96, :], c_X[:, :], [t["ksx"].b], [t["ksx"].b])
        t["kwz"] = sb("kwz", [128, S], BF16)
        V("pool", "memset", [], [t["kwz"].b], t["kwz"][:, :], 0.0)
        t["vsa"] = sb("vsa", [128, NT, 65], BF16)
        t["vwa"] = sb("vwa", [128, NT, 65], BF16)
        V("pool", "memset", [], [t["vsa"].b], t["vsa"][:, :, :], 1.0)
        V("pool", "memset", [], [t["vwa"].b], t["vwa"][:, :, :], 1.0)
        t["gate"] = sb("gate", [128, NT, 12])
        accraw = sb("acc", [128, max(NT * 256, 4096)])
        t["acc"] = TT(accraw[:, 0:NT * 256].rearrange("p (i c) -> p i c", i=NT), "acc")
        t["acc"].b = accraw.b
        t["wbig"] = TT(accraw[:, 0:4096].bitcast(BF16).rearrange("p (c n) -> p c n", c=8), "wbig")
        t["wbig"].b = accraw.b
        t["ha"] = [sb("ha%d" % i, [128, 128], BF16) for i in range(2)]
        t["kcmp"] = sb("kcmp", [128, 128], BF16)
        t["Ec"] = [sb("Ec%d" % i, [127, 512], BF16) for i in range(3)]
        t["cm"] = sb("cm", [128, 4, 4, 97])
        t["sm"] = [sb("sm0", [128, 32]), sb("sm1", [128, 512]), sb("sm2", [128, 288])]
        t["penb"] = sb("penb", [128, 4, 96], BF16)
        V("pool", "memset", [], [t["penb"].b], t["penb"][:, :, :], 0.0)
        t["tmp"] = [sb("tmpo%d" % i, [128, 4, 64]) for i in range(2)]
        t["th"] = [sb("th%d" % i, [128, 256], BF16) for i in range(2)]
        t["zsg"] = sb("zsg", [128, NT, 256], BF16)
        t["og"] = [sb("og%d" % i, [128, 256], BF16) for i in range(2)]
        DMA("sp", t["wgA"][:, :, :], WG[0][:, :, 0:GA], [B_WG[0]], [t["wgA"].b])
        DMA("sp", t["wgZ"][:, :, :], WG[0][:, :, GA:908], [B_WG[0]], [t["wgZ"].b])
        return t

    def nsa_layer(t, s):
        phase_norm_T(lambda i: x_in[s, i * 128:(i + 1) * 128, :], lambda i: [NOB], cst["gpre"])
        wgA, wgZ = t["wgA"], t["wgZ"]
        for g in range(4):
            g_next = (g + 1) % 4
            has_next = (g < 3) or (s + 1 < nseq)
            for j in range(4):
                qa_ = t["qa"][j]
                proj_fm(wgA[:, :, 64 * j:64 * j + 64], wgA.b, [(slice(0, 64), (lambda Q, qa_=qa_: qa_[0:64, Q * 512:(Q + 1) * 512]), qa_.b)], scale=0.125, M=64)
            proj_fm(wgA[:, :, 256:384], wgA.b, [(slice(0, 128), (lambda Q: t["cT"][:, Q * 512:(Q + 1) * 512]), t["cT"].b)])
            proj_fm(wgA[:, :, 384:448], wgA.b, [(slice(0, 64), (lambda Q: t["ksx"][0:64, Q * 512:(Q + 1) * 512]), t["ksx"].b)], M=64)
            proj_fm(wgA[:, :, 448:512], wgA.b, [(slice(0, 64), (lambda Q: t["kwz"][0:64, Q * 512:(Q + 1) * 512]), t["kwz"].b)], M=64)
            for i in range(NT):
                bk = banks[3 + (i % 2)]
                for c in range(8):
                    MM(bk[:, 0:140], uT[:, c, i * 128:(i + 1) * 128], wgA[:, c, 512:652], c == 0, c == 7, [uT.b, wgA.b], [bk.b])
                V("dve", "tensor_copy", [bk.b], [t["vsa"].b], out=t["vsa"][:, i, 0:64], in_=bk[:, 0:64])
                V("dve", "tensor_copy", [bk.b], [t["vwa"].b], out=t["vwa"][:, i, 0:64], in_=bk[:, 64:128])
                ACT(t["gate"][:, i, :], bk[:, 128:140], AF.Sigmoid, [bk.b], [t["gate"].b])
            if has_next:
                DMA("sp", wgA[:, :, :], WG[g_next][:, :, 0:GA], [B_WG[g_next]], [wgA.b])
            dbg("qT0", t["qa"][0], t["qa"][0][:, :], BF16)
            dbg("cT", t["cT"], t["cT"][:, :], BF16)
            dbg("vsa", t["vsa"], t["vsa"][:, :, :], BF16)
            dbg("gate", t["gate"], t["gate"][:, :, :])
            for kv in range(2):
                ps_ = slice(64 * kv, 64 * kv + 64)
                bk = banks[3 + kv]
                for l in range(32):
                    MM(bk[:, 0:NCMP], t["w1"][ps_, l, :], t["cT"][ps_, l:l + 16 * (NCMP - 1) + 1:16], l == 0, l == 31, [t["w1"].b, t["cT"].b], [bk.b])
                ACT(t["ha"][kv][:, 0:NCMP], bk[:, 0:NCMP], AF.Silu, [bk.b, t["peh"].b], [t["ha"][kv].b], bias=t["peh"][:, kv:kv + 1])
            MM(banks[5][:, 0:NCMP], t["w2k"][:, :], t["ha"][0][:, 0:NCMP], True, True, [t["w2k"].b, t["ha"][0].b], [banks[5].b])
            V("dve", "tensor_copy", [banks[5].b], [t["kcmp"].b], out=t["kcmp"][:, 0:NCMP], in_=banks[5][:, 0:NCMP])
            MM(banks[6][0:NCMP, 0:64], t["ha"][1][:, 0:NCMP], t["w2"][:, 64:128], True, True, [t["w2"].b, t["ha"][1].b], [banks[6].b])
            V("dve", "tensor_copy", [banks[6].b], [t["Vca"].b], out=t["Vca"][0:NCMP, 0:64], in_=banks[6][0:NCMP, 0:64])
            dbg("kcmp", t["kcmp"], t["kcmp"][:, 0:NCMP], BF16)
            dbg("Vca", t["Vca"], t["Vca"][0:NCMP, :], BF16)
            def zproj_gen():
                for i in range(NT):
                    bk = banks[6 + (i % 2)]
                    for c in range(8):
                        MM(bk[:, 0:256], uT[:, c, i * 128:(i + 1) * 128], wgZ[:, c, :], c == 0, c == 7, [uT.b, wgZ.b], [bk.b])
                    th = t["th"][i % 2]
                    ACT(th[:, :], bk[:, 0:256], AF.Tanh, [bk.b], [th.b], scale=0.5)
                    V("dve", "scalar_tensor_tensor", [th.b, bk.b], [t["zsg"].b], out=t["zsg"][:, i, :], in0=th[:, :], scalar=1.0, in1=bk[:, 0:256],
                      op0=ALU.add, op1=ALU.mult)
                    yield
                if has_next:
                    DMA("sp", wgZ[:, :, :], WG[g_next][:, :, GA:908], [B_WG[g_next]], [wgZ.b])
                yield
            zgen = zproj_gen()
            ec_rr = 0
            for Q in range(NQ):
                for j in range(4):
                    next(zgen, None)
                    h = 4 * g + j
                    qa_ = t["qa"][j]
                    ec = t["Ec"][ec_rr % 3]
                    ec_rr += 1
                    p0, p1 = Ec_rows(Q)
                    DMA("sp", ec[p0:p1, :], Ec_src(h, Q), [B_Dc], [ec.b])
                    sbk = S_BANKS[s_rr[0]]
                    s_rr[0] = (s_rr[0] + 1) % 3
                    ptb = PT[pt_rr[0]]
                    pt_rr[0] = (pt_rr[0] + 1) % len(PT)
                    MM(sbk[0:p1, :], t["kcmp"][:, 0:p1], qa_[:, Q * 512:(Q + 1) * 512], True, True, [t["kcmp"].b, qa_.b], [sbk.b])
                    ACT(ptb[0:p1, :], sbk[0:p1, :], AF.Exp, [sbk.b], [ptb.b])
                    segs = [(p0, p1)] if p0 != 32 else [(32, min(64, p1))] + ([(64, p1)] if p1 > 64 else [])
                    for (a0, a1) in segs:
                        V("dve", "tensor_tensor", [ptb.b, ec.b], [ptb.b], out=ptb[a0:a1, :], in0=ptb[a0:a1, :], in1=ec[a0:a1, :], op=ALU.mult)
                    ob = banks[3 + (j % 2)]
                    for r in range(4):
                        MM(ob[:, r * 97:(r + 1) * 97], ptb[0:p1, r * 128:(r + 1) * 128], t["Vca"][0:p1, :], True, True, [ptb.b, t["Vca"].b], [ob.b])
                    ACT(t["cm"][:, :, j, :], ob[:, 0:388].rearrange("p (r c) -> p r c", r=4), AF.Copy, [ob.b], [t["cm"].b])
                cm = t["cm"]
                sm0, sm1, sm2 = t["sm"]
                rinv = sm0[:, 0:16].rearrange("p (r j) -> p r j", r=4)
                coef = sm0[:, 16:32].rearrange("p (r j) -> p r j", r=4)
                V("dve", "tensor_scalar", [cm.b], [sm0.b], out=rinv, in0=cm[:, :, :, 64], scalar1=1e-30, scalar2=None, op0=ALU.add)
                V("dve", "reciprocal", [sm0.b], [sm0.b], out=rinv, in_=rinv)
                gv = t["gate"][:, 4 * Q:4 * Q + 4, :].rearrange("p r (j b) -> p r j b", b=3)
                V("dve", "tensor_tensor", [sm0.b, t["gate"].b], [sm0.b], out=coef, in0=rinv, in1=gv[:, :, :, 0], op=ALU.mult)
                accv = t["acc"][:, 4 * Q:4 * Q + 4, :].rearrange("p r (j d) -> p r j d", j=4)
                V("pool", "tensor_tensor", [cm.b, sm0.b], [t["acc"].b], out=accv, in0=cm[:, :, :, 0:64],
                  in1=coef.unsqueeze(3).broadcast_to([128, 4, 4, 64]), op=ALU.mult)
                next(zgen, None)
                impw = sm1[:, :].rearrange("p (r j n) -> p r j n", r=4, j=4)
                V("dve", "tensor_tensor", [cm.b, sm0.b], [sm1.b], out=impw, in0=cm[:, :, :, 65:97],
                  in1=rinv.unsqueeze(3).broadcast_to([128, 4, 4, 32]), op=ALU.mult)
                imp = sm2[:, 0:128].rearrange("p (r n) -> p r n", r=4)
                V("dve", "tensor_reduce", [sm1.b], [sm2.b], out=imp, in_=sm1[:, :].rearrange("p (r j n) -> p r n j", r=4, j=4), axis=AX.X, op=ALU.add)
                V("dve", "tensor_tensor", [sm2.b, t["keep"].b], [sm2.b], out=imp, in0=imp, in1=t["keep"][:, 4 * Q:4 * Q + 4, :], op=ALU.mult)
                V("dve", "tensor_tensor", [sm2.b, t["addc"].b], [sm2.b], out=imp, in0=imp, in1=t["addc"][:, 4 * Q:4 * Q + 4, :], op=ALU.add)
                next(zgen, None)
                top8 = sm2[:, 128:160].rearrange("p (r e) -> p r e", r=4)
                for r in range(4):
                    V("dve", "max", [sm2.b], [sm2.b], out=top8[:, r, :], in_=imp[:, r, :])
                pen32 = sm2[:, 160:288].rearrange("p (r n) -> p r n", r=4)
                V("dve", "tensor_tensor", [sm2.b], [sm2.b], out=pen32, in0=imp, in1=top8[:, :, 7:8].broadcast_to([128, 4, 32]), op=ALU.is_lt)
                V("dve", "tensor_scalar", [sm2.b], [t["penb"].b], out=t["penb"][:, :, 64:96], in0=pen32, scalar1=PEN, scalar2=None, op0=ALU.mult)
                next(zgen, None)
                bkp = banks[5]
                bkpv = bkp[:, :].bitcast(BF16)
                for r in range(4):
                    TR(bkpv[0:96, r * 128:(r + 1) * 128], t["penb"][:, r, :], [t["penb"].b], [bkp.b])
                cols = slice(Q * 512, (Q + 1) * 512)
                V("dve", "tensor_copy", [bkp.b], [t["qa"][0].b], out=t["qa"][0][64:96, cols], in_=bkpv[64:96, 0:512])
                for j in range(1, 4):
                    V("pool", "tensor_copy", [t["qa"][0].b], [t["qa"][j].b], out=t["qa"][j][64:96, cols], in_=t["qa"][0][64:96, cols])
            for _ in zgen:
                pass
            dbg("cm", t["cm"], t["cm"][:, :, :, :])
            dbg("acc_c", t["acc"], t["acc"][:, :, :])
            streams = []
            o_rr = 0
            for Q in range(NQ):
                for j in range(4):
                    for br in (1, 2):
                        st_ = Stream()
                        st_.Q = Q
                        st_.band = (br == 2)
                        qa_ = t["qa"][j]
                        kt_ = t["ksx"] if br == 1 else t["kwz"]
                        va_ = t["vsa"] if br == 1 else t["vwa"]
                        st_.q_ap = lambda gc, qa_=qa_: qa_[:, gc]
                        st_.k_ap = lambda kj, kt_=kt_: kt_[:, kj * 128:(kj + 1) * 128]
                        st_.qk_bufs = [qa_.b, kt_.b]
                        st_.pen = None
                        st_.pen_buf = None
                        st_.v_ap = lambda kj, va_=va_: va_[:, kj, :]
                        st_.v_bufs = [va_.b]
                        st_.emap = 4 * g + j
                        ob = banks[3 + (o_rr % 2)]
                        o_rr += 1
                        st_.O = [(ob[:, r * 65:(r + 1) * 65], ob) for r in range(4)]
                        st_.ob = ob
                        st_.j = j
                        st_.br = br

                        def fin(st_):
                            ob = st_.ob
                            Q, j, br = st_.Q, st_.j, st_.br
                            ov = ob[:, 0:260].rearrange("p (r c) -> p r c", r=4)
                            sm = get_stat()
                            V("dve", "reciprocal", [ob.b], [sm.b], out=sm[:, 0:4], in_=ov[:, :, 64])
                            gv = t["gate"][:, 4 * Q:4 * Q + 4, :].rearrange("p r (j b) -> p r j b", b=3)
                            V("dve", "tensor_tensor", [sm.b, t["gate"].b], [sm.b], out=sm[:, 4:8], in0=sm[:, 0:4], in1=gv[:, :, j, br], op=ALU.mult)
                            tm = t["tmp"][(2 * j + br) % 2]
                            V("dve", "tensor_tensor", [ob.b, sm.b], [tm.b], out=tm[:, :, :], in0=ov[:, :, 0:64],
                              in1=sm[:, 4:8].unsqueeze(2).broadcast_to([128, 4, 64]), op=ALU.mult)
                            accv = t["acc"][:, 4 * Q:4 * Q + 4, 64 * j:64 * j + 64]
                            V("pool", "tensor_tensor", [tm.b, t["acc"].b], [t["acc"].b], out=accv, in0=accv, in1=tm[:, :, :], op=ALU.add)
                        st_.finalize = fin
                        streams.append(st_)
            run_streams(streams)
            dbg("acc_f", t["acc"], t["acc"][:, :, :])
            for i in range(NT):
                bk = banks[5 + (i % 2)]
                og = t["og"][i % 2]
                V("dve", "scalar_tensor_tensor", [t["zsg"].b, t["acc"].b], [og.b], out=og[:, :], in0=t["acc"][:, i, :], scalar=0.5, in1=t["zsg"][:, i, :],
                  op0=ALU.mult, op1=ALU.mult)
                bkv = bk[:, :].bitcast(BF16)
                for pr in range(2):
                    TR(bkv[:, 512 + pr * 128:512 + (pr + 1) * 128], og[:, pr * 128:(pr + 1) * 128], [og.b], [bk.b])
                EVAC(oT[:, 2 * g:2 * g + 2, i * 128:(i + 1) * 128], bkv[:, 512:768].rearrange("p (c t) -> p c t", c=2), [bk.b], [oT.b])
        dbg("oT", oT, oT[:, :, :], BF16)
        dst, dstb = (x1_d, B_x1[s]) if 1 in layers else (out, B_out[s])
        phase_out(t["wbig"], 0, cst["gpost"], lambda i: x_in[s, i * 128:(i + 1) * 128, :], lambda i: [NOB],
                  lambda i: dst[s, i * 128:(i + 1) * 128, :], lambda i: [dstb[i]])

    def diff_setup():
        t = {}
        load_norm_gains(1)
        stgs["stg"] = [sb("dstg%d" % i, [128, STG_N]) for i in range(1)]
        stgs["rr"] = 0
        stgs["queues"] = ("sp",)
        cast_engs[0] = ("dve",)
        raw = sb("dscr", [128, 4096])
        t["sq"] = TT(raw[:, 0:NT * 128].rearrange("p (i c) -> p i c", i=NT), "sq")
        t["og"] = TT(raw[:, 2048:3072].bitcast(BF16)[:, 0:NT * 128].rearrange("p (i c) -> p i c", i=NT), "og")
        t["wbig"] = TT(raw[:, :].bitcast(BF16).rearrange("p (c n) -> p c n", c=8), "wbig")
        t["sq"].b = t["og"].b = t["wbig"].b = raw.b
        lam4 = sb("lam4", [128, 4, 64])
        for idx, src in enumerate((lq1, lk1, lq2, lk2)):
            DMA("sp", lam4[:, idx, :], src[0:1, :].partition_broadcast(128), [NOB], [lam4.b], join=(idx > 0))
        lm = sb("lm", [128, 8])
        prod = sb("lprod", [128, 2, 64])
        V("dve", "tensor_tensor", [lam4.b], [prod.b], out=prod[:, 0, :], in0=lam4[:, 0, :], in1=lam4[:, 1, :], op=ALU.mult)
        V("dve", "tensor_tensor", [lam4.b], [prod.b], out=prod[:, 1, :], in0=lam4[:, 2, :], in1=lam4[:, 3, :], op=ALU.mult)
        V("dve", "reduce_sum", [prod.b], [lm.b], out=lm[:, 0:2], in_=prod[:, :, :], axis=AX.X)
        ACT(lm[:, 2:4], lm[:, 0:2], AF.Exp, [lm.b], [lm.b])
        V("dve", "tensor_tensor", [lm.b], [lm.b], out=lm[:, 4:5], in0=lm[:, 3:4], in1=lm[:, 2:3], op=ALU.subtract)
        V("dve", "tensor_scalar", [lm.b], [lm.b], out=lm[:, 5:6], in0=lm[:, 4:5], scalar1=-LAMBDA_INIT, scalar2=None, op0=ALU.add)
        t["lm"] = lm
        t["subln"] = sb("sublnb", [128, D])
        DMA("sp", t["subln"][:, :], subln[0:1, :].partition_broadcast(128), [NOB], [t["subln"].b])
        t["w"] = [sb("wd%d" % i, [128, 8, 512], BF16) for i in range(2)]
        t["qT"] = [sb("dqT%d" % i, [128, S], BF16) for i in range(2)]
        t["kz"] = [[sb("dkz%d_%d" % (b_, i), [128, S], BF16) for i in range(2)] for b_ in range(2)]
        for b_ in range(2):
            for i in range(2):
                V("pool", "memset", [], [t["kz"][b_][i].b], t["kz"][b_][i][:, :], 0.0)
        t["va"] = [sb("dva%d" % i, [128, NT, 129], BF16) for i in range(2)]
        for i in range(2):
            V("pool", "memset", [], [t["va"][i].b], t["va"][i][:, :, :], 1.0)
        t["od"] = [sb("od%d" % i, [128, NT, 128]) for i in range(2)]
        t["zs"] = [sb("dzs%d" % i, [128, NT, 128], BF16) for i in range(2)]
        t["tmp"] = [sb("dtmp%d" % i, [128, 2, 128]) for i in range(2)]
        t["rs"] = sb("drs", [128, 4, NT])
        t["th"] = [sb("dth0", [128, 512], BF16)] * 2
        return t

    def diff_layer(t, s):
        phase_norm_T(lambda i: x1_d[s, i * 128:(i + 1) * 128, :], lambda i: [B_x1[s][i]], cst["gpre"])
        dv3 = diff_w_in.rearrange("(c p) n -> p c n", p=128)
        lm = t["lm"]

        def load_w(h):
            w = t["w"][h % 2]
            for part in range(4):
                LOADW(w[:, :, 128 * part:128 * part + 128], dv3[:, :, 1024 * part + 128 * h:1024 * part + 128 * h + 128], w.b, join=(part > 0))

        def proj_gen(h, part="all"):
            hb = h % 2
            w = t["w"][hb]
            qT, kz, va, zs = t["qT"][hb], t["kz"][hb], t["va"][hb], t["zs"][hb]
            for Q in range(NQ if part in ("all", "qkv") else 0):
                bk = banks[7]
                for c in range(8):
                    MM(bk[:, :], w[:, c, 0:128], uT[:, c, Q * 512:(Q + 1) * 512], c == 0, c == 7, [w.b, uT.b], [bk.b])
                EVAC(qT[:, Q * 512:(Q + 1) * 512], bk[:, :], [bk.b], [qT.b], scale=0.125)
                yield
            for Q in range(NQ if part in ("all", "qkv") else 0):
                bk = banks[7]
                for c in range(8):
                    MM(bk[:, :], w[:, c, 128:256], uT[:, c, Q * 512:(Q + 1) * 512], c == 0, c == 7, [w.b, uT.b], [bk.b])
                EVAC(kz[0][0:64, Q * 512:(Q + 1) * 512], bk[0:64, :], [bk.b], [kz[0].b])
                EVAC(kz[1][64:128, Q * 512:(Q + 1) * 512], bk[64:128, :], [bk.b], [kz[1].b])
                yield
            for i4 in range(NT // 4 if part in ("all", "qkv") else 0):
                bk = banks[7]
                for r in range(4):
                    i = 4 * i4 + r
                    for c in range(8):
                        MM(bk[:, r * 128:(r + 1) * 128], uT[:, c, i * 128:(i + 1) * 128], w[:, c, 256:384], c == 0, c == 7, [uT.b, w.b], [bk.b])
                V("dve", "tensor_copy", [bk.b], [va.b], out=va[:, 4 * i4:4 * i4 + 4, 0:128], in_=bk[:, :].rearrange("p (r c) -> p r c", r=4))
                yield
            for i4 in range(NT // 4 if part in ("all", "z") else 0):
                bk = banks[7]
                for r in range(4):
                    i = 4 * i4 + r
                    for c in range(8):
                        MM(bk[:, r * 128:(r + 1) * 128], uT[:, c, i * 128:(i + 1) * 128], w[:, c, 384:512], c == 0, c == 7, [uT.b, w.b], [bk.b])
                th = t["th"][i4 % 2]
                ACT(th[:, :], bk[:, :], AF.Tanh, [bk.b], [th.b], scale=0.5)
                V("dve", "scalar_tensor_tensor", [th.b, bk.b], [zs.b], out=zs[:, 4 * i4:4 * i4 + 4, :], in0=th[:, :].rearrange("p (r c) -> p r c", r=4),
                  scalar=1.0, in1=bk[:, :].rearrange("p (r c) -> p r c", r=4), op0=ALU.add, op1=ALU.mult)
                yield
            if h + 2 < 8 and part in ("all", "z"):
                load_w(h + 2)
            yield

        def tail_gen(h):
            hb = h % 2
            od, zs, sq, rs = t["od"][hb], t["zs"][hb], t["sq"], t["rs"]
            V("dve", "tensor_tensor", [od.b], [sq.b], out=sq[:, :, :], in0=od[:, :, :], in1=od[:, :, :], op=ALU.mult)
            yield
            V("dve", "reduce_sum", [sq.b], [rs.b], out=rs[:, 0, :], in_=sq[:, :, :], axis=AX.X)
            yield
            V("dve", "tensor_scalar", [rs.b], [rs.b], out=rs[:, 1, :], in0=rs[:, 0, :], scalar1=1.0 / 128, scalar2=EPS, op0=ALU.mult, op1=ALU.add)
            ACT(rs[:, 2, :], rs[:, 1, :], AF.Sqrt, [rs.b], [rs.b])
            V("dve", "reciprocal", [rs.b], [rs.b], out=rs[:, 3, :], in_=rs[:, 2, :])
            yield
            V("dve", "tensor_tensor", [od.b, rs.b], [sq.b], out=sq[:, :, :], in0=od[:, :, :], in1=rs[:, 3, :].unsqueeze(2).broadcast_to([128, NT, 128]), op=ALU.mult)
            yield
            V("dve", "scalar_tensor_tensor", [sq.b, t["subln"].b], [sq.b], out=sq[:, :, :], in0=sq[:, :, :], scalar=0.5 * (1.0 - LAMBDA_INIT),
              in1=t["subln"][:, 128 * h:128 * h + 128].unsqueeze(1).broadcast_to([128, NT, 128]), op0=ALU.mult, op1=ALU.mult)
            yield
            V("dve", "tensor_tensor", [sq.b, zs.b], [t["og"].b], out=t["og"][:, :, :], in0=sq[:, :, :], in1=zs[:, :, :], op=ALU.mult)
            yield
            for i4 in range(NT // 4):
                bk = banks[7]
                bkv = bk[:, :].bitcast(BF16)
                for r in range(4):
                    TR(bkv[:, r * 128:(r + 1) * 128], t["og"][:, 4 * i4 + r, :], [t["og"].b], [bk.b])
                EVAC(oT[:, h, i4 * 512:(i4 + 1) * 512], bkv[:, 0:512], [bk.b], [oT.b])
                yield

        def chain(*gens):
            for g_ in gens:
                if g_ is not None:
                    for _ in g_:
                        yield

        def head_streams(h):
            hb = h % 2
            qT, kz, va, od = t["qT"][hb], t["kz"][hb], t["va"][hb], t["od"][hb]
            streams = []
            for Q in range(NQ):
                for m in range(2):
                    st_ = Stream()
                    st_.Q = Q
                    st_.band = False
                    kz_ = kz[m]
                    st_.q_ap = lambda gc, qT=qT: qT[:, gc]
                    st_.k_ap = lambda kj, kz_=kz_: kz_[:, kj * 128:(kj + 1) * 128]
                    st_.qk_bufs = [qT.b, kz_.b]
                    st_.pen = None
                    st_.pen_buf = None
                    st_.v_ap = lambda kj, va=va: va[:, kj, :]
                    st_.v_bufs = [va.b]
                    st_.emap = 2 * h + m
                    obs = [banks[3 + 2 * m], banks[4 + 2 * m]]
                    st_.O = [(obs[r // 2][:, (r % 2) * 129:(r % 2) * 129 + 129], obs[r // 2]) for r in range(4)]
                    st_.obs = obs
                    st_.m = m
                    st_.od = od

                    def fin(st_):
                        Q, m, od = st_.Q, st_.m, st_.od
                        for half in range(2):
                            ob = st_.obs[half]
                            ov = ob[:, 0:258].rearrange("p (r c) -> p r c", r=2)
                            i0 = 4 * Q + 2 * half
                            sm = get_stat()
                            V("dve", "reciprocal", [ob.b], [sm.b], out=sm[:, 0:2], in_=ov[:, :, 128])
                            if m == 0:
                                V("dve", "tensor_tensor", [ob.b, sm.b], [od.b], out=od[:, i0:i0 + 2, :], in0=ov[:, :, 0:128],
                                  in1=sm[:, 0:2].unsqueeze(2).broadcast_to([128, 2, 128]), op=ALU.mult)
                            else:
                                V("dve", "tensor_scalar", [sm.b, lm.b], [sm.b], out=sm[:, 2:4], in0=sm[:, 0:2], scalar1=lm[:, 5:6], scalar2=None, op0=ALU.mult)
                                tm = t["tmp"][half]
                                V("dve", "tensor_tensor", [ob.b, sm.b], [tm.b], out=tm[:, :, :], in0=ov[:, :, 0:128],
                                  in1=sm[:, 2:4].unsqueeze(2).broadcast_to([128, 2, 128]), op=ALU.mult)
                                V("pool", "tensor_tensor", [tm.b, od.b], [od.b], out=od[:, i0:i0 + 2, :], in0=od[:, i0:i0 + 2, :],
                                  in1=tm[:, :, :], op=ALU.add)
                    st_.finalize = fin
                    streams.append(st_)
            return streams

        load_w(0)
        load_w(1)
        for _ in proj_gen(0):
            pass
        for h in range(8):
            fill = chain(proj_gen(h + 1, "qkv") if h < 7 else None, tail_gen(h - 1) if h > 0 else None,
                         proj_gen(h + 1, "z") if h < 7 else None)
            run_streams(head_streams(h), filler=fill, every=2)
        for _ in tail_gen(7):
            pass
        cast_engs[0] = ("dve", "act")
        wo3 = diff_w_out.rearrange("(c p) n -> p c n", p=128)
        for q4 in range(4):
            LOADW(t["wbig"][:, :, 256 * q4:256 * q4 + 256], wo3[:, :, 256 * q4:256 * q4 + 256], t["wbig"].b, join=(q4 > 0))
        cast_engs[0] = ("dve",)
        phase_out(t["wbig"], None, cst["gpost"], lambda i: x1_d[s, i * 128:(i + 1) * 128, :], lambda i: [B_x1[s][i]],
                  lambda i: out[s, i * 128:(i + 1) * 128, :], lambda i: [B_out[s][i]])

    with es:
        with ExitStack() as es1:
            cur_es[0] = es1
            setup_bias()
            setup_weights()
            setup_bias_load()
        cur_es[0] = es
        p.barrier()
        if 0 in layers:
            with ExitStack() as es2:
                cur_es[0] = es2
                nt_ = nsa_setup()
                for s in range(nseq):
                    nsa_layer(nt_, s)
            cur_es[0] = es
            p.barrier()
        if 1 in layers:
            with ExitStack() as es3:
                cur_es[0] = es3
                dt_ = diff_setup()
                for s in range(nseq):
                    if 0 not in layers:
                        for i in range(NT):
                            xb_ = xt[i % 2]
                            DMA("sp", xb_[:, :], x_in[s, i * 128:(i + 1) * 128, :], [NOB], [xb_.b])
                            DMA("sp", x1_d[s, i * 128:(i + 1) * 128, :], xb_[:, :], [xb_.b], [B_x1[s][i]])
                    diff_layer(dt_, s)
            cur_es[0] = es
        p.emit()
    return nc


INPUT_NAMES = ["rel_bias_table", "norm_pre", "norm_post", "nsa_w_in", "nsa_cmp_pe_k", "nsa_cmp_w1_k", "nsa_cmp_w2_k",
               "nsa_cmp_pe_v", "nsa_cmp_w1_v", "nsa_cmp_w2_v", "nsa_w_out", "diff_w_in", "diff_lambda_q1", "diff_lambda_k1",
               "diff_lambda_q2", "diff_lambda_k2", "diff_subln", "diff_w_out"]


def make_in_maps(inputs, n_cores, nseq, S):
    consts = host_consts(S)
    shared = {}
    for k in INPUT_NAMES:
        a = np.ascontiguousarray(np.asarray(inputs[k], dtype=np.float32))
        if a.ndim == 3:
            a = a[0]
        elif k.startswith("diff_lambda") or k == "diff_subln":
            a = a.reshape(1, -1)
        shared[k] = np.ascontiguousarray(a)
    shared.update(consts)
    x = np.asarray(inputs["x"], dtype=np.float32)
    maps = []
    for c in range(n_cores):
        m = dict(shared)
        m["x"] = np.ascontiguousarray(x[c * nseq:(c + 1) * nseq])
        maps.append(m)
    return maps


def kernel(**inputs):
    x = np.asarray(inputs["x"])
    B, S, _ = x.shape
    n_cores = 8
    nseq = B // n_cores
    nc = build(nseq, S)
    in_maps = make_in_maps(inputs, n_cores, nseq, S)
    res = run_bass_kernel_spmd(nc, in_maps, core_ids=list(range(n_cores)))
    return np.concatenate([np.asarray(r["out"]) for r in res.results], axis=0).astype(np.float32)
```

```python
import math
from contextlib import ExitStack

import numpy as np
import ml_dtypes

import concourse.bass as bass
import concourse.mybir as mybir
from concourse.bass_utils import run_bass_kernel_spmd

F32 = mybir.dt.float32
BF16 = mybir.dt.bfloat16
AF = mybir.ActivationFunctionType
ALU = mybir.AluOpType
AX = mybir.AxisListType

D = 1024
NSA_IN = 3632
EPS = 1e-6
PEN = -30000.0
LAMBDA_INIT = 0.8 - 0.6 * math.exp(-0.3 * 1)


class Buf:
    __slots__ = ("name", "writers", "readers", "prev")

    def __init__(self, name=""):
        self.name = name
        self.writers = []
        self.readers = []
        self.prev = []


class Op:
    __slots__ = ("eng", "fn", "dma", "deps", "needs_sig", "sig")

    def __init__(self, eng, fn, dma):
        self.eng = eng
        self.fn = fn
        self.dma = dma
        self.deps = []
        self.needs_sig = False
        self.sig = None


ENGS = ("pe", "act", "dve", "pool", "sp")


class Prog:
    def __init__(self, nc, n_dma_sems=48):
        self.nc = nc
        self.ops = {e: [] for e in ENGS}
        self.n_dma_sems = n_dma_sems
        self.dma_last = [None] * n_dma_sems
        self.dma_cnt = [0] * n_dma_sems
        self.dma_rr = 0
        self.pending = {}

    def barrier(self):
        lasts = []
        for e in ENGS:
            for op in reversed(self.ops[e]):
                if not op.dma:
                    op.needs_sig = True
                    lasts.append(op)
                    break
        for j in range(self.n_dma_sems):
            if self.dma_last[j] is not None:
                lasts.append(self.dma_last[j])
        self.pending = {e: list(lasts) for e in ENGS}

    def add(self, eng, fn, reads=(), writes=(), dma=False, join=False):
        op = Op(eng, fn, dma)
        if self.pending.get(eng):
            op.deps.extend(self.pending.pop(eng))
        for b in reads:
            for w in b.writers:
                self._dep(w, op, True)
        for b in writes:
            if not join:
                for w in b.writers:
                    self._dep(w, op, False)
            else:
                for r in b.prev:
                    self._dep(r, op, False)
            for r in b.readers:
                self._dep(r, op, False)
        for b in writes:
            if join:
                b.writers.append(op)
            else:
                b.prev = list(b.writers) + list(b.readers)
                b.writers = [op]
                b.readers = []
        for b in reads:
            b.readers.append(op)
        if dma:
            j = self.dma_rr
            self.dma_rr = (j + 1) % self.n_dma_sems
            prev = self.dma_last[j]
            if prev is not None:
                op.deps.append(prev)
            self.dma_cnt[j] += 1
            op.sig = (("d", j), 16 * self.dma_cnt[j])
            op.needs_sig = True
            self.dma_last[j] = op
        self.ops[eng].append(op)
        return op

    def _dep(self, p, c, raw):
        if p is c:
            return
        if (not p.dma) and (not c.dma) and p.eng == c.eng:
            if p.eng == "pe" or not raw:
                return
        p.needs_sig = True
        c.deps.append(p)

    def emit(self):
        nc = self.nc
        with ExitStack() as es:
            esem = {e: es.enter_context(nc.semaphore("s_" + e)) for e in ENGS}
            dsem = [es.enter_context(nc.semaphore("d%d" % j)) for j in range(self.n_dma_sems)]
            for e in ENGS:
                cnt = 0
                for op in self.ops[e]:
                    if op.dma:
                        continue
                    if op.needs_sig:
                        cnt += 1
                        op.sig = (("e", e), cnt)

            def semof(key):
                return esem[key[1]] if key[0] == "e" else dsem[key[1]]

            block = es.enter_context(nc.Block())
            engobj = {"pe": "tensor", "act": "scalar", "dve": "vector", "pool": "gpsimd", "sp": "sync"}

            def make(e):
                def body(eng):
                    waited = {}
                    for op in self.ops[e]:
                        need = {}
                        for p in op.deps:
                            k, v = p.sig
                            if need.get(k, 0) < v:
                                need[k] = v
                        for k, v in need.items():
                            if waited.get(k, 0) >= v:
                                continue
                            eng.wait_ge(semof(k), v)
                            waited[k] = v
                        ins = op.fn(eng)
                        if op.needs_sig:
                            k, v = op.sig
                            ins.then_inc(semof(k), 16 if op.dma else 1)
                    if e == "sp":
                        for j in range(self.n_dma_sems):
                            if self.dma_cnt[j] > 0:
                                eng.wait_ge(dsem[j], 16 * self.dma_cnt[j])
                return body

            for e in ENGS:
                getattr(block, engobj[e])(make(e))


def _rel_bucket(n):
    n = np.maximum(n, 0)
    nf = np.maximum(n, 1).astype(np.float32)
    large = 16 + (np.log(nf / np.float32(16)) / np.float32(math.log(8.0)) * np.float32(16)).astype(np.int32)
    large = np.minimum(large, 31)
    return np.where(n < 16, n, large)


def host_consts(S):
    NT = S // 128
    ncmp = S // 16 - 1
    bf = ml_dtypes.bfloat16
    c = {}
    c["c_ident"] = np.eye(128, dtype=np.float32).astype(bf)
    b = _rel_bucket(np.arange(128))
    oh = np.zeros((32, 128), np.float32)
    oh[b, np.arange(128)] = 1.0
    oh[31, :] -= 1.0
    c["c_onehot"] = oh
    cmp_lo = np.arange(ncmp) * 16
    sel_lo = np.arange(S // 64) * 64
    ov = np.clip(np.minimum(cmp_lo[:, None] + 32, sel_lo[None, :] + 64)
                 - np.maximum(cmp_lo[:, None], sel_lo[None, :]), 0, None).astype(np.float32) / 32.0
    ovp = np.zeros((127, 32), np.float32)
    ovp[:ncmp, :S // 64] = ov
    c["c_ovl"] = ovp.astype(bf)
    X = np.zeros((32, S), np.float32)
    X[np.arange(S) // 64, np.arange(S)] = 1.0
    c["c_X"] = X.astype(bf)
    k = np.arange(128)[:, None]
    q = np.arange(128)[None, :]
    c["c_M4"] = (q < k).astype(np.float32).astype(bf)
    t = np.arange(S)
    cur = t // 64
    n = np.arange(32)[None, :]
    forced = (n == 0) | (n == cur[:, None]) | (n == cur[:, None] - 1)
    future = n > cur[:, None]
    keep = (~(forced | future)).astype(np.float32)
    addc = np.where(forced, 1e9, np.where(future, -1e9, 0.0)).astype(np.float32)
    c["c_keep"] = np.ascontiguousarray(keep.reshape(NT, 128, 32).transpose(1, 0, 2))
    c["c_addc"] = np.ascontiguousarray(addc.reshape(NT, 128, 32).transpose(1, 0, 2))
    return c


def build(nseq, S, layers=(0, 1)):
    NT = S // 128
    NQ = S // 512
    NCMP = S // 16 - 1
    nc = bass.Bass("TRN2", target_bir_lowering=False)
    p = Prog(nc)

    def din(name, shape, dt=F32):
        return nc.dram_tensor(name, list(shape), dt, kind="ExternalInput").ap()

    x_in = din("x", [nseq, S, D])
    table = din("rel_bias_table", [32, 16])
    norm_pre = din("norm_pre", [2, D])
    norm_post = din("norm_post", [2, D])
    nsa_w_in = din("nsa_w_in", [D, NSA_IN])
    pe_k = din("nsa_cmp_pe_k", [32, 64])
    w1_k = din("nsa_cmp_w1_k", [2048, 128])
    w2_k = din("nsa_cmp_w2_k", [128, 64])
    pe_v = din("nsa_cmp_pe_v", [32, 64])
    w1_v = din("nsa_cmp_w1_v", [2048, 128])
    w2_v = din("nsa_cmp_w2_v", [128, 64])
    nsa_w_out = din("nsa_w_out", [D, D])
    diff_w_in = din("diff_w_in", [D, 4096])
    lq1 = din("diff_lambda_q1", [1, 64])
    lk1 = din("diff_lambda_k1", [1, 64])
    lq2 = din("diff_lambda_q2", [1, 64])
    lk2 = din("diff_lambda_k2", [1, 64])
    subln = din("diff_subln", [1, D])
    diff_w_out = din("diff_w_out", [D, D])
    c_ident = din("c_ident", [128, 128], BF16)
    c_onehot = din("c_onehot", [32, 128])
    c_ovl = din("c_ovl", [127, 32], BF16)
    c_X = din("c_X", [32, S], BF16)
    c_M4 = din("c_M4", [128, 128], BF16)
    c_keep = din("c_keep", [128, NT, 32])
    c_addc = din("c_addc", [128, NT, 32])
    out = nc.dram_tensor("out", [nseq, S, D], F32, kind="ExternalOutput").ap()
    x1_d = nc.dram_tensor("x1_scr", [nseq, S, D], F32).ap()
    De = nc.dram_tensor("De_scr", [16, 128, 512], BF16).ap()
    Dc = nc.dram_tensor("Dc_scr", [16, 64, 2048], BF16).ap()
    B_x1 = [[Buf("x1d%d_%d" % (s, i)) for i in range(NT)] for s in range(nseq)]
    B_out = [[Buf("out%d_%d" % (s, i)) for i in range(NT)] for s in range(nseq)]
    B_De = Buf("De")
    B_Dc = Buf("Dc")
    NOB = Buf("const_in")

    import os as _os
    es = ExitStack()
    cur_es = [es]
    DEBUG = bool(_os.environ.get("KDEBUG"))
    dbg_seen = set()

    def dbg(name, tt, ap, dt=F32):
        if not DEBUG or name in dbg_seen:
            return
        dbg_seen.add(name)
        o = nc.dram_tensor("dbg_" + name, list(ap.shape), dt, kind="ExternalOutput").ap()
        p.add("sp", lambda e: e.dma_start(out=o, in_=ap), reads=[tt.b], writes=[Buf()], dma=True)

    class TT:
        def __init__(self, t, name):
            self.t = t
            self.b = Buf(name)

        def __getitem__(self, k):
            return self.t[k]

    def sb(name, shape, dt=F32):
        if _os.environ.get("KDEBUG_SB"):
            print("SB", name, shape, "remaining", nc.sbuf_bytes_remaining)
        return TT(cur_es[0].enter_context(nc.sbuf_tensor(name, list(shape), dt)), name)

    banks = [TT(es.enter_context(nc.psum_tensor("bank%d" % i, [128, 512], F32)), "bank%d" % i) for i in range(8)]

    def DMA(eng, out_ap, in_ap, reads, writes, join=False, **kw):
        p.add(eng, lambda e: e.dma_start(out=out_ap, in_=in_ap, **kw), reads=reads, writes=writes, dma=True, join=join)

    def MM(out_ap, lhsT, rhs, start, stop, reads, writes, skip=False):
        if skip:
            p.add("pe", lambda e: e.matmul(out_ap, lhsT=lhsT, rhs=rhs, start=start, stop=stop, skip_group_check=True), reads=reads, writes=writes)
        else:
            p.add("pe", lambda e: e.matmul(out_ap, lhsT=lhsT, rhs=rhs, start=start, stop=stop), reads=reads, writes=writes)

    def TR(out_ap, in_ap, reads, writes):
        p.add("pe", lambda e: e.transpose(out=out_ap, in_=in_ap, identity=ident[:, :]), reads=list(reads) + [ident.b], writes=writes)

    def ACT(out_ap, in_ap, func, reads, writes, **kw):
        p.add("act", lambda e: e.activation(out=out_ap, in_=in_ap, func=func, **kw), reads=reads, writes=writes)

    def V(eng, name, reads, writes, *a, **kw):
        p.add(eng, lambda e: getattr(e, name)(*a, **kw), reads=reads, writes=writes)

    evac_rr = [0]

    def EVAC(out_ap, in_ap, reads, writes, scale=None, eng=None):
        evac_rr[0] ^= 1
        if (eng == "act") or (eng is None and evac_rr[0]):
            if scale is None:
                ACT(out_ap, in_ap, AF.Copy, reads, writes)
            else:
                ACT(out_ap, in_ap, AF.Copy, reads, writes, scale=float(scale))
        else:
            if scale is None:
                V("dve", "tensor_copy", reads, writes, out=out_ap, in_=in_ap)
            else:
                V("dve", "tensor_scalar", reads, writes, out=out_ap, in0=in_ap, scalar1=float(scale), scalar2=None, op0=ALU.mult)

    ident = sb("ident", [128, 128], BF16)
    DMA("sp", ident[:, :], c_ident[:, :], [NOB], [ident.b])
    M4 = sb("M4", [128, 128], BF16)
    DMA("sp", M4[:, :], c_M4[:, :], [NOB], [M4.b])
    Ee = sb("Ee", [128, 16, 256], BF16)
    uT = sb("uT", [128, 8, S], BF16)
    oT = sb("oT", [128, 8, S], BF16)
    xt = [sb("xt%d" % i, [128, D]) for i in range(2)]
    ub = [sb("ub%d" % i, [128, D], BF16) for i in range(2)]
    stat = [sb("stat%d" % i, [128, 8]) for i in range(4)]
    NPT = 8
    PT = [sb("PT%d" % i, [128, 512], BF16) for i in range(NPT)]
    yout = [sb("yout0", [128, D])]
    cst = {}

    def setup_bias():
        tab = sb("tab", [32, 16])
        oh = sb("oh", [32, 128])
        fse = sb("fse", [16, 512], BF16)
        fsc = sb("fsc", [16, 2048], BF16)
        DMA("sp", tab[:, :], table[:, :], [NOB], [tab.b])
        DMA("sp", oh[:, :], c_onehot[:, :], [NOB], [oh.b])
        MM(banks[0][0:16, 0:128], tab[:, :], oh[:, :], True, True, [tab.b, oh.b], [banks[0].b])
        V("pool", "memset", [], [fse.b], fse[:, :], 0.0)
        V("pool", "memset", [fse.b], [fse.b], fse[:, 128:384], 1.0)
        V("pool", "memset", [], [fsc.b], fsc[:, :], 0.0)
        V("pool", "memset", [fsc.b], [fsc.b], fsc[:, 0:512], 1.0)
        V("pool", "memset", [fsc.b], [fsc.b], fsc[:, 1695:2048], 1.0)
        ACT(fse[:, 0:128], banks[0][0:16, 0:128], AF.Exp, [banks[0].b, fse.b], [fse.b])
        ACT(fsc[:, 1567:1695], banks[0][0:16, 0:128], AF.Exp, [banks[0].b, fsc.b], [fsc.b])
        DMA("act", De, fse[:, :].unsqueeze(1).broadcast_to([16, 128, 512]), [fse.b], [B_De])
        DMA("act", Dc, fsc[:, :].unsqueeze(1).broadcast_to([16, 64, 2048]), [fsc.b], [B_Dc])

    def setup_bias_load():
        while stgs.get("jobs"):
            stgs["jobs"].pop(0)()
        for h in range(16):
            DMA("sp", Ee[:, h, :], bass.AP(De.tensor, h * 128 * 512, [[511, 128], [1, 256]]), [B_De], [Ee.b], join=(h > 0))

    def load_norm_gains(l):
        cst["gpre"] = sb("gpre%d" % l, [128, D])
        cst["gpost"] = sb("gpost%d" % l, [128, D])
        DMA("sp", cst["gpre"][:, :], norm_pre[l:l + 1, :].partition_broadcast(128), [NOB], [cst["gpre"].b])
        DMA("sp", cst["gpost"][:, :], norm_post[l:l + 1, :].partition_broadcast(128), [NOB], [cst["gpost"].b])

    def Ec_rows(Q):
        p0 = max(0, 32 * (Q - 1))
        p1 = min(NCMP, 32 * Q + 32)
        return p0, p1

    def Ec_src(h, Q):
        p0, p1 = Ec_rows(Q)
        i0 = p0 - 32 * Q + 32
        return bass.AP(Dc.tensor, h * 64 * 2048 + i0 * 2032, [[2032, p1 - p0], [1, 512]])

    STG_N = 1024
    stgs = {}
    cast_rr = [0]

    cast_engs = [("dve", "act")]

    def CAST(out_ap, in_ap, reads, writes, join=False):
        e = cast_engs[0][cast_rr[0] % len(cast_engs[0])]
        cast_rr[0] += 1
        if e == "act":
            p.add("act", lambda en: en.activation(out=out_ap, in_=in_ap, func=AF.Copy), reads=reads, writes=writes, join=join)
        else:
            p.add(e, lambda en: en.tensor_copy(out=out_ap, in_=in_ap), reads=reads, writes=writes, join=join)

    def LOADW(dst_ap, src_ap, dst_buf, join=False):
        stg = stgs["stg"]
        shp = list(dst_ap.shape)
        P_ = shp[0]
        mid = 1
        for d_ in shp[1:-1]:
            mid *= d_
        last = shp[-1]
        step = max(1, STG_N // mid)
        bp = dst_ap.base_partition()
        first = True
        for c0 in range(0, last, step):
            c1 = min(last, c0 + step)
            n = mid * (c1 - c0)
            assert n <= STG_N, shp
            st_ = stg[stgs["rr"]]
            stgs["rr"] = (stgs["rr"] + 1) % len(stg)
            sv = st_[bp:bp + P_, 0:n]
            if len(shp) == 3:
                sv = sv.rearrange("p (a b) -> p a b", a=shp[1])
                d_ap, s_ap = dst_ap[:, :, c0:c1], src_ap[:, :, c0:c1]
            else:
                d_ap, s_ap = dst_ap[:, c0:c1], src_ap[:, c0:c1]
            dq = stgs.get("queues", ("sp",))
            DMA(dq[stgs["rr"] % len(dq)], sv, s_ap, [NOB], [st_.b])
            CAST(d_ap, sv, [st_.b], [dst_buf], join=(join or not first))
            first = False
            if stgs.get("jobs"):
                stgs["jobs"].pop(0)()

    GA = 652
    WG = nc.dram_tensor("WG_scr", [4, 128, 8, 908], BF16).ap()
    WD = nc.dram_tensor("WD_scr", [8, 128, 8, 512], BF16).ap()
    WO = nc.dram_tensor("WO_scr", [2, 128, 8, 1024], BF16).ap()
    W1S = nc.dram_tensor("W1_scr", [128, 32, 128], BF16).ap()
    W2S = nc.dram_tensor("W2_scr", [128, 128], BF16).ap()
    B_WG = [Buf("WG%d" % g) for g in range(4)]
    B_WD = [Buf("WD%d" % h) for h in range(8)]
    B_WO = [Buf("WO%d" % l) for l in range(2)]
    B_W1 = Buf("W1S")
    B_W2 = Buf("W2S")

    def setup_weights():
        stgs["stg"] = [sb("stg%d" % i, [128, STG_N]) for i in range(4)]
        stgs["rr"] = 0
        stgs["queues"] = ("sp",)
        asm = [sb("asm%d" % i, [128, 8, 1024], BF16) for i in range(2)]
        k = 0
        wv3 = nsa_w_in.rearrange("(c p) n -> p c n", p=128)
        if 0 in layers:
            for g in range(4):
                a_ = asm[k % 2]
                k += 1
                pieces = [(0, 256 * g, 256), (256, 1024 + 64 * g, 64), (320, 1280 + 64 * g, 64), (384, 1536 + 64 * g, 64),
                          (448, 2048 + 64 * g, 64), (512, 1792 + 64 * g, 64), (576, 2304 + 64 * g, 64), (640, 2560 + 12 * g, 12),
                          (652, 2608 + 256 * g, 256)]
                for pi, (d0, s0, n) in enumerate(pieces):
                    LOADW(a_[:, :, d0:d0 + n], wv3[:, :, s0:s0 + n], a_.b, join=(pi > 0))
                DMA("act", WG[g], a_[:, :, 0:908], [a_.b], [B_WG[g]])
            a_ = asm[k % 2]
            k += 1
            for lh in range(2):
                ls = slice(16 * lh, 16 * lh + 16)
                LOADW(a_[0:64, :, :].rearrange("p c n -> p (c n)")[:, 0:4096].rearrange("p (l h) -> p l h", l=32)[:, ls, :],
                      w1_k.rearrange("(l d) h -> d l h", d=64)[:, ls, :], a_.b, join=(lh > 0))
                LOADW(a_[64:128, :, :].rearrange("p c n -> p (c n)")[:, 0:4096].rearrange("p (l h) -> p l h", l=32)[:, ls, :],
                      w1_v.rearrange("(l d) h -> d l h", d=64)[:, ls, :], a_.b, join=True)
            LOADW(a_[:, 4, 0:64], w2_k[:, :], a_.b, join=True)
            LOADW(a_[:, 4, 64:128], w2_v[:, :], a_.b, join=True)
            DMA("act", W1S, a_[:, :, :].rearrange("p c n -> p (c n)")[:, 0:4096].rearrange("p (l h) -> p l h", l=32), [a_.b], [B_W1])
            DMA("act", W2S, a_[:, 4, 0:128], [a_.b], [B_W2])
        for l, w_ in enumerate((nsa_w_out,)):
            if l not in layers:
                continue
            a_ = asm[k % 2]
            k += 1
            wo3 = w_.rearrange("(c p) n -> p c n", p=128)
            for q4 in range(4):
                LOADW(a_[:, :, 256 * q4:256 * q4 + 256], wo3[:, :, 256 * q4:256 * q4 + 256], a_.b, join=(q4 > 0))
            DMA("act", WO[l], a_[:, :, :], [a_.b], [B_WO[l]])

    stat_rr = [0]

    def get_stat():
        stat_rr[0] = (stat_rr[0] + 1) % 4
        return stat[stat_rr[0]]

    def phase_norm_T(src_ap_fn, src_bufs, g_tile):
        for i in range(NT):
            xb_ = xt[i % 2]
            u_ = ub[i % 2]
            DMA("sp", xb_[:, :], src_ap_fn(i), src_bufs(i), [xb_.b])
            st = get_stat()
            ACT(u_[:, :], xb_[:, :], AF.Square, [xb_.b], [u_.b, st.b], accum_out=st[:, 0:1])
            V("dve", "tensor_scalar", [st.b], [st.b], out=st[:, 1:2], in0=st[:, 0:1], scalar1=1.0 / D, scalar2=EPS, op0=ALU.mult, op1=ALU.add)
            ACT(st[:, 2:3], st[:, 1:2], AF.Sqrt, [st.b], [st.b])
            V("dve", "reciprocal", [st.b], [st.b], out=st[:, 3:4], in_=st[:, 2:3])
            V("dve", "scalar_tensor_tensor", [xb_.b, st.b, g_tile.b], [u_.b], out=u_[:, :], in0=xb_[:, :], scalar=st[:, 3:4], in1=g_tile[:, :], op0=ALU.mult, op1=ALU.mult)
            bk = banks[6 + (i % 2)]
            bkv = bk[:, :].bitcast(BF16)
            for c in range(8):
                TR(bkv[:, c * 128:(c + 1) * 128], u_[:, c * 128:(c + 1) * 128], [u_.b], [bk.b])
            EVAC(uT[:, :, i * 128:(i + 1) * 128], bkv.rearrange("p (c t) -> p c t", c=8), [bk.b], [uT.b])

    pf_rr = [0]

    def proj_fm(wt, wb, outs, scale=None, bank_ids=(5, 6, 7), M=128):
        for Q in range(NQ):
            bk = banks[bank_ids[pf_rr[0] % len(bank_ids)]]
            pf_rr[0] += 1
            for c in range(8):
                MM(bk[0:M, :], wt[:, c, :], uT[:, c, Q * 512:(Q + 1) * 512], c == 0, c == 7, [wb, uT.b], [bk.b])
            for (rs_, fn_, ob_) in outs:
                EVAC(fn_(Q), bk[rs_, :], [bk.b], [ob_], scale=scale)

    def phase_out(wbig, w_out_d, gp, res_fn, res_bufs, dst_fn, dst_bufs):
        if w_out_d is not None:
            DMA("sp", wbig[:, :, :], WO[w_out_d], [B_WO[w_out_d]], [wbig.b])
        for i in range(NT):
            xb_ = xt[i % 2]
            DMA("sp", xb_[:, :], res_fn(i), res_bufs(i), [xb_.b])
            bk = [banks[2 + 2 * (i % 3)], banks[3 + 2 * (i % 3)]]
            for half in range(2):
                for c in range(8):
                    MM(bk[half][:, :], oT[:, c, i * 128:(i + 1) * 128], wbig[:, c, half * 512:(half + 1) * 512], c == 0, c == 7, [oT.b, wbig.b], [bk[half].b])
            st = get_stat()
            ACT(ub[0][:, 0:512], bk[0][:, :], AF.Square, [bk[0].b], [ub[0].b, st.b], accum_out=st[:, 0:1])
            ACT(ub[0][:, 512:1024], bk[1][:, :], AF.Square, [bk[1].b], [ub[0].b, st.b], accum_out=st[:, 1:2])
            V("dve", "tensor_tensor", [st.b], [st.b], out=st[:, 2:3], in0=st[:, 0:1], in1=st[:, 1:2], op=ALU.add)
            V("dve", "tensor_scalar", [st.b], [st.b], out=st[:, 3:4], in0=st[:, 2:3], scalar1=1.0 / D, scalar2=EPS, op0=ALU.mult, op1=ALU.add)
            ACT(st[:, 4:5], st[:, 3:4], AF.Sqrt, [st.b], [st.b])
            V("dve", "reciprocal", [st.b], [st.b], out=st[:, 5:6], in_=st[:, 4:5])
            yo = yout[0]
            for half in range(2):
                sl = slice(half * 512, (half + 1) * 512)
                V("dve", "scalar_tensor_tensor", [bk[half].b, st.b, gp.b], [yo.b], out=yo[:, sl], in0=bk[half][:, :], scalar=st[:, 5:6], in1=gp[:, sl], op0=ALU.mult, op1=ALU.mult)
            V("pool", "tensor_tensor", [yo.b, xb_.b], [xb_.b], out=xb_[:, :], in0=yo[:, :], in1=xb_[:, :], op=ALU.add)
            DMA("sp", dst_fn(i), xb_[:, :], [xb_.b], dst_bufs(i))


    class Stream:
        pass

    S_BANKS = [banks[0], banks[1], banks[2]]
    s_rr = [0]
    pt_rr = [0]
    LOOK = 6

    def run_streams(streams, filler=None, every=3):
        pending = []
        ntile = [0]

        def emit_pv(item):
            st_, kj, c0, c1, ptb = item
            for r in range(c0, c1):
                qi = 4 * st_.Q + r
                first = max(0, qi - 4) if st_.band else 0
                o_ap, o_tt = st_.O[r]
                if not hasattr(st_, "started"):
                    st_.started = set()
                is_first = id(o_tt) not in st_.started
                st_.started.add(id(o_tt))
                MM(o_ap, ptb[:, r * 128:(r + 1) * 128], st_.v_ap(kj), is_first, kj == qi,
                   [ptb.b] + st_.v_bufs, [o_tt.b], skip=True)
            if kj == st_.last_kj:
                st_.finalize(st_)

        for st_ in streams:
            Q = st_.Q
            kj_lo = max(0, 4 * Q - 4) if st_.band else 0
            st_.last_kj = 4 * Q + 3
            for kj in range(kj_lo, 4 * Q + 4):
                rd0 = kj - 4 * Q
                c0 = max(0, rd0)
                c1 = min(4, rd0 + 5) if st_.band else 4
                sbk = S_BANKS[s_rr[0]]
                s_rr[0] = (s_rr[0] + 1) % len(S_BANKS)
                ptb = PT[pt_rr[0]]
                pt_rr[0] = (pt_rr[0] + 1) % len(PT)
                cols = slice(c0 * 128, c1 * 128)
                gcols = slice(Q * 512 + c0 * 128, Q * 512 + c1 * 128)
                MM(sbk[:, cols], st_.k_ap(kj), st_.q_ap(gcols), True, st_.pen is None, st_.qk_bufs, [sbk.b])
                if st_.pen is not None:
                    MM(sbk[:, cols], cst["Xs"][0:32, kj * 128:(kj + 1) * 128], st_.pen[0:32, gcols], False, True,
                       [cst["Xs"].b, st_.pen_buf], [sbk.b])
                ACT(ptb[:, cols], sbk[:, cols], AF.Exp, [sbk.b], [ptb.b])
                if -1 <= rd0 <= 3:
                    lo = max(rd0, 0)
                    hi = min(rd0 + 2, 4)
                    eo = (lo - rd0) * 128
                    V("dve", "tensor_tensor", [ptb.b, Ee.b], [ptb.b], out=ptb[:, lo * 128:hi * 128], in0=ptb[:, lo * 128:hi * 128],
                      in1=Ee[:, st_.emap, eo:eo + (hi - lo) * 128], op=ALU.mult)
                if st_.band:
                    r4 = rd0 + 4
                    if 0 <= r4 <= 3:
                        V("dve", "tensor_tensor", [ptb.b, M4.b], [ptb.b], out=ptb[:, r4 * 128:(r4 + 1) * 128], in0=ptb[:, r4 * 128:(r4 + 1) * 128],
                          in1=M4[:, :], op=ALU.mult)
                pending.append((st_, kj, c0, c1, ptb))
                if len(pending) > LOOK:
                    emit_pv(pending.pop(0))
                ntile[0] += 1
                if filler is not None and ntile[0] % every == 0:
                    next(filler, None)
        while pending:
            emit_pv(pending.pop(0))
        if filler is not None:
            for _ in filler:
                pass

    def nsa_setup():
        t = {}
        load_norm_gains(0)
        t["keep"] = sb("keep", [128, NT, 32])
        t["addc"] = sb("addc", [128, NT, 32])
        DMA("sp", t["keep"][:, :, :], c_keep[:, :, :], [NOB], [t["keep"].b])
        DMA("sp", t["addc"][:, :, :], c_addc[:, :, :], [NOB], [t["addc"].b])
        t["w1"] = sb("w1", [128, 32, 128], BF16)
        DMA("sp", t["w1"][:, :, :], W1S, [B_W1], [t["w1"].b])
        t["w2"] = sb("w2", [128, 128], BF16)
        DMA("sp", t["w2"][:, :], W2S, [B_W2], [t["w2"].b])
        t["w2k"] = sb("w2k", [128, 128], BF16)
        V("pool", "memset", [], [t["w2k"].b], t["w2k"][:, :], 0.0)
        V("pool", "tensor_copy", [t["w2"].b, t["w2k"].b], [t["w2k"].b], out=t["w2k"][:, 0:64], in_=t["w2"][:, 0:64])
        pes = sb("pes", [32, 128])
        DMA("sp", pes[:, 0:64], pe_k[:, :], [NOB], [pes.b])
        DMA("sp", pes[:, 64:128], pe_v[:, :], [NOB], [pes.b], join=True)
        pesb = sb("pesb", [32, 128], BF16)
        V("dve", "tensor_copy", [pes.b], [pesb.b], out=pesb[:, :], in_=pes[:, :])
        peT = sb("peT", [128, 32], BF16)
        bkv = banks[3][:, :].bitcast(BF16)
        p.add("pe", lambda e: e.transpose(out=bkv[:, 0:32], in_=pesb[:, :], identity=ident[0:32, 0:32]), reads=[pesb.b, ident.b], writes=[banks[3].b])
        V("dve", "tensor_copy", [banks[3].b], [peT.b], out=peT[:, :], in_=bkv[:, 0:32])
        t["peh"] = sb("peh", [128, 2])
        for kv in range(2):
            ps_ = slice(64 * kv, 64 * kv + 64)
            for l in range(32):
                MM(banks[4 + kv][:, 0:1], t["w1"][ps_, l, :], peT[ps_, l:l + 1], l == 0, l == 31, [t["w1"].b, peT.b], [banks[4 + kv].b])
        for kv in range(2):
            V("dve", "tensor_copy", [banks[4 + kv].b], [t["peh"].b], out=t["peh"][:, kv:kv + 1], in_=banks[4 + kv][:, 0:1])
        t["Vca"] = sb("Vca", [127, 97], BF16)
        V("pool", "memset", [], [t["Vca"].b], t["Vca"][:, :], 1.0)
        DMA("sp", t["Vca"][:, 65:97], c_ovl[:, :], [t["Vca"].b], [t["Vca"].b])
        t["wgA"] = sb("wgA", [128, 8, GA], BF16)
        t["wgZ"] = sb("wgZ", [128, 8, 256], BF16)
        t["qa"] = [sb("qa%d" % i, [128, S], BF16) for i in range(4)]
        for i in range(4):
            V("pool", "memset", [], [t["qa"][i].b], t["qa"][i][:, :], 0.0)
        t["cT"] = sb("cT", [128, S], BF16)
        t["ksx"] = sb("ksx", [128, S], BF16)
        V("pool", "memset", [], [t["ksx"].b], t["ksx"][:, :], 0.0)
        DMA("sp", t["ksx"][64:96, :], c_X[:, :], [t["ksx"].b], [t["ksx"].b])
        t["kwz"] = sb("kwz", [128, S], BF16)
        V("pool", "memset", [], [t["kwz"].b], t["kwz"][:, :], 0.0)
        t["vsa"] = sb("vsa", [128, NT, 65], BF16)
        t["vwa"] = sb("vwa", [128, NT, 65], BF16)
        V("pool", "memset", [], [t["vsa"].b], t["vsa"][:, :, :], 1.0)
        V("pool", "memset", [], [t["vwa"].b], t["vwa"][:, :, :], 1.0)
        t["gate"] = sb("gate", [128, NT, 12])
        accraw = sb("acc", [128, max(NT * 256, 4096)])
        t["acc"] = TT(accraw[:, 0:NT * 256].rearrange("p (i c) -> p i c", i=NT), "acc")
        t["acc"].b = accraw.b
        t["wbig"] = TT(accraw[:, 0:4096].bitcast(BF16).rearrange("p (c n) -> p c n", c=8), "wbig")
        t["wbig"].b = accraw.b
        t["ha"] = [sb("ha%d" % i, [128, 128], BF16) for i in range(2)]
        t["kcmp"] = sb("kcmp", [128, 128], BF16)
        t["Ec"] = [sb("Ec%d" % i, [127, 512], BF16) for i in range(3)]
        t["cm"] = sb("cm", [128, 4, 4, 97])
        t["sm"] = [sb("sm0", [128, 32]), sb("sm1", [128, 512]), sb("sm2", [128, 288])]
        t["penb"] = sb("penb", [128, 4, 96], BF16)
        V("pool", "memset", [], [t["penb"].b], t["penb"][:, :, :], 0.0)
        t["tmp"] = [sb("tmpo%d" % i, [128, 4, 64]) for i in range(2)]
        t["th"] = [sb("th%d" % i, [128, 256], BF16) for i in range(2)]
        t["zsg"] = sb("zsg", [128, NT, 256], BF16)
        t["og"] = [sb("og%d" % i, [128, 256], BF16) for i in range(2)]
        DMA("sp", t["wgA"][:, :, :], WG[0][:, :, 0:GA], [B_WG[0]], [t["wgA"].b])
        DMA("sp", t["wgZ"][:, :, :], WG[0][:, :, GA:908], [B_WG[0]], [t["wgZ"].b])
        return t

    def nsa_layer(t, s):
        phase_norm_T(lambda i: x_in[s, i * 128:(i + 1) * 128, :], lambda i: [NOB], cst["gpre"])
        wgA, wgZ = t["wgA"], t["wgZ"]
        for g in range(4):
            g_next = (g + 1) % 4
            has_next = (g < 3) or (s + 1 < nseq)
            for j in range(4):
                qa_ = t["qa"][j]
                proj_fm(wgA[:, :, 64 * j:64 * j + 64], wgA.b, [(slice(0, 64), (lambda Q, qa_=qa_: qa_[0:64, Q * 512:(Q + 1) * 512]), qa_.b)], scale=0.125, M=64)
            proj_fm(wgA[:, :, 256:384], wgA.b, [(slice(0, 128), (lambda Q: t["cT"][:, Q * 512:(Q + 1) * 512]), t["cT"].b)])
            proj_fm(wgA[:, :, 384:448], wgA.b, [(slice(0, 64), (lambda Q: t["ksx"][0:64, Q * 512:(Q + 1) * 512]), t["ksx"].b)], M=64)
            proj_fm(wgA[:, :, 448:512], wgA.b, [(slice(0, 64), (lambda Q: t["kwz"][0:64, Q * 512:(Q + 1) * 512]), t["kwz"].b)], M=64)
            for i in range(NT):
                bk = banks[3 + (i % 2)]
                for c in range(8):
                    MM(bk[:, 0:140], uT[:, c, i * 128:(i + 1) * 128], wgA[:, c, 512:652], c == 0, c == 7, [uT.b, wgA.b], [bk.b])
                V("dve", "tensor_copy", [bk.b], [t["vsa"].b], out=t["vsa"][:, i, 0:64], in_=bk[:, 0:64])
                V("dve", "tensor_copy", [bk.b], [t["vwa"].b], out=t["vwa"][:, i, 0:64], in_=bk[:, 64:128])
                ACT(t["gate"][:, i, :], bk[:, 128:140], AF.Sigmoid, [bk.b], [t["gate"].b])
            if has_next:
                DMA("sp", wgA[:, :, :], WG[g_next][:, :, 0:GA], [B_WG[g_next]], [wgA.b])
            dbg("qT0", t["qa"][0], t["qa"][0][:, :], BF16)
            dbg("cT", t["cT"], t["cT"][:, :], BF16)
            dbg("vsa", t["vsa"], t["vsa"][:, :, :], BF16)
            dbg("gate", t["gate"], t["gate"][:, :, :])
            for kv in range(2):
                ps_ = slice(64 * kv, 64 * kv + 64)
                bk = banks[3 + kv]
                for l in range(32):
                    MM(bk[:, 0:NCMP], t["w1"][ps_, l, :], t["cT"][ps_, l:l + 16 * (NCMP - 1) + 1:16], l == 0, l == 31, [t["w1"].b, t["cT"].b], [bk.b])
                ACT(t["ha"][kv][:, 0:NCMP], bk[:, 0:NCMP], AF.Silu, [bk.b, t["peh"].b], [t["ha"][kv].b], bias=t["peh"][:, kv:kv + 1])
            MM(banks[5][:, 0:NCMP], t["w2k"][:, :], t["ha"][0][:, 0:NCMP], True, True, [t["w2k"].b, t["ha"][0].b], [banks[5].b])
            V("dve", "tensor_copy", [banks[5].b], [t["kcmp"].b], out=t["kcmp"][:, 0:NCMP], in_=banks[5][:, 0:NCMP])
            MM(banks[6][0:NCMP, 0:64], t["ha"][1][:, 0:NCMP], t["w2"][:, 64:128], True, True, [t["w2"].b, t["ha"][1].b], [banks[6].b])
            V("dve", "tensor_copy", [banks[6].b], [t["Vca"].b], out=t["Vca"][0:NCMP, 0:64], in_=banks[6][0:NCMP, 0:64])
            dbg("kcmp", t["kcmp"], t["kcmp"][:, 0:NCMP], BF16)
            dbg("Vca", t["Vca"], t["Vca"][0:NCMP, :], BF16)
            def zproj_gen():
                for i in range(NT):
                    bk = banks[6 + (i % 2)]
                    for c in range(8):
                        MM(bk[:, 0:256], uT[:, c, i * 128:(i + 1) * 128], wgZ[:, c, :], c == 0, c == 7, [uT.b, wgZ.b], [bk.b])
                    th = t["th"][i % 2]
                    ACT(th[:, :], bk[:, 0:256], AF.Tanh, [bk.b], [th.b], scale=0.5)
                    V("dve", "scalar_tensor_tensor", [th.b, bk.b], [t["zsg"].b], out=t["zsg"][:, i, :], in0=th[:, :], scalar=1.0, in1=bk[:, 0:256],
                      op0=ALU.add, op1=ALU.mult)
                    yield
                if has_next:
                    DMA("sp", wgZ[:, :, :], WG[g_next][:, :, GA:908], [B_WG[g_next]], [wgZ.b])
                yield
            zgen = zproj_gen()
            ec_rr = 0
            for Q in range(NQ):
                for j in range(4):
                    next(zgen, None)
                    h = 4 * g + j
                    qa_ = t["qa"][j]
                    ec = t["Ec"][ec_rr % 3]
                    ec_rr += 1
                    p0, p1 = Ec_rows(Q)
                    DMA("sp", ec[p0:p1, :], Ec_src(h, Q), [B_Dc], [ec.b])
                    sbk = S_BANKS[s_rr[0]]
                    s_rr[0] = (s_rr[0] + 1) % 3
                    ptb = PT[pt_rr[0]]
                    pt_rr[0] = (pt_rr[0] + 1) % len(PT)
                    MM(sbk[0:p1, :], t["kcmp"][:, 0:p1], qa_[:, Q * 512:(Q + 1) * 512], True, True, [t["kcmp"].b, qa_.b], [sbk.b])
                    ACT(ptb[0:p1, :], sbk[0:p1, :], AF.Exp, [sbk.b], [ptb.b])
                    segs = [(p0, p1)] if p0 != 32 else [(32, min(64, p1))] + ([(64, p1)] if p1 > 64 else [])
                    for (a0, a1) in segs:
                        V("dve", "tensor_tensor", [ptb.b, ec.b], [ptb.b], out=ptb[a0:a1, :], in0=ptb[a0:a1, :], in1=ec[a0:a1, :], op=ALU.mult)
                    ob = banks[3 + (j % 2)]
                    for r in range(4):
                        MM(ob[:, r * 97:(r + 1) * 97], ptb[0:p1, r * 128:(r + 1) * 128], t["Vca"][0:p1, :], True, True, [ptb.b, t["Vca"].b], [ob.b])
                    ACT(t["cm"][:, :, j, :], ob[:, 0:388].rearrange("p (r c) -> p r c", r=4), AF.Copy, [ob.b], [t["cm"].b])
                cm = t["cm"]
                sm0, sm1, sm2 = t["sm"]
                rinv = sm0[:, 0:16].rearrange("p (r j) -> p r j", r=4)
                coef = sm0[:, 16:32].rearrange("p (r j) -> p r j", r=4)
                V("dve", "tensor_scalar", [cm.b], [sm0.b], out=rinv, in0=cm[:, :, :, 64], scalar1=1e-30, scalar2=None, op0=ALU.add)
                V("dve", "reciprocal", [sm0.b], [sm0.b], out=rinv, in_=rinv)
                gv = t["gate"][:, 4 * Q:4 * Q + 4, :].rearrange("p r (j b) -> p r j b", b=3)
                V("dve", "tensor_tensor", [sm0.b, t["gate"].b], [sm0.b], out=coef, in0=rinv, in1=gv[:, :, :, 0], op=ALU.mult)
                accv = t["acc"][:, 4 * Q:4 * Q + 4, :].rearrange("p r (j d) -> p r j d", j=4)
                V("pool", "tensor_tensor", [cm.b, sm0.b], [t["acc"].b], out=accv, in0=cm[:, :, :, 0:64],
                  in1=coef.unsqueeze(3).broadcast_to([128, 4, 4, 64]), op=ALU.mult)
                next(zgen, None)
                impw = sm1[:, :].rearrange("p (r j n) -> p r j n", r=4, j=4)
                V("dve", "tensor_tensor", [cm.b, sm0.b], [sm1.b], out=impw, in0=cm[:, :, :, 65:97],
                  in1=rinv.unsqueeze(3).broadcast_to([128, 4, 4, 32]), op=ALU.mult)
                imp = sm2[:, 0:128].rearrange("p (r n) -> p r n", r=4)
                V("dve", "tensor_reduce", [sm1.b], [sm2.b], out=imp, in_=sm1[:, :].rearrange("p (r j n) -> p r n j", r=4, j=4), axis=AX.X, op=ALU.add)
                V("dve", "tensor_tensor", [sm2.b, t["keep"].b], [sm2.b], out=imp, in0=imp, in1=t["keep"][:, 4 * Q:4 * Q + 4, :], op=ALU.mult)
                V("dve", "tensor_tensor", [sm2.b, t["addc"].b], [sm2.b], out=imp, in0=imp, in1=t["addc"][:, 4 * Q:4 * Q + 4, :], op=ALU.add)
                next(zgen, None)
                top8 = sm2[:, 128:160].rearrange("p (r e) -> p r e", r=4)
                for r in range(4):
                    V("dve", "max", [sm2.b], [sm2.b], out=top8[:, r, :], in_=imp[:, r, :])
                pen32 = sm2[:, 160:288].rearrange("p (r n) -> p r n", r=4)
                V("dve", "tensor_tensor", [sm2.b], [sm2.b], out=pen32, in0=imp, in1=top8[:, :, 7:8].broadcast_to([128, 4, 32]), op=ALU.is_lt)
                V("dve", "tensor_scalar", [sm2.b], [t["penb"].b], out=t["penb"][:, :, 64:96], in0=pen32, scalar1=PEN, scalar2=None, op0=ALU.mult)
                next(zgen, None)
                bkp = banks[5]
                bkpv = bkp[:, :].bitcast(BF16)
                for r in range(4):
                    TR(bkpv[0:96, r * 128:(r + 1) * 128], t["penb"][:, r, :], [t["penb"].b], [bkp.b])
                cols = slice(Q * 512, (Q + 1) * 512)
                V("dve", "tensor_copy", [bkp.b], [t["qa"][0].b], out=t["qa"][0][64:96, cols], in_=bkpv[64:96, 0:512])
                for j in range(1, 4):
                    V("pool", "tensor_copy", [t["qa"][0].b], [t["qa"][j].b], out=t["qa"][j][64:96, cols], in_=t["qa"][0][64:96, cols])
            for _ in zgen:
                pass
            dbg("cm", t["cm"], t["cm"][:, :, :, :])
            dbg("acc_c", t["acc"], t["acc"][:, :, :])
            streams = []
            o_rr = 0
            for Q in range(NQ):
                for j in range(4):
                    for br in (1, 2):
                        st_ = Stream()
                        st_.Q = Q
                        st_.band = (br == 2)
                        qa_ = t["qa"][j]
                        kt_ = t["ksx"] if br == 1 else t["kwz"]
                        va_ = t["vsa"] if br == 1 else t["vwa"]
                        st_.q_ap = lambda gc, qa_=qa_: qa_[:, gc]
                        st_.k_ap = lambda kj, kt_=kt_: kt_[:, kj * 128:(kj + 1) * 128]
                        st_.qk_bufs = [qa_.b, kt_.b]
                        st_.pen = None
                        st_.pen_buf = None
                        st_.v_ap = lambda kj, va_=va_: va_[:, kj, :]
                        st_.v_bufs = [va_.b]
                        st_.emap = 4 * g + j
                        ob = banks[3 + (o_rr % 2)]
                        o_rr += 1
                        st_.O = [(ob[:, r * 65:(r + 1) * 65], ob) for r in range(4)]
                        st_.ob = ob
                        st_.j = j
                        st_.br = br

                        def fin(st_):
                            ob = st_.ob
                            Q, j, br = st_.Q, st_.j, st_.br
                            ov = ob[:, 0:260].rearrange("p (r c) -> p r c", r=4)
                            sm = get_stat()
                            V("dve", "reciprocal", [ob.b], [sm.b], out=sm[:, 0:4], in_=ov[:, :, 64])
                            gv = t["gate"][:, 4 * Q:4 * Q + 4, :].rearrange("p r (j b) -> p r j b", b=3)
                            V("dve", "tensor_tensor", [sm.b, t["gate"].b], [sm.b], out=sm[:, 4:8], in0=sm[:, 0:4], in1=gv[:, :, j, br], op=ALU.mult)
                            tm = t["tmp"][(2 * j + br) % 2]
                            V("dve", "tensor_tensor", [ob.b, sm.b], [tm.b], out=tm[:, :, :], in0=ov[:, :, 0:64],
                              in1=sm[:, 4:8].unsqueeze(2).broadcast_to([128, 4, 64]), op=ALU.mult)
                            accv = t["acc"][:, 4 * Q:4 * Q + 4, 64 * j:64 * j + 64]
                            V("pool", "tensor_tensor", [tm.b, t["acc"].b], [t["acc"].b], out=accv, in0=accv, in1=tm[:, :, :], op=ALU.add)
                        st_.finalize = fin
                        streams.append(st_)
            run_streams(streams)
            dbg("acc_f", t["acc"], t["acc"][:, :, :])
            for i in range(NT):
                bk = banks[5 + (i % 2)]
                og = t["og"][i % 2]
                V("dve", "scalar_tensor_tensor", [t["zsg"].b, t["acc"].b], [og.b], out=og[:, :], in0=t["acc"][:, i, :], scalar=0.5, in1=t["zsg"][:, i, :],
                  op0=ALU.mult, op1=ALU.mult)
                bkv = bk[:, :].bitcast(BF16)
                for pr in range(2):
                    TR(bkv[:, 512 + pr * 128:512 + (pr + 1) * 128], og[:, pr * 128:(pr + 1) * 128], [og.b], [bk.b])
                EVAC(oT[:, 2 * g:2 * g + 2, i * 128:(i + 1) * 128], bkv[:, 512:768].rearrange("p (c t) -> p c t", c=2), [bk.b], [oT.b])
        dbg("oT", oT, oT[:, :, :], BF16)
        dst, dstb = (x1_d, B_x1[s]) if 1 in layers else (out, B_out[s])
        phase_out(t["wbig"], 0, cst["gpost"], lambda i: x_in[s, i * 128:(i + 1) * 128, :], lambda i: [NOB],
                  lambda i: dst[s, i * 128:(i + 1) * 128, :], lambda i: [dstb[i]])

    def diff_setup():
        t = {}
        load_norm_gains(1)
        stgs["stg"] = [sb("dstg%d" % i, [128, STG_N]) for i in range(1)]
        stgs["rr"] = 0
        stgs["queues"] = ("sp",)
        cast_engs[0] = ("dve",)
        raw = sb("dscr", [128, 4096])
        t["sq"] = TT(raw[:, 0:NT * 128].rearrange("p (i c) -> p i c", i=NT), "sq")
        t["og"] = TT(raw[:, 2048:3072].bitcast(BF16)[:, 0:NT * 128].rearrange("p (i c) -> p i c", i=NT), "og")
        t["wbig"] = TT(raw[:, :].bitcast(BF16).rearrange("p (c n) -> p c n", c=8), "wbig")
        t["sq"].b = t["og"].b = t["wbig"].b = raw.b
        lam4 = sb("lam4", [128, 4, 64])
        for idx, src in enumerate((lq1, lk1, lq2, lk2)):
            DMA("sp", lam4[:, idx, :], src[0:1, :].partition_broadcast(128), [NOB], [lam4.b], join=(idx > 0))
        lm = sb("lm", [128, 8])
        prod = sb("lprod", [128, 2, 64])
        V("dve", "tensor_tensor", [lam4.b], [prod.b], out=prod[:, 0, :], in0=lam4[:, 0, :], in1=lam4[:, 1, :], op=ALU.mult)
        V("dve", "tensor_tensor", [lam4.b], [prod.b], out=prod[:, 1, :], in0=lam4[:, 2, :], in1=lam4[:, 3, :], op=ALU.mult)
        V("dve", "reduce_sum", [prod.b], [lm.b], out=lm[:, 0:2], in_=prod[:, :, :], axis=AX.X)
        ACT(lm[:, 2:4], lm[:, 0:2], AF.Exp, [lm.b], [lm.b])
        V("dve", "tensor_tensor", [lm.b], [lm.b], out=lm[:, 4:5], in0=lm[:, 3:4], in1=lm[:, 2:3], op=ALU.subtract)
        V("dve", "tensor_scalar", [lm.b], [lm.b], out=lm[:, 5:6], in0=lm[:, 4:5], scalar1=-LAMBDA_INIT, scalar2=None, op0=ALU.add)
        t["lm"] = lm
        t["subln"] = sb("sublnb", [128, D])
        DMA("sp", t["subln"][:, :], subln[0:1, :].partition_broadcast(128), [NOB], [t["subln"].b])
        t["w"] = [sb("wd%d" % i, [128, 8, 512], BF16) for i in range(2)]
        t["qT"] = [sb("dqT%d" % i, [128, S], BF16) for i in range(2)]
        t["kz"] = [[sb("dkz%d_%d" % (b_, i), [128, S], BF16) for i in range(2)] for b_ in range(2)]
        for b_ in range(2):
            for i in range(2):
                V("pool", "memset", [], [t["kz"][b_][i].b], t["kz"][b_][i][:, :], 0.0)
        t["va"] = [sb("dva%d" % i, [128, NT, 129], BF16) for i in range(2)]
        for i in range(2):
            V("pool", "memset", [], [t["va"][i].b], t["va"][i][:, :, :], 1.0)
        t["od"] = [sb("od%d" % i, [128, NT, 128]) for i in range(2)]
        t["zs"] = [sb("dzs%d" % i, [128, NT, 128], BF16) for i in range(2)]
        t["tmp"] = [sb("dtmp%d" % i, [128, 2, 128]) for i in range(2)]
        t["rs"] = sb("drs", [128, 4, NT])
        t["th"] = [sb("dth0", [128, 512], BF16)] * 2
        return t

    def diff_layer(t, s):
        phase_norm_T(lambda i: x1_d[s, i * 128:(i + 1) * 128, :], lambda i: [B_x1[s][i]], cst["gpre"])
        dv3 = diff_w_in.rearrange("(c p) n -> p c n", p=128)
        lm = t["lm"]

        def load_w(h):
            w = t["w"][h % 2]
            for part in range(4):
                LOADW(w[:, :, 128 * part:128 * part + 128], dv3[:, :, 1024 * part + 128 * h:1024 * part + 128 * h + 128], w.b, join=(part > 0))

        def proj_gen(h, part="all"):
            hb = h % 2
            w = t["w"][hb]
            qT, kz, va, zs = t["qT"][hb], t["kz"][hb], t["va"][hb], t["zs"][hb]
            for Q in range(NQ if part in ("all", "qkv") else 0):
                bk = banks[7]
                for c in range(8):
                    MM(bk[:, :], w[:, c, 0:128], uT[:, c, Q * 512:(Q + 1) * 512], c == 0, c == 7, [w.b, uT.b], [bk.b])
                EVAC(qT[:, Q * 512:(Q + 1) * 512], bk[:, :], [bk.b], [qT.b], scale=0.125, eng="act")
                yield
            for Q in range(NQ if part in ("all", "qkv") else 0):
                bk = banks[7]
                for c in range(8):
                    MM(bk[:, :], w[:, c, 128:256], uT[:, c, Q * 512:(Q + 1) * 512], c == 0, c == 7, [w.b, uT.b], [bk.b])
                EVAC(kz[0][0:64, Q * 512:(Q + 1) * 512], bk[0:64, :], [bk.b], [kz[0].b], eng="act")
                EVAC(kz[1][64:128, Q * 512:(Q + 1) * 512], bk[64:128, :], [bk.b], [kz[1].b], eng="act")
                yield
            for i4 in range(NT // 4 if part in ("all", "qkv") else 0):
                bk = banks[7]
                for r in range(4):
                    i = 4 * i4 + r
                    for c in range(8):
                        MM(bk[:, r * 128:(r + 1) * 128], uT[:, c, i * 128:(i + 1) * 128], w[:, c, 256:384], c == 0, c == 7, [uT.b, w.b], [bk.b])
                V("dve", "tensor_copy", [bk.b], [va.b], out=va[:, 4 * i4:4 * i4 + 4, 0:128], in_=bk[:, :].rearrange("p (r c) -> p r c", r=4))
                yield
            for i4 in range(NT // 4 if part in ("all", "z") else 0):
                bk = banks[7]
                for r in range(4):
                    i = 4 * i4 + r
                    for c in range(8):
                        MM(bk[:, r * 128:(r + 1) * 128], uT[:, c, i * 128:(i + 1) * 128], w[:, c, 384:512], c == 0, c == 7, [uT.b, w.b], [bk.b])
                th = t["th"][i4 % 2]
                ACT(th[:, :], bk[:, :], AF.Tanh, [bk.b], [th.b], scale=0.5)
                V("dve", "scalar_tensor_tensor", [th.b, bk.b], [zs.b], out=zs[:, 4 * i4:4 * i4 + 4, :], in0=th[:, :].rearrange("p (r c) -> p r c", r=4),
                  scalar=1.0, in1=bk[:, :].rearrange("p (r c) -> p r c", r=4), op0=ALU.add, op1=ALU.mult)
                yield
            if h + 2 < 8 and part in ("all", "z"):
                load_w(h + 2)
            yield

        def tail_gen(h):
            hb = h % 2
            od, zs, sq, rs = t["od"][hb], t["zs"][hb], t["sq"], t["rs"]
            for i in range(NT):
                ACT(sq[:, i, :], od[:, i, :], AF.Square, [od.b], [sq.b, rs.b], accum_out=rs[:, 0, i:i + 1])
                if i % 4 == 3:
                    yield
            V("dve", "tensor_scalar", [rs.b], [rs.b], out=rs[:, 1, :], in0=rs[:, 0, :], scalar1=1.0 / 128, scalar2=EPS, op0=ALU.mult, op1=ALU.add)
            ACT(rs[:, 2, :], rs[:, 1, :], AF.Sqrt, [rs.b], [rs.b])
            V("dve", "reciprocal", [rs.b], [rs.b], out=rs[:, 3, :], in_=rs[:, 2, :])
            yield
            V("dve", "tensor_tensor", [od.b, rs.b], [sq.b], out=sq[:, :, :], in0=od[:, :, :], in1=rs[:, 3, :].unsqueeze(2).broadcast_to([128, NT, 128]), op=ALU.mult)
            yield
            V("dve", "scalar_tensor_tensor", [sq.b, t["subln"].b], [sq.b], out=sq[:, :, :], in0=sq[:, :, :], scalar=0.5 * (1.0 - LAMBDA_INIT),
              in1=t["subln"][:, 128 * h:128 * h + 128].unsqueeze(1).broadcast_to([128, NT, 128]), op0=ALU.mult, op1=ALU.mult)
            yield
            V("dve", "tensor_tensor", [sq.b, zs.b], [t["og"].b], out=t["og"][:, :, :], in0=sq[:, :, :], in1=zs[:, :, :], op=ALU.mult)
            yield
            for i4 in range(NT // 4):
                bk = banks[7]
                bkv = bk[:, :].bitcast(BF16)
                for r in range(4):
                    TR(bkv[:, r * 128:(r + 1) * 128], t["og"][:, 4 * i4 + r, :], [t["og"].b], [bk.b])
                EVAC(oT[:, h, i4 * 512:(i4 + 1) * 512], bkv[:, 0:512], [bk.b], [oT.b], eng="act")
                yield

        def chain(*gens):
            for g_ in gens:
                if g_ is not None:
                    for _ in g_:
                        yield

        def head_streams(h):
            hb = h % 2
            qT, kz, va, od = t["qT"][hb], t["kz"][hb], t["va"][hb], t["od"][hb]
            streams = []
            for Q in range(NQ):
                for m in range(2):
                    st_ = Stream()
                    st_.Q = Q
                    st_.band = False
                    kz_ = kz[m]
                    st_.q_ap = lambda gc, qT=qT: qT[:, gc]
                    st_.k_ap = lambda kj, kz_=kz_: kz_[:, kj * 128:(kj + 1) * 128]
                    st_.qk_bufs = [qT.b, kz_.b]
                    st_.pen = None
                    st_.pen_buf = None
                    st_.v_ap = lambda kj, va=va: va[:, kj, :]
                    st_.v_bufs = [va.b]
                    st_.emap = 2 * h + m
                    obs = [banks[3 + 2 * m], banks[4 + 2 * m]]
                    st_.O = [(obs[r // 2][:, (r % 2) * 129:(r % 2) * 129 + 129], obs[r // 2]) for r in range(4)]
                    st_.obs = obs
                    st_.m = m
                    st_.od = od

                    def fin(st_):
                        Q, m, od = st_.Q, st_.m, st_.od
                        for half in range(2):
                            ob = st_.obs[half]
                            ov = ob[:, 0:258].rearrange("p (r c) -> p r c", r=2)
                            i0 = 4 * Q + 2 * half
                            sm = get_stat()
                            V("dve", "reciprocal", [ob.b], [sm.b], out=sm[:, 0:2], in_=ov[:, :, 128])
                            if m == 0:
                                V("dve", "tensor_tensor", [ob.b, sm.b], [od.b], out=od[:, i0:i0 + 2, :], in0=ov[:, :, 0:128],
                                  in1=sm[:, 0:2].unsqueeze(2).broadcast_to([128, 2, 128]), op=ALU.mult)
                            else:
                                V("dve", "tensor_scalar", [sm.b, lm.b], [sm.b], out=sm[:, 2:4], in0=sm[:, 0:2], scalar1=lm[:, 5:6], scalar2=None, op0=ALU.mult)
                                tm = t["tmp"][half]
                                V("dve", "tensor_tensor", [ob.b, sm.b], [tm.b], out=tm[:, :, :], in0=ov[:, :, 0:128],
                                  in1=sm[:, 2:4].unsqueeze(2).broadcast_to([128, 2, 128]), op=ALU.mult)
                                V("pool", "tensor_tensor", [tm.b, od.b], [od.b], out=od[:, i0:i0 + 2, :], in0=od[:, i0:i0 + 2, :],
                                  in1=tm[:, :, :], op=ALU.add)
                    st_.finalize = fin
                    streams.append(st_)
            return streams

        load_w(0)
        load_w(1)
        for _ in proj_gen(0):
            pass
        for h in range(8):
            fill = chain(proj_gen(h + 1, "qkv") if h < 7 else None, tail_gen(h - 1) if h > 0 else None,
                         proj_gen(h + 1, "z") if h < 7 else None)
            run_streams(head_streams(h), filler=fill, every=2)
        for _ in tail_gen(7):
            pass
        cast_engs[0] = ("dve", "act")
        wo3 = diff_w_out.rearrange("(c p) n -> p c n", p=128)
        for q4 in range(4):
            LOADW(t["wbig"][:, :, 256 * q4:256 * q4 + 256], wo3[:, :, 256 * q4:256 * q4 + 256], t["wbig"].b, join=(q4 > 0))
        cast_engs[0] = ("dve",)
        phase_out(t["wbig"], None, cst["gpost"], lambda i: x1_d[s, i * 128:(i + 1) * 128, :], lambda i: [B_x1[s][i]],
                  lambda i: out[s, i * 128:(i + 1) * 128, :], lambda i: [B_out[s][i]])

    with es:
        with ExitStack() as es1:
            cur_es[0] = es1
            setup_bias()
            setup_weights()
            setup_bias_load()
        cur_es[0] = es
        p.barrier()
        if 0 in layers:
            with ExitStack() as es2:
                cur_es[0] = es2
                nt_ = nsa_setup()
                for s in range(nseq):
                    nsa_layer(nt_, s)
            cur_es[0] = es
            p.barrier()
        if 1 in layers:
            with ExitStack() as es3:
                cur_es[0] = es3
                dt_ = diff_setup()
                for s in range(nseq):
                    if 0 not in layers:
                        for i in range(NT):
                            xb_ = xt[i % 2]
                            DMA("sp", xb_[:, :], x_in[s, i * 128:(i + 1) * 128, :], [NOB], [xb_.b])
                            DMA("sp", x1_d[s, i * 128:(i + 1) * 128, :], xb_[:, :], [xb_.b], [B_x1[s][i]])
                    diff_layer(dt_, s)
            cur_es[0] = es
        p.emit()
    return nc


INPUT_NAMES = ["rel_bias_table", "norm_pre", "norm_post", "nsa_w_in", "nsa_cmp_pe_k", "nsa_cmp_w1_k", "nsa_cmp_w2_k",
               "nsa_cmp_pe_v", "nsa_cmp_w1_v", "nsa_cmp_w2_v", "nsa_w_out", "diff_w_in", "diff_lambda_q1", "diff_lambda_k1",
               "diff_lambda_q2", "diff_lambda_k2", "diff_subln", "diff_w_out"]


def make_in_maps(inputs, n_cores, nseq, S):
    consts = host_consts(S)
    shared = {}
    for k in INPUT_NAMES:
        a = np.ascontiguousarray(np.asarray(inputs[k], dtype=np.float32))
        if a.ndim == 3:
            a = a[0]
        elif k.startswith("diff_lambda") or k == "diff_subln":
            a = a.reshape(1, -1)
        shared[k] = np.ascontiguousarray(a)
    shared.update(consts)
    x = np.asarray(inputs["x"], dtype=np.float32)
    maps = []
    for c in range(n_cores):
        m = dict(shared)
        m["x"] = np.ascontiguousarray(x[c * nseq:(c + 1) * nseq])
        maps.append(m)
    return maps


def kernel(**inputs):
    x = np.asarray(inputs["x"])
    B, S, _ = x.shape
    n_cores = 8
    nseq = B // n_cores
    nc = build(nseq, S)
    in_maps = make_in_maps(inputs, n_cores, nseq, S)
    res = run_bass_kernel_spmd(nc, in_maps, core_ids=list(range(n_cores)))
    return np.concatenate([np.asarray(r["out"]) for r in res.results], axis=0).astype(np.float32)
```

```python
import math
from contextlib import ExitStack

import numpy as np
import ml_dtypes

import concourse.bass as bass
import concourse.mybir as mybir
from concourse.bass_utils import run_bass_kernel_spmd

F32 = mybir.dt.float32
BF16 = mybir.dt.bfloat16
AF = mybir.ActivationFunctionType
ALU = mybir.AluOpType
AX = mybir.AxisListType

D = 1024
NSA_IN = 3632
EPS = 1e-6
PEN = -30000.0
LAMBDA_INIT = 0.8 - 0.6 * math.exp(-0.3 * 1)


class Buf:
    __slots__ = ("name", "writers", "readers", "prev")

    def __init__(self, name=""):
        self.name = name
        self.writers = []
        self.readers = []
        self.prev = []


class Op:
    __slots__ = ("eng", "fn", "dma", "deps", "needs_sig", "sig")

    def __init__(self, eng, fn, dma):
        self.eng = eng
        self.fn = fn
        self.dma = dma
        self.deps = []
        self.needs_sig = False
        self.sig = None


ENGS = ("pe", "act", "dve", "pool", "sp")


class Prog:
    def __init__(self, nc, n_dma_sems=48):
        self.nc = nc
        self.ops = {e: [] for e in ENGS}
        self.n_dma_sems = n_dma_sems
        self.dma_last = [None] * n_dma_sems
        self.dma_cnt = [0] * n_dma_sems
        self.dma_rr = 0
        self.pending = {}

    def barrier(self):
        lasts = []
        for e in ENGS:
            for op in reversed(self.ops[e]):
                if not op.dma:
                    op.needs_sig = True
                    lasts.append(op)
                    break
        for j in range(self.n_dma_sems):
            if self.dma_last[j] is not None:
                lasts.append(self.dma_last[j])
        self.pending = {e: list(lasts) for e in ENGS}

    def add(self, eng, fn, reads=(), writes=(), dma=False, join=False):
        op = Op(eng, fn, dma)
        if self.pending.get(eng):
            op.deps.extend(self.pending.pop(eng))
        for b in reads:
            for w in b.writers:
                self._dep(w, op, True)
        for b in writes:
            if not join:
                for w in b.writers:
                    self._dep(w, op, False)
            else:
                for r in b.prev:
                    self._dep(r, op, False)
            for r in b.readers:
                self._dep(r, op, False)
        for b in writes:
            if join:
                b.writers.append(op)
            else:
                b.prev = list(b.writers) + list(b.readers)
                b.writers = [op]
                b.readers = []
        for b in reads:
            b.readers.append(op)
        if dma:
            j = self.dma_rr
            self.dma_rr = (j + 1) % self.n_dma_sems
            prev = self.dma_last[j]
            if prev is not None:
                op.deps.append(prev)
            self.dma_cnt[j] += 1
            op.sig = (("d", j), 16 * self.dma_cnt[j])
            op.needs_sig = True
            self.dma_last[j] = op
        self.ops[eng].append(op)
        return op

    def _dep(self, p, c, raw):
        if p is c:
            return
        if (not p.dma) and (not c.dma) and p.eng == c.eng:
            if p.eng == "pe" or not raw:
                return
        p.needs_sig = True
        c.deps.append(p)

    def emit(self):
        nc = self.nc
        with ExitStack() as es:
            esem = {e: es.enter_context(nc.semaphore("s_" + e)) for e in ENGS}
            dsem = [es.enter_context(nc.semaphore("d%d" % j)) for j in range(self.n_dma_sems)]
            for e in ENGS:
                cnt = 0
                for op in self.ops[e]:
                    if op.dma:
                        continue
                    if op.needs_sig:
                        cnt += 1
                        op.sig = (("e", e), cnt)

            def semof(key):
                return esem[key[1]] if key[0] == "e" else dsem[key[1]]

            block = es.enter_context(nc.Block())
            engobj = {"pe": "tensor", "act": "scalar", "dve": "vector", "pool": "gpsimd", "sp": "sync"}

            def make(e):
                def body(eng):
                    waited = {}
                    for op in self.ops[e]:
                        need = {}
                        for p in op.deps:
                            k, v = p.sig
                            if need.get(k, 0) < v:
                                need[k] = v
                        for k, v in need.items():
                            if waited.get(k, 0) >= v:
                                continue
                            eng.wait_ge(semof(k), v)
                            waited[k] = v
                        ins = op.fn(eng)
                        if op.needs_sig:
                            k, v = op.sig
                            ins.then_inc(semof(k), 16 if op.dma else 1)
                    if e == "sp":
                        for j in range(self.n_dma_sems):
                            if self.dma_cnt[j] > 0:
                                eng.wait_ge(dsem[j], 16 * self.dma_cnt[j])
                return body

            for e in ENGS:
                getattr(block, engobj[e])(make(e))


def _rel_bucket(n):
    n = np.maximum(n, 0)
    nf = np.maximum(n, 1).astype(np.float32)
    large = 16 + (np.log(nf / np.float32(16)) / np.float32(math.log(8.0)) * np.float32(16)).astype(np.int32)
    large = np.minimum(large, 31)
    return np.where(n < 16, n, large)


def host_consts(S):
    NT = S // 128
    ncmp = S // 16 - 1
    bf = ml_dtypes.bfloat16
    c = {}
    c["c_ident"] = np.eye(128, dtype=np.float32).astype(bf)
    b = _rel_bucket(np.arange(128))
    oh = np.zeros((32, 128), np.float32)
    oh[b, np.arange(128)] = 1.0
    oh[31, :] -= 1.0
    c["c_onehot"] = oh
    cmp_lo = np.arange(ncmp) * 16
    sel_lo = np.arange(S // 64) * 64
    ov = np.clip(np.minimum(cmp_lo[:, None] + 32, sel_lo[None, :] + 64)
                 - np.maximum(cmp_lo[:, None], sel_lo[None, :]), 0, None).astype(np.float32) / 32.0
    ovp = np.zeros((127, 32), np.float32)
    ovp[:ncmp, :S // 64] = ov
    c["c_ovl"] = ovp.astype(bf)
    X = np.zeros((32, S), np.float32)
    X[np.arange(S) // 64, np.arange(S)] = 1.0
    c["c_X"] = X.astype(bf)
    k = np.arange(128)[:, None]
    q = np.arange(128)[None, :]
    c["c_M4"] = (q < k).astype(np.float32).astype(bf)
    t = np.arange(S)
    cur = t // 64
    n = np.arange(32)[None, :]
    forced = (n == 0) | (n == cur[:, None]) | (n == cur[:, None] - 1)
    future = n > cur[:, None]
    keep = (~(forced | future)).astype(np.float32)
    addc = np.where(forced, 1e9, np.where(future, -1e9, 0.0)).astype(np.float32)
    c["c_keep"] = np.ascontiguousarray(keep.reshape(NT, 128, 32).transpose(1, 0, 2))
    c["c_addc"] = np.ascontiguousarray(addc.reshape(NT, 128, 32).transpose(1, 0, 2))
    return c


def build(nseq, S, layers=(0, 1)):
    NT = S // 128
    NQ = S // 512
    NCMP = S // 16 - 1
    nc = bass.Bass("TRN2", target_bir_lowering=False)
    p = Prog(nc)

    def din(name, shape, dt=F32):
        return nc.dram_tensor(name, list(shape), dt, kind="ExternalInput").ap()

    x_in = din("x", [nseq, S, D])
    table = din("rel_bias_table", [32, 16])
    norm_pre = din("norm_pre", [2, D])
    norm_post = din("norm_post", [2, D])
    nsa_w_in = din("nsa_w_in", [D, NSA_IN])
    pe_k = din("nsa_cmp_pe_k", [32, 64])
    w1_k = din("nsa_cmp_w1_k", [2048, 128])
    w2_k = din("nsa_cmp_w2_k", [128, 64])
    pe_v = din("nsa_cmp_pe_v", [32, 64])
    w1_v = din("nsa_cmp_w1_v", [2048, 128])
    w2_v = din("nsa_cmp_w2_v", [128, 64])
    nsa_w_out = din("nsa_w_out", [D, D])
    diff_w_in = din("diff_w_in", [D, 4096])
    lq1 = din("diff_lambda_q1", [1, 64])
    lk1 = din("diff_lambda_k1", [1, 64])
    lq2 = din("diff_lambda_q2", [1, 64])
    lk2 = din("diff_lambda_k2", [1, 64])
    subln = din("diff_subln", [1, D])
    diff_w_out = din("diff_w_out", [D, D])
    c_ident = din("c_ident", [128, 128], BF16)
    c_onehot = din("c_onehot", [32, 128])
    c_ovl = din("c_ovl", [127, 32], BF16)
    c_X = din("c_X", [32, S], BF16)
    c_M4 = din("c_M4", [128, 128], BF16)
    c_keep = din("c_keep", [128, NT, 32])
    c_addc = din("c_addc", [128, NT, 32])
    out = nc.dram_tensor("out", [nseq, S, D], F32, kind="ExternalOutput").ap()
    x1_d = nc.dram_tensor("x1_scr", [nseq, S, D], F32).ap()
    De = nc.dram_tensor("De_scr", [16, 128, 512], BF16).ap()
    Dc = nc.dram_tensor("Dc_scr", [16, 64, 2048], BF16).ap()
    B_x1 = [[Buf("x1d%d_%d" % (s, i)) for i in range(NT)] for s in range(nseq)]
    B_out = [[Buf("out%d_%d" % (s, i)) for i in range(NT)] for s in range(nseq)]
    B_De = Buf("De")
    B_Dc = Buf("Dc")
    NOB = Buf("const_in")

    import os as _os
    es = ExitStack()
    cur_es = [es]
    DEBUG = bool(_os.environ.get("KDEBUG"))
    dbg_seen = set()

    def dbg(name, tt, ap, dt=F32):
        if not DEBUG or name in dbg_seen:
            return
        dbg_seen.add(name)
        o = nc.dram_tensor("dbg_" + name, list(ap.shape), dt, kind="ExternalOutput").ap()
        p.add("sp", lambda e: e.dma_start(out=o, in_=ap), reads=[tt.b], writes=[Buf()], dma=True)

    class TT:
        def __init__(self, t, name):
            self.t = t
            self.b = Buf(name)

        def __getitem__(self, k):
            return self.t[k]

    def sb(name, shape, dt=F32):
        if _os.environ.get("KDEBUG_SB"):
            print("SB", name, shape, "remaining", nc.sbuf_bytes_remaining)
        return TT(cur_es[0].enter_context(nc.sbuf_tensor(name, list(shape), dt)), name)

    banks = [TT(es.enter_context(nc.psum_tensor("bank%d" % i, [128, 512], F32)), "bank%d" % i) for i in range(8)]

    def DMA(eng, out_ap, in_ap, reads, writes, join=False, **kw):
        p.add(eng, lambda e: e.dma_start(out=out_ap, in_=in_ap, **kw), reads=reads, writes=writes, dma=True, join=join)

    def MM(out_ap, lhsT, rhs, start, stop, reads, writes, skip=False):
        if skip:
            p.add("pe", lambda e: e.matmul(out_ap, lhsT=lhsT, rhs=rhs, start=start, stop=stop, skip_group_check=True), reads=reads, writes=writes)
        else:
            p.add("pe", lambda e: e.matmul(out_ap, lhsT=lhsT, rhs=rhs, start=start, stop=stop), reads=reads, writes=writes)

    def TR(out_ap, in_ap, reads, writes):
        p.add("pe", lambda e: e.transpose(out=out_ap, in_=in_ap, identity=ident[:, :]), reads=list(reads) + [ident.b], writes=writes)

    def ACT(out_ap, in_ap, func, reads, writes, **kw):
        p.add("act", lambda e: e.activation(out=out_ap, in_=in_ap, func=func, **kw), reads=reads, writes=writes)

    def V(eng, name, reads, writes, *a, **kw):
        p.add(eng, lambda e: getattr(e, name)(*a, **kw), reads=reads, writes=writes)

    evac_rr = [0]

    def EVAC(out_ap, in_ap, reads, writes, scale=None):
        evac_rr[0] ^= 1
        if evac_rr[0]:
            if scale is None:
                ACT(out_ap, in_ap, AF.Copy, reads, writes)
            else:
                ACT(out_ap, in_ap, AF.Copy, reads, writes, scale=float(scale))
        else:
            if scale is None:
                V("dve", "tensor_copy", reads, writes, out=out_ap, in_=in_ap)
            else:
                V("dve", "tensor_scalar", reads, writes, out=out_ap, in0=in_ap, scalar1=float(scale), scalar2=None, op0=ALU.mult)

    ident = sb("ident", [128, 128], BF16)
    DMA("sp", ident[:, :], c_ident[:, :], [NOB], [ident.b])
    M4 = sb("M4", [128, 128], BF16)
    DMA("sp", M4[:, :], c_M4[:, :], [NOB], [M4.b])
    Ee = sb("Ee", [128, 16, 256], BF16)
    uT = sb("uT", [128, 8, S], BF16)
    oT = sb("oT", [128, 8, S], BF16)
    xt = [sb("xt%d" % i, [128, D]) for i in range(2)]
    ub = [sb("ub%d" % i, [128, D], BF16) for i in range(2)]
    stat = [sb("stat%d" % i, [128, 8]) for i in range(4)]
    NPT = 8
    PT = [sb("PT%d" % i, [128, 512], BF16) for i in range(NPT)]
    yout = [sb("yout0", [128, D])]
    cst = {}

    def setup_bias():
        tab = sb("tab", [32, 16])
        oh = sb("oh", [32, 128])
        fse = sb("fse", [16, 512], BF16)
        fsc = sb("fsc", [16, 2048], BF16)
        DMA("sp", tab[:, :], table[:, :], [NOB], [tab.b])
        DMA("sp", oh[:, :], c_onehot[:, :], [NOB], [oh.b])
        MM(banks[0][0:16, 0:128], tab[:, :], oh[:, :], True, True, [tab.b, oh.b], [banks[0].b])
        V("pool", "memset", [], [fse.b], fse[:, :], 0.0)
        V("pool", "memset", [fse.b], [fse.b], fse[:, 128:384], 1.0)
        V("pool", "memset", [], [fsc.b], fsc[:, :], 0.0)
        V("pool", "memset", [fsc.b], [fsc.b], fsc[:, 0:512], 1.0)
        V("pool", "memset", [fsc.b], [fsc.b], fsc[:, 1695:2048], 1.0)
        ACT(fse[:, 0:128], banks[0][0:16, 0:128], AF.Exp, [banks[0].b, fse.b], [fse.b])
        ACT(fsc[:, 1567:1695], banks[0][0:16, 0:128], AF.Exp, [banks[0].b, fsc.b], [fsc.b])
        DMA("act", De, fse[:, :].unsqueeze(1).broadcast_to([16, 128, 512]), [fse.b], [B_De])
        DMA("act", Dc, fsc[:, :].unsqueeze(1).broadcast_to([16, 64, 2048]), [fsc.b], [B_Dc])

    def setup_bias_load():
        while stgs.get("jobs"):
            stgs["jobs"].pop(0)()
        for h in range(16):
            DMA("sp", Ee[:, h, :], bass.AP(De.tensor, h * 128 * 512, [[511, 128], [1, 256]]), [B_De], [Ee.b], join=(h > 0))

    def load_norm_gains(l):
        cst["gpre"] = sb("gpre%d" % l, [128, D])
        cst["gpost"] = sb("gpost%d" % l, [128, D])
        DMA("sp", cst["gpre"][:, :], norm_pre[l:l + 1, :].partition_broadcast(128), [NOB], [cst["gpre"].b])
        DMA("sp", cst["gpost"][:, :], norm_post[l:l + 1, :].partition_broadcast(128), [NOB], [cst["gpost"].b])

    def Ec_rows(Q):
        p0 = max(0, 32 * (Q - 1))
        p1 = min(NCMP, 32 * Q + 32)
        return p0, p1

    def Ec_src(h, Q):
        p0, p1 = Ec_rows(Q)
        i0 = p0 - 32 * Q + 32
        return bass.AP(Dc.tensor, h * 64 * 2048 + i0 * 2032, [[2032, p1 - p0], [1, 512]])

    STG_N = 1024
    stgs = {}
    cast_rr = [0]

    cast_engs = [("dve", "act")]

    def CAST(out_ap, in_ap, reads, writes, join=False):
        e = cast_engs[0][cast_rr[0] % len(cast_engs[0])]
        cast_rr[0] += 1
        if e == "act":
            p.add("act", lambda en: en.activation(out=out_ap, in_=in_ap, func=AF.Copy), reads=reads, writes=writes, join=join)
        else:
            p.add(e, lambda en: en.tensor_copy(out=out_ap, in_=in_ap), reads=reads, writes=writes, join=join)

    def LOADW(dst_ap, src_ap, dst_buf, join=False):
        stg = stgs["stg"]
        shp = list(dst_ap.shape)
        P_ = shp[0]
        mid = 1
        for d_ in shp[1:-1]:
            mid *= d_
        last = shp[-1]
        step = max(1, STG_N // mid)
        bp = dst_ap.base_partition()
        first = True
        for c0 in range(0, last, step):
            c1 = min(last, c0 + step)
            n = mid * (c1 - c0)
            assert n <= STG_N, shp
            st_ = stg[stgs["rr"]]
            stgs["rr"] = (stgs["rr"] + 1) % len(stg)
            sv = st_[bp:bp + P_, 0:n]
            if len(shp) == 3:
                sv = sv.rearrange("p (a b) -> p a b", a=shp[1])
                d_ap, s_ap = dst_ap[:, :, c0:c1], src_ap[:, :, c0:c1]
            else:
                d_ap, s_ap = dst_ap[:, c0:c1], src_ap[:, c0:c1]
            dq = stgs.get("queues", ("sp",))
            DMA(dq[stgs["rr"] % len(dq)], sv, s_ap, [NOB], [st_.b])
            CAST(d_ap, sv, [st_.b], [dst_buf], join=(join or not first))
            first = False
            if stgs.get("jobs"):
                stgs["jobs"].pop(0)()

    GA = 652
    WG = nc.dram_tensor("WG_scr", [4, 128, 8, 908], BF16).ap()
    WD = nc.dram_tensor("WD_scr", [8, 128, 8, 512], BF16).ap()
    WO = nc.dram_tensor("WO_scr", [2, 128, 8, 1024], BF16).ap()
    W1S = nc.dram_tensor("W1_scr", [128, 32, 128], BF16).ap()
    W2S = nc.dram_tensor("W2_scr", [128, 128], BF16).ap()
    B_WG = [Buf("WG%d" % g) for g in range(4)]
    B_WD = [Buf("WD%d" % h) for h in range(8)]
    B_WO = [Buf("WO%d" % l) for l in range(2)]
    B_W1 = Buf("W1S")
    B_W2 = Buf("W2S")

    def setup_weights():
        stgs["stg"] = [sb("stg%d" % i, [128, STG_N]) for i in range(4)]
        stgs["rr"] = 0
        stgs["queues"] = ("sp",)
        asm = [sb("asm%d" % i, [128, 8, 1024], BF16) for i in range(2)]
        k = 0
        wv3 = nsa_w_in.rearrange("(c p) n -> p c n", p=128)
        if 0 in layers:
            for g in range(4):
                a_ = asm[k % 2]
                k += 1
                pieces = [(0, 256 * g, 256), (256, 1024 + 64 * g, 64), (320, 1280 + 64 * g, 64), (384, 1536 + 64 * g, 64),
                          (448, 2048 + 64 * g, 64), (512, 1792 + 64 * g, 64), (576, 2304 + 64 * g, 64), (640, 2560 + 12 * g, 12),
                          (652, 2608 + 256 * g, 256)]
                for pi, (d0, s0, n) in enumerate(pieces):
                    LOADW(a_[:, :, d0:d0 + n], wv3[:, :, s0:s0 + n], a_.b, join=(pi > 0))
                DMA("act", WG[g], a_[:, :, 0:908], [a_.b], [B_WG[g]])
            a_ = asm[k % 2]
            k += 1
            for lh in range(2):
                ls = slice(16 * lh, 16 * lh + 16)
                LOADW(a_[0:64, :, :].rearrange("p c n -> p (c n)")[:, 0:4096].rearrange("p (l h) -> p l h", l=32)[:, ls, :],
                      w1_k.rearrange("(l d) h -> d l h", d=64)[:, ls, :], a_.b, join=(lh > 0))
                LOADW(a_[64:128, :, :].rearrange("p c n -> p (c n)")[:, 0:4096].rearrange("p (l h) -> p l h", l=32)[:, ls, :],
                      w1_v.rearrange("(l d) h -> d l h", d=64)[:, ls, :], a_.b, join=True)
            LOADW(a_[:, 4, 0:64], w2_k[:, :], a_.b, join=True)
            LOADW(a_[:, 4, 64:128], w2_v[:, :], a_.b, join=True)
            DMA("act", W1S, a_[:, :, :].rearrange("p c n -> p (c n)")[:, 0:4096].rearrange("p (l h) -> p l h", l=32), [a_.b], [B_W1])
            DMA("act", W2S, a_[:, 4, 0:128], [a_.b], [B_W2])
        for l, w_ in enumerate((nsa_w_out,)):
            if l not in layers:
                continue
            a_ = asm[k % 2]
            k += 1
            wo3 = w_.rearrange("(c p) n -> p c n", p=128)
            for q4 in range(4):
                LOADW(a_[:, :, 256 * q4:256 * q4 + 256], wo3[:, :, 256 * q4:256 * q4 + 256], a_.b, join=(q4 > 0))
            DMA("act", WO[l], a_[:, :, :], [a_.b], [B_WO[l]])

    stat_rr = [0]

    def get_stat():
        stat_rr[0] = (stat_rr[0] + 1) % 4
        return stat[stat_rr[0]]

    def phase_norm_T(src_ap_fn, src_bufs, g_tile):
        for i in range(NT):
            xb_ = xt[i % 2]
            u_ = ub[i % 2]
            DMA("sp", xb_[:, :], src_ap_fn(i), src_bufs(i), [xb_.b])
            st = get_stat()
            ACT(u_[:, :], xb_[:, :], AF.Square, [xb_.b], [u_.b, st.b], accum_out=st[:, 0:1])
            V("dve", "tensor_scalar", [st.b], [st.b], out=st[:, 1:2], in0=st[:, 0:1], scalar1=1.0 / D, scalar2=EPS, op0=ALU.mult, op1=ALU.add)
            ACT(st[:, 2:3], st[:, 1:2], AF.Sqrt, [st.b], [st.b])
            V("dve", "reciprocal", [st.b], [st.b], out=st[:, 3:4], in_=st[:, 2:3])
            V("dve", "scalar_tensor_tensor", [xb_.b, st.b, g_tile.b], [u_.b], out=u_[:, :], in0=xb_[:, :], scalar=st[:, 3:4], in1=g_tile[:, :], op0=ALU.mult, op1=ALU.mult)
            bk = banks[6 + (i % 2)]
            bkv = bk[:, :].bitcast(BF16)
            for c in range(8):
                TR(bkv[:, c * 128:(c + 1) * 128], u_[:, c * 128:(c + 1) * 128], [u_.b], [bk.b])
            EVAC(uT[:, :, i * 128:(i + 1) * 128], bkv.rearrange("p (c t) -> p c t", c=8), [bk.b], [uT.b])

    pf_rr = [0]

    def proj_fm(wt, wb, outs, scale=None, bank_ids=(5, 6, 7), M=128):
        for Q in range(NQ):
            bk = banks[bank_ids[pf_rr[0] % len(bank_ids)]]
            pf_rr[0] += 1
            for c in range(8):
                MM(bk[0:M, :], wt[:, c, :], uT[:, c, Q * 512:(Q + 1) * 512], c == 0, c == 7, [wb, uT.b], [bk.b])
            for (rs_, fn_, ob_) in outs:
                EVAC(fn_(Q), bk[rs_, :], [bk.b], [ob_], scale=scale)

    def phase_out(wbig, w_out_d, gp, res_fn, res_bufs, dst_fn, dst_bufs):
        if w_out_d is not None:
            DMA("sp", wbig[:, :, :], WO[w_out_d], [B_WO[w_out_d]], [wbig.b])
        for i in range(NT):
            xb_ = xt[i % 2]
            DMA("sp", xb_[:, :], res_fn(i), res_bufs(i), [xb_.b])
            bk = [banks[2 + 2 * (i % 3)], banks[3 + 2 * (i % 3)]]
            for half in range(2):
                for c in range(8):
                    MM(bk[half][:, :], oT[:, c, i * 128:(i + 1) * 128], wbig[:, c, half * 512:(half + 1) * 512], c == 0, c == 7, [oT.b, wbig.b], [bk[half].b])
            st = get_stat()
            ACT(ub[0][:, 0:512], bk[0][:, :], AF.Square, [bk[0].b], [ub[0].b, st.b], accum_out=st[:, 0:1])
            ACT(ub[0][:, 512:1024], bk[1][:, :], AF.Square, [bk[1].b], [ub[0].b, st.b], accum_out=st[:, 1:2])
            V("dve", "tensor_tensor", [st.b], [st.b], out=st[:, 2:3], in0=st[:, 0:1], in1=st[:, 1:2], op=ALU.add)
            V("dve", "tensor_scalar", [st.b], [st.b], out=st[:, 3:4], in0=st[:, 2:3], scalar1=1.0 / D, scalar2=EPS, op0=ALU.mult, op1=ALU.add)
            ACT(st[:, 4:5], st[:, 3:4], AF.Sqrt, [st.b], [st.b])
            V("dve", "reciprocal", [st.b], [st.b], out=st[:, 5:6], in_=st[:, 4:5])
            yo = yout[0]
            for half in range(2):
                sl = slice(half * 512, (half + 1) * 512)
                V("dve", "scalar_tensor_tensor", [bk[half].b, st.b, gp.b], [yo.b], out=yo[:, sl], in0=bk[half][:, :], scalar=st[:, 5:6], in1=gp[:, sl], op0=ALU.mult, op1=ALU.mult)
            V("pool", "tensor_tensor", [yo.b, xb_.b], [xb_.b], out=xb_[:, :], in0=yo[:, :], in1=xb_[:, :], op=ALU.add)
            DMA("sp", dst_fn(i), xb_[:, :], [xb_.b], dst_bufs(i))


    class Stream:
        pass

    S_BANKS = [banks[0], banks[1], banks[2]]
    s_rr = [0]
    pt_rr = [0]
    LOOK = 6

    def run_streams(streams, filler=None, every=3):
        pending = []
        ntile = [0]

        def emit_pv(item):
            st_, kj, c0, c1, ptb = item
            for r in range(c0, c1):
                qi = 4 * st_.Q + r
                first = max(0, qi - 4) if st_.band else 0
                o_ap, o_tt = st_.O[r]
                if not hasattr(st_, "started"):
                    st_.started = set()
                is_first = id(o_tt) not in st_.started
                st_.started.add(id(o_tt))
                MM(o_ap, ptb[:, r * 128:(r + 1) * 128], st_.v_ap(kj), is_first, kj == qi,
                   [ptb.b] + st_.v_bufs, [o_tt.b], skip=True)
            if kj == st_.last_kj:
                st_.finalize(st_)

        for st_ in streams:
            Q = st_.Q
            kj_lo = max(0, 4 * Q - 4) if st_.band else 0
            st_.last_kj = 4 * Q + 3
            for kj in range(kj_lo, 4 * Q + 4):
                rd0 = kj - 4 * Q
                c0 = max(0, rd0)
                c1 = min(4, rd0 + 5) if st_.band else 4
                sbk = S_BANKS[s_rr[0]]
                s_rr[0] = (s_rr[0] + 1) % len(S_BANKS)
                ptb = PT[pt_rr[0]]
                pt_rr[0] = (pt_rr[0] + 1) % len(PT)
                cols = slice(c0 * 128, c1 * 128)
                gcols = slice(Q * 512 + c0 * 128, Q * 512 + c1 * 128)
                MM(sbk[:, cols], st_.k_ap(kj), st_.q_ap(gcols), True, st_.pen is None, st_.qk_bufs, [sbk.b])
                if st_.pen is not None:
                    MM(sbk[:, cols], cst["Xs"][0:32, kj * 128:(kj + 1) * 128], st_.pen[0:32, gcols], False, True,
                       [cst["Xs"].b, st_.pen_buf], [sbk.b])
                ACT(ptb[:, cols], sbk[:, cols], AF.Exp, [sbk.b], [ptb.b])
                if -1 <= rd0 <= 3:
                    lo = max(rd0, 0)
                    hi = min(rd0 + 2, 4)
                    eo = (lo - rd0) * 128
                    V("dve", "tensor_tensor", [ptb.b, Ee.b], [ptb.b], out=ptb[:, lo * 128:hi * 128], in0=ptb[:, lo * 128:hi * 128],
                      in1=Ee[:, st_.emap, eo:eo + (hi - lo) * 128], op=ALU.mult)
                if st_.band:
                    r4 = rd0 + 4
                    if 0 <= r4 <= 3:
                        V("dve", "tensor_tensor", [ptb.b, M4.b], [ptb.b], out=ptb[:, r4 * 128:(r4 + 1) * 128], in0=ptb[:, r4 * 128:(r4 + 1) * 128],
                          in1=M4[:, :], op=ALU.mult)
                pending.append((st_, kj, c0, c1, ptb))
                if len(pending) > LOOK:
                    emit_pv(pending.pop(0))
                ntile[0] += 1
                if filler is not None and ntile[0] % every == 0:
                    next(filler, None)
        while pending:
            emit_pv(pending.pop(0))
        if filler is not None:
            for _ in filler:
                pass

    def nsa_setup():
        t = {}
        load_norm_gains(0)
        t["keep"] = sb("keep", [128, NT, 32])
        t["addc"] = sb("addc", [128, NT, 32])
        DMA("sp", t["keep"][:, :, :], c_keep[:, :, :], [NOB], [t["keep"].b])
        DMA("sp", t["addc"][:, :, :], c_addc[:, :, :], [NOB], [t["addc"].b])
        t["w1"] = sb("w1", [128, 32, 128], BF16)
        DMA("sp", t["w1"][:, :, :], W1S, [B_W1], [t["w1"].b])
        t["w2"] = sb("w2", [128, 128], BF16)
        DMA("sp", t["w2"][:, :], W2S, [B_W2], [t["w2"].b])
        t["w2k"] = sb("w2k", [128, 128], BF16)
        V("pool", "memset", [], [t["w2k"].b], t["w2k"][:, :], 0.0)
        V("pool", "tensor_copy", [t["w2"].b, t["w2k"].b], [t["w2k"].b], out=t["w2k"][:, 0:64], in_=t["w2"][:, 0:64])
        pes = sb("pes", [32, 128])
        DMA("sp", pes[:, 0:64], pe_k[:, :], [NOB], [pes.b])
        DMA("sp", pes[:, 64:128], pe_v[:, :], [NOB], [pes.b], join=True)
        pesb = sb("pesb", [32, 128], BF16)
        V("dve", "tensor_copy", [pes.b], [pesb.b], out=pesb[:, :], in_=pes[:, :])
        peT = sb("peT", [128, 32], BF16)
        bkv = banks[3][:, :].bitcast(BF16)
        p.add("pe", lambda e: e.transpose(out=bkv[:, 0:32], in_=pesb[:, :], identity=ident[0:32, 0:32]), reads=[pesb.b, ident.b], writes=[banks[3].b])
        V("dve", "tensor_copy", [banks[3].b], [peT.b], out=peT[:, :], in_=bkv[:, 0:32])
        t["peh"] = sb("peh", [128, 2])
        for kv in range(2):
            ps_ = slice(64 * kv, 64 * kv + 64)
            for l in range(32):
                MM(banks[4 + kv][:, 0:1], t["w1"][ps_, l, :], peT[ps_, l:l + 1], l == 0, l == 31, [t["w1"].b, peT.b], [banks[4 + kv].b])
        for kv in range(2):
            V("dve", "tensor_copy", [banks[4 + kv].b], [t["peh"].b], out=t["peh"][:, kv:kv + 1], in_=banks[4 + kv][:, 0:1])
        t["Vca"] = sb("Vca", [127, 97], BF16)
        V("pool", "memset", [], [t["Vca"].b], t["Vca"][:, :], 1.0)
        DMA("sp", t["Vca"][:, 65:97], c_ovl[:, :], [t["Vca"].b], [t["Vca"].b])
        t["wgA"] = sb("wgA", [128, 8, GA], BF16)
        t["wgZ"] = sb("wgZ", [128, 8, 256], BF16)
        t["qa"] = [sb("qa%d" % i, [128, S], BF16) for i in range(4)]
        for i in range(4):
            V("pool", "memset", [], [t["qa"][i].b], t["qa"][i][:, :], 0.0)
        t["cT"] = sb("cT", [128, S], BF16)
        t["ksx"] = sb("ksx", [128, S], BF16)
        V("pool", "memset", [], [t["ksx"].b], t["ksx"][:, :], 0.0)
        DMA("sp", t["ksx"][64:96, :], c_X[:, :], [t["ksx"].b], [t["ksx"].b])
        t["kwz"] = sb("kwz", [128, S], BF16)
        V("pool", "memset", [], [t["kwz"].b], t["kwz"][:, :], 0.0)
        t["vsa"] = sb("vsa", [128, NT, 65], BF16)
        t["vwa"] = sb("vwa", [128, NT, 65], BF16)
        V("pool", "memset", [], [t["vsa"].b], t["vsa"][:, :, :], 1.0)
        V("pool", "memset", [], [t["vwa"].b], t["vwa"][:, :, :], 1.0)
        t["gate"] = sb("gate", [128, NT, 12])
        accraw = sb("acc", [128, max(NT * 256, 4096)])
        t["acc"] = TT(accraw[:, 0:NT * 256].rearrange("p (i c) -> p i c", i=NT), "acc")
        t["acc"].b = accraw.b
        t["wbig"] = TT(accraw[:, 0:4096].bitcast(BF16).rearrange("p (c n) -> p c n", c=8), "wbig")
        t["wbig"].b = accraw.b
        t["ha"] = [sb("ha%d" % i, [128, 128], BF16) for i in range(2)]
        t["kcmp"] = sb("kcmp", [128, 128], BF16)
        t["Ec"] = [sb("Ec%d" % i, [127, 512], BF16) for i in range(3)]
        t["cm"] = sb("cm", [128, 4, 4, 97])
        t["sm"] = [sb("sm0", [128, 32]), sb("sm1", [128, 512]), sb("sm2", [128, 288])]
        t["penb"] = sb("penb", [128, 4, 96], BF16)
        V("pool", "memset", [], [t["penb"].b], t["penb"][:, :, :], 0.0)
        t["tmp"] = [sb("tmpo%d" % i, [128, 4, 64]) for i in range(2)]
        t["th"] = [sb("th%d" % i, [128, 256], BF16) for i in range(2)]
        t["zsg"] = sb("zsg", [128, NT, 256], BF16)
        t["og"] = [sb("og%d" % i, [128, 256], BF16) for i in range(2)]
        DMA("sp", t["wgA"][:, :, :], WG[0][:, :, 0:GA], [B_WG[0]], [t["wgA"].b])
        DMA("sp", t["wgZ"][:, :, :], WG[0][:, :, GA:908], [B_WG[0]], [t["wgZ"].b])
        return t

    def nsa_layer(t, s):
        phase_norm_T(lambda i: x_in[s, i * 128:(i + 1) * 128, :], lambda i: [NOB], cst["gpre"])
        wgA, wgZ = t["wgA"], t["wgZ"]
        for g in range(4):
            g_next = (g + 1) % 4
            has_next = (g < 3) or (s + 1 < nseq)
            for j in range(4):
                qa_ = t["qa"][j]
                proj_fm(wgA[:, :, 64 * j:64 * j + 64], wgA.b, [(slice(0, 64), (lambda Q, qa_=qa_: qa_[0:64, Q * 512:(Q + 1) * 512]), qa_.b)], scale=0.125, M=64)
            proj_fm(wgA[:, :, 256:384], wgA.b, [(slice(0, 128), (lambda Q: t["cT"][:, Q * 512:(Q + 1) * 512]), t["cT"].b)])
            proj_fm(wgA[:, :, 384:448], wgA.b, [(slice(0, 64), (lambda Q: t["ksx"][0:64, Q * 512:(Q + 1) * 512]), t["ksx"].b)], M=64)
            proj_fm(wgA[:, :, 448:512], wgA.b, [(slice(0, 64), (lambda Q: t["kwz"][0:64, Q * 512:(Q + 1) * 512]), t["kwz"].b)], M=64)
            for i in range(NT):
                bk = banks[3 + (i % 2)]
                for c in range(8):
                    MM(bk[:, 0:140], uT[:, c, i * 128:(i + 1) * 128], wgA[:, c, 512:652], c == 0, c == 7, [uT.b, wgA.b], [bk.b])
                V("dve", "tensor_copy", [bk.b], [t["vsa"].b], out=t["vsa"][:, i, 0:64], in_=bk[:, 0:64])
                V("dve", "tensor_copy", [bk.b], [t["vwa"].b], out=t["vwa"][:, i, 0:64], in_=bk[:, 64:128])
                ACT(t["gate"][:, i, :], bk[:, 128:140], AF.Sigmoid, [bk.b], [t["gate"].b])
            if has_next:
                DMA("sp", wgA[:, :, :], WG[g_next][:, :, 0:GA], [B_WG[g_next]], [wgA.b])
            dbg("qT0", t["qa"][0], t["qa"][0][:, :], BF16)
            dbg("cT", t["cT"], t["cT"][:, :], BF16)
            dbg("vsa", t["vsa"], t["vsa"][:, :, :], BF16)
            dbg("gate", t["gate"], t["gate"][:, :, :])
            for kv in range(2):
                ps_ = slice(64 * kv, 64 * kv + 64)
                bk = banks[3 + kv]
                for l in range(32):
                    MM(bk[:, 0:NCMP], t["w1"][ps_, l, :], t["cT"][ps_, l:l + 16 * (NCMP - 1) + 1:16], l == 0, l == 31, [t["w1"].b, t["cT"].b], [bk.b])
                ACT(t["ha"][kv][:, 0:NCMP], bk[:, 0:NCMP], AF.Silu, [bk.b, t["peh"].b], [t["ha"][kv].b], bias=t["peh"][:, kv:kv + 1])
            MM(banks[5][:, 0:NCMP], t["w2k"][:, :], t["ha"][0][:, 0:NCMP], True, True, [t["w2k"].b, t["ha"][0].b], [banks[5].b])
            V("dve", "tensor_copy", [banks[5].b], [t["kcmp"].b], out=t["kcmp"][:, 0:NCMP], in_=banks[5][:, 0:NCMP])
            MM(banks[6][0:NCMP, 0:64], t["ha"][1][:, 0:NCMP], t["w2"][:, 64:128], True, True, [t["w2"].b, t["ha"][1].b], [banks[6].b])
            V("dve", "tensor_copy", [banks[6].b], [t["Vca"].b], out=t["Vca"][0:NCMP, 0:64], in_=banks[6][0:NCMP, 0:64])
            dbg("kcmp", t["kcmp"], t["kcmp"][:, 0:NCMP], BF16)
            dbg("Vca", t["Vca"], t["Vca"][0:NCMP, :], BF16)
            def zproj_gen():
                for i in range(NT):
                    bk = banks[6 + (i % 2)]
                    for c in range(8):
                        MM(bk[:, 0:256], uT[:, c, i * 128:(i + 1) * 128], wgZ[:, c, :], c == 0, c == 7, [uT.b, wgZ.b], [bk.b])
                    th = t["th"][i % 2]
                    ACT(th[:, :], bk[:, 0:256], AF.Tanh, [bk.b], [th.b], scale=0.5)
                    V("dve", "scalar_tensor_tensor", [th.b, bk.b], [t["zsg"].b], out=t["zsg"][:, i, :], in0=th[:, :], scalar=1.0, in1=bk[:, 0:256],
                      op0=ALU.add, op1=ALU.mult)
                    yield
                if has_next:
                    DMA("sp", wgZ[:, :, :], WG[g_next][:, :, GA:908], [B_WG[g_next]], [wgZ.b])
                yield
            zgen = zproj_gen()
            ec_rr = 0
            for Q in range(NQ):
                for j in range(4):
                    next(zgen, None)
                    h = 4 * g + j
                    qa_ = t["qa"][j]
                    ec = t["Ec"][ec_rr % 3]
                    ec_rr += 1
                    p0, p1 = Ec_rows(Q)
                    DMA("sp", ec[p0:p1, :], Ec_src(h, Q), [B_Dc], [ec.b])
                    sbk = S_BANKS[s_rr[0]]
                    s_rr[0] = (s_rr[0] + 1) % 3
                    ptb = PT[pt_rr[0]]
                    pt_rr[0] = (pt_rr[0] + 1) % len(PT)
                    MM(sbk[0:p1, :], t["kcmp"][:, 0:p1], qa_[:, Q * 512:(Q + 1) * 512], True, True, [t["kcmp"].b, qa_.b], [sbk.b])
                    ACT(ptb[0:p1, :], sbk[0:p1, :], AF.Exp, [sbk.b], [ptb.b])
                    segs = [(p0, p1)] if p0 != 32 else [(32, min(64, p1))] + ([(64, p1)] if p1 > 64 else [])
                    for (a0, a1) in segs:
                        V("dve", "tensor_tensor", [ptb.b, ec.b], [ptb.b], out=ptb[a0:a1, :], in0=ptb[a0:a1, :], in1=ec[a0:a1, :], op=ALU.mult)
                    ob = banks[3 + (j % 2)]
                    for r in range(4):
                        MM(ob[:, r * 97:(r + 1) * 97], ptb[0:p1, r * 128:(r + 1) * 128], t["Vca"][0:p1, :], True, True, [ptb.b, t["Vca"].b], [ob.b])
                    ACT(t["cm"][:, :, j, :], ob[:, 0:388].rearrange("p (r c) -> p r c", r=4), AF.Copy, [ob.b], [t["cm"].b])
                cm = t["cm"]
                sm0, sm1, sm2 = t["sm"]
                rinv = sm0[:, 0:16].rearrange("p (r j) -> p r j", r=4)
                coef = sm0[:, 16:32].rearrange("p (r j) -> p r j", r=4)
                V("dve", "tensor_scalar", [cm.b], [sm0.b], out=rinv, in0=cm[:, :, :, 64], scalar1=1e-30, scalar2=None, op0=ALU.add)
                V("dve", "reciprocal", [sm0.b], [sm0.b], out=rinv, in_=rinv)
                gv = t["gate"][:, 4 * Q:4 * Q + 4, :].rearrange("p r (j b) -> p r j b", b=3)
                V("dve", "tensor_tensor", [sm0.b, t["gate"].b], [sm0.b], out=coef, in0=rinv, in1=gv[:, :, :, 0], op=ALU.mult)
                accv = t["acc"][:, 4 * Q:4 * Q + 4, :].rearrange("p r (j d) -> p r j d", j=4)
                V("pool", "tensor_tensor", [cm.b, sm0.b], [t["acc"].b], out=accv, in0=cm[:, :, :, 0:64],
                  in1=coef.unsqueeze(3).broadcast_to([128, 4, 4, 64]), op=ALU.mult)
                next(zgen, None)
                impw = sm1[:, :].rearrange("p (r j n) -> p r j n", r=4, j=4)
                V("dve", "tensor_tensor", [cm.b, sm0.b], [sm1.b], out=impw, in0=cm[:, :, :, 65:97],
                  in1=rinv.unsqueeze(3).broadcast_to([128, 4, 4, 32]), op=ALU.mult)
                imp = sm2[:, 0:128].rearrange("p (r n) -> p r n", r=4)
                V("dve", "tensor_reduce", [sm1.b], [sm2.b], out=imp, in_=sm1[:, :].rearrange("p (r j n) -> p r n j", r=4, j=4), axis=AX.X, op=ALU.add)
                V("dve", "tensor_tensor", [sm2.b, t["keep"].b], [sm2.b], out=imp, in0=imp, in1=t["keep"][:, 4 * Q:4 * Q + 4, :], op=ALU.mult)
                V("dve", "tensor_tensor", [sm2.b, t["addc"].b], [sm2.b], out=imp, in0=imp, in1=t["addc"][:, 4 * Q:4 * Q + 4, :], op=ALU.add)
                next(zgen, None)
                top8 = sm2[:, 128:160].rearrange("p (r e) -> p r e", r=4)
                for r in range(4):
                    V("dve", "max", [sm2.b], [sm2.b], out=top8[:, r, :], in_=imp[:, r, :])
                pen32 = sm2[:, 160:288].rearrange("p (r n) -> p r n", r=4)
                V("dve", "tensor_tensor", [sm2.b], [sm2.b], out=pen32, in0=imp, in1=top8[:, :, 7:8].broadcast_to([128, 4, 32]), op=ALU.is_lt)
                V("dve", "tensor_scalar", [sm2.b], [t["penb"].b], out=t["penb"][:, :, 64:96], in0=pen32, scalar1=PEN, scalar2=None, op0=ALU.mult)
                next(zgen, None)
                bkp = banks[5]
                bkpv = bkp[:, :].bitcast(BF16)
                for r in range(4):
                    TR(bkpv[0:96, r * 128:(r + 1) * 128], t["penb"][:, r, :], [t["penb"].b], [bkp.b])
                cols = slice(Q * 512, (Q + 1) * 512)
                V("dve", "tensor_copy", [bkp.b], [t["qa"][0].b], out=t["qa"][0][64:96, cols], in_=bkpv[64:96, 0:512])
                for j in range(1, 4):
                    V("pool", "tensor_copy", [t["qa"][0].b], [t["qa"][j].b], out=t["qa"][j][64:96, cols], in_=t["qa"][0][64:96, cols])
            for _ in zgen:
                pass
            dbg("cm", t["cm"], t["cm"][:, :, :, :])
            dbg("acc_c", t["acc"], t["acc"][:, :, :])
            streams = []
            o_rr = 0
            for Q in range(NQ):
                for j in range(4):
                    for br in (1, 2):
                        st_ = Stream()
                        st_.Q = Q
                        st_.band = (br == 2)
                        qa_ = t["qa"][j]
                        kt_ = t["ksx"] if br == 1 else t["kwz"]
                        va_ = t["vsa"] if br == 1 else t["vwa"]
                        st_.q_ap = lambda gc, qa_=qa_: qa_[:, gc]
                        st_.k_ap = lambda kj, kt_=kt_: kt_[:, kj * 128:(kj + 1) * 128]
                        st_.qk_bufs = [qa_.b, kt_.b]
                        st_.pen = None
                        st_.pen_buf = None
                        st_.v_ap = lambda kj, va_=va_: va_[:, kj, :]
                        st_.v_bufs = [va_.b]
                        st_.emap = 4 * g + j
                        ob = banks[3 + (o_rr % 2)]
                        o_rr += 1
                        st_.O = [(ob[:, r * 65:(r + 1) * 65], ob) for r in range(4)]
                        st_.ob = ob
                        st_.j = j
                        st_.br = br

                        def fin(st_):
                            ob = st_.ob
                            Q, j, br = st_.Q, st_.j, st_.br
                            ov = ob[:, 0:260].rearrange("p (r c) -> p r c", r=4)
                            sm = get_stat()
                            V("dve", "reciprocal", [ob.b], [sm.b], out=sm[:, 0:4], in_=ov[:, :, 64])
                            gv = t["gate"][:, 4 * Q:4 * Q + 4, :].rearrange("p r (j b) -> p r j b", b=3)
                            V("dve", "tensor_tensor", [sm.b, t["gate"].b], [sm.b], out=sm[:, 4:8], in0=sm[:, 0:4], in1=gv[:, :, j, br], op=ALU.mult)
                            tm = t["tmp"][(2 * j + br) % 2]
                            V("dve", "tensor_tensor", [ob.b, sm.b], [tm.b], out=tm[:, :, :], in0=ov[:, :, 0:64],
                              in1=sm[:, 4:8].unsqueeze(2).broadcast_to([128, 4, 64]), op=ALU.mult)
                            accv = t["acc"][:, 4 * Q:4 * Q + 4, 64 * j:64 * j + 64]
                            V("pool", "tensor_tensor", [tm.b, t["acc"].b], [t["acc"].b], out=accv, in0=accv, in1=tm[:, :, :], op=ALU.add)
                        st_.finalize = fin
                        streams.append(st_)
            run_streams(streams)
            dbg("acc_f", t["acc"], t["acc"][:, :, :])
            for i in range(NT):
                bk = banks[5 + (i % 2)]
                og = t["og"][i % 2]
                V("dve", "scalar_tensor_tensor", [t["zsg"].b, t["acc"].b], [og.b], out=og[:, :], in0=t["acc"][:, i, :], scalar=0.5, in1=t["zsg"][:, i, :],
                  op0=ALU.mult, op1=ALU.mult)
                bkv = bk[:, :].bitcast(BF16)
                for pr in range(2):
                    TR(bkv[:, 512 + pr * 128:512 + (pr + 1) * 128], og[:, pr * 128:(pr + 1) * 128], [og.b], [bk.b])
                EVAC(oT[:, 2 * g:2 * g + 2, i * 128:(i + 1) * 128], bkv[:, 512:768].rearrange("p (c t) -> p c t", c=2), [bk.b], [oT.b])
        dbg("oT", oT, oT[:, :, :], BF16)
        dst, dstb = (x1_d, B_x1[s]) if 1 in layers else (out, B_out[s])
        phase_out(t["wbig"], 0, cst["gpost"], lambda i: x_in[s, i * 128:(i + 1) * 128, :], lambda i: [NOB],
                  lambda i: dst[s, i * 128:(i + 1) * 128, :], lambda i: [dstb[i]])

    def diff_setup():
        t = {}
        load_norm_gains(1)
        stgs["stg"] = [sb("dstg%d" % i, [128, STG_N]) for i in range(1)]
        stgs["rr"] = 0
        stgs["queues"] = ("sp",)
        cast_engs[0] = ("dve",)
        raw = sb("dscr", [128, 4096])
        t["sq"] = TT(raw[:, 0:NT * 128].rearrange("p (i c) -> p i c", i=NT), "sq")
        t["og"] = TT(raw[:, 2048:3072].bitcast(BF16)[:, 0:NT * 128].rearrange("p (i c) -> p i c", i=NT), "og")
        t["wbig"] = TT(raw[:, :].bitcast(BF16).rearrange("p (c n) -> p c n", c=8), "wbig")
        t["sq"].b = t["og"].b = t["wbig"].b = raw.b
        lam4 = sb("lam4", [128, 4, 64])
        for idx, src in enumerate((lq1, lk1, lq2, lk2)):
            DMA("sp", lam4[:, idx, :], src[0:1, :].partition_broadcast(128), [NOB], [lam4.b], join=(idx > 0))
        lm = sb("lm", [128, 8])
        prod = sb("lprod", [128, 2, 64])
        V("dve", "tensor_tensor", [lam4.b], [prod.b], out=prod[:, 0, :], in0=lam4[:, 0, :], in1=lam4[:, 1, :], op=ALU.mult)
        V("dve", "tensor_tensor", [lam4.b], [prod.b], out=prod[:, 1, :], in0=lam4[:, 2, :], in1=lam4[:, 3, :], op=ALU.mult)
        V("dve", "reduce_sum", [prod.b], [lm.b], out=lm[:, 0:2], in_=prod[:, :, :], axis=AX.X)
        ACT(lm[:, 2:4], lm[:, 0:2], AF.Exp, [lm.b], [lm.b])
        V("dve", "tensor_tensor", [lm.b], [lm.b], out=lm[:, 4:5], in0=lm[:, 3:4], in1=lm[:, 2:3], op=ALU.subtract)
        V("dve", "tensor_scalar", [lm.b], [lm.b], out=lm[:, 5:6], in0=lm[:, 4:5], scalar1=-LAMBDA_INIT, scalar2=None, op0=ALU.add)
        t["lm"] = lm
        t["subln"] = sb("sublnb", [128, D])
        DMA("sp", t["subln"][:, :], subln[0:1, :].partition_broadcast(128), [NOB], [t["subln"].b])
        t["w"] = [sb("wd%d" % i, [128, 8, 512], BF16) for i in range(2)]
        t["qT"] = [sb("dqT%d" % i, [128, S], BF16) for i in range(2)]
        t["kz"] = [[sb("dkz%d_%d" % (b_, i), [128, S], BF16) for i in range(2)] for b_ in range(2)]
        for b_ in range(2):
            for i in range(2):
                V("pool", "memset", [], [t["kz"][b_][i].b], t["kz"][b_][i][:, :], 0.0)
        t["va"] = [sb("dva%d" % i, [128, NT, 129], BF16) for i in range(2)]
        for i in range(2):
            V("pool", "memset", [], [t["va"][i].b], t["va"][i][:, :, :], 1.0)
        t["od"] = [sb("od%d" % i, [128, NT, 128]) for i in range(2)]
        t["zs"] = [sb("dzs%d" % i, [128, NT, 128], BF16) for i in range(2)]
        t["tmp"] = [sb("dtmp%d" % i, [128, 2, 128]) for i in range(2)]
        t["rs"] = sb("drs", [128, 4, NT])
        t["th"] = [sb("dth0", [128, 512], BF16)] * 2
        return t

    def diff_layer(t, s):
        phase_norm_T(lambda i: x1_d[s, i * 128:(i + 1) * 128, :], lambda i: [B_x1[s][i]], cst["gpre"])
        dv3 = diff_w_in.rearrange("(c p) n -> p c n", p=128)
        lm = t["lm"]

        def load_w(h):
            w = t["w"][h % 2]
            for part in range(4):
                LOADW(w[:, :, 128 * part:128 * part + 128], dv3[:, :, 1024 * part + 128 * h:1024 * part + 128 * h + 128], w.b, join=(part > 0))

        def proj_gen(h, part="all"):
            hb = h % 2
            w = t["w"][hb]
            qT, kz, va, zs = t["qT"][hb], t["kz"][hb], t["va"][hb], t["zs"][hb]
            for Q in range(NQ if part in ("all", "qkv") else 0):
                bk = banks[7]
                for c in range(8):
                    MM(bk[:, :], w[:, c, 0:128], uT[:, c, Q * 512:(Q + 1) * 512], c == 0, c == 7, [w.b, uT.b], [bk.b])
                EVAC(qT[:, Q * 512:(Q + 1) * 512], bk[:, :], [bk.b], [qT.b], scale=0.125)
                yield
            for Q in range(NQ if part in ("all", "qkv") else 0):
                bk = banks[7]
                for c in range(8):
                    MM(bk[:, :], w[:, c, 128:256], uT[:, c, Q * 512:(Q + 1) * 512], c == 0, c == 7, [w.b, uT.b], [bk.b])
                EVAC(kz[0][0:64, Q * 512:(Q + 1) * 512], bk[0:64, :], [bk.b], [kz[0].b])
                EVAC(kz[1][64:128, Q * 512:(Q + 1) * 512], bk[64:128, :], [bk.b], [kz[1].b])
                yield
            for i4 in range(NT // 4 if part in ("all", "qkv") else 0):
                bk = banks[7]
                for r in range(4):
                    i = 4 * i4 + r
                    for c in range(8):
                        MM(bk[:, r * 128:(r + 1) * 128], uT[:, c, i * 128:(i + 1) * 128], w[:, c, 256:384], c == 0, c == 7, [uT.b, w.b], [bk.b])
                V("dve", "tensor_copy", [bk.b], [va.b], out=va[:, 4 * i4:4 * i4 + 4, 0:128], in_=bk[:, :].rearrange("p (r c) -> p r c", r=4))
                yield
            for i4 in range(NT // 4 if part in ("all", "z") else 0):
                bk = banks[7]
                for r in range(4):
                    i = 4 * i4 + r
                    for c in range(8):
                        MM(bk[:, r * 128:(r + 1) * 128], uT[:, c, i * 128:(i + 1) * 128], w[:, c, 384:512], c == 0, c == 7, [uT.b, w.b], [bk.b])
                th = t["th"][i4 % 2]
                ACT(th[:, :], bk[:, :], AF.Tanh, [bk.b], [th.b], scale=0.5)
                V("dve", "scalar_tensor_tensor", [th.b, bk.b], [zs.b], out=zs[:, 4 * i4:4 * i4 + 4, :], in0=th[:, :].rearrange("p (r c) -> p r c", r=4),
                  scalar=1.0, in1=bk[:, :].rearrange("p (r c) -> p r c", r=4), op0=ALU.add, op1=ALU.mult)
                yield
            yield

        def tail_gen(h):
            hb = h % 2
            od, zs, sq, rs = t["od"][hb], t["zs"][hb], t["sq"], t["rs"]
            V("dve", "tensor_tensor", [od.b], [sq.b], out=sq[:, :, :], in0=od[:, :, :], in1=od[:, :, :], op=ALU.mult)
            yield
            V("dve", "reduce_sum", [sq.b], [rs.b], out=rs[:, 0, :], in_=sq[:, :, :], axis=AX.X)
            yield
            V("dve", "tensor_scalar", [rs.b], [rs.b], out=rs[:, 1, :], in0=rs[:, 0, :], scalar1=1.0 / 128, scalar2=EPS, op0=ALU.mult, op1=ALU.add)
            ACT(rs[:, 2, :], rs[:, 1, :], AF.Sqrt, [rs.b], [rs.b])
            V("dve", "reciprocal", [rs.b], [rs.b], out=rs[:, 3, :], in_=rs[:, 2, :])
            yield
            V("dve", "tensor_tensor", [od.b, rs.b], [sq.b], out=sq[:, :, :], in0=od[:, :, :], in1=rs[:, 3, :].unsqueeze(2).broadcast_to([128, NT, 128]), op=ALU.mult)
            yield
            V("dve", "scalar_tensor_tensor", [sq.b, t["subln"].b], [sq.b], out=sq[:, :, :], in0=sq[:, :, :], scalar=0.5 * (1.0 - LAMBDA_INIT),
              in1=t["subln"][:, 128 * h:128 * h + 128].unsqueeze(1).broadcast_to([128, NT, 128]), op0=ALU.mult, op1=ALU.mult)
            yield
            V("dve", "tensor_tensor", [sq.b, zs.b], [t["og"].b], out=t["og"][:, :, :], in0=sq[:, :, :], in1=zs[:, :, :], op=ALU.mult)
            yield
            for i4 in range(NT // 4):
                bk = banks[7]
                bkv = bk[:, :].bitcast(BF16)
                for r in range(4):
                    TR(bkv[:, r * 128:(r + 1) * 128], t["og"][:, 4 * i4 + r, :], [t["og"].b], [bk.b])
                EVAC(oT[:, h, i4 * 512:(i4 + 1) * 512], bkv[:, 0:512], [bk.b], [oT.b])
                yield

        def chain(*gens):
            for g_ in gens:
                if g_ is not None:
                    for _ in g_:
                        yield

        def head_streams(h):
            hb = h % 2
            qT, kz, va, od = t["qT"][hb], t["kz"][hb], t["va"][hb], t["od"][hb]
            streams = []
            for Q in range(NQ):
                for m in range(2):
                    st_ = Stream()
                    st_.Q = Q
                    st_.band = False
                    kz_ = kz[m]
                    st_.q_ap = lambda gc, qT=qT: qT[:, gc]
                    st_.k_ap = lambda kj, kz_=kz_: kz_[:, kj * 128:(kj + 1) * 128]
                    st_.qk_bufs = [qT.b, kz_.b]
                    st_.pen = None
                    st_.pen_buf = None
                    st_.v_ap = lambda kj, va=va: va[:, kj, :]
                    st_.v_bufs = [va.b]
                    st_.emap = 2 * h + m
                    obs = [banks[3 + 2 * m], banks[4 + 2 * m]]
                    st_.O = [(obs[r // 2][:, (r % 2) * 129:(r % 2) * 129 + 129], obs[r // 2]) for r in range(4)]
                    st_.obs = obs
                    st_.m = m
                    st_.od = od

                    def fin(st_):
                        Q, m, od = st_.Q, st_.m, st_.od
                        for half in range(2):
                            ob = st_.obs[half]
                            ov = ob[:, 0:258].rearrange("p (r c) -> p r c", r=2)
                            i0 = 4 * Q + 2 * half
                            sm = get_stat()
                            V("dve", "reciprocal", [ob.b], [sm.b], out=sm[:, 0:2], in_=ov[:, :, 128])
                            if m == 0:
                                V("dve", "tensor_tensor", [ob.b, sm.b], [od.b], out=od[:, i0:i0 + 2, :], in0=ov[:, :, 0:128],
                                  in1=sm[:, 0:2].unsqueeze(2).broadcast_to([128, 2, 128]), op=ALU.mult)
                            else:
                                V("dve", "tensor_scalar", [sm.b, lm.b], [sm.b], out=sm[:, 2:4], in0=sm[:, 0:2], scalar1=lm[:, 5:6], scalar2=None, op0=ALU.mult)
                                tm = t["tmp"][half]
                                V("dve", "tensor_tensor", [ob.b, sm.b], [tm.b], out=tm[:, :, :], in0=ov[:, :, 0:128],
                                  in1=sm[:, 2:4].unsqueeze(2).broadcast_to([128, 2, 128]), op=ALU.mult)
                                V("pool", "tensor_tensor", [tm.b, od.b], [od.b], out=od[:, i0:i0 + 2, :], in0=od[:, i0:i0 + 2, :],
                                  in1=tm[:, :, :], op=ALU.add)
                    st_.finalize = fin
                    streams.append(st_)
            return streams

        load_w(0)
        load_w(1)
        for _ in proj_gen(0):
            pass
        for h in range(8):
            def lw_gen(hh):
                load_w(hh)
                yield
            fill = chain(lw_gen(h + 2) if h + 2 < 8 else None, proj_gen(h + 1, "qkv") if h < 7 else None,
                         tail_gen(h - 1) if h > 0 else None, proj_gen(h + 1, "z") if h < 7 else None)
            run_streams(head_streams(h), filler=fill, every=3)
        for _ in tail_gen(7):
            pass
        cast_engs[0] = ("dve", "act")
        wo3 = diff_w_out.rearrange("(c p) n -> p c n", p=128)
        for q4 in range(4):
            LOADW(t["wbig"][:, :, 256 * q4:256 * q4 + 256], wo3[:, :, 256 * q4:256 * q4 + 256], t["wbig"].b, join=(q4 > 0))
        cast_engs[0] = ("dve",)
        phase_out(t["wbig"], None, cst["gpost"], lambda i: x1_d[s, i * 128:(i + 1) * 128, :], lambda i: [B_x1[s][i]],
                  lambda i: out[s, i * 128:(i + 1) * 128, :], lambda i: [B_out[s][i]])

    with es:
        with ExitStack() as es1:
            cur_es[0] = es1
            setup_bias()
            setup_weights()
            setup_bias_load()
        cur_es[0] = es
        p.barrier()
        if 0 in layers:
            with ExitStack() as es2:
                cur_es[0] = es2
                nt_ = nsa_setup()
                for s in range(nseq):
                    nsa_layer(nt_, s)
            cur_es[0] = es
            p.barrier()
        if 1 in layers:
            with ExitStack() as es3:
                cur_es[0] = es3
                dt_ = diff_setup()
                for s in range(nseq):
                    if 0 not in layers:
                        for i in range(NT):
                            xb_ = xt[i % 2]
                            DMA("sp", xb_[:, :], x_in[s, i * 128:(i + 1) * 128, :], [NOB], [xb_.b])
                            DMA("sp", x1_d[s, i * 128:(i + 1) * 128, :], xb_[:, :], [xb_.b], [B_x1[s][i]])
                    diff_layer(dt_, s)
            cur_es[0] = es
        p.emit()
    return nc


INPUT_NAMES = ["rel_bias_table", "norm_pre", "norm_post", "nsa_w_in", "nsa_cmp_pe_k", "nsa_cmp_w1_k", "nsa_cmp_w2_k",
               "nsa_cmp_pe_v", "nsa_cmp_w1_v", "nsa_cmp_w2_v", "nsa_w_out", "diff_w_in", "diff_lambda_q1", "diff_lambda_k1",
               "diff_lambda_q2", "diff_lambda_k2", "diff_subln", "diff_w_out"]


def make_in_maps(inputs, n_cores, nseq, S):
    consts = host_consts(S)
    shared = {}
    for k in INPUT_NAMES:
        a = np.ascontiguousarray(np.asarray(inputs[k], dtype=np.float32))
        if a.ndim == 3:
            a = a[0]
        elif k.startswith("diff_lambda") or k == "diff_subln":
            a = a.reshape(1, -1)
        shared[k] = np.ascontiguousarray(a)
    shared.update(consts)
    x = np.asarray(inputs["x"], dtype=np.float32)
    maps = []
    for c in range(n_cores):
        m = dict(shared)
        m["x"] = np.ascontiguousarray(x[c * nseq:(c + 1) * nseq])
        maps.append(m)
    return maps


def kernel(**inputs):
    x = np.asarray(inputs["x"])
    B, S, _ = x.shape
    n_cores = 8
    nseq = B // n_cores
    nc = build(nseq, S)
    in_maps = make_in_maps(inputs, n_cores, nseq, S)
    res = run_bass_kernel_spmd(nc, in_maps, core_ids=list(range(n_cores)))
    return np.concatenate([np.asarray(r["out"]) for r in res.results], axis=0).astype(np.float32)
```
